# Optimizing a Trainium2 kernel written in Bass

```python
import math
import jax
import jax.numpy as jnp
from jax import lax
import numpy as np

D_MODEL = 1024
BATCH = 8
SEQ = 2048
DEPTH = 4

GRID_W = 64
CTX_LEN = 256
F32 = jnp.float32

N_EVEN = (DEPTH + 1) // 2
N_ODD = DEPTH // 2

A_HEADS = 4
A_DK = 128
A_DV = 128
A_WIDTH = A_HEADS * A_DK
A_CHUNK = 64
A_IN = 5 * A_WIDTH

B_HEADS = 8
B_KV_HEADS = 2
B_HEAD_DIM = 64
B_QW = B_HEADS * B_HEAD_DIM
B_KVW = B_KV_HEADS * B_HEAD_DIM
B_IN = B_QW + 2 * B_KVW
B_WINDOW = 128
B_BLOCK = 128
ROPE_BASE = 10000.0

EVEN_IN = A_IN + B_IN
EVEN_OUT = A_HEADS * A_DV + B_QW

C_GROUP = 16
C_GROUPS = 32
C_WIDTH = C_GROUP * C_GROUPS
C_STATE = 64

R_HEADS = 8
R_HEAD = 64
R_WIDTH = R_HEADS * R_HEAD
R_LORA_W = 64
R_LORA_A = 64
R_LORA_G = 128
R_SPLITS = (R_WIDTH, R_WIDTH, R_WIDTH, R_LORA_W, R_LORA_W, R_LORA_A, R_LORA_A, R_LORA_G)
R_IN = sum(R_SPLITS)

ODD_IN = C_WIDTH + R_IN
ODD_OUT = C_WIDTH + R_WIDTH

D_FF = 2816
N_EXPERTS = 8
TOP_K = 2
E_FF = 2816

DEEPNORM_ALPHA = (2 * DEPTH) ** 0.25
DEEPNORM_BETA = (8 * DEPTH) ** -0.25
LN_EPS = 1e-5
RWKV_GN_EPS = 64e-5

kernel_name = "hybrid_hgrn2_swa_s5_rwkv7_moe_dit"


def _split(x, sizes):
    idx = np.cumsum(sizes)[:-1].tolist()
    return jnp.split(x, idx, axis=-1)


def layer_norm(x, g, b):
    xf = x.astype(F32)
    mu = jnp.mean(xf, -1, keepdims=True)
    var = jnp.mean(jnp.square(xf - mu), -1, keepdims=True)
    return ((xf - mu) * lax.rsqrt(var + LN_EPS)).astype(x.dtype) * g + b


def axial_rope_tables(rows):
    row = jnp.repeat(jnp.arange(rows, dtype=F32), GRID_W)
    col = jnp.tile(jnp.arange(GRID_W, dtype=F32), rows)
    n_freq = B_HEAD_DIM // 4
    inv = ROPE_BASE ** (-jnp.arange(n_freq, dtype=F32) / n_freq)
    ang = jnp.concatenate([row[:, None] * inv, col[:, None] * inv], axis=-1)
    return jnp.cos(ang), jnp.sin(ang)


def apply_rope(x, cos, sin):
    half = x.shape[-1] // 2
    x1, x2 = x[..., :half], x[..., half:]
    cos = cos.astype(x.dtype)
    sin = sin.astype(x.dtype)
    return jnp.concatenate([x1 * cos - x2 * sin, x1 * sin + x2 * cos], axis=-1)


def _gla_chunk_scan(q, k, v, logf, s0):
    bsz, h, L, _ = q.shape
    n = L // A_CHUNK

    def to_chunks(t):
        return t.reshape(bsz, h, n, A_CHUNK, t.shape[-1]).transpose(2, 0, 1, 3, 4)

    tri = jnp.tril(jnp.ones((A_CHUNK, A_CHUNK), bool))[:, :, None]

    def step(s, inp):
        qi, ki, vi, gi = inp
        b = jnp.cumsum(gi, axis=-2)
        diff = b[..., :, None, :] - b[..., None, :, :]
        decay = jnp.where(tri, jnp.exp(jnp.where(tri, diff, 0.0)), 0.0)
        att = jnp.einsum('bhtd,bhsd,bhtsd->bhts', qi, ki, decay)
        o = att @ vi + jnp.einsum('bhtd,bhdv->bhtv', qi * jnp.exp(b), s)
        b_last = b[..., -1:, :]
        s_new = jnp.exp(b_last[..., 0, :])[..., None] * s + jnp.einsum(
            'bhsd,bhsv->bhdv', ki * jnp.exp(b_last - b), vi)
        return s_new, o

    s_fin, o = lax.scan(step, s0, (to_chunks(q), to_chunks(k), to_chunks(v), to_chunks(logf)))
    o = o.transpose(1, 2, 0, 3, 4).reshape(bsz, h, L, -1)
    return o, s_fin


def hgrn2_mixer(pc, pl, lb, norm_g, need_ctx):
    def heads(t):
        b, l, _ = t.shape
        return t.astype(F32).reshape(b, l, A_HEADS, -1).transpose(0, 2, 1, 3)

    def prep(p):
        q, f_fw, f_bw, i, g = _split(p, (A_WIDTH,) * 5)
        q = heads(jax.nn.silu(q)) * A_DK ** -0.5
        dirs = []
        for fl in (f_fw, f_bw):
            f = lb + (1.0 - lb) * jax.nn.sigmoid(fl.astype(F32))
            dirs.append((heads(1.0 - f), heads(jnp.log(f))))
        return q, heads(i), dirs, g

    qc, ic, dc, gc = prep(pc)
    ql, il, dl, gl = prep(pl)
    zero = jnp.zeros((pl.shape[0], A_HEADS, A_DK, A_DV), F32)
    rev = lambda t: jnp.flip(t, axis=2)
    oc_f, sc_f = _gla_chunk_scan(qc, dc[0][0], ic, dc[0][1], zero)
    ol_f, _ = _gla_chunk_scan(ql, dl[0][0], il, dl[0][1], sc_f)
    oc_b, sc_b = _gla_chunk_scan(rev(qc), rev(dc[1][0]), rev(ic), rev(dc[1][1]), zero)
    ol_b, _ = _gla_chunk_scan(rev(ql), rev(dl[1][0]), rev(il), rev(dl[1][1]), sc_b)

    def finish(o, g):
        o = o * lax.rsqrt(jnp.mean(jnp.square(o), -1, keepdims=True) + 1e-6)
        b, h, l, dv = o.shape
        o = o.transpose(0, 2, 1, 3).reshape(b, l, h * dv).astype(g.dtype)
        return o * norm_g * jax.nn.silu(g)

    out_l = finish(ol_f + rev(ol_b), gl)
    out_c = finish(oc_f + rev(oc_b), gc) if need_ctx else None
    return out_c, out_l


def window_gqa_mixer(pc, pl, sink, cos, sin, need_ctx):
    bsz, L, _ = pl.shape
    Lc = pc.shape[1]
    G, R, dh = B_KV_HEADS, B_HEADS // B_KV_HEADS, B_HEAD_DIM
    scale = dh ** -0.5

    def qkv(p):
        b, l, _ = p.shape
        q, k, v = _split(p, (B_QW, B_KVW, B_KVW))
        return q.reshape(b, l, G, R, dh), k.reshape(b, l, G, dh), v.reshape(b, l, G, dh)

    qc, kc, vc = qkv(pc)
    ql, kl, vl = qkv(pl)
    ql = apply_rope(ql, cos[:, None, None, :], sin[:, None, None, :])
    kl = apply_rope(kl, cos[:, None, :], sin[:, None, :])
    sink_gr = sink.astype(F32).reshape(G, R)

    nb = L // B_BLOCK
    qb = ql.reshape(bsz, nb, B_BLOCK, G, R, dh)

    def band(t):
        tp = jnp.pad(t, ((0, 0), (B_BLOCK, B_BLOCK), (0, 0), (0, 0))).reshape(bsz, nb + 2, B_BLOCK, G, dh)
        return jnp.concatenate([tp[:, :-2], tp[:, 1:-1], tp[:, 2:]], axis=2)

    kb, vb = band(kl), band(vl)
    blk = jnp.arange(nb)[:, None]
    qpos = blk * B_BLOCK + jnp.arange(B_BLOCK)[None, :]
    kpos = (blk - 1) * B_BLOCK + jnp.arange(3 * B_BLOCK)[None, :]
    mask = (jnp.abs(qpos[:, :, None] - kpos[:, None, :]) <= B_WINDOW) & ((kpos >= 0) & (kpos < L))[:, None, :]
    s_win = jnp.einsum('bnqgrd,bnkgd->bngrqk', qb, kb).astype(F32) * scale
    s_win = jnp.where(mask[None, :, None, None], s_win, -jnp.inf)
    s_ctx = jnp.einsum('bnqgrd,bcgd->bngrqc', qb, kc).astype(F32) * scale
    s_sink = jnp.broadcast_to(sink_gr[None, None, :, :, None, None], s_win.shape[:-1] + (1,))
    p = jax.nn.softmax(jnp.concatenate([s_win, s_ctx, s_sink], axis=-1), axis=-1).astype(pl.dtype)
    nk = 3 * B_BLOCK
    ol = (jnp.einsum('bngrqk,bnkgd->bnqgrd', p[..., :nk], vb)
          + jnp.einsum('bngrqc,bcgd->bnqgrd', p[..., nk:nk + Lc], vc)).reshape(bsz, L, B_QW)

    oc = None
    if need_ctx:
        s_cc = jnp.einsum('bqgrd,bkgd->bgrqk', qc, kc).astype(F32) * scale
        sk = jnp.broadcast_to(sink_gr[None, :, :, None, None], s_cc.shape[:-1] + (1,))
        pcc = jax.nn.softmax(jnp.concatenate([s_cc, sk], axis=-1), axis=-1).astype(pc.dtype)
        oc = jnp.einsum('bgrqk,bkgd->bqgrd', pcc[..., :Lc], vc).reshape(bsz, Lc, B_QW)
    return oc, ol


def _cplx_combine(e1, e2):
    a1r, a1i, b1r, b1i = e1
    a2r, a2i, b2r, b2i = e2
    return (a1r * a2r - a1i * a2i, a1r * a2i + a1i * a2r,
            a2r * b1r - a2i * b1i + b2r, a2r * b1i + a2i * b1r + b2i)


def s5_discretize(lam_re, lam_im, log_dt, b_re, b_im):
    lam_re = jnp.minimum(lam_re.astype(F32), -1e-4)
    lam_im = lam_im.astype(F32)
    dt = jnp.exp(log_dt.astype(F32))[:, None]
    mag = jnp.exp(lam_re * dt)
    ab_re, ab_im = mag * jnp.cos(lam_im * dt), mag * jnp.sin(lam_im * dt)
    den = lam_re ** 2 + lam_im ** 2
    nr = ab_re - 1.0
    co_re = (nr * lam_re + ab_im * lam_im) / den
    co_im = (ab_im * lam_re - nr * lam_im) / den
    b_re, b_im = b_re.astype(F32), b_im.astype(F32)
    bb_re = co_re[..., None] * b_re - co_im[..., None] * b_im
    bb_im = co_re[..., None] * b_im + co_im[..., None] * b_re
    return ab_re, ab_im, bb_re, bb_im


def s5_scan(u, ab_re, ab_im, bb_re, bb_im, x0_re, x0_im):
    bu_re = jnp.einsum('lbgh,gph->lbgp', u, bb_re)
    bu_im = jnp.einsum('lbgh,gph->lbgp', u, bb_im)
    shape = (u.shape[0], 1) + ab_re.shape
    a_re, a_im = jnp.broadcast_to(ab_re, shape), jnp.broadcast_to(ab_im, shape)
    ac_re, ac_im, x_re, x_im = lax.associative_scan(_cplx_combine, (a_re, a_im, bu_re, bu_im), axis=0)
    x_re = x_re + ac_re * x0_re - ac_im * x0_im
    x_im = x_im + ac_re * x0_im + ac_im * x0_re
    return x_re, x_im


def s5_mixer(uc, ul, lam_re, lam_im, log_dt, b_re, b_im, c_re, c_im, d_skip, glu_w, need_ctx):
    def groups(u):
        b, l, _ = u.shape
        return u.astype(F32).reshape(b, l, C_GROUPS, C_GROUP).transpose(1, 0, 2, 3)

    g_c, g_l = groups(uc), groups(ul)
    zero = jnp.zeros((ul.shape[0], C_GROUPS, C_STATE), F32)
    fw = s5_discretize(lam_re[0], lam_im[0], log_dt[0], b_re, b_im)
    bw = s5_discretize(lam_re[1], lam_im[1], log_dt[1], b_re, b_im)
    xcf = s5_scan(g_c, *fw, zero, zero)
    xlf = s5_scan(g_l, *fw, xcf[0][-1], xcf[1][-1])
    xcb = s5_scan(g_c[::-1], *bw, zero, zero)
    xlb = s5_scan(g_l[::-1], *bw, xcb[0][-1], xcb[1][-1])
    cr, ci = c_re.astype(F32), c_im.astype(F32)
    dsk = d_skip.astype(F32).reshape(C_GROUPS, C_GROUP)

    def readout(u, xf, xb, dtype):
        x_re = xf[0] + xb[0][::-1]
        x_im = xf[1] + xb[1][::-1]
        y = jnp.einsum('lbgp,ghp->lbgh', x_re, cr) - jnp.einsum('lbgp,ghp->lbgh', x_im, ci) + dsk * u
        l, b = y.shape[:2]
        y = jax.nn.gelu(y.transpose(1, 0, 2, 3).reshape(b, l, C_WIDTH).astype(dtype))
        return y * jax.nn.sigmoid(y @ glu_w)

    out_l = readout(g_l, xlf, xlb, ul.dtype)
    out_c = readout(g_c, xcf, xcb, uc.dtype) if need_ctx else None
    return out_c, out_l


def centred_shift(x):
    prev = jnp.pad(x[:, :-1], ((0, 0), (1, 0), (0, 0)))
    nxt = jnp.pad(x[:, 1:], ((0, 0), (0, 1), (0, 0)))
    return 0.5 * (prev + nxt)


def _rwkv7_scan(r, w, k, v, a, b, s0):
    def step(s, inp):
        rt, wt, kt, vt, at, bt = inp
        sa = jnp.einsum('bhij,bhj->bhi', s, at)
        s = s * wt[:, :, None, :] + sa[..., None] * bt[:, :, None, :] + vt[..., None] * kt[:, :, None, :]
        return s, jnp.einsum('bhij,bhj->bhi', s, rt)

    xs = tuple(jnp.swapaxes(t, 0, 1) for t in (r, w, k, v, a, b))
    s_fin, o = lax.scan(step, s0, xs)
    return jnp.swapaxes(o, 0, 1), s_fin


def rwkv7_mixer(pc, pl, mu, w0, w2, a0, a2, g2, k_k, k_a, r_k, lnx_g, lnx_b, need_ctx):
    def heads(t):
        b, l, _ = t.shape
        return t.astype(F32).reshape(b, l, R_HEADS, R_HEAD)

    def prep(p):
        p = p + mu * (centred_shift(p) - p)
        r, k, v, wd_f, wd_b, ad_f, ad_b, gd = _split(p, R_SPLITS)
        g = jax.nn.sigmoid(gd) @ g2
        kk = heads(k * k_k)
        kk = kk / jnp.maximum(jnp.sqrt(jnp.sum(jnp.square(kk), -1, keepdims=True)), 1e-12)
        dirs = []
        for d, (wd, ad) in enumerate(((wd_f, ad_f), (wd_b, ad_b))):
            w = -jax.nn.softplus(-(w0[d] + jnp.tanh(wd) @ w2[d])) - 0.5
            decay = jnp.exp(-jnp.exp(heads(w)))
            a = jax.nn.sigmoid(a0[d] + ad @ a2[d])
            kd = heads(k * (1.0 + (a - 1.0) * k_a))
            dirs.append((decay, kd, -kk, kk * heads(a)))
        return heads(r), heads(k), heads(v), dirs, g

    rc, kc, vc, dc, gc = prep(pc)
    rl, kl, vl, dl, gl = prep(pl)
    zero = jnp.zeros((pl.shape[0], R_HEADS, R_HEAD, R_HEAD), F32)
    rev = lambda t: jnp.flip(t, axis=1)
    oc_f, sc_f = _rwkv7_scan(rc, dc[0][0], dc[0][1], vc, dc[0][2], dc[0][3], zero)
    ol_f, _ = _rwkv7_scan(rl, dl[0][0], dl[0][1], vl, dl[0][2], dl[0][3], sc_f)
    oc_b, sc_b = _rwkv7_scan(*[rev(t) for t in (rc, dc[1][0], dc[1][1], vc, dc[1][2], dc[1][3])], zero)
    ol_b, _ = _rwkv7_scan(*[rev(t) for t in (rl, dl[1][0], dl[1][1], vl, dl[1][2], dl[1][3])], sc_b)

    def finish(o, r, k, v, g):
        m = jnp.mean(o, -1, keepdims=True)
        var = jnp.mean(jnp.square(o - m), -1, keepdims=True)
        o = (o - m) * lax.rsqrt(var + RWKV_GN_EPS)
        b, l = o.shape[:2]
        o = o.reshape(b, l, R_WIDTH).astype(g.dtype) * lnx_g + lnx_b
        bonus = (jnp.sum(r * k * r_k, -1, keepdims=True) * v).reshape(b, l, R_WIDTH).astype(g.dtype)
        return (o + bonus) * g

    out_l = finish(ol_f + rev(ol_b), rl, kl, vl, gl)
    out_c = finish(oc_f + rev(oc_b), rc, kc, vc, gc) if need_ctx else None
    return out_c, out_l


def swiglu(h, wg, wu, wd):
    return (jax.nn.silu(h @ wg) * (h @ wu)) @ wd


def moe_swiglu(h, router_w, router_b, wg, wu, wd):
    logits = (h @ router_w + router_b).astype(F32)
    top_v, top_i = lax.top_k(logits, TOP_K)
    top_p = jax.nn.softmax(top_v, axis=-1)
    gates = jnp.sum(jax.nn.one_hot(top_i, N_EXPERTS, dtype=F32) * top_p[..., None], axis=-2).astype(h.dtype)
    out = jnp.zeros_like(h)
    for e in range(N_EXPERTS):
        out = out + gates[..., e:e + 1] * swiglu(h, wg[e], wu[e], wd[e])
    return out


def setup_inputs(seed: int = 0) -> dict:
    key = jax.random.key(seed)
    keys = iter(jax.random.split(key, 48))

    def nrm(shape, std):
        return std * jax.random.normal(next(keys), shape, F32)

    def uni(shape, lo, hi):
        return jax.random.uniform(next(keys), shape, F32, lo, hi)

    D = D_MODEL
    beta = DEEPNORM_BETA
    lam_im = jnp.pi * jnp.arange(C_STATE, dtype=F32) + nrm((N_ODD, 2, C_GROUPS, C_STATE), 0.01)
    return {
        "x": nrm((BATCH, SEQ, D), 1.0),
        "c": nrm((BATCH, D), 1.0),
        "ctx": nrm((BATCH, CTX_LEN, D), 1.0),
        "c_ctx": nrm((D,), 1.0),
        "ada_w": nrm((DEPTH, D, 6 * D), 0.5 * D ** -0.5),
        "ada_b": nrm((DEPTH, 6 * D), 0.02),
        "ln_g": 1.0 + nrm((DEPTH, 2, D), 0.02),
        "ln_b": nrm((DEPTH, 2, D), 0.02),
        "ev_w_in": nrm((N_EVEN, D, EVEN_IN), D ** -0.5),
        "ev_w_out": nrm((N_EVEN, EVEN_OUT, D), beta * EVEN_OUT ** -0.5),
        "hg_lb": 1.0 + nrm((N_EVEN, A_WIDTH), 0.1),
        "hg_norm_g": 1.0 + nrm((N_EVEN, A_WIDTH), 0.02),
        "attn_sink": nrm((N_EVEN, B_HEADS), 0.5),
        "ffn_w_gate": nrm((N_EVEN, D, D_FF), D ** -0.5),
        "ffn_w_up": nrm((N_EVEN, D, D_FF), D ** -0.5),
        "ffn_w_down": nrm((N_EVEN, D_FF, D), beta * D_FF ** -0.5),
        "od_w_in": nrm((N_ODD, D, ODD_IN), D ** -0.5),
        "od_w_out": nrm((N_ODD, ODD_OUT, D), beta * ODD_OUT ** -0.5),
        "s5_lam_re": -0.5 + nrm((N_ODD, 2, C_GROUPS, C_STATE), 0.01),
        "s5_lam_im": lam_im,
        "s5_log_dt": uni((N_ODD, 2, C_GROUPS), math.log(0.001), math.log(0.1)),
        "s5_b_re": nrm((N_ODD, C_GROUPS, C_STATE, C_GROUP), (2 * C_GROUP) ** -0.5),
        "s5_b_im": nrm((N_ODD, C_GROUPS, C_STATE, C_GROUP), (2 * C_GROUP) ** -0.5),
        "s5_c_re": nrm((N_ODD, C_GROUPS, C_GROUP, C_STATE), C_STATE ** -0.5),
        "s5_c_im": nrm((N_ODD, C_GROUPS, C_GROUP, C_STATE), C_STATE ** -0.5),
        "s5_d": nrm((N_ODD, C_WIDTH), 1.0),
        "s5_glu_w": nrm((N_ODD, C_WIDTH, C_WIDTH), C_WIDTH ** -0.5),
        "rwkv_mu": uni((N_ODD, R_IN), 0.0, 1.0),
        "rwkv_w0": uni((N_ODD, 2, R_WIDTH), -5.5, -0.5),
        "rwkv_w2": nrm((N_ODD, 2, R_LORA_W, R_WIDTH), 0.5 * R_LORA_W ** -0.5),
        "rwkv_a0": nrm((N_ODD, 2, R_WIDTH), 0.5),
        "rwkv_a2": nrm((N_ODD, 2, R_LORA_A, R_WIDTH), 0.5 * R_LORA_A ** -0.5),
        "rwkv_g2": nrm((N_ODD, R_LORA_G, R_WIDTH), R_LORA_G ** -0.5),
        "rwkv_k_k": 0.85 + nrm((N_ODD, R_WIDTH), 0.05),
        "rwkv_k_a": 1.0 + nrm((N_ODD, R_WIDTH), 0.05),
        "rwkv_r_k": nrm((N_ODD, R_HEADS, R_HEAD), 0.1),
        "rwkv_ln_g": 1.0 + nrm((N_ODD, R_WIDTH), 0.02),
        "rwkv_ln_b": nrm((N_ODD, R_WIDTH), 0.02),
        "moe_router_w": nrm((N_ODD, D, N_EXPERTS), D ** -0.5),
        "moe_router_b": nrm((N_ODD, N_EXPERTS), 0.01),
        "moe_w_gate": nrm((N_ODD, N_EXPERTS, D, E_FF), D ** -0.5),
        "moe_w_up": nrm((N_ODD, N_EXPERTS, D, E_FF), D ** -0.5),
        "moe_w_down": nrm((N_ODD, N_EXPERTS, E_FF, D), beta * E_FF ** -0.5),
    }


def reference(x, c, ctx, c_ctx, ada_w, ada_b, ln_g, ln_b,
              ev_w_in, ev_w_out, hg_lb, hg_norm_g, attn_sink, ffn_w_gate, ffn_w_up, ffn_w_down,
              od_w_in, od_w_out, s5_lam_re, s5_lam_im, s5_log_dt, s5_b_re, s5_b_im, s5_c_re, s5_c_im,
              s5_d, s5_glu_w, rwkv_mu, rwkv_w0, rwkv_w2, rwkv_a0, rwkv_a2, rwkv_g2, rwkv_k_k, rwkv_k_a,
              rwkv_r_k, rwkv_ln_g, rwkv_ln_b, moe_router_w, moe_router_b, moe_w_gate, moe_w_up, moe_w_down):
    L = x.shape[1]
    rows = L // GRID_W
    cos, sin = axial_rope_tables(rows)
    lb_soft = jax.nn.softmax(hg_lb.astype(F32), axis=0)
    lb_all = jnp.cumsum(lb_soft, axis=0) - lb_soft[0:1]
    cond_l = jax.nn.silu(c)[:, None, :]
    cond_c = jax.nn.silu(c_ctx)[None, None, :]
    xl, xc = x, ctx
    Lc = ctx.shape[1]
    for layer in range(DEPTH):
        j = layer // 2
        need_ctx = layer < DEPTH - 1
        sh1, sc1, gt1, sh2, sc2, gt2 = _split(cond_l @ ada_w[layer] + ada_b[layer], (D_MODEL,) * 6)
        csh1, csc1, cgt1, csh2, csc2, cgt2 = _split(cond_c @ ada_w[layer] + ada_b[layer], (D_MODEL,) * 6)
        hl = xl * (1.0 + sc1) + sh1
        hc = xc * (1.0 + csc1) + csh1
        if layer % 2 == 0:
            w_in, w_out = ev_w_in[j], ev_w_out[j]
            pc, pl = hc @ w_in, hl @ w_in
            oa_c, oa_l = hgrn2_mixer(pc[..., :A_IN], pl[..., :A_IN], lb_all[j], hg_norm_g[j], need_ctx)
            ob_c, ob_l = window_gqa_mixer(pc[..., A_IN:], pl[..., A_IN:], attn_sink[j], cos, sin, need_ctx)
            yl = jnp.concatenate([oa_l, ob_l], axis=-1)
            yc = jnp.concatenate([oa_c, ob_c], axis=-1) if need_ctx else None
        else:
            w_in, w_out = od_w_in[j], od_w_out[j]
            pc, pl = hc @ w_in, hl @ w_in
            oc_c, oc_l = s5_mixer(pc[..., :C_WIDTH], pl[..., :C_WIDTH], s5_lam_re[j], s5_lam_im[j],
                                  s5_log_dt[j], s5_b_re[j], s5_b_im[j], s5_c_re[j], s5_c_im[j],
                                  s5_d[j], s5_glu_w[j], need_ctx)
            od_c, od_l = rwkv7_mixer(pc[..., C_WIDTH:], pl[..., C_WIDTH:], rwkv_mu[j], rwkv_w0[j], rwkv_w2[j],
                                     rwkv_a0[j], rwkv_a2[j], rwkv_g2[j], rwkv_k_k[j], rwkv_k_a[j],
                                     rwkv_r_k[j], rwkv_ln_g[j], rwkv_ln_b[j], need_ctx)
            yl = jnp.concatenate([oc_l, od_l], axis=-1)
            yc = jnp.concatenate([oc_c, od_c], axis=-1) if need_ctx else None
        xl = layer_norm(DEEPNORM_ALPHA * xl + gt1 * (yl @ w_out), ln_g[layer, 0], ln_b[layer, 0])
        hl = xl * (1.0 + sc2) + sh2
        if need_ctx:
            xc = layer_norm(DEEPNORM_ALPHA * xc + cgt1 * (yc @ w_out), ln_g[layer, 0], ln_b[layer, 0])
            h = jnp.concatenate([xc * (1.0 + csc2) + csh2, hl], axis=1)
        else:
            h = hl
        if layer % 2 == 0:
            f = swiglu(h, ffn_w_gate[j], ffn_w_up[j], ffn_w_down[j])
        else:
            f = moe_swiglu(h, moe_router_w[j], moe_router_b[j], moe_w_gate[j], moe_w_up[j], moe_w_down[j])
        if need_ctx:
            xc = layer_norm(DEEPNORM_ALPHA * xc + cgt2 * f[:, :Lc], ln_g[layer, 1], ln_b[layer, 1])
            f = f[:, Lc:]
        xl = layer_norm(DEEPNORM_ALPHA * xl + gt2 * f, ln_g[layer, 1], ln_b[layer, 1])
    return xl
```

```python
import contextlib, math, os
import numpy as np
import concourse.bass as bass
import concourse.mybir as mybir
from concourse.bass_utils import run_bass_kernel_spmd

F32 = mybir.dt.float32
BF16 = mybir.dt.bfloat16
F32R = mybir.dt.float32r
ALU = mybir.AluOpType
AF = mybir.ActivationFunctionType
AX = mybir.AxisListType

EPOCH = 30000
NDMA = 24


class Reg:
    __slots__ = ("w", "r", "name")

    def __init__(self, name=""):
        self.w = {}
        self.r = {}
        self.name = name


class Eng:
    def __init__(self, kb, name, self_sync):
        self.kb, self.name, self.self_sync = kb, name, self_sync
        self.ops = []
        self.seen = {}
        self.sems = [kb.new_sem(f"{name}_e0")]
        self.count = 0

    def cur(self):
        return self.sems[-1]


class KB:
    def __init__(self, nc, stack):
        self.nc, self.stack = nc, stack
        self.semh = {}
        self.nsem = 0
        self.pe = Eng(self, "pe", False)
        self.dve = Eng(self, "dve", True)
        self.act = Eng(self, "act", True)
        self.pool = Eng(self, "pool", True)
        self.sp = Eng(self, "sp", False)
        self.engs = [self.pe, self.dve, self.act, self.pool, self.sp]
        self.dma_sems = [self.new_sem(f"dma{i}") for i in range(NDMA)]
        self.dma_tot = [0] * NDMA
        self.dma_i = 0
        self.n_ops = 0

    def new_sem(self, name):
        h = self.stack.enter_context(self.nc.semaphore(name))
        k = self.nsem
        self.nsem += 1
        self.semh[k] = h
        return k

    def _waits(self, E, reads, writes):
        need = {}
        for r in reads:
            for s, v in r.w.items():
                if need.get(s, 0) < v:
                    need[s] = v
        for w in writes:
            for d in (w.w, w.r):
                for s, v in d.items():
                    if need.get(s, 0) < v:
                        need[s] = v
        out = []
        for s, v in need.items():
            if (not E.self_sync) and s in E.sems:
                continue
            if E.seen.get(s, 0) < v:
                E.seen[s] = v
                out.append((s, v))
        return out

    def _mark(self, ev, reads, writes):
        s, v = ev
        for r in reads:
            r.r[s] = v
        for w in writes:
            w.w = {s: v}
            w.r = {}

    def op(self, E, fn, reads=(), writes=()):
        waits = self._waits(E, reads, writes)
        if E.count >= EPOCH:
            E.sems.append(self.new_sem(f"{E.name}_e{len(E.sems)}"))
            E.count = 0
        E.count += 1
        ev = (E.cur(), E.count)
        E.ops.append((waits, fn, ev[0], 1))
        self._mark(ev, reads, writes)
        self.n_ops += 1
        return ev

    def dma(self, Q, out, in_, reads=(), writes=(), **kw):
        i = self.dma_i
        self.dma_i = (i + 1) % NDMA
        s = self.dma_sems[i]
        waits = self._waits(Q, reads, writes)
        if self.dma_tot[i] > 0 and Q.seen.get(s, 0) < self.dma_tot[i]:
            Q.seen[s] = self.dma_tot[i]
            waits.append((s, self.dma_tot[i]))
        self.dma_tot[i] += 16
        ev = (s, self.dma_tot[i])
        Q.ops.append((waits, lambda e: e.dma_start(out=out, in_=in_, **kw), s, 16))
        self._mark(ev, reads, writes)
        self.n_ops += 1
        return ev

    def emit(self):
        nc = self.nc
        fin = [(self.dma_sems[i], self.dma_tot[i]) for i in range(NDMA) if self.dma_tot[i] > 0]
        semh = self.semh

        def run(E, e):
            for waits, fn, s, inc in E.ops:
                for ws, wv in waits:
                    e.wait_ge(semh[ws], wv)
                inst = fn(e)
                inst.then_inc(semh[s], inc)

        with nc.Block() as block:
            @block.tensor
            def _(e):
                run(self.pe, e)

            @block.vector
            def _(e):
                run(self.dve, e)

            @block.scalar
            def _(e):
                run(self.act, e)

            @block.gpsimd
            def _(e):
                run(self.pool, e)

            @block.sync
            def _(e):
                run(self.sp, e)
                for ws, wv in fin:
                    e.wait_ge(semh[ws], wv)


class _LazyW:
    def __init__(self, prog, shapes):
        self.p, self.shapes, self.c = prog, shapes, {}

    def __getitem__(self, k):
        if k not in self.c:
            self.c[k] = self.p.inp(k, self.shapes[k])
        return self.c[k]


T = 2304; LC = 256; NT = 18; D = 1024; KC = 8; CH = 32; NCH = 72; NG = 10
NTILES = [(0, 256), (256, 512), (768, 512), (1280, 512), (1792, 512)]
ALPHA = 8 ** 0.25
DFF = 2816; NFC = 22


def host_consts():
    c = {}
    c["ident"] = np.eye(128, dtype=np.float32)
    s = np.arange(128)[:, None]; t = np.arange(128)[None, :]
    same = (s // CH) == (t // CH)
    c["tri_le"] = (same & (s <= t)).astype(np.float32)
    c["tri_ge"] = (same & (s >= t)).astype(np.float32)
    tf = np.arange(T, dtype=np.float32)
    tb = np.concatenate([255.0 - np.arange(256), 256.0 + (2303.0 - np.arange(256, T))]).astype(np.float32)
    c["tauF"] = np.ascontiguousarray(np.broadcast_to(tf, (128, T))); c["tauB"] = np.ascontiguousarray(np.broadcast_to(tb, (128, T)))
    a_ = np.arange(128)[:, None]; b_ = np.arange(128)[None, :]
    mf = np.stack([(a_ < b_), (a_ <= b_), (b_ < a_)], 1).astype(np.float32)
    mb = np.stack([(a_ > b_), (a_ >= b_), (b_ > a_)], 1).astype(np.float32)
    c["rw_masks"] = np.ascontiguousarray(np.stack([mf, mb], 0))
    c["bdones"] = ((a_ // 64) == (b_ // 64)).astype(np.float32)
    c["halfm"] = (np.arange(128)[:, None] // 64 == np.arange(2)[None, :]).astype(np.float32)
    c["rowmask"] = (np.arange(128)[:, None] // CH == np.arange(4)[None, :]).astype(np.float32)
    kk = np.arange(128)[:, None]; qq = np.arange(128)[None, :]
    c["mprev4"] = (kk >= qq).astype(np.float32)
    c["mnext4"] = (kk <= qq).astype(np.float32)
    rm = np.ones((128, T), np.float32); rm[:, ::CH] = 0.0
    c["resetm"] = rm
    rows = 2048 // 64
    row = np.repeat(np.arange(rows, dtype=np.float32), 64); col = np.tile(np.arange(64, dtype=np.float32), rows)
    inv = (np.float32(10000.0) ** (-np.arange(16, dtype=np.float32) / np.float32(16))).astype(np.float32)
    ang = np.concatenate([row[:, None] * inv, col[:, None] * inv], axis=-1).astype(np.float32)
    c["cosF"] = np.ascontiguousarray(np.concatenate([np.cos(ang), np.cos(ang)], -1).T.astype(np.float32))
    c["sinF"] = np.ascontiguousarray(np.concatenate([np.sin(ang), np.sin(ang)], -1).T.astype(np.float32))
    pm = np.zeros((64, 64), np.float32)
    for r in range(32):
        pm[r + 32, r] = -1.0
        pm[r, r + 32] = 1.0
    pm2 = np.zeros((128, 128), np.float32); pm2[:64, :64] = pm; pm2[64:, 64:] = pm
    c["Pm2"] = pm2
    c["cosF"] = np.ascontiguousarray(np.concatenate([c["cosF"], c["cosF"]], 0))
    c["sinF"] = np.ascontiguousarray(np.concatenate([c["sinF"], c["sinF"]], 0))
    return c


class Buf:
    def __init__(self, t, name):
        self.t = t; self.r = Reg(name)

    def __getitem__(self, i):
        return self.t[i]


class Prog:
    def __init__(self, layers, taps=(), stop=99):
        self.layers = layers; self.taps = set(taps); self.stop = stop
        self.nc = bass.Bass("TRN2", target_bir_lowering=False)
        self.st = contextlib.ExitStack()
        self.kb = KB(self.nc, self.st)
        self.din = {}; self.dout = {}
        self.psi = 0

    def inp(self, name, shape, dt=F32):
        a = self.nc.dram_tensor(name, list(shape), dt, kind="ExternalInput").ap()
        self.din[name] = a
        return a

    def outp(self, name, shape, dt=F32):
        a = self.nc.dram_tensor(name, list(shape), dt, kind="ExternalOutput").ap()
        self.dout[name] = a
        return a

    def scratch(self, name, shape, dt=F32):
        if name in self.taps:
            return Buf(self.outp(name, shape, dt), name)
        return Buf(self.nc.dram_tensor(name, list(shape), dt, kind="Internal").ap(), name)

    def sb(self, name, shape, dt=F32):
        return Buf(self.st.enter_context(self.nc.sbuf_tensor(name, list(shape), dt)), name)

    def next_ps(self):
        b = self.ps[self.psi]; self.psi = (self.psi + 1) % 8
        return b

    def _rw(self, R, W):
        return [b.r for b in R], [b.r for b in W]

    def MM(self, out, lhsT, rhs, R, W, start=True, stop=True):
        r, w = self._rw(R, W)
        self.kb.op(self.kb.pe, lambda e: e.matmul(out, lhsT=lhsT, rhs=rhs, start=start, stop=stop), r, w)

    def TR(self, out, in_, R, W, n=128):
        r, w = self._rw(R + [self.ident], W)
        idn = self.ident[0:n, 0:n]
        self.kb.op(self.kb.pe, lambda e: e.transpose(out=out, in_=in_, identity=idn), r, w)

    def ACT(self, out, in_, func, R, W, bias=None, scale=None):
        r, w = self._rw(R, W)
        kw = {}
        if bias is not None: kw["bias"] = bias
        if scale is not None: kw["scale"] = scale
        self.kb.op(self.kb.act, lambda e: e.activation(out=out, in_=in_, func=func, **kw), r, w)

    def TT(self, out, a, b, op, R, W, eng=None):
        r, w = self._rw(R, W)
        self.kb.op(eng or self.kb.dve, lambda e: e.tensor_tensor(out=out, in0=a, in1=b, op=op), r, w)

    def TS(self, out, a, s1, op0, R, W, s2=None, op1=None, eng=None):
        r, w = self._rw(R, W)
        if op1 is None:
            self.kb.op(eng or self.kb.dve, lambda e: e.tensor_scalar(out=out, in0=a, scalar1=s1, scalar2=None, op0=op0), r, w)
        else:
            self.kb.op(eng or self.kb.dve, lambda e: e.tensor_scalar(out=out, in0=a, scalar1=s1, scalar2=s2, op0=op0, op1=op1), r, w)

    def STT(self, out, a, s, b, op0, op1, R, W):
        r, w = self._rw(R, W)
        self.kb.op(self.kb.dve, lambda e: e.scalar_tensor_tensor(out=out, in0=a, scalar=s, in1=b, op0=op0, op1=op1), r, w)

    def CP(self, out, in_, R, W, eng=None):
        r, w = self._rw(R, W)
        E = eng or self.kb.dve
        if E is self.kb.act:
            self.kb.op(E, lambda e: e.copy(out=out, in_=in_), r, w)
        else:
            self.kb.op(E, lambda e: e.tensor_copy(out=out, in_=in_), r, w)

    def MSET(self, ap, val, W, eng=None):
        r, w = self._rw([], W)
        self.kb.op(eng or self.kb.pool, lambda e: e.memset(ap, val), r, w)

    def RECIP(self, out, in_, R, W):
        r, w = self._rw(R, W)
        self.kb.op(self.kb.dve, lambda e: e.reciprocal(out=out, in_=in_), r, w)

    def SCAN(self, out, d0, d1, R, W, init=0.0):
        r, w = self._rw(R, W)
        self.kb.op(self.kb.dve, lambda e: e.tensor_tensor_scan(out=out, data0=d0, data1=d1, initial=init, op0=ALU.mult, op1=ALU.add), r, w)

    def DMA(self, out, in_, R, W, q=None, **kw):
        r, w = self._rw(R, W)
        self.kb.dma(q or self.kb.sp, out, in_, r, w, **kw)

    def tap(self, name, src_ap, R, shape):
        if name in self.taps:
            o = self.outp("tap_" + name, shape)
            self.DMA(o, src_ap, R, [])

    def setup(self):
        P = self
        nc = self.nc
        P.xin = Buf(P.inp("xin", [T, D]), "xin")
        P.condT = P.inp("condT", [128, 8, 2])
        hc = host_consts()
        P.cd = {k: Buf(P.inp("c_" + k, v.shape), "c_" + k) for k, v in hc.items()}
        I = lambda name, shape: (name, shape)
        wdecl = dict(
            ada_w=I("ada_w", [4, D, 6 * D]), ada_b_fm=I("ada_b_fm", [4, 128, 48]),
            ln_g=I("ln_g", [4, 2, D]), ln_b=I("ln_b", [4, 2, D]),
            ev_w_in=I("ev_w_in", [2, D, 3328]), ev_w_out=I("ev_w_out", [2, D, D]),
            hg_lb_fm=I("hg_lb_fm", [128, 4, 2]), hg_ng_fm=I("hg_ng_fm", [2, 128, 4]), attn_sink=I("attn_sink", [2, 8]),
            od_w_in=I("od_w_in", [2, D, 2432]), od_w_out=I("od_w_out", [2, D, D]),
            s5_lre=I("s5_lre", [2, 2, 128, 16]), s5_lim=I("s5_lim", [2, 2, 128, 16]), s5_ldt=I("s5_ldt", [2, 2, 128, 16]),
            s5_bT=I("s5_bT", [2, 128, 16, 2, 16]), s5_cT=I("s5_cT", [2, 128, 16, 2, 16]), s5_d_fm=I("s5_d_fm", [2, 128, 4]),
            s5_glu_w=I("s5_glu_w", [2, 512, 512]),
            rw_mu_fm=I("rw_mu_fm", [2, 128, 15]), rw_w0_fm=I("rw_w0_fm", [2, 2, 128, 4]), rw_a0_fm=I("rw_a0_fm", [2, 2, 128, 4]),
            rw_w2pad=I("rw_w2pad", [2, 2, 128, 512]), rw_a2pad=I("rw_a2pad", [2, 2, 128, 512]), rw_g2=I("rw_g2", [2, 128, 512]),
            rw_kk_fm=I("rw_kk_fm", [2, 128, 4]), rw_ka_fm=I("rw_ka_fm", [2, 128, 4]), rw_rk_fm=I("rw_rk_fm", [2, 128, 4]),
            rw_lng=I("rw_lng", [2, 512]), rw_lnb=I("rw_lnb", [2, 512]),
            moe_router_w=I("moe_router_w", [2, D, 8]), moe_router_b=I("moe_router_b", [2, 8]),
            moe_w_gate=I("moe_w_gate", [2, 8, D, DFF]), moe_w_up=I("moe_w_up", [2, 8, D, DFF]), moe_w_down=I("moe_w_down", [2, 8, DFF, D]),
            ffn_w_gate=I("ffn_w_gate", [2, D, DFF]), ffn_w_up=I("ffn_w_up", [2, D, DFF]), ffn_w_down=I("ffn_w_down", [2, DFF, D]),
        )
        P.w = _LazyW(P, {k: v[1] for k, v in wdecl.items()})
        P.out = Buf(P.outp("out", [T, D]), "out")
        P.xs = [P.scratch("xs0", [T, D]), P.scratch("xs1", [T, D])]
        if "inject_y" in P.taps:
            P.yT = Buf(P.inp("yT_inject", [D, T]), "yT_inject")
        else:
            P.yT = P.scratch("yTd", [D, T])
        P.ident = P.sb("ident", [128, 128]); P.ones = P.sb("ones", [128, 128]); P.onesdiv = P.sb("onesdiv", [128, 128])
        P.tri_le = P.sb("tri_le", [128, 128]); P.tri_ge = P.sb("tri_ge", [128, 128])
        P.mprev4 = P.sb("mprev4", [128, 128]); P.mnext4 = P.sb("mnext4", [128, 128])
        P.cst = P.sb("cst", [128, 8])
        P.hT = P.sb("hT", [128, KC, T], BF16)
        P.G = [None] * NG
        P.Gt = self.st.enter_context(nc.sbuf_tensor("G", [128, NG, T], F32))
        for i in range(NG):
            P.G[i] = Buf(P.Gt[:, i, :], f"G{i}")
        P.wA = [P.sb(f"wA{i}", [128, KC, 128], BF16) for i in range(5)]
        P.wAi = 0
        P.xb = [P.sb(f"xb{i}", [128, D]) for i in range(3)]
        P.xbi = 0
        P.zb = [P.sb("zb0", [128, D])] * 2
        P.bc = [P.sb(f"bc{i}", [128, D]) for i in range(4)]
        P.condS = P.sb("condS", [128, 8, 2]); P.adab = P.sb("adab", [128, 48])
        P.fm = P.sb("fm", [128, 48, 2]); P.sc1p = P.sb("sc1p", [128, 8, 2]); P.sc2p = P.sb("sc2p", [128, 8, 2])
        P.small = [P.sb(f"small{i}", [128, 512]) for i in range(5)]
        P.smi = 0
        P.S = P.sb("S", [128, 128]); P.tmpS = P.sb("tmpS", [128, 128])
        P.stat = P.sb("stat", [128, 32])
        P.s5t = P.sb("s5t", [128, 2, 12, 16]); P.s5i = P.sb("s5i", [128, 16], mybir.dt.int32); P.s5d = P.sb("s5d", [128, 4])
        P.s5bc = P.sb("s5bc", [128, 64]); P.s5zc = P.sb("s5zc", [128, 256])
        P.rwm = P.sb("rwm", [128, 3, 128]); P.bdones = P.sb("bdones", [128, 128])
        P.rwar = [P.sb(f"rwar{h}", [128, 256]) for h in range(2)]
        P.rwxm = [P.sb(f"rwxm{h}", [128, 4, 128]) for h in range(2)]
        P.rwxz = [P.sb(f"rwxz{h}", [128, 3, 128]) for h in range(2)]
        P.rwu = P.sb("rwu", [128, 128]); P.rwp = P.sb("rwp", [128, 48]); P.rwgc = P.sb("rwgc", [128, NT]); P.rwln = P.sb("rwln", [128, 2, 128])
        P.gates = P.sb("gates", [128, NT, 8]); P.rw = P.sb("rw", [128, KC, 8]); P.rb = P.sb("rb", [128, 8])
        P.h32 = [P.sb("h32_0", [128, 4, 128])] * 2; P.rt = P.sb("rt", [128, 64])
        P.lbt = P.sb("lbt", [128, 16]); P.ngt = P.sb("ngt", [128, 4]); P.sk = P.sb("sk", [128, 8])
        P.Pm2 = P.sb("Pm2", [128, 128])
        P.attm = [P.sb(f"attm{i}", [128, 128]) for i in range(2)]; P.attmi = 0
        P.ktok = [P.sb(f"ktok{i}", [128, 4, 128]) for i in range(2)]
        P.rowmask = P.sb("rowmask", [128, 4]); P.halfm = P.sb("halfm", [128, 2])
        P.ps = [Buf(self.st.enter_context(nc.psum_tensor(f"ps{i}", [128, 512], F32)), f"ps{i}") for i in range(8)]
        for k, dst in (("ident", P.ident), ("tri_le", P.tri_le), ("tri_ge", P.tri_ge), ("mprev4", P.mprev4),
                       ("mnext4", P.mnext4), ("Pm2", P.Pm2), ("rowmask", P.rowmask), ("halfm", P.halfm), ("bdones", P.bdones)):
            P.DMA(dst[:], P.cd[k][:], [], [dst])
        P.MSET(P.ones[:], 1.0, [P.ones]); P.MSET(P.onesdiv[:], 1.0 / 128.0, [P.onesdiv])
        P.MSET(P.cst[:, 0:1], 1e-6, [P.cst]); P.MSET(P.cst[:, 1:2], 1e-5, [P.cst]); P.MSET(P.cst[:, 2:3], 64e-5, [P.cst])
        P.MSET(P.cst[:, 3:4], 0.0, [P.cst]); P.MSET(P.cst[:, 4:5], 1.0, [P.cst])
        P.DMA(P.condS[:], P.condT, [], [P.condS])
        P.ACT(P.condS[:], P.condS[:], AF.Silu, [P.condS], [P.condS])

    def gflat(self, slot0, nelem, dt=F32):
        flat = self.Gt[:].rearrange("p a n -> p (a n)")[:, slot0 * T:slot0 * T + nelem]
        ns = (nelem + T - 1) // T
        regs = [self.G[slot0 + i] for i in range(ns)]
        if dt is not F32:
            flat = flat.bitcast(dt)
        return flat, regs

    def sm(self):
        b = self.small[self.smi]; self.smi = (self.smi + 1) % len(self.small)
        return b

    def loadw(self, src, ncols, kc=KC):
        b = self.wA[self.wAi]; self.wAi = (self.wAi + 1) % len(self.wA)
        self.DMA(b[:, 0:kc, 0:ncols], src.rearrange("(c p) n -> p c n", p=128), [], [b], q=self.kb.pool)
        return b

    def phase_mod(self, l):
        P = self
        P.DMA(P.adab[:], P.w["ada_b_fm"][l], [], [P.adab])
        pM = P.next_ps()
        for blk in range(12):
            fl, regs = P.gflat((blk % 2) * 2, 4096)
            stg = fl.rearrange("p (c n) -> p c n", n=512)
            for ch in range(8):
                P.DMA(stg[:, ch, :], P.w["ada_w"][l, ch * 128:(ch + 1) * 128, blk * 512:(blk + 1) * 512], [], regs,
                      q=(P.kb.sp if ch % 2 == 0 else P.kb.act))
            for s in range(4):
                k = blk * 4 + s
                for ch in range(8):
                    P.MM(pM[:, 2 * k:2 * k + 2], stg[:, ch, s * 128:(s + 1) * 128], P.condS[:, ch, :], regs + [P.condS], [pM],
                         start=(ch == 0), stop=(ch == 7))
        for cond in range(2):
            P.TT(P.fm[:, :, cond], pM[:, cond:96:2], P.adab[:], ALU.add, [pM, P.adab], [P.fm])
        P.TS(P.sc1p[:], P.fm[:, 8:16, :], 1.0, ALU.add, [P.fm], [P.sc1p])
        P.TS(P.sc2p[:], P.fm[:, 32:40, :], 1.0, ALU.add, [P.fm], [P.sc2p])

    def gate_bcast(self, q, cond, dst):
        P = self
        for half in range(2):
            pg = P.next_ps()
            for cc in range(4):
                c = half * 4 + cc
                dg = P.sm()
                P.TS(dg[:, 0:128], P.ident[:], P.fm[:, q * 8 + c, cond:cond + 1], ALU.mult, [P.ident, P.fm], [dg])
                P.MM(pg[:, cc * 128:(cc + 1) * 128], P.ones[:], dg[:, 0:128], [P.ones, dg], [pg])
            P.CP(dst[:, half * 512:(half + 1) * 512], pg[:, :], [pg], [dst], eng=P.kb.act)

    def hT_tile(self, xt, ti, scp, shq, router=False):
        P = self
        cond = 1 if ti < 2 else 0
        pl = P.next_ps() if router else None
        for half in range(2):
            pt = P.next_ps()
            for cc in range(4):
                c = half * 4 + cc
                P.TR(pt[:, cc * 128:(cc + 1) * 128], xt[:, c * 128:(c + 1) * 128], [xt], [pt])
            h32 = P.h32[half]
            for cc in range(4):
                c = half * 4 + cc
                if router:
                    P.TS(h32[:, cc, :], pt[:, cc * 128:(cc + 1) * 128], scp[:, c, cond:cond + 1], ALU.mult, [pt, scp, P.fm], [h32],
                         s2=P.fm[:, shq * 8 + c, cond:cond + 1], op1=ALU.add)
                    P.CP(P.hT[:, c, ti * 128:(ti + 1) * 128], h32[:, cc, :], [h32], [P.hT], eng=P.kb.act)
                else:
                    P.ACT(P.hT[:, c, ti * 128:(ti + 1) * 128], pt[:, cc * 128:(cc + 1) * 128], AF.Identity,
                          [pt, scp, P.fm], [P.hT], scale=scp[:, c, cond:cond + 1], bias=P.fm[:, shq * 8 + c, cond:cond + 1])
            if router:
                for cc in range(4):
                    c = half * 4 + cc
                    P.MM(pl[:, half * 8:half * 8 + 8], h32[:, cc, :], P.rw[:, c, :], [h32, P.rw], [pl], start=(cc == 0), stop=(cc == 3))
        if router and "1" != "2":
            P.top2(pl, ti)

    def top2(self, pl, ti):
        P = self
        rt = P.rt
        R_, W_ = [rt], [rt]
        lg, e1, l2, e2 = rt[:, 0:8], rt[:, 8:16], rt[:, 16:24], rt[:, 24:32]
        m1, m2, dd, p1, p2 = rt[:, 32:33], rt[:, 33:34], rt[:, 34:35], rt[:, 35:36], rt[:, 36:37]
        P.TT(lg, pl[:, 0:8], P.rb[:], ALU.add, [pl, P.rb], W_)
        P.TT(lg, lg, pl[:, 8:16], ALU.add, [pl, rt], W_)
        P.kb.op(P.kb.dve, lambda e: e.reduce_max(out=m1, in_=lg, axis=AX.X), [rt.r], [rt.r])
        P.TS(e1, lg, m1, ALU.is_equal, R_, W_)
        P.STT(l2, e1, -1e30, lg, ALU.mult, ALU.add, R_, W_)
        P.kb.op(P.kb.dve, lambda e: e.reduce_max(out=m2, in_=l2, axis=AX.X), [rt.r], [rt.r])
        P.TS(e2, l2, m2, ALU.is_equal, R_, W_)
        P.TT(dd, m2, m1, ALU.subtract, R_, W_)
        P.ACT(dd, dd, AF.Exp, R_, W_)
        P.TS(p1, dd, 1.0, ALU.add, R_, W_)
        P.RECIP(p1, p1, R_, W_)
        P.TT(p2, dd, p1, ALU.mult, R_, W_)
        P.TS(e1, e1, p1, ALU.mult, R_, W_)
        P.STT(P.gates[:, ti, :], e2, p2, e1, ALU.mult, ALU.add, R_, [P.gates])

    def phase_hT(self, xsrc):
        P = self
        for ti in range(NT):
            xt = P.xb[P.xbi]; P.xbi = (P.xbi + 1) % 3
            P.DMA(xt[:], xsrc[ti * 128:(ti + 1) * 128, :], [xsrc], [xt])
            P.hT_tile(xt, ti, P.sc1p, 0)

    def proj_fm(self, wb, M, evac, col0=0):
        P = self
        for (t0, n) in NTILES:
            pb = P.next_ps()
            for c in range(KC):
                P.MM(pb[0:M, 0:n], wb[:, c, col0:col0 + M], P.hT[:, c, t0:t0 + n], [wb, P.hT], [pb], start=(c == 0), stop=(c == 7))
            evac(pb, t0, n)

    def hgrn2(self, l, j):
        P = self
        G = P.G
        W = P.w["ev_w_in"][j]
        lbt = P.lbt; ngt = P.ngt
        P.DMA(ngt[:, 0:4], P.w["hg_ng_fm"][j], [], [ngt])
        if j == 0:
            P.MSET(lbt[:, 0:4], 0.0, [lbt]); P.MSET(lbt[:, 4:8], 1.0, [lbt])
        else:
            P.DMA(lbt[:, 8:16], P.w["hg_lb_fm"].rearrange("p h j -> p (h j)"), [], [lbt])
            P.TT(lbt[:, 0:4], lbt[:, 9:16:2], lbt[:, 8:16:2], ALU.subtract, [lbt], [lbt])
            P.ACT(lbt[:, 0:4], lbt[:, 0:4], AF.Sigmoid, [lbt], [lbt])
            P.TS(lbt[:, 4:8], lbt[:, 0:4], -1.0, ALU.mult, [lbt], [lbt], s2=1.0, op1=ALU.add)
        qT, sgT, X1, X2, X3, X4, oacc = G[0], G[1], G[2], G[3], G[4], G[5], G[8]
        P.resetm = G[9]
        P.DMA(P.resetm[:], P.cd["resetm"][:], [], [P.resetm])
        itok = P.Gt[:, 6, :].rearrange("p (b c) -> p b c", c=128)
        itr = [G[6]]
        for hd in range(4):
            cs = lambda k: W[:, k * 512 + hd * 128:k * 512 + (hd + 1) * 128]
            wq = P.loadw(cs(0), 128)
            P.proj_fm(wq, 128, lambda pb, t0, n: P.ACT(qT[:, t0:t0 + n], pb[:, 0:n], AF.Silu, [pb], [qT]))
            wg = P.loadw(cs(4), 128)
            P.proj_fm(wg, 128, lambda pb, t0, n: P.ACT(sgT[:, t0:t0 + n], pb[:, 0:n], AF.Silu, [pb], [sgT]))
            wi = P.loadw(cs(3), 128)
            for b0 in range(0, NT, 4):
                nb = min(4, NT - b0)
                pi = P.next_ps()
                for q in range(nb):
                    ti = b0 + q
                    for c in range(KC):
                        P.MM(pi[:, q * 128:(q + 1) * 128], P.hT[:, c, ti * 128:(ti + 1) * 128], wi[:, c, :], [P.hT, wi], [pi],
                             start=(c == 0), stop=(c == 7))
                P.CP(itok[:, b0:b0 + nb, :], pi[:, 0:nb * 128].rearrange("p (a b) -> p a b", b=128), [pi], itr, eng=P.kb.act)
            for d in range(2):
                fwd = d == 0
                wf = P.loadw(cs(1 + d), 128)
                P.proj_fm(wf, 128, lambda pb, t0, n: P.ACT(X1[:, t0:t0 + n], pb[:, 0:n], AF.Sigmoid, [pb], [X1]))
                P.TS(X1[:], X1[:], lbt[:, 4 + hd:5 + hd], ALU.mult, [X1, lbt], [X1], s2=lbt[:, hd:hd + 1], op1=ALU.add)
                P.TS(X2[:], X1[:], -1.0, ALU.mult, [X1], [X2], s2=1.0, op1=ALU.add, eng=P.kb.pool)
                P.ACT(X1[:], X1[:], AF.Ln, [X1], [X1])
                if fwd:
                    P.SCAN(X3[:], P.resetm[:], X1[:], [P.resetm, X1], [X3])
                else:
                    P.SCAN(X3[:, ::-1], P.resetm[:], X1[:, ::-1], [P.resetm, X1], [X3])
                P.ACT(X1[:], X3[:], AF.Exp, [X3], [X1])
                P.STT(X4[:], qT[:], 128.0 ** -0.5, X1[:], ALU.mult, ALU.mult, [qT, X1], [X4])
                P.ACT(X3[:], X3[:], AF.Exp, [X3], [X3], scale=-1.0)
                P.TT(X2[:], X2[:], X3[:], ALU.mult, [X2, X3], [X2], eng=P.kb.pool)
                P.MSET(P.S[:], 0.0, [P.S])
                tiles = list(range(NT)) if fwd else [1, 0] + list(range(NT - 1, 1, -1))
                mask = P.tri_le if fwd else P.tri_ge
                for ti in tiles:
                    tsl = slice(ti * 128, (ti + 1) * 128)
                    pA = P.next_ps()
                    P.MM(pA[:, 0:128], X2[:, tsl], X4[:, tsl], [X2, X4], [pA])
                    attm = P.attm[P.attmi]; ktok = P.ktok[P.attmi]; P.attmi ^= 1
                    P.TT(attm[:], pA[:, 0:128], mask[:], ALU.mult, [pA, mask], [attm])
                    pB = P.next_ps()
                    P.TR(pB[:, 0:128], X2[:, tsl], [X2], [pB])
                    for q in range(4):
                        P.ACT(ktok[:, q, :], pB[:, 0:128], AF.Identity, [pB, P.rowmask], [ktok], scale=P.rowmask[:, q:q + 1])
                    pC = P.next_ps()
                    P.MM(pC[:, 0:128], itok[:, ti, :], attm[:], itr + [attm], [pC], start=True, stop=False)
                    for q in (range(4) if fwd else range(3, -1, -1)):
                        c0 = ti * 128 + q * CH
                        csl = slice(c0, c0 + CH); prt = slice(q * CH, (q + 1) * CH)
                        P.MM(pC[:, q * CH:(q + 1) * CH], P.S[:], X4[:, csl], [P.S, X4], [pC], start=False, stop=True)
                        pD = P.next_ps()
                        P.MM(pD[:, 0:128], ktok[:, q, :], itok[:, ti, :], [ktok] + itr, [pD])
                        P.TT(P.tmpS[:], pD[:, 0:128], P.S[:], ALU.add, [pD, P.S], [P.tmpS])
                        last = c0 + CH - 1 if fwd else c0
                        P.ACT(P.S[:], P.tmpS[:], AF.Identity, [P.tmpS, X1], [P.S], scale=X1[:, last:last + 1])
                    if fwd:
                        P.CP(oacc[:, tsl], pC[:, 0:128], [pC], [oacc], eng=P.kb.act)
                    else:
                        P.TT(oacc[:, tsl], oacc[:, tsl], pC[:, 0:128], ALU.add, [oacc, pC], [oacc])
            for (t0, n) in NTILES:
                sq = P.sm()
                P.ACT(sq[:, 0:n], oacc[:, t0:t0 + n], AF.Square, [oacc], [sq])
                pE = P.next_ps()
                P.MM(pE[:, 0:n], P.onesdiv[:], sq[:, 0:n], [P.onesdiv, sq], [pE])
                rs = P.sm()
                P.ACT(rs[:, 0:n], pE[:, 0:n], AF.Sqrt, [pE, P.cst], [rs], bias=P.cst[:, 0:1])
                P.RECIP(rs[:, 0:n], rs[:, 0:n], [rs], [rs])
                yo = P.sm()
                P.STT(yo[:, 0:n], oacc[:, t0:t0 + n], ngt[:, hd:hd + 1], rs[:, 0:n], ALU.mult, ALU.mult, [oacc, ngt, rs], [yo])
                P.TT(yo[:, 0:n], yo[:, 0:n], sgT[:, t0:t0 + n], ALU.mult, [yo, sgT], [yo], eng=P.kb.pool)
                P.DMA(P.yT[hd * 128:(hd + 1) * 128, t0:t0 + n], yo[:, 0:n], [yo], [P.yT])

    def attention(self, l, j):
        P = self
        G = P.G
        W = P.w["ev_w_in"][j]
        sk = P.sk
        P.DMA(sk[:, 0:8], P.w["attn_sink"][j].partition_broadcast(128), [], [sk])
        P.ACT(sk[:, 0:8], sk[:, 0:8], AF.Exp, [sk], [sk])
        cosT, sinT, kT2, q2 = G[0], G[1], G[2], G[3]
        qm = [G[4], G[5], G[6], G[7]]
        ptb = [(P.Gt[:, 8, i * 512:(i + 1) * 512], G[8]) for i in range(4)] + [(P.Gt[:, 9, 0:512], G[9])]
        vaug = P.Gt[:, 9, 512:512 + NT * 65].rearrange("p (t e) -> p t e", e=65)
        vreg = [G[9]]
        P.DMA(cosT[:, 0:2048], P.cd["cosF"][:], [], [cosT]); P.DMA(sinT[:, 0:2048], P.cd["sinF"][:], [], [sinT])

        def rope(src):
            for (t0, n) in NTILES[1:]:
                pr = P.next_ps()
                P.MM(pr[:, 0:n], P.Pm2[:], src[:, t0:t0 + n], [P.Pm2, src], [pr])
                t1 = P.sm(); t2 = P.sm()
                P.TT(t1[:, 0:n], src[:, t0:t0 + n], cosT[:, t0 - LC:t0 - LC + n], ALU.mult, [src, cosT], [t1])
                P.TT(t2[:, 0:n], pr[:, 0:n], sinT[:, t0 - LC:t0 - LC + n], ALU.mult, [pr, sinT], [t2])
                P.TT(src[:, t0:t0 + n], t1[:, 0:n], t2[:, 0:n], ALU.add, [t1, t2], [src], eng=P.kb.pool)

        for g in range(2):
            wv = P.loadw(W[:, 3200 + g * 64:3200 + (g + 1) * 64], 64)
            P.MSET(vaug[:, :, 64:65], 1.0, vreg)
            for ti in range(NT):
                pv = P.next_ps()
                for c in range(KC):
                    P.MM(pv[:, 0:64], P.hT[:, c, ti * 128:(ti + 1) * 128], wv[:, c, 0:64], [P.hT, wv], [pv], start=(c == 0), stop=(c == 7))
                P.CP(vaug[:, ti, 0:64], pv[:, 0:64], [pv], vreg, eng=P.kb.act)
            wk = P.wA[P.wAi]; P.wAi = (P.wAi + 1) % len(P.wA)
            ksrc = W[:, 3072 + g * 64:3072 + (g + 1) * 64].rearrange("(c p) n -> p c n", p=128)
            P.DMA(wk[:, :, 0:64], ksrc, [], [wk], q=P.kb.pool)
            P.DMA(wk[:, :, 64:128], ksrc, [], [wk], q=P.kb.pool)
            P.proj_fm(wk, 128, lambda pb, t0, n: P.CP(kT2[:, t0:t0 + n], pb[:, 0:n], [pb], [kT2], eng=P.kb.act))
            rope(kT2)
            for pair in range(2):
                h0 = g * 4 + pair * 2
                wq = P.loadw(W[:, 2560 + h0 * 64:2560 + (h0 + 2) * 64], 128)
                P.proj_fm(wq, 128, lambda pb, t0, n: P.CP(q2[:, t0:t0 + n], pb[:, 0:n], [pb], [q2], eng=P.kb.act))
                rope(q2)
                for half in range(2):
                    dst = qm[pair * 2 + half]
                    P.TS(dst[:], q2[:], P.halfm[:, half:half + 1], ALU.mult, [q2, P.halfm], [dst], eng=(P.kb.pool if half else P.kb.dve))
            for ti in range(NT):
                if ti < 2:
                    keys = [(0, 'c'), (1, 'c')]
                else:
                    keys = [(kt, kd) for kt, kd in ((ti - 1, 'p'), (ti, 's'), (ti + 1, 'n')) if 2 <= kt < NT] + [(0, 'c'), (1, 'c')]
                pts = []
                for ki, (kt, kd) in enumerate(keys):
                    pS = P.next_ps()
                    for hh in range(4):
                        P.MM(pS[:, hh * 128:(hh + 1) * 128], kT2[:, kt * 128:(kt + 1) * 128], qm[hh][:, ti * 128:(ti + 1) * 128],
                             [kT2, qm[hh]], [pS])
                    pt, pr_ = ptb[ki]
                    P.ACT(pt, pS[:, 0:512], AF.Exp, [pS], [pr_], scale=0.125)
                    if kd in ('p', 'n'):
                        mk_ = P.mprev4 if kd == 'p' else P.mnext4
                        for hh in range(4):
                            P.TT(pt[:, hh * 128:(hh + 1) * 128], pt[:, hh * 128:(hh + 1) * 128], mk_[:], ALU.mult, [pr_, mk_], [pr_],
                                 eng=(P.kb.pool if hh % 2 else P.kb.dve))
                    pts.append((pt, pr_, kt))
                pO = P.next_ps()
                for hh in range(4):
                    for ki, (pt, pr_, kt) in enumerate(pts):
                        P.MM(pO[:, hh * 65:(hh + 1) * 65], pt[:, hh * 128:(hh + 1) * 128], vaug[:, kt, :], [pr_] + vreg, [pO],
                             start=(ki == 0), stop=(ki == len(pts) - 1))
                den = P.sm()
                P.TT(den[:, 0:4], pO[:, 64:260:65], sk[:, g * 4:(g + 1) * 4], ALU.add, [pO, sk], [den])
                P.RECIP(den[:, 0:4], den[:, 0:4], [den], [den])
                ob = P.sm()
                for hh in range(4):
                    P.TS(ob[:, hh * 64:(hh + 1) * 64], pO[:, hh * 65:hh * 65 + 64], den[:, hh:hh + 1], ALU.mult, [pO, den], [ob])
                for half in range(2):
                    pT = P.next_ps()
                    P.TR(pT[:, 0:128], ob[:, half * 128:(half + 1) * 128], [ob], [pT])
                    yo = P.sm()
                    P.CP(yo[:, 0:128], pT[:, 0:128], [pT], [yo], eng=P.kb.act)
                    r0 = 512 + g * 256 + half * 128
                    P.DMA(P.yT[r0:r0 + 128, ti * 128:(ti + 1) * 128], yo[:, 0:128], [yo], [P.yT])

    def resid_ln(self, z, xt, ti, xn):
        P = self
        P.STT(z[:], xt[:], ALPHA, z[:], ALU.mult, ALU.add, [xt, z], [z])
        st = P.stat
        for hf in range(2):
            P.kb.op(P.kb.dve, lambda e, hf=hf: e.bn_stats(out=st[:, hf * 6:(hf + 1) * 6], in_=z[:, hf * 512:(hf + 1) * 512]), [z.r], [st.r])
        P.kb.op(P.kb.dve, lambda e: e.bn_aggr(out=st[:, 12:14], in_=st[:, 0:12]), [st.r], [st.r])
        P.ACT(st[:, 14:15], st[:, 13:14], AF.Sqrt, [st, P.cst], [st], bias=P.cst[:, 1:2])
        P.RECIP(st[:, 14:15], st[:, 14:15], [st], [st])
        P.TS(z[:], z[:], st[:, 12:13], ALU.subtract, [z, st], [z], s2=st[:, 14:15], op1=ALU.mult)
        P.TT(z[:], z[:], P.bc[2][:], ALU.mult, [z, P.bc[2]], [z], eng=P.kb.pool)
        P.TT(xn[:], z[:], P.bc[3][:], ALU.add, [z, P.bc[3]], [xn], eng=P.kb.pool)

    def load_ln(self, l, k):
        P = self
        P.DMA(P.bc[2][:], P.w["ln_g"][l, k].partition_broadcast(128), [], [P.bc[2]])
        P.DMA(P.bc[3][:], P.w["ln_b"][l, k].partition_broadcast(128), [], [P.bc[3]])

    def phase_out(self, l, wout_dram, xsrc, xdst, router_j=None):
        P = self
        P.gate_bcast(2, 0, P.bc[0]); P.gate_bcast(2, 1, P.bc[1]); P.load_ln(l, 0)
        if router_j is not None:
            P.DMA(P.rw[:], P.w["moe_router_w"][router_j].rearrange("(c p) n -> p c n", p=128), [], [P.rw])
            P.DMA(P.rb[:], P.w["moe_router_b"][router_j].partition_broadcast(128), [], [P.rb])
        wof, wor = P.gflat(0, 4096, BF16)
        wo = wof.rearrange("p (c n) -> p c n", n=1024)
        P.DMA(wo, wout_dram.rearrange("(c p) n -> p c n", p=128), [], wor, q=P.kb.pool)
        ytb = [P.gflat(2 + i, 512, BF16)[0].rearrange("p (c n) -> p c n", n=128) for i in range(2)]
        for ti in range(NT):
            cond = 1 if ti < 2 else 0
            yt, yr = ytb[ti % 2], P.G[2 + ti % 2]
            P.DMA(yt, P.yT[:, ti * 128:(ti + 1) * 128].rearrange("(c p) n -> p c n", p=128), [P.yT], [yr], q=P.kb.pool)
            xt = P.xb[P.xbi]; P.xbi = (P.xbi + 1) % 3
            P.DMA(xt[:], xsrc[ti * 128:(ti + 1) * 128, :], [xsrc], [xt])
            z = P.zb[ti % 2]
            for hf in range(2):
                po = P.next_ps()
                for c in range(KC):
                    P.MM(po[:, :], yt[:, c, :], wo[:, c, hf * 512:(hf + 1) * 512], [yr] + wor, [po], start=(c == 0), stop=(c == 7))
                P.TT(z[:, hf * 512:(hf + 1) * 512], po[:, :], P.bc[cond][:, hf * 512:(hf + 1) * 512], ALU.mult, [po, P.bc[cond]], [z])
            xn = P.xb[P.xbi]; P.xbi = (P.xbi + 1) % 3
            P.resid_ln(z, xt, ti, xn)
            P.DMA(xdst[ti * 128:(ti + 1) * 128, :], xn[:], [xn], [xdst])
            P.hT_tile(xn, ti, P.sc2p, 3, router=(router_j is not None))

    def ffn_tile_weights(self, wg, wu, wd):
        pass

    def phase_ffn(self, l, j, xsrc, xdst, lat_only_out=None):
        P = self
        P.gate_bcast(5, 0, P.bc[0]); P.gate_bcast(5, 1, P.bc[1]); P.load_ln(l, 1)
        Wg, Wu, Wd = P.w["ffn_w_gate"][j], P.w["ffn_w_up"][j], P.w["ffn_w_down"][j]
        wdf, wdr = P.gflat(0, NFC * 512, BF16)
        wd = wdf.rearrange("p (f n) -> p f n", n=1024)
        P.DMA(wd, Wd.rearrange("(f p) n -> p f n", p=128), [], wdr, q=P.kb.pool)
        acf, acr = P.gflat(5, NFC * 256, BF16)
        actT = acf.rearrange("p (f n) -> p f n", n=512)
        for (t0, n) in NTILES:
            for fc in range(NFC):
                wgb = P.loadw(Wg[:, fc * 128:(fc + 1) * 128], 128)
                wub = P.loadw(Wu[:, fc * 128:(fc + 1) * 128], 128)
                pg = P.next_ps(); pu = P.next_ps()
                for c in range(KC):
                    P.MM(pg[:, 0:n], wgb[:, c, :], P.hT[:, c, t0:t0 + n], [wgb, P.hT], [pg], start=(c == 0), stop=(c == 7))
                for c in range(KC):
                    P.MM(pu[:, 0:n], wub[:, c, :], P.hT[:, c, t0:t0 + n], [wub, P.hT], [pu], start=(c == 0), stop=(c == 7))
                sg = P.sm()
                P.ACT(sg[:, 0:n], pg[:, 0:n], AF.Silu, [pg], [sg])
                P.TT(actT[:, fc, 0:n], sg[:, 0:n], pu[:, 0:n], ALU.mult, [sg, pu], acr)
            for sub in range(n // 128):
                ti = t0 // 128 + sub
                cond = 1 if ti < 2 else 0
                xt = P.xb[P.xbi]; P.xbi = (P.xbi + 1) % 3
                P.DMA(xt[:], xsrc[ti * 128:(ti + 1) * 128, :], [xsrc], [xt])
                z = P.zb[ti % 2]
                for hf in range(2):
                    po = P.next_ps()
                    for fc in range(NFC):
                        P.MM(po[:, :], actT[:, fc, sub * 128:(sub + 1) * 128], wd[:, fc, hf * 512:(hf + 1) * 512], acr + wdr, [po],
                             start=(fc == 0), stop=(fc == NFC - 1))
                    P.TT(z[:, hf * 512:(hf + 1) * 512], po[:, :], P.bc[cond][:, hf * 512:(hf + 1) * 512], ALU.mult, [po, P.bc[cond]], [z])
                xn = P.xb[P.xbi]; P.xbi = (P.xbi + 1) % 3
                P.resid_ln(z, xt, ti, xn)
                P.DMA(xdst[ti * 128:(ti + 1) * 128, :], xn[:], [xn], [xdst])

    def phase_moe(self, l, j, xsrc, xdst):
        P = self
        P.gate_bcast(5, 0, P.bc[0]); P.gate_bcast(5, 1, P.bc[1]); P.load_ln(l, 1)
        wdf, wdr = P.gflat(0, NFC * 512, BF16)
        wd = wdf.rearrange("p (f n) -> p f n", n=1024)
        acf, acr = P.gflat(5, NFC * 256, BF16)
        actT = acf.rearrange("p (f n) -> p f n", n=512)
        accf, accr = P.gflat(8, 4096)
        acc = accf.rearrange("p (s n) -> p s n", n=1024)
        for (t0, n) in NTILES:
            nsub = n // 128
            for ex in range(8):
                Wg, Wu, Wd = P.w["moe_w_gate"][j, ex], P.w["moe_w_up"][j, ex], P.w["moe_w_down"][j, ex]
                P.DMA(wd, Wd.rearrange("(f p) n -> p f n", p=128), [], wdr, q=P.kb.pool)
                for fc in range(NFC):
                    wgb = P.loadw(Wg[:, fc * 128:(fc + 1) * 128], 128)
                    wub = P.loadw(Wu[:, fc * 128:(fc + 1) * 128], 128)
                    pg = P.next_ps(); pu = P.next_ps()
                    for c in range(KC):
                        P.MM(pg[:, 0:n], wgb[:, c, :], P.hT[:, c, t0:t0 + n], [wgb, P.hT], [pg], start=(c == 0), stop=(c == 7))
                    for c in range(KC):
                        P.MM(pu[:, 0:n], wub[:, c, :], P.hT[:, c, t0:t0 + n], [wub, P.hT], [pu], start=(c == 0), stop=(c == 7))
                    sg = P.sm()
                    P.ACT(sg[:, 0:n], pg[:, 0:n], AF.Silu, [pg], [sg])
                    P.TT(actT[:, fc, 0:n], sg[:, 0:n], pu[:, 0:n], ALU.mult, [sg, pu], acr)
                for sub in range(nsub):
                    ti = t0 // 128 + sub
                    for hf in range(2):
                        po = P.next_ps()
                        for fc in range(NFC):
                            P.MM(po[:, :], actT[:, fc, sub * 128:(sub + 1) * 128], wd[:, fc, hf * 512:(hf + 1) * 512], acr + wdr, [po],
                                 start=(fc == 0), stop=(fc == NFC - 1))
                        a = acc[:, sub, hf * 512:(hf + 1) * 512]
                        if ex == 0:
                            P.TS(a, po[:, :], P.gates[:, ti, ex:ex + 1], ALU.mult, [po, P.gates], accr)
                        else:
                            P.STT(a, po[:, :], P.gates[:, ti, ex:ex + 1], a, ALU.mult, ALU.add, [po, P.gates] + accr, accr)
            for sub in range(nsub):
                ti = t0 // 128 + sub
                cond = 1 if ti < 2 else 0
                xt = P.xb[P.xbi]; P.xbi = (P.xbi + 1) % 3
                P.DMA(xt[:], xsrc[ti * 128:(ti + 1) * 128, :], [xsrc], [xt])
                z = P.zb[ti % 2]
                P.TT(z[:], acc[:, sub, :], P.bc[cond][:], ALU.mult, accr + [P.bc[cond]], [z])
                xn = P.xb[P.xbi]; P.xbi = (P.xbi + 1) % 3
                P.resid_ln(z, xt, ti, xn)
                P.DMA(xdst[ti * 128:(ti + 1) * 128, :], xn[:], [xn], [xdst])

    def s5(self, l, j):
        P = self
        G = P.G
        I32 = mybir.dt.int32
        TWO_PI = 2.0 * math.pi
        W = P.w["od_w_in"][j]
        st_ = P.s5t
        R_, W_ = [st_, P.s5i], [st_]
        P.DMA(P.s5d[:], P.w["s5_d_fm"][j], [], [P.s5d])
        LRE, LIM, DT, MAG, THN, SN, CS, CRE, CIM, NCIM, TA, TB = range(12)
        for d in range(2):
            q = lambda k: st_[:, d, k, :]
            P.DMA(q(LRE), P.w["s5_lre"][j, d], [], W_); P.DMA(q(LIM), P.w["s5_lim"][j, d], [], W_); P.DMA(q(DT), P.w["s5_ldt"][j, d], [], W_)
            P.ACT(q(DT), q(DT), AF.Exp, R_, W_)
            P.TS(q(LRE), q(LRE), -1e-4, ALU.min, R_, W_)
            P.TT(q(TA), q(LRE), q(DT), ALU.mult, R_, W_)
            P.ACT(q(MAG), q(TA), AF.Exp, R_, W_)
            P.TT(q(THN), q(LIM), q(DT), ALU.mult, R_, W_)
            P.TS(q(THN), q(THN), 1.0 / TWO_PI, ALU.mult, R_, W_)
            P.CP(P.s5i[:], q(THN), R_, [P.s5i]); P.CP(q(TA), P.s5i[:], R_, W_)
            P.TT(q(TA), q(THN), q(TA), ALU.subtract, R_, W_)
            P.ACT(q(SN), q(TA), AF.Sin, R_, W_, scale=TWO_PI)
            P.TS(q(TA), q(TA), 0.25, ALU.add, R_, W_)
            P.CP(P.s5i[:], q(TA), R_, [P.s5i]); P.CP(q(TB), P.s5i[:], R_, W_)
            P.TT(q(TA), q(TA), q(TB), ALU.subtract, R_, W_)
            P.ACT(q(CS), q(TA), AF.Sin, R_, W_, scale=TWO_PI)
            P.TT(q(CS), q(CS), q(MAG), ALU.mult, R_, W_)
            P.TT(q(SN), q(SN), q(MAG), ALU.mult, R_, W_)
            P.TT(q(TA), q(LRE), q(LRE), ALU.mult, R_, W_); P.TT(q(TB), q(LIM), q(LIM), ALU.mult, R_, W_)
            P.TT(q(TA), q(TA), q(TB), ALU.add, R_, W_); P.RECIP(q(TA), q(TA), R_, W_)
            P.TS(q(CS), q(CS), -1.0, ALU.add, R_, W_)
            P.TT(q(CRE), q(CS), q(LRE), ALU.mult, R_, W_); P.TT(q(TB), q(SN), q(LIM), ALU.mult, R_, W_)
            P.TT(q(CRE), q(CRE), q(TB), ALU.add, R_, W_); P.TT(q(CRE), q(CRE), q(TA), ALU.mult, R_, W_)
            P.TT(q(CIM), q(SN), q(LRE), ALU.mult, R_, W_); P.TT(q(TB), q(CS), q(LIM), ALU.mult, R_, W_)
            P.TT(q(CIM), q(CIM), q(TB), ALU.subtract, R_, W_); P.TT(q(CIM), q(CIM), q(TA), ALU.mult, R_, W_)
            P.TS(q(NCIM), q(CIM), -1.0, ALU.mult, R_, W_)
        uT, yacc, A, B, Cs, Sn, t1, t2 = [G[i] for i in range(8)]
        t2i = P.Gt[:, 7, :].bitcast(I32)
        ygf, ygr = P.gflat(8, 2 * T, BF16)
        ygT = ygf.rearrange("p (c n) -> p c n", n=T)
        for ut in range(4):
            wu = P.loadw(W[:, ut * 128:(ut + 1) * 128], 128)
            P.proj_fm(wu, 128, lambda pb, t0, n: P.CP(uT[:, t0:t0 + n], pb[:, 0:n], [pb], [uT], eng=P.kb.act))
            P.TS(yacc[:], uT[:], P.s5d[:, ut:ut + 1], ALU.mult, [uT, P.s5d], [yacc])
            for sl in range(4):
                stt = ut * 4 + sl
                r0 = sl * 32
                bc_ = P.s5bc
                P.DMA(bc_[:, 0:32], P.w["s5_bT"][j, :, stt].rearrange("p a h -> p (a h)"), [], [bc_])
                P.DMA(bc_[:, 32:64], P.w["s5_cT"][j, :, stt].rearrange("p a h -> p (a h)"), [], [bc_])
                Zc = P.s5zc
                P.MSET(Zc[:, 0:256], 0.0, [Zc])
                for gl in range(2):
                    pr = slice(gl * 64, gl * 64 + 64); cc = slice(r0 + gl * 16, r0 + gl * 16 + 16)
                    P.CP(Zc[pr, cc], bc_[pr, 32:48], [bc_], [Zc])
                    P.TS(Zc[pr, 128 + cc.start:128 + cc.stop], bc_[pr, 48:64], -1.0, ALU.mult, [bc_], [Zc])
                for d in range(2):
                    q = lambda k: st_[:, d, k, stt:stt + 1]
                    Z = P.sm(); tmp = P.sm(); L = P.sm()
                    P.MSET(Z[:, 0:256], 0.0, [Z])
                    for gl in range(2):
                        pr = slice(gl * 64, gl * 64 + 64); c0 = r0 + gl * 16
                        P.TS(tmp[pr, 0:16], bc_[pr, 0:16], q(CRE)[pr], ALU.mult, [bc_, st_], [tmp])
                        P.STT(Z[pr, c0:c0 + 16], bc_[pr, 16:32], q(NCIM)[pr], tmp[pr, 0:16], ALU.mult, ALU.add, [bc_, st_, tmp], [Z])
                        P.TS(tmp[pr, 16:32], bc_[pr, 16:32], q(CRE)[pr], ALU.mult, [bc_, st_], [tmp])
                        P.STT(Z[pr, 128 + c0:128 + c0 + 16], bc_[pr, 0:16], q(CIM)[pr], tmp[pr, 16:32], ALU.mult, ALU.add, [bc_, st_, tmp], [Z])
                    for k in range(2):
                        pz = P.next_ps()
                        P.TR(pz[:, 0:128], Z[:, k * 128:(k + 1) * 128], [Z], [pz])
                        P.CP(L[:, k * 128:(k + 1) * 128], pz[:, 0:128], [pz], [L], eng=P.kb.act)
                    P.DMA(t1[:], P.cd["tauF" if d == 0 else "tauB"][:], [], [t1])
                    P.TS(t1[:], t1[:], q(THN), ALU.mult, [t1, st_], [t1])
                    P.CP(t2i, t1[:], [t1], [t2]); P.CP(Cs[:], t2i, [t2], [Cs], eng=P.kb.pool)
                    P.TT(t1[:], t1[:], Cs[:], ALU.subtract, [t1, Cs], [t1])
                    P.ACT(Sn[:], t1[:], AF.Sin, [t1], [Sn], scale=TWO_PI)
                    P.TS(t1[:], t1[:], 0.25, ALU.add, [t1], [t1])
                    P.CP(t2i, t1[:], [t1], [t2]); P.CP(Cs[:], t2i, [t2], [Cs], eng=P.kb.pool)
                    P.TT(t1[:], t1[:], Cs[:], ALU.subtract, [t1, Cs], [t1])
                    P.ACT(Cs[:], t1[:], AF.Sin, [t1], [Cs], scale=TWO_PI)
                    for (t0, n) in NTILES:
                        pa = P.next_ps(); pb_ = P.next_ps()
                        P.MM(pa[:, 0:n], L[:, 0:128], uT[:, t0:t0 + n], [L, uT], [pa])
                        P.MM(pb_[:, 0:n], L[:, 128:256], uT[:, t0:t0 + n], [L, uT], [pb_])
                        P.CP(A[:, t0:t0 + n], pa[:, 0:n], [pa], [A], eng=P.kb.act)
                        P.CP(B[:, t0:t0 + n], pb_[:, 0:n], [pb_], [B])
                    P.TT(t1[:], A[:], Cs[:], ALU.mult, [A, Cs], [t1]); P.TT(t2[:], B[:], Sn[:], ALU.mult, [B, Sn], [t2], eng=P.kb.pool)
                    P.TT(A[:], A[:], Sn[:], ALU.mult, [A, Sn], [A]); P.TT(B[:], B[:], Cs[:], ALU.mult, [B, Cs], [B], eng=P.kb.pool)
                    P.TT(t1[:], t1[:], t2[:], ALU.add, [t1, t2], [t1]); P.TT(B[:], B[:], A[:], ALU.subtract, [B, A], [B], eng=P.kb.pool)
                    mg = q(MAG)
                    for src, dst in ((t1, t2), (B, A)):
                        if d == 0:
                            P.SCAN(dst[:], mg.broadcast_to([128, T]), src[:], [st_, src], [dst])
                        else:
                            P.SCAN(dst[:, 255::-1], mg.broadcast_to([128, 256]), src[:, 255::-1], [st_, src], [dst])
                            P.SCAN(dst[:, T - 1:255:-1], mg.broadcast_to([128, T - 256]), src[:, T - 1:255:-1], [st_, src, dst], [dst], init=dst[:, 0:1])
                    P.TT(t1[:], t2[:], Cs[:], ALU.mult, [t2, Cs], [t1]); P.TT(B[:], A[:], Sn[:], ALU.mult, [A, Sn], [B], eng=P.kb.pool)
                    P.TT(t1[:], t1[:], B[:], ALU.subtract, [t1, B], [t1])
                    P.TT(t2[:], t2[:], Sn[:], ALU.mult, [t2, Sn], [t2]); P.TT(A[:], A[:], Cs[:], ALU.mult, [A, Cs], [A], eng=P.kb.pool)
                    P.TT(t2[:], t2[:], A[:], ALU.add, [t2, A], [t2])
                    for (t0, n) in NTILES:
                        py = P.next_ps()
                        P.MM(py[:, 0:n], Zc[:, 0:128], t1[:, t0:t0 + n], [Zc, t1], [py], start=True, stop=False)
                        P.MM(py[:, 0:n], Zc[:, 128:256], t2[:, t0:t0 + n], [Zc, t2], [py], start=False, stop=True)
                        P.TT(yacc[:, t0:t0 + n], yacc[:, t0:t0 + n], py[:, 0:n], ALU.add, [yacc, py], [yacc])
            P.ACT(t1[:], yacc[:], AF.Square, [yacc], [t1])
            P.TS(t1[:], t1[:], 0.044715, ALU.mult, [t1], [t1], s2=1.0, op1=ALU.add)
            P.TT(t1[:], t1[:], yacc[:], ALU.mult, [t1, yacc], [t1])
            P.ACT(t1[:], t1[:], AF.Sigmoid, [t1], [t1], scale=2.0 * math.sqrt(2.0 / math.pi))
            P.TT(ygT[:, ut, :], yacc[:], t1[:], ALU.mult, [yacc, t1], ygr)
        for nt in range(4):
            wg = P.loadw(P.w["s5_glu_w"][j][:, nt * 128:(nt + 1) * 128], 128, kc=4)
            for (t0, n) in NTILES:
                pg = P.next_ps()
                for c in range(4):
                    P.MM(pg[:, 0:n], wg[:, c, :], ygT[:, c, t0:t0 + n], [wg] + ygr, [pg], start=(c == 0), stop=(c == 3))
                sg = P.sm()
                P.ACT(sg[:, 0:n], pg[:, 0:n], AF.Sigmoid, [pg], [sg])
                yo = P.sm()
                P.TT(yo[:, 0:n], ygT[:, nt, t0:t0 + n], sg[:, 0:n], ALU.mult, ygr + [sg], [yo])
                P.DMA(P.yT[nt * 128:(nt + 1) * 128, t0:t0 + n], yo[:, 0:n], [yo], [P.yT])

    def rwkv(self, l, j):
        P = self
        G = P.G
        W = P.w["od_w_in"][j]
        C0 = 512
        prm = P.rwp
        P.DMA(prm[:, 0:15], P.w["rw_mu_fm"][j], [], [prm])
        P.DMA(prm[:, 16:20], P.w["rw_kk_fm"][j], [], [prm]); P.DMA(prm[:, 20:24], P.w["rw_ka_fm"][j], [], [prm])
        P.DMA(prm[:, 24:28], P.w["rw_rk_fm"][j], [], [prm])
        for d in range(2):
            P.DMA(prm[:, 28 + d * 4:32 + d * 4], P.w["rw_w0_fm"][j, d], [], [prm])
            P.DMA(prm[:, 36 + d * 4:40 + d * 4], P.w["rw_a0_fm"][j, d], [], [prm])
        MU = lambda t_: prm[:, t_:t_ + 1]
        rT, kT, vtokS, kkT, L_, Gc, E1, E2, As, Oacc = [G[i] for i in range(10)]
        vtok = vtokS[:].rearrange("p (t c) -> p t c", c=128)
        oacc = Oacc[:].rearrange("p (t c) -> p t c", c=128)
        Hbd = P.S

        def proj_shifted(tile_idx, dst, tmp):
            wb = P.loadw(W[:, C0 + tile_idx * 128:C0 + (tile_idx + 1) * 128], 128)
            P.proj_fm(wb, 128, lambda pb, t0, n: P.CP(tmp[:, t0:t0 + n], pb[:, 0:n], [pb], [tmp], eng=P.kb.act))
            for (a, b) in ((0, LC), (LC, T)):
                P.CP(dst[:, a:a + 1], tmp[:, a + 1:a + 2], [tmp], [dst], eng=P.kb.pool)
                P.CP(dst[:, b - 1:b], tmp[:, b - 2:b - 1], [tmp], [dst], eng=P.kb.pool)
                P.TT(dst[:, a + 1:b - 1], tmp[:, a:b - 2], tmp[:, a + 2:b], ALU.add, [tmp], [dst])
            P.STT(dst[:], dst[:], 0.5, tmp[:], ALU.mult, ALU.subtract, [dst, tmp], [dst])
            P.STT(dst[:], dst[:], MU(tile_idx), tmp[:], ALU.mult, ALU.add, [dst, prm, tmp], [dst])

        P.DMA(P.rwm[:], P.cd["rw_masks"][0], [], [P.rwm])
        for pp in range(4):
            proj_shifted(0 + pp, rT, E1)
            proj_shifted(4 + pp, kT, E1)
            proj_shifted(8 + pp, E2, E1)
            for ti in range(NT):
                pv = P.next_ps()
                P.TR(pv[:, 0:128], E2[:, ti * 128:(ti + 1) * 128], [E2], [pv])
                P.CP(vtok[:, ti, :], pv[:, 0:128], [pv], [vtokS], eng=P.kb.act)
            P.TS(kkT[:], kT[:], prm[:, 16 + pp:17 + pp], ALU.mult, [kT, prm], [kkT])
            for (t0, n) in NTILES:
                sq = P.sm()
                P.ACT(sq[:, 0:n], kkT[:, t0:t0 + n], AF.Square, [kkT], [sq])
                pn = P.next_ps()
                P.MM(pn[:, 0:n], P.bdones[:], sq[:, 0:n], [P.bdones, sq], [pn])
                nr_ = P.sm()
                P.ACT(nr_[:, 0:n], pn[:, 0:n], AF.Sqrt, [pn], [nr_])
                P.TS(nr_[:, 0:n], nr_[:, 0:n], 1e-12, ALU.max, [nr_], [nr_])
                P.RECIP(nr_[:, 0:n], nr_[:, 0:n], [nr_], [nr_])
                P.TT(kkT[:, t0:t0 + n], kkT[:, t0:t0 + n], nr_[:, 0:n], ALU.mult, [kkT, nr_], [kkT])
            P.DMA(P.rwln[:, 0, :], P.w["rw_lng"][j, pp * 128:(pp + 1) * 128].partition_broadcast(128), [], [P.rwln])
            P.DMA(P.rwln[:, 1, :], P.w["rw_lnb"][j, pp * 128:(pp + 1) * 128].partition_broadcast(128), [], [P.rwln])
            for d in range(2):
                fwd = d == 0
                P.DMA(P.rwm[:], P.cd["rw_masks"][d], [], [P.rwm])
                for (lt, padk, biasc, dstb, fn) in ((12, "rw_w2pad", 28 + d * 4 + pp, L_, AF.Tanh), (13, "rw_a2pad", 36 + d * 4 + pp, As, None)):
                    proj_shifted(lt, E1, E2)
                    if fn is not None:
                        P.ACT(E1[:], E1[:], fn, [E1], [E1])
                    wpad = P.attm[1]
                    P.DMA(wpad[:, 0:128], P.w[padk][j, d, :, pp * 128:(pp + 1) * 128], [], [wpad])
                    for (t0, n) in NTILES:
                        pz = P.next_ps()
                        P.MM(pz[:, 0:n], wpad[:, 0:128], E1[:, t0:t0 + n], [wpad, E1], [pz])
                        P.ACT(dstb[:, t0:t0 + n], pz[:, 0:n], AF.Sigmoid, [pz, prm], [dstb], bias=prm[:, biasc:biasc + 1])
                P.TS(L_[:], L_[:], -math.exp(-0.5), ALU.mult, [L_], [L_])
                for ti in range(NT):
                    tsl = slice(ti * 128, (ti + 1) * 128)
                    if fwd:
                        P.SCAN(Gc[:, tsl], P.ones[:], L_[:, tsl], [P.ones, L_], [Gc])
                    else:
                        hi, lo = (ti + 1) * 128 - 1, ti * 128 - 1
                        rs = slice(hi, lo if lo >= 0 else None, -1)
                        P.SCAN(Gc[:, rs], P.ones[:], L_[:, rs], [P.ones, L_], [Gc])
                P.ACT(E1[:], Gc[:], AF.Exp, [Gc], [E1])
                ecol = E1[:, 127:T:128] if fwd else E1[:, 0:T:128]
                P.CP(P.rwgc[:], ecol, [E1], [P.rwgc])
                P.TT(E1[:], E1[:], rT[:], ALU.mult, [E1, rT], [E1])
                P.TT(E2[:], Gc[:], L_[:], ALU.subtract, [Gc, L_], [E2])
                P.ACT(E2[:], E2[:], AF.Exp, [E2], [E2])
                P.STT(E2[:], kkT[:], -1.0, E2[:], ALU.mult, ALU.mult, [kkT, E2], [E2])
                P.ACT(Gc[:], Gc[:], AF.Exp, [Gc], [Gc], scale=-1.0)
                P.TT(L_[:], kkT[:], As[:], ALU.mult, [kkT, As], [L_], eng=P.kb.pool)
                P.TT(L_[:], L_[:], Gc[:], ALU.mult, [L_, Gc], [L_], eng=P.kb.pool)
                P.TS(As[:], As[:], -1.0, ALU.add, [As, prm], [As], s2=prm[:, 20 + pp:21 + pp], op1=ALU.mult)
                P.STT(As[:], As[:], 1.0, kT[:], ALU.add, ALU.mult, [As, kT], [As])
                P.TT(As[:], As[:], Gc[:], ALU.mult, [As, Gc], [As])
                rt_, at_, bt_, kt_ = E1, E2, L_, As
                P.MSET(Hbd[:], 0.0, [Hbd])
                tiles = list(range(NT)) if fwd else [1, 0] + list(range(NT - 1, 1, -1))
                for ti in tiles:
                    tsl = slice(ti * 128, (ti + 1) * 128)
                    hs = [slice(0, 64), slice(64, 128)]
                    for hh in range(2):
                        ar = P.rwar[hh]
                        P.TS(ar[:, 0:128], at_[:, tsl], P.halfm[:, hh:hh + 1], ALU.mult, [at_, P.halfm], [ar])
                        P.TS(ar[:, 128:256], rt_[:, tsl], P.halfm[:, hh:hh + 1], ALU.mult, [rt_, P.halfm], [ar], eng=P.kb.pool)
                    for hh in range(2):
                        ar, xm, xz = P.rwar[hh], P.rwxm[hh], P.rwxz[hh]
                        p1 = P.next_ps(); p2 = P.next_ps(); p3 = P.next_ps()
                        P.MM(p1[:, 0:256], bt_[:, tsl], ar[:, 0:256], [bt_, ar], [p1])
                        P.MM(p2[:, 0:256], kt_[:, tsl], ar[:, 0:256], [kt_, ar], [p2])
                        P.MM(p3[:, 0:128], ar[:, 0:128], bt_[:, tsl], [ar, bt_], [p3])
                        P.TT(xm[:, 0:2, :], p1[:, 0:256].rearrange("p (a n) -> p a n", n=128), P.rwm[:, 0:2, :], ALU.mult, [p1, P.rwm], [xm])
                        P.TT(xm[:, 2:4, :], p2[:, 0:256].rearrange("p (a n) -> p a n", n=128), P.rwm[:, 0:2, :], ALU.mult, [p2, P.rwm], [xm])
                        P.TT(xz[:, 0, :], p3[:, 0:128], P.rwm[:, 2, :], ALU.mult, [p3, P.rwm], [xz])
                    pY = P.next_ps()
                    for hh in range(2):
                        xm = P.rwxm[hh]
                        P.MM(pY[:, hs[hh]], at_[:, tsl], Hbd[:, hs[hh]], [at_, Hbd], [pY], start=True, stop=False)
                        P.MM(pY[:, hs[hh]], xm[:, 2, :], vtok[:, ti, hs[hh]], [xm, vtokS], [pY], start=False, stop=True)
                    U = P.rwu
                    P.CP(U[:], pY[:, 0:128], [pY], [U])
                    cur = [(P.rwxm[hh][:, 0, :], P.rwxz[hh][:, 0, :]) for hh in range(2)]
                    nxt = [(P.rwxz[hh][:, 1, :], P.rwxz[hh][:, 2, :]) for hh in range(2)]
                    for k in range(7):
                        pU = P.next_ps()
                        for hh in range(2):
                            P.MM(pU[:, hs[hh]], cur[hh][0], U[:, hs[hh]], [P.rwxm[hh], P.rwxz[hh], U], [pU])
                        if k < 6:
                            for hh in range(2):
                                X, Z = cur[hh]; Xn, Zn = nxt[hh]
                                pX = P.next_ps(); pZ = P.next_ps()
                                P.MM(pX[:, 0:128], Z, X, [P.rwxm[hh], P.rwxz[hh]], [pX])
                                P.MM(pZ[:, 0:128], X, Z, [P.rwxm[hh], P.rwxz[hh]], [pZ])
                                P.CP(Xn, pX[:, 0:128], [pX], [P.rwxm[hh], P.rwxz[hh]], eng=P.kb.act)
                                P.CP(Zn, pZ[:, 0:128], [pZ], [P.rwxm[hh], P.rwxz[hh]])
                        P.TT(U[:], U[:], pU[:, 0:128], ALU.add, [U, pU], [U])
                        if k < 6:
                            if k == 0:
                                cur, nxt = nxt, [(P.rwxm[hh][:, 0, :], P.rwxz[hh][:, 0, :]) for hh in range(2)]
                            else:
                                cur, nxt = nxt, cur
                    pO = P.next_ps()
                    for hh in range(2):
                        xm = P.rwxm[hh]
                        P.MM(pO[:, hs[hh]], rt_[:, tsl], Hbd[:, hs[hh]], [rt_, Hbd], [pO], start=True, stop=False)
                        P.MM(pO[:, hs[hh]], xm[:, 1, :], U[:, hs[hh]], [xm, U], [pO], start=False, stop=False)
                        P.MM(pO[:, hs[hh]], xm[:, 3, :], vtok[:, ti, hs[hh]], [xm, vtokS], [pO], start=False, stop=True)
                    if fwd:
                        P.CP(oacc[:, ti, :], pO[:, 0:128], [pO], [Oacc], eng=P.kb.act)
                    else:
                        P.TT(oacc[:, ti, :], oacc[:, ti, :], pO[:, 0:128], ALU.add, [Oacc, pO], [Oacc])
                    tk = P.ktok[0]
                    for q_, src in ((0, bt_), (1, kt_)):
                        pt_ = P.next_ps()
                        P.TR(pt_[:, 0:128], src[:, tsl], [src], [pt_])
                        P.CP(tk[:, q_, :], pt_[:, 0:128], [pt_], [tk], eng=P.kb.act)
                    pH = P.next_ps()
                    P.MM(pH[:, 0:128], tk[:, 0, :], U[:], [tk, U], [pH], start=True, stop=False)
                    P.MM(pH[:, 0:128], tk[:, 1, :], vtok[:, ti, :], [tk, vtokS], [pH], start=False, stop=True)
                    P.TT(P.tmpS[:], pH[:, 0:128], P.bdones[:], ALU.mult, [pH, P.bdones], [P.tmpS])
                    P.TT(P.tmpS[:], P.tmpS[:], Hbd[:], ALU.add, [P.tmpS, Hbd], [P.tmpS])
                    P.ACT(Hbd[:], P.tmpS[:], AF.Identity, [P.tmpS, P.rwgc], [Hbd], scale=P.rwgc[:, ti:ti + 1])
            P.STT(E1[:], rT[:], prm[:, 24 + pp:25 + pp], kT[:], ALU.mult, ALU.mult, [rT, prm, kT], [E1])
            proj_shifted(14, E2, Gc)
            P.ACT(E2[:], E2[:], AF.Sigmoid, [E2], [E2])
            g2b = P.attm[0]
            P.DMA(g2b[:, 0:128], P.w["rw_g2"][j, :, pp * 128:(pp + 1) * 128], [], [g2b])
            for ti in range(NT):
                tsl = slice(ti * 128, (ti + 1) * 128)
                o = P.sm(); st = P.stat
                P.CP(o[:, 0:128], oacc[:, ti, :], [Oacc], [o], eng=P.kb.pool)
                o3 = o[:, 0:128].rearrange("p (h i) -> p h i", i=64)
                P.kb.op(P.kb.dve, lambda e, o3=o3: e.reduce_sum(out=st[:, 16:18], in_=o3, axis=AX.X), [o.r], [st.r])
                P.TS(st[:, 16:18], st[:, 16:18], 1.0 / 64.0, ALU.mult, [st], [st])
                for hh in range(2):
                    P.TS(o3[:, hh, :], o3[:, hh, :], st[:, 16 + hh:17 + hh], ALU.subtract, [o, st], [o])
                sq = P.sm()
                P.TT(sq[:, 0:128], o[:, 0:128], o[:, 0:128], ALU.mult, [o], [sq])
                sq3 = sq[:, 0:128].rearrange("p (h i) -> p h i", i=64)
                P.kb.op(P.kb.dve, lambda e, sq3=sq3: e.reduce_sum(out=st[:, 18:20], in_=sq3, axis=AX.X), [sq.r], [st.r])
                P.ACT(st[:, 18:20], st[:, 18:20], AF.Sqrt, [st, P.cst], [st], scale=1.0 / 64.0, bias=P.cst[:, 2:3])
                P.RECIP(st[:, 18:20], st[:, 18:20], [st], [st])
                for hh in range(2):
                    P.TS(o3[:, hh, :], o3[:, hh, :], st[:, 18 + hh:19 + hh], ALU.mult, [o, st], [o])
                P.TT(o[:, 0:128], o[:, 0:128], P.rwln[:, 0, :], ALU.mult, [o, P.rwln], [o])
                P.TT(o[:, 0:128], o[:, 0:128], P.rwln[:, 1, :], ALU.add, [o, P.rwln], [o])
                pb_ = P.next_ps()
                P.MM(pb_[:, 0:2], E1[:, tsl], P.halfm[:], [E1, P.halfm], [pb_])
                P.CP(st[:, 20:22], pb_[:, 0:2], [pb_], [st])
                for hh in range(2):
                    P.STT(o3[:, hh, :], vtok[:, ti, hh * 64:(hh + 1) * 64], st[:, 20 + hh:21 + hh], o3[:, hh, :], ALU.mult, ALU.add, [vtokS, st, o], [o])
                pg = P.next_ps()
                P.MM(pg[:, 0:128], E2[:, tsl], g2b[:, 0:128], [E2, g2b], [pg])
                P.TT(o[:, 0:128], o[:, 0:128], pg[:, 0:128], ALU.mult, [o, pg], [o])
                pT = P.next_ps()
                P.TR(pT[:, 0:128], o[:, 0:128], [o], [pT])
                yo = P.sm()
                P.CP(yo[:, 0:128], pT[:, 0:128], [pT], [yo], eng=P.kb.act)
                P.DMA(P.yT[512 + pp * 128:512 + (pp + 1) * 128, tsl], yo[:, 0:128], [yo], [P.yT])

    def build(self):
        P = self
        P.setup()
        xcur = P.xin
        for li, l in enumerate(P.layers):
            j = l // 2
            last = li == len(P.layers) - 1
            xmid = P.xs[0]
            xnext = P.out if last else P.xs[1]
            if P.stop >= 1:
                P.phase_mod(l)
                if "fm" in P.taps:
                    P.DMA(P.outp("tap_fm", [128, 48, 2]), P.fm[:], [P.fm], [])
            if P.stop >= 2:
                P.phase_hT(xcur)
                if "hTd" in P.taps:
                    P.DMA(P.outp("tap_hT", [128, KC, T]), P.hT[:], [P.hT], [], q=P.kb.pool)
            if l % 2 == 0:
                if P.stop >= 3: P.hgrn2(l, j)
                if P.stop >= 4: P.attention(l, j)
                if P.stop >= 5: P.phase_out(l, P.w["ev_w_out"][j], xcur, xmid)
                if P.stop >= 6: P.phase_ffn(l, j, xmid, xnext)
            else:
                if "inject_y" in P.taps:
                    pass
                else:
                    if P.stop >= 3: P.s5(l, j)
                    if P.stop >= 4: P.rwkv(l, j)
                if P.stop >= 5: P.phase_out(l, P.w["od_w_out"][j], xcur, xmid, router_j=(j if "1" != "0" else None))
                if "gates" in P.taps:
                    P.DMA(P.outp("tap_gates", [128, NT, 8]), P.gates[:], [P.gates], [])
                if P.stop >= 6: P.phase_moe(l, j, xmid, xnext)
            xcur = xnext
        P.kb.emit()
        self.st.close()
        return self.nc


def host_inputs(inp, b):
    m = {}
    m["xin"] = np.ascontiguousarray(np.concatenate([inp["ctx"][b], inp["x"][b]], 0))
    ct = np.stack([inp["c"][b].reshape(8, 128).T, inp["c_ctx"].reshape(8, 128).T], -1)
    m["condT"] = np.ascontiguousarray(ct.astype(np.float32))
    for k, v in host_consts().items():
        m["c_" + k] = v
    m["ada_w"] = inp["ada_w"]
    m["ada_b_fm"] = np.ascontiguousarray(inp["ada_b"].reshape(4, 48, 128).transpose(0, 2, 1))
    m["ln_g"] = inp["ln_g"]; m["ln_b"] = inp["ln_b"]
    m["ev_w_in"] = inp["ev_w_in"]; m["ev_w_out"] = inp["ev_w_out"]
    m["hg_lb_fm"] = np.ascontiguousarray(inp["hg_lb"].reshape(2, 4, 128).transpose(2, 1, 0))
    m["hg_ng_fm"] = np.ascontiguousarray(inp["hg_norm_g"].reshape(2, 4, 128).transpose(0, 2, 1))
    m["attn_sink"] = inp["attn_sink"]
    def fm16(a):
        return np.ascontiguousarray(a.reshape(2, 2, 16, 2, 64).transpose(0, 1, 3, 4, 2).reshape(2, 2, 128, 16))
    m["s5_lre"] = fm16(inp["s5_lam_re"]); m["s5_lim"] = fm16(inp["s5_lam_im"])
    m["s5_ldt"] = fm16(np.ascontiguousarray(np.broadcast_to(inp["s5_log_dt"][..., None], (2, 2, 32, 64))))
    bb = np.stack([inp["s5_b_re"], inp["s5_b_im"]], 3)
    m["s5_bT"] = np.ascontiguousarray(bb.reshape(2, 16, 2, 64, 2, 16).transpose(0, 2, 3, 1, 4, 5).reshape(2, 128, 16, 2, 16))
    cc = np.stack([inp["s5_c_re"], inp["s5_c_im"]], 2).transpose(0, 1, 4, 2, 3)
    m["s5_cT"] = np.ascontiguousarray(cc.reshape(2, 16, 2, 64, 2, 16).transpose(0, 2, 3, 1, 4, 5).reshape(2, 128, 16, 2, 16))
    m["s5_d_fm"] = np.ascontiguousarray(inp["s5_d"].reshape(2, 4, 128).transpose(0, 2, 1))
    m["s5_glu_w"] = inp["s5_glu_w"]
    fm4 = lambda a: np.ascontiguousarray(a.reshape(a.shape[:-1] + (4, 128)).swapaxes(-1, -2))
    m["rw_mu_fm"] = np.ascontiguousarray(inp["rwkv_mu"].reshape(2, 15, 128).transpose(0, 2, 1))
    m["rw_w0_fm"] = fm4(inp["rwkv_w0"]); m["rw_a0_fm"] = fm4(inp["rwkv_a0"])
    def pad2(a):
        o = np.zeros((2, 2, 128, 512), np.float32)
        o[:, 0, 0:64] = a[:, 0]; o[:, 1, 64:128] = a[:, 1]
        return o
    m["rw_w2pad"] = pad2(inp["rwkv_w2"]); m["rw_a2pad"] = pad2(inp["rwkv_a2"]); m["rw_g2"] = inp["rwkv_g2"]
    m["rw_kk_fm"] = fm4(inp["rwkv_k_k"]); m["rw_ka_fm"] = fm4(inp["rwkv_k_a"]); m["rw_rk_fm"] = fm4(inp["rwkv_r_k"].reshape(2, 512))
    m["rw_lng"] = inp["rwkv_ln_g"]; m["rw_lnb"] = inp["rwkv_ln_b"]
    for k in ("ffn_w_gate", "ffn_w_up", "ffn_w_down", "od_w_in", "od_w_out", "moe_router_w", "moe_router_b", "moe_w_gate", "moe_w_up", "moe_w_down"):
        m[k] = inp[k]
    return m


FUSED = os.environ.get("MK_FUSED", "1") == "1"
N_CORES = 8


def _run(layers, inputs, xin_per_core):
    P = Prog(layers)
    nc = P.build()
    in_maps = []
    for b in range(N_CORES):
        m = host_inputs(inputs, b)
        m["xin"] = xin_per_core[b]
        in_maps.append({k: v for k, v in m.items() if k in P.din})
    res = run_bass_kernel_spmd(nc, in_maps, core_ids=list(range(N_CORES)))
    return [r["out"] for r in res.results]


def kernel(**inputs):
    inputs = {k: np.asarray(v) for k, v in inputs.items()}
    xs = [np.ascontiguousarray(np.concatenate([inputs["ctx"][b], inputs["x"][b]], 0)).astype(np.float32) for b in range(N_CORES)]
    if FUSED:
        xs = _run([0, 1, 2, 3], inputs, xs)
    else:
        for l in range(4):
            xs = _run([l], inputs, xs)
    return np.stack([x[LC:] for x in xs], 0).astype(np.float32)
```

```python
import contextlib, math, os
import numpy as np
import concourse.bass as bass
import concourse.mybir as mybir
from concourse.bass_utils import run_bass_kernel_spmd

F32 = mybir.dt.float32
BF16 = mybir.dt.bfloat16
F32R = mybir.dt.float32r
ALU = mybir.AluOpType
AF = mybir.ActivationFunctionType
AX = mybir.AxisListType

EPOCH = 30000
NDMA = 24


class Reg:
    __slots__ = ("w", "r", "name")

    def __init__(self, name=""):
        self.w = {}
        self.r = {}
        self.name = name


class Eng:
    def __init__(self, kb, name, self_sync):
        self.kb, self.name, self.self_sync = kb, name, self_sync
        self.ops = []
        self.seen = {}
        self.sems = [kb.new_sem(f"{name}_e0")]
        self.count = 0

    def cur(self):
        return self.sems[-1]


class KB:
    def __init__(self, nc, stack):
        self.nc, self.stack = nc, stack
        self.semh = {}
        self.nsem = 0
        self.pe = Eng(self, "pe", False)
        self.dve = Eng(self, "dve", True)
        self.act = Eng(self, "act", True)
        self.pool = Eng(self, "pool", True)
        self.sp = Eng(self, "sp", False)
        self.engs = [self.pe, self.dve, self.act, self.pool, self.sp]
        self.dma_sems = [self.new_sem(f"dma{i}") for i in range(NDMA)]
        self.dma_tot = [0] * NDMA
        self.dma_i = 0
        self.n_ops = 0

    def new_sem(self, name):
        h = self.stack.enter_context(self.nc.semaphore(name))
        k = self.nsem
        self.nsem += 1
        self.semh[k] = h
        return k

    def _waits(self, E, reads, writes):
        need = {}
        for r in reads:
            for s, v in r.w.items():
                if need.get(s, 0) < v:
                    need[s] = v
        for w in writes:
            for d in (w.w, w.r):
                for s, v in d.items():
                    if need.get(s, 0) < v:
                        need[s] = v
        out = []
        for s, v in need.items():
            if (not E.self_sync) and s in E.sems:
                continue
            if E.seen.get(s, 0) < v:
                E.seen[s] = v
                out.append((s, v))
        return out

    def _mark(self, ev, reads, writes):
        s, v = ev
        for r in reads:
            r.r[s] = v
        for w in writes:
            w.w = {s: v}
            w.r = {}

    def op(self, E, fn, reads=(), writes=()):
        waits = self._waits(E, reads, writes)
        if E.count >= EPOCH:
            E.sems.append(self.new_sem(f"{E.name}_e{len(E.sems)}"))
            E.count = 0
        E.count += 1
        ev = (E.cur(), E.count)
        E.ops.append((waits, fn, ev[0], 1))
        self._mark(ev, reads, writes)
        self.n_ops += 1
        return ev

    def dma(self, Q, out, in_, reads=(), writes=(), **kw):
        i = self.dma_i
        self.dma_i = (i + 1) % NDMA
        s = self.dma_sems[i]
        waits = self._waits(Q, reads, writes)
        if self.dma_tot[i] > 0 and Q.seen.get(s, 0) < self.dma_tot[i]:
            Q.seen[s] = self.dma_tot[i]
            waits.append((s, self.dma_tot[i]))
        self.dma_tot[i] += 16
        ev = (s, self.dma_tot[i])
        Q.ops.append((waits, lambda e: e.dma_start(out=out, in_=in_, **kw), s, 16))
        self._mark(ev, reads, writes)
        self.n_ops += 1
        return ev

    def emit(self):
        nc = self.nc
        fin = [(self.dma_sems[i], self.dma_tot[i]) for i in range(NDMA) if self.dma_tot[i] > 0]
        semh = self.semh

        def run(E, e):
            for waits, fn, s, inc in E.ops:
                for ws, wv in waits:
                    e.wait_ge(semh[ws], wv)
                inst = fn(e)
                inst.then_inc(semh[s], inc)

        with nc.Block() as block:
            @block.tensor
            def _(e):
                run(self.pe, e)

            @block.vector
            def _(e):
                run(self.dve, e)

            @block.scalar
            def _(e):
                run(self.act, e)

            @block.gpsimd
            def _(e):
                run(self.pool, e)

            @block.sync
            def _(e):
                run(self.sp, e)
                for ws, wv in fin:
                    e.wait_ge(semh[ws], wv)


class _LazyW:
    def __init__(self, prog, shapes):
        self.p, self.shapes, self.c = prog, shapes, {}

    def __getitem__(self, k):
        if k not in self.c:
            self.c[k] = self.p.inp(k, self.shapes[k])
        return self.c[k]


T = 2304; LC = 256; NT = 18; D = 1024; KC = 8; CH = 32; NCH = 72; NG = 10
NTILES = [(0, 256), (256, 512), (768, 512), (1280, 512), (1792, 512)]
ALPHA = 8 ** 0.25
DFF = 2816; NFC = 22


def host_consts():
    c = {}
    c["ident"] = np.eye(128, dtype=np.float32)
    s = np.arange(128)[:, None]; t = np.arange(128)[None, :]
    same = (s // CH) == (t // CH)
    c["tri_le"] = (same & (s <= t)).astype(np.float32)
    c["tri_ge"] = (same & (s >= t)).astype(np.float32)
    tf = np.arange(T, dtype=np.float32)
    tb = np.concatenate([255.0 - np.arange(256), 256.0 + (2303.0 - np.arange(256, T))]).astype(np.float32)
    c["tauF"] = np.ascontiguousarray(np.broadcast_to(tf, (128, T))); c["tauB"] = np.ascontiguousarray(np.broadcast_to(tb, (128, T)))
    a_ = np.arange(128)[:, None]; b_ = np.arange(128)[None, :]
    mf = np.stack([(a_ < b_), (a_ <= b_), (b_ < a_)], 1).astype(np.float32)
    mb = np.stack([(a_ > b_), (a_ >= b_), (b_ > a_)], 1).astype(np.float32)
    c["rw_masks"] = np.ascontiguousarray(np.stack([mf, mb], 0))
    c["bdones"] = ((a_ // 64) == (b_ // 64)).astype(np.float32)
    c["halfm"] = (np.arange(128)[:, None] // 64 == np.arange(2)[None, :]).astype(np.float32)
    c["rowmask"] = (np.arange(128)[:, None] // CH == np.arange(4)[None, :]).astype(np.float32)
    kk = np.arange(128)[:, None]; qq = np.arange(128)[None, :]
    c["mprev4"] = (kk >= qq).astype(np.float32)
    c["mnext4"] = (kk <= qq).astype(np.float32)
    rm = np.ones((128, T), np.float32); rm[:, ::CH] = 0.0
    c["resetm"] = rm
    rows = 2048 // 64
    row = np.repeat(np.arange(rows, dtype=np.float32), 64); col = np.tile(np.arange(64, dtype=np.float32), rows)
    inv = (np.float32(10000.0) ** (-np.arange(16, dtype=np.float32) / np.float32(16))).astype(np.float32)
    ang = np.concatenate([row[:, None] * inv, col[:, None] * inv], axis=-1).astype(np.float32)
    c["cosF"] = np.ascontiguousarray(np.concatenate([np.cos(ang), np.cos(ang)], -1).T.astype(np.float32))
    c["sinF"] = np.ascontiguousarray(np.concatenate([np.sin(ang), np.sin(ang)], -1).T.astype(np.float32))
    pm = np.zeros((64, 64), np.float32)
    for r in range(32):
        pm[r + 32, r] = -1.0
        pm[r, r + 32] = 1.0
    pm2 = np.zeros((128, 128), np.float32); pm2[:64, :64] = pm; pm2[64:, 64:] = pm
    c["Pm2"] = pm2
    c["cosF"] = np.ascontiguousarray(np.concatenate([c["cosF"], c["cosF"]], 0))
    c["sinF"] = np.ascontiguousarray(np.concatenate([c["sinF"], c["sinF"]], 0))
    return c


class Buf:
    def __init__(self, t, name):
        self.t = t; self.r = Reg(name)

    def __getitem__(self, i):
        return self.t[i]


class Prog:
    def __init__(self, layers, taps=(), stop=99):
        self.layers = layers; self.taps = set(taps); self.stop = stop
        self.nc = bass.Bass("TRN2", target_bir_lowering=False)
        self.st = contextlib.ExitStack()
        self.kb = KB(self.nc, self.st)
        self.din = {}; self.dout = {}
        self.psi = 0

    def inp(self, name, shape, dt=F32):
        a = self.nc.dram_tensor(name, list(shape), dt, kind="ExternalInput").ap()
        self.din[name] = a
        return a

    def outp(self, name, shape, dt=F32):
        a = self.nc.dram_tensor(name, list(shape), dt, kind="ExternalOutput").ap()
        self.dout[name] = a
        return a

    def scratch(self, name, shape, dt=F32):
        if name in self.taps:
            return Buf(self.outp(name, shape, dt), name)
        return Buf(self.nc.dram_tensor(name, list(shape), dt, kind="Internal").ap(), name)

    def sb(self, name, shape, dt=F32):
        return Buf(self.st.enter_context(self.nc.sbuf_tensor(name, list(shape), dt)), name)

    def next_ps(self):
        b = self.ps[self.psi]; self.psi = (self.psi + 1) % 8
        return b

    def _rw(self, R, W):
        return [b.r for b in R], [b.r for b in W]

    def MM(self, out, lhsT, rhs, R, W, start=True, stop=True):
        r, w = self._rw(R, W)
        self.kb.op(self.kb.pe, lambda e: e.matmul(out, lhsT=lhsT, rhs=rhs, start=start, stop=stop), r, w)

    def TR(self, out, in_, R, W, n=128):
        r, w = self._rw(R + [self.ident], W)
        idn = self.ident[0:n, 0:n]
        self.kb.op(self.kb.pe, lambda e: e.transpose(out=out, in_=in_, identity=idn), r, w)

    def ACT(self, out, in_, func, R, W, bias=None, scale=None):
        r, w = self._rw(R, W)
        kw = {}
        if bias is not None: kw["bias"] = bias
        if scale is not None: kw["scale"] = scale
        self.kb.op(self.kb.act, lambda e: e.activation(out=out, in_=in_, func=func, **kw), r, w)

    def TT(self, out, a, b, op, R, W, eng=None):
        r, w = self._rw(R, W)
        self.kb.op(eng or self.kb.dve, lambda e: e.tensor_tensor(out=out, in0=a, in1=b, op=op), r, w)

    def TS(self, out, a, s1, op0, R, W, s2=None, op1=None, eng=None):
        r, w = self._rw(R, W)
        if op1 is None:
            self.kb.op(eng or self.kb.dve, lambda e: e.tensor_scalar(out=out, in0=a, scalar1=s1, scalar2=None, op0=op0), r, w)
        else:
            self.kb.op(eng or self.kb.dve, lambda e: e.tensor_scalar(out=out, in0=a, scalar1=s1, scalar2=s2, op0=op0, op1=op1), r, w)

    def STT(self, out, a, s, b, op0, op1, R, W):
        r, w = self._rw(R, W)
        self.kb.op(self.kb.dve, lambda e: e.scalar_tensor_tensor(out=out, in0=a, scalar=s, in1=b, op0=op0, op1=op1), r, w)

    def CP(self, out, in_, R, W, eng=None):
        r, w = self._rw(R, W)
        E = eng or self.kb.dve
        if E is self.kb.act:
            self.kb.op(E, lambda e: e.copy(out=out, in_=in_), r, w)
        else:
            self.kb.op(E, lambda e: e.tensor_copy(out=out, in_=in_), r, w)

    def MSET(self, ap, val, W, eng=None):
        r, w = self._rw([], W)
        self.kb.op(eng or self.kb.pool, lambda e: e.memset(ap, val), r, w)

    def RECIP(self, out, in_, R, W):
        r, w = self._rw(R, W)
        self.kb.op(self.kb.dve, lambda e: e.reciprocal(out=out, in_=in_), r, w)

    def SCAN(self, out, d0, d1, R, W, init=0.0):
        r, w = self._rw(R, W)
        self.kb.op(self.kb.dve, lambda e: e.tensor_tensor_scan(out=out, data0=d0, data1=d1, initial=init, op0=ALU.mult, op1=ALU.add), r, w)

    def DMA(self, out, in_, R, W, q=None, **kw):
        r, w = self._rw(R, W)
        self.kb.dma(q or self.kb.sp, out, in_, r, w, **kw)

    def tap(self, name, src_ap, R, shape):
        if name in self.taps:
            o = self.outp("tap_" + name, shape)
            self.DMA(o, src_ap, R, [])

    def setup(self):
        P = self
        nc = self.nc
        P.xin = Buf(P.inp("xin", [T, D]), "xin")
        P.condT = P.inp("condT", [128, 8, 2])
        hc = host_consts()
        P.cd = {k: Buf(P.inp("c_" + k, v.shape), "c_" + k) for k, v in hc.items()}
        I = lambda name, shape: (name, shape)
        wdecl = dict(
            ada_w=I("ada_w", [4, D, 6 * D]), ada_b_fm=I("ada_b_fm", [4, 128, 48]),
            ln_g=I("ln_g", [4, 2, D]), ln_b=I("ln_b", [4, 2, D]),
            ev_w_in=I("ev_w_in", [2, D, 3328]), ev_w_out=I("ev_w_out", [2, D, D]),
            hg_lb_fm=I("hg_lb_fm", [128, 4, 2]), hg_ng_fm=I("hg_ng_fm", [2, 128, 4]), attn_sink=I("attn_sink", [2, 8]),
            od_w_in=I("od_w_in", [2, D, 2432]), od_w_out=I("od_w_out", [2, D, D]),
            s5_lre=I("s5_lre", [2, 2, 128, 16]), s5_lim=I("s5_lim", [2, 2, 128, 16]), s5_ldt=I("s5_ldt", [2, 2, 128, 16]),
            s5_bT=I("s5_bT", [2, 128, 16, 2, 16]), s5_cT=I("s5_cT", [2, 128, 16, 2, 16]), s5_d_fm=I("s5_d_fm", [2, 128, 4]),
            s5_glu_w=I("s5_glu_w", [2, 512, 512]),
            rw_mu_fm=I("rw_mu_fm", [2, 128, 15]), rw_w0_fm=I("rw_w0_fm", [2, 2, 128, 4]), rw_a0_fm=I("rw_a0_fm", [2, 2, 128, 4]),
            rw_w2pad=I("rw_w2pad", [2, 2, 128, 512]), rw_a2pad=I("rw_a2pad", [2, 2, 128, 512]), rw_g2=I("rw_g2", [2, 128, 512]),
            rw_kk_fm=I("rw_kk_fm", [2, 128, 4]), rw_ka_fm=I("rw_ka_fm", [2, 128, 4]), rw_rk_fm=I("rw_rk_fm", [2, 128, 4]),
            rw_lng=I("rw_lng", [2, 512]), rw_lnb=I("rw_lnb", [2, 512]),
            moe_router_w=I("moe_router_w", [2, D, 8]), moe_router_b=I("moe_router_b", [2, 8]),
            moe_w_gate=I("moe_w_gate", [2, 8, D, DFF]), moe_w_up=I("moe_w_up", [2, 8, D, DFF]), moe_w_down=I("moe_w_down", [2, 8, DFF, D]),
            ffn_w_gate=I("ffn_w_gate", [2, D, DFF]), ffn_w_up=I("ffn_w_up", [2, D, DFF]), ffn_w_down=I("ffn_w_down", [2, DFF, D]),
        )
        P.w = _LazyW(P, {k: v[1] for k, v in wdecl.items()})
        P.out = Buf(P.outp("out", [T, D]), "out")
        P.xs = [P.scratch("xs0", [T, D]), P.scratch("xs1", [T, D])]
        if "inject_y" in P.taps:
            P.yT = Buf(P.inp("yT_inject", [D, T]), "yT_inject")
        else:
            P.yT = P.scratch("yTd", [D, T])
        P.ident = P.sb("ident", [128, 128]); P.ones = P.sb("ones", [128, 128]); P.onesdiv = P.sb("onesdiv", [128, 128])
        P.tri_le = P.sb("tri_le", [128, 128]); P.tri_ge = P.sb("tri_ge", [128, 128])
        P.mprev4 = P.sb("mprev4", [128, 128]); P.mnext4 = P.sb("mnext4", [128, 128])
        P.cst = P.sb("cst", [128, 8])
        P.hT = P.sb("hT", [128, KC, T], BF16)
        P.G = [None] * NG
        P.Gt = self.st.enter_context(nc.sbuf_tensor("G", [128, NG, T], F32))
        for i in range(NG):
            P.G[i] = Buf(P.Gt[:, i, :], f"G{i}")
        P.wA = [P.sb(f"wA{i}", [128, KC, 128], BF16) for i in range(5)]
        P.wAi = 0
        P.xb = [P.sb(f"xb{i}", [128, D]) for i in range(3)]
        P.xbi = 0
        P.zb = [P.sb("zb0", [128, D])] * 2
        P.bc = [P.sb(f"bc{i}", [128, D]) for i in range(4)]
        P.condS = P.sb("condS", [128, 8, 2]); P.adab = P.sb("adab", [128, 48])
        P.fm = P.sb("fm", [128, 48, 2]); P.sc1p = P.sb("sc1p", [128, 8, 2]); P.sc2p = P.sb("sc2p", [128, 8, 2])
        P.small = [P.sb(f"small{i}", [128, 512]) for i in range(5)]
        P.smi = 0
        P.S = P.sb("S", [128, 128]); P.tmpS = P.sb("tmpS", [128, 128])
        P.stat = P.sb("stat", [128, 32])
        P.s5t = P.sb("s5t", [128, 2, 12, 16]); P.s5i = P.sb("s5i", [128, 16], mybir.dt.int32); P.s5d = P.sb("s5d", [128, 4])
        P.s5bc = P.sb("s5bc", [128, 64]); P.s5zc = P.sb("s5zc", [128, 256])
        P.rwm = P.sb("rwm", [128, 3, 128]); P.bdones = P.sb("bdones", [128, 128])
        P.rwar = [P.sb(f"rwar{h}", [128, 256]) for h in range(2)]
        P.rwxm = [P.sb(f"rwxm{h}", [128, 4, 128]) for h in range(2)]
        P.rwxz = [P.sb(f"rwxz{h}", [128, 3, 128]) for h in range(2)]
        P.rwu = P.sb("rwu", [128, 128]); P.rwp = P.sb("rwp", [128, 48]); P.rwgc = P.sb("rwgc", [128, NT]); P.rwln = P.sb("rwln", [128, 2, 128])
        P.gates = P.sb("gates", [128, NT, 8]); P.rw = P.sb("rw", [128, KC, 8]); P.rb = P.sb("rb", [128, 8])
        P.h32 = [P.sb("h32_0", [128, 4, 128])] * 2; P.rt = P.sb("rt", [128, 64])
        P.lbt = P.sb("lbt", [128, 16]); P.ngt = P.sb("ngt", [128, 4]); P.sk = P.sb("sk", [128, 8])
        P.Pm2 = P.sb("Pm2", [128, 128])
        P.attm = [P.sb(f"attm{i}", [128, 128]) for i in range(2)]; P.attmi = 0
        P.ktok = [P.sb(f"ktok{i}", [128, 4, 128]) for i in range(2)]
        P.rowmask = P.sb("rowmask", [128, 4]); P.halfm = P.sb("halfm", [128, 2])
        P.ps = [Buf(self.st.enter_context(nc.psum_tensor(f"ps{i}", [128, 512], F32)), f"ps{i}") for i in range(8)]
        for k, dst in (("ident", P.ident), ("tri_le", P.tri_le), ("tri_ge", P.tri_ge), ("mprev4", P.mprev4),
                       ("mnext4", P.mnext4), ("Pm2", P.Pm2), ("rowmask", P.rowmask), ("halfm", P.halfm), ("bdones", P.bdones)):
            P.DMA(dst[:], P.cd[k][:], [], [dst])
        P.MSET(P.ones[:], 1.0, [P.ones]); P.MSET(P.onesdiv[:], 1.0 / 128.0, [P.onesdiv])
        P.MSET(P.cst[:, 0:1], 1e-6, [P.cst]); P.MSET(P.cst[:, 1:2], 1e-5, [P.cst]); P.MSET(P.cst[:, 2:3], 64e-5, [P.cst])
        P.MSET(P.cst[:, 3:4], 0.0, [P.cst]); P.MSET(P.cst[:, 4:5], 1.0, [P.cst])
        P.DMA(P.condS[:], P.condT, [], [P.condS])
        P.ACT(P.condS[:], P.condS[:], AF.Silu, [P.condS], [P.condS])

    def gflat(self, slot0, nelem, dt=F32):
        flat = self.Gt[:].rearrange("p a n -> p (a n)")[:, slot0 * T:slot0 * T + nelem]
        ns = (nelem + T - 1) // T
        regs = [self.G[slot0 + i] for i in range(ns)]
        if dt is not F32:
            flat = flat.bitcast(dt)
        return flat, regs

    def sm(self):
        b = self.small[self.smi]; self.smi = (self.smi + 1) % len(self.small)
        return b

    def loadw(self, src, ncols, kc=KC):
        b = self.wA[self.wAi]; self.wAi = (self.wAi + 1) % len(self.wA)
        self.DMA(b[:, 0:kc, 0:ncols], src.rearrange("(c p) n -> p c n", p=128), [], [b], q=self.kb.pool)
        return b

    def phase_mod(self, l):
        P = self
        P.DMA(P.adab[:], P.w["ada_b_fm"][l], [], [P.adab])
        pM = P.next_ps()
        for blk in range(12):
            fl, regs = P.gflat((blk % 2) * 2, 4096)
            stg = fl.rearrange("p (c n) -> p c n", n=512)
            for ch in range(8):
                P.DMA(stg[:, ch, :], P.w["ada_w"][l, ch * 128:(ch + 1) * 128, blk * 512:(blk + 1) * 512], [], regs,
                      q=(P.kb.sp if ch % 2 == 0 else P.kb.act))
            for s in range(4):
                k = blk * 4 + s
                for ch in range(8):
                    P.MM(pM[:, 2 * k:2 * k + 2], stg[:, ch, s * 128:(s + 1) * 128], P.condS[:, ch, :], regs + [P.condS], [pM],
                         start=(ch == 0), stop=(ch == 7))
        for cond in range(2):
            P.TT(P.fm[:, :, cond], pM[:, cond:96:2], P.adab[:], ALU.add, [pM, P.adab], [P.fm])
        P.TS(P.sc1p[:], P.fm[:, 8:16, :], 1.0, ALU.add, [P.fm], [P.sc1p])
        P.TS(P.sc2p[:], P.fm[:, 32:40, :], 1.0, ALU.add, [P.fm], [P.sc2p])

    def gate_bcast(self, q, cond, dst):
        P = self
        for half in range(2):
            pg = P.next_ps()
            for cc in range(4):
                c = half * 4 + cc
                dg = P.sm()
                P.TS(dg[:, 0:128], P.ident[:], P.fm[:, q * 8 + c, cond:cond + 1], ALU.mult, [P.ident, P.fm], [dg])
                P.MM(pg[:, cc * 128:(cc + 1) * 128], P.ones[:], dg[:, 0:128], [P.ones, dg], [pg])
            P.CP(dst[:, half * 512:(half + 1) * 512], pg[:, :], [pg], [dst], eng=P.kb.act)

    def hT_tile(self, xt, ti, scp, shq, router=False):
        P = self
        cond = 1 if ti < 2 else 0
        pl = P.next_ps() if router else None
        for half in range(2):
            pt = P.next_ps()
            for cc in range(4):
                c = half * 4 + cc
                P.TR(pt[:, cc * 128:(cc + 1) * 128], xt[:, c * 128:(c + 1) * 128], [xt], [pt])
            h32 = P.h32[half]
            for cc in range(4):
                c = half * 4 + cc
                if router:
                    P.TS(h32[:, cc, :], pt[:, cc * 128:(cc + 1) * 128], scp[:, c, cond:cond + 1], ALU.mult, [pt, scp, P.fm], [h32],
                         s2=P.fm[:, shq * 8 + c, cond:cond + 1], op1=ALU.add)
                    P.CP(P.hT[:, c, ti * 128:(ti + 1) * 128], h32[:, cc, :], [h32], [P.hT], eng=P.kb.act)
                else:
                    P.ACT(P.hT[:, c, ti * 128:(ti + 1) * 128], pt[:, cc * 128:(cc + 1) * 128], AF.Identity,
                          [pt, scp, P.fm], [P.hT], scale=scp[:, c, cond:cond + 1], bias=P.fm[:, shq * 8 + c, cond:cond + 1])
            if router:
                for cc in range(4):
                    c = half * 4 + cc
                    P.MM(pl[:, half * 8:half * 8 + 8], h32[:, cc, :], P.rw[:, c, :], [h32, P.rw], [pl], start=(cc == 0), stop=(cc == 3))
        if router and "1" != "2":
            P.top2(pl, ti)

    def top2(self, pl, ti):
        P = self
        rt = P.rt
        R_, W_ = [rt], [rt]
        lg, e1, l2, e2 = rt[:, 0:8], rt[:, 8:16], rt[:, 16:24], rt[:, 24:32]
        m1, m2, dd, p1, p2 = rt[:, 32:33], rt[:, 33:34], rt[:, 34:35], rt[:, 35:36], rt[:, 36:37]
        P.TT(lg, pl[:, 0:8], P.rb[:], ALU.add, [pl, P.rb], W_)
        P.TT(lg, lg, pl[:, 8:16], ALU.add, [pl, rt], W_)
        P.kb.op(P.kb.dve, lambda e: e.reduce_max(out=m1, in_=lg, axis=AX.X), [rt.r], [rt.r])
        P.TS(e1, lg, m1, ALU.is_equal, R_, W_)
        P.STT(l2, e1, -1e30, lg, ALU.mult, ALU.add, R_, W_)
        P.kb.op(P.kb.dve, lambda e: e.reduce_max(out=m2, in_=l2, axis=AX.X), [rt.r], [rt.r])
        P.TS(e2, l2, m2, ALU.is_equal, R_, W_)
        P.TT(dd, m2, m1, ALU.subtract, R_, W_)
        P.ACT(dd, dd, AF.Exp, R_, W_)
        P.TS(p1, dd, 1.0, ALU.add, R_, W_)
        P.RECIP(p1, p1, R_, W_)
        P.TT(p2, dd, p1, ALU.mult, R_, W_)
        P.TS(e1, e1, p1, ALU.mult, R_, W_)
        P.STT(P.gates[:, ti, :], e2, p2, e1, ALU.mult, ALU.add, R_, [P.gates])

    def phase_hT(self, xsrc):
        P = self
        for ti in range(NT):
            xt = P.xb[P.xbi]; P.xbi = (P.xbi + 1) % 3
            P.DMA(xt[:], xsrc[ti * 128:(ti + 1) * 128, :], [xsrc], [xt])
            P.hT_tile(xt, ti, P.sc1p, 0)

    def proj_fm(self, wb, M, evac, col0=0):
        P = self
        for (t0, n) in NTILES:
            pb = P.next_ps()
            for c in range(KC):
                P.MM(pb[0:M, 0:n], wb[:, c, col0:col0 + M], P.hT[:, c, t0:t0 + n], [wb, P.hT], [pb], start=(c == 0), stop=(c == 7))
            evac(pb, t0, n)

    def hgrn2(self, l, j):
        P = self
        G = P.G
        W = P.w["ev_w_in"][j]
        lbt = P.lbt; ngt = P.ngt
        P.DMA(ngt[:, 0:4], P.w["hg_ng_fm"][j], [], [ngt])
        if j == 0:
            P.MSET(lbt[:, 0:4], 0.0, [lbt]); P.MSET(lbt[:, 4:8], 1.0, [lbt])
        else:
            P.DMA(lbt[:, 8:16], P.w["hg_lb_fm"].rearrange("p h j -> p (h j)"), [], [lbt])
            P.TT(lbt[:, 0:4], lbt[:, 9:16:2], lbt[:, 8:16:2], ALU.subtract, [lbt], [lbt])
            P.ACT(lbt[:, 0:4], lbt[:, 0:4], AF.Sigmoid, [lbt], [lbt])
            P.TS(lbt[:, 4:8], lbt[:, 0:4], -1.0, ALU.mult, [lbt], [lbt], s2=1.0, op1=ALU.add)
        qT, sgT, X1, X2, X3, X4, oacc = G[0], G[1], G[2], G[3], G[4], G[5], G[8]
        P.resetm = G[9]
        P.DMA(P.resetm[:], P.cd["resetm"][:], [], [P.resetm])
        itok = P.Gt[:, 6, :].rearrange("p (b c) -> p b c", c=128)
        itr = [G[6]]
        for hd in range(4):
            cs = lambda k: W[:, k * 512 + hd * 128:k * 512 + (hd + 1) * 128]
            wq = P.loadw(cs(0), 128)
            P.proj_fm(wq, 128, lambda pb, t0, n: P.ACT(qT[:, t0:t0 + n], pb[:, 0:n], AF.Silu, [pb], [qT]))
            wg = P.loadw(cs(4), 128)
            P.proj_fm(wg, 128, lambda pb, t0, n: P.ACT(sgT[:, t0:t0 + n], pb[:, 0:n], AF.Silu, [pb], [sgT]))
            wi = P.loadw(cs(3), 128)
            for b0 in range(0, NT, 4):
                nb = min(4, NT - b0)
                pi = P.next_ps()
                for q in range(nb):
                    ti = b0 + q
                    for c in range(KC):
                        P.MM(pi[:, q * 128:(q + 1) * 128], P.hT[:, c, ti * 128:(ti + 1) * 128], wi[:, c, :], [P.hT, wi], [pi],
                             start=(c == 0), stop=(c == 7))
                P.CP(itok[:, b0:b0 + nb, :], pi[:, 0:nb * 128].rearrange("p (a b) -> p a b", b=128), [pi], itr, eng=P.kb.act)
            for d in range(2):
                fwd = d == 0
                wf = P.loadw(cs(1 + d), 128)
                P.proj_fm(wf, 128, lambda pb, t0, n: P.ACT(X1[:, t0:t0 + n], pb[:, 0:n], AF.Sigmoid, [pb], [X1]))
                P.TS(X1[:], X1[:], lbt[:, 4 + hd:5 + hd], ALU.mult, [X1, lbt], [X1], s2=lbt[:, hd:hd + 1], op1=ALU.add)
                P.TS(X2[:], X1[:], -1.0, ALU.mult, [X1], [X2], s2=1.0, op1=ALU.add, eng=P.kb.pool)
                P.ACT(X1[:], X1[:], AF.Ln, [X1], [X1])
                if fwd:
                    P.SCAN(X3[:], P.resetm[:], X1[:], [P.resetm, X1], [X3])
                else:
                    P.SCAN(X3[:, ::-1], P.resetm[:], X1[:, ::-1], [P.resetm, X1], [X3])
                P.ACT(X1[:], X3[:], AF.Exp, [X3], [X1])
                P.STT(X4[:], qT[:], 128.0 ** -0.5, X1[:], ALU.mult, ALU.mult, [qT, X1], [X4])
                P.ACT(X3[:], X3[:], AF.Exp, [X3], [X3], scale=-1.0)
                P.TT(X2[:], X2[:], X3[:], ALU.mult, [X2, X3], [X2], eng=P.kb.pool)
                P.MSET(P.S[:], 0.0, [P.S])
                tiles = list(range(NT)) if fwd else [1, 0] + list(range(NT - 1, 1, -1))
                mask = P.tri_le if fwd else P.tri_ge
                for ti in tiles:
                    tsl = slice(ti * 128, (ti + 1) * 128)
                    pA = P.next_ps()
                    P.MM(pA[:, 0:128], X2[:, tsl], X4[:, tsl], [X2, X4], [pA])
                    attm = P.attm[P.attmi]; ktok = P.ktok[P.attmi]; P.attmi ^= 1
                    P.TT(attm[:], pA[:, 0:128], mask[:], ALU.mult, [pA, mask], [attm])
                    pB = P.next_ps()
                    P.TR(pB[:, 0:128], X2[:, tsl], [X2], [pB])
                    for q in range(4):
                        P.ACT(ktok[:, q, :], pB[:, 0:128], AF.Identity, [pB, P.rowmask], [ktok], scale=P.rowmask[:, q:q + 1])
                    pC = P.next_ps()
                    P.MM(pC[:, 0:128], itok[:, ti, :], attm[:], itr + [attm], [pC], start=True, stop=False)
                    for q in (range(4) if fwd else range(3, -1, -1)):
                        c0 = ti * 128 + q * CH
                        csl = slice(c0, c0 + CH); prt = slice(q * CH, (q + 1) * CH)
                        P.MM(pC[:, q * CH:(q + 1) * CH], P.S[:], X4[:, csl], [P.S, X4], [pC], start=False, stop=True)
                        pD = P.next_ps()
                        P.MM(pD[:, 0:128], ktok[:, q, :], itok[:, ti, :], [ktok] + itr, [pD])
                        P.TT(P.tmpS[:], pD[:, 0:128], P.S[:], ALU.add, [pD, P.S], [P.tmpS])
                        last = c0 + CH - 1 if fwd else c0
                        P.ACT(P.S[:], P.tmpS[:], AF.Identity, [P.tmpS, X1], [P.S], scale=X1[:, last:last + 1])
                    if fwd:
                        P.CP(oacc[:, tsl], pC[:, 0:128], [pC], [oacc], eng=P.kb.act)
                    else:
                        P.TT(oacc[:, tsl], oacc[:, tsl], pC[:, 0:128], ALU.add, [oacc, pC], [oacc])
            for (t0, n) in NTILES:
                sq = P.sm()
                P.ACT(sq[:, 0:n], oacc[:, t0:t0 + n], AF.Square, [oacc], [sq])
                pE = P.next_ps()
                P.MM(pE[:, 0:n], P.onesdiv[:], sq[:, 0:n], [P.onesdiv, sq], [pE])
                rs = P.sm()
                P.ACT(rs[:, 0:n], pE[:, 0:n], AF.Sqrt, [pE, P.cst], [rs], bias=P.cst[:, 0:1])
                P.RECIP(rs[:, 0:n], rs[:, 0:n], [rs], [rs])
                yo = P.sm()
                P.STT(yo[:, 0:n], oacc[:, t0:t0 + n], ngt[:, hd:hd + 1], rs[:, 0:n], ALU.mult, ALU.mult, [oacc, ngt, rs], [yo])
                P.TT(yo[:, 0:n], yo[:, 0:n], sgT[:, t0:t0 + n], ALU.mult, [yo, sgT], [yo], eng=P.kb.pool)
                P.DMA(P.yT[hd * 128:(hd + 1) * 128, t0:t0 + n], yo[:, 0:n], [yo], [P.yT])

    def attention(self, l, j):
        P = self
        G = P.G
        W = P.w["ev_w_in"][j]
        sk = P.sk
        P.DMA(sk[:, 0:8], P.w["attn_sink"][j].partition_broadcast(128), [], [sk])
        P.ACT(sk[:, 0:8], sk[:, 0:8], AF.Exp, [sk], [sk])
        cosT, sinT, kT2, q2 = G[0], G[1], G[2], G[3]
        qm = [G[4], G[5], G[6], G[7]]
        ptb = [(P.Gt[:, 8, i * 512:(i + 1) * 512], G[8]) for i in range(4)] + [(P.Gt[:, 9, 0:512], G[9])]
        vaug = P.Gt[:, 9, 512:512 + NT * 65].rearrange("p (t e) -> p t e", e=65)
        vreg = [G[9]]
        P.DMA(cosT[:, 0:2048], P.cd["cosF"][:], [], [cosT]); P.DMA(sinT[:, 0:2048], P.cd["sinF"][:], [], [sinT])

        def rope(src):
            for (t0, n) in NTILES[1:]:
                pr = P.next_ps()
                P.MM(pr[:, 0:n], P.Pm2[:], src[:, t0:t0 + n], [P.Pm2, src], [pr])
                t1 = P.sm(); t2 = P.sm()
                P.TT(t1[:, 0:n], src[:, t0:t0 + n], cosT[:, t0 - LC:t0 - LC + n], ALU.mult, [src, cosT], [t1])
                P.TT(t2[:, 0:n], pr[:, 0:n], sinT[:, t0 - LC:t0 - LC + n], ALU.mult, [pr, sinT], [t2])
                P.TT(src[:, t0:t0 + n], t1[:, 0:n], t2[:, 0:n], ALU.add, [t1, t2], [src], eng=P.kb.pool)

        for g in range(2):
            wv = P.loadw(W[:, 3200 + g * 64:3200 + (g + 1) * 64], 64)
            P.MSET(vaug[:, :, 64:65], 1.0, vreg)
            for ti in range(NT):
                pv = P.next_ps()
                for c in range(KC):
                    P.MM(pv[:, 0:64], P.hT[:, c, ti * 128:(ti + 1) * 128], wv[:, c, 0:64], [P.hT, wv], [pv], start=(c == 0), stop=(c == 7))
                P.CP(vaug[:, ti, 0:64], pv[:, 0:64], [pv], vreg, eng=P.kb.act)
            wk = P.wA[P.wAi]; P.wAi = (P.wAi + 1) % len(P.wA)
            ksrc = W[:, 3072 + g * 64:3072 + (g + 1) * 64].rearrange("(c p) n -> p c n", p=128)
            P.DMA(wk[:, :, 0:64], ksrc, [], [wk], q=P.kb.pool)
            P.DMA(wk[:, :, 64:128], ksrc, [], [wk], q=P.kb.pool)
            P.proj_fm(wk, 128, lambda pb, t0, n: P.CP(kT2[:, t0:t0 + n], pb[:, 0:n], [pb], [kT2], eng=P.kb.act))
            rope(kT2)
            for pair in range(2):
                h0 = g * 4 + pair * 2
                wq = P.loadw(W[:, 2560 + h0 * 64:2560 + (h0 + 2) * 64], 128)
                P.proj_fm(wq, 128, lambda pb, t0, n: P.CP(q2[:, t0:t0 + n], pb[:, 0:n], [pb], [q2], eng=P.kb.act))
                rope(q2)
                for half in range(2):
                    dst = qm[pair * 2 + half]
                    P.TS(dst[:], q2[:], P.halfm[:, half:half + 1], ALU.mult, [q2, P.halfm], [dst], eng=(P.kb.pool if half else P.kb.dve))
            for ti in range(NT):
                if ti < 2:
                    keys = [(0, 'c'), (1, 'c')]
                else:
                    keys = [(kt, kd) for kt, kd in ((ti - 1, 'p'), (ti, 's'), (ti + 1, 'n')) if 2 <= kt < NT] + [(0, 'c'), (1, 'c')]
                pts = []
                for ki, (kt, kd) in enumerate(keys):
                    pS = P.next_ps()
                    for hh in range(4):
                        P.MM(pS[:, hh * 128:(hh + 1) * 128], kT2[:, kt * 128:(kt + 1) * 128], qm[hh][:, ti * 128:(ti + 1) * 128],
                             [kT2, qm[hh]], [pS])
                    pt, pr_ = ptb[ki]
                    P.ACT(pt, pS[:, 0:512], AF.Exp, [pS], [pr_], scale=0.125)
                    if kd in ('p', 'n'):
                        mk_ = P.mprev4 if kd == 'p' else P.mnext4
                        for hh in range(4):
                            P.TT(pt[:, hh * 128:(hh + 1) * 128], pt[:, hh * 128:(hh + 1) * 128], mk_[:], ALU.mult, [pr_, mk_], [pr_],
                                 eng=(P.kb.pool if hh % 2 else P.kb.dve))
                    pts.append((pt, pr_, kt))
                pO = P.next_ps()
                for hh in range(4):
                    for ki, (pt, pr_, kt) in enumerate(pts):
                        P.MM(pO[:, hh * 65:(hh + 1) * 65], pt[:, hh * 128:(hh + 1) * 128], vaug[:, kt, :], [pr_] + vreg, [pO],
                             start=(ki == 0), stop=(ki == len(pts) - 1))
                den = P.sm()
                P.TT(den[:, 0:4], pO[:, 64:260:65], sk[:, g * 4:(g + 1) * 4], ALU.add, [pO, sk], [den])
                P.RECIP(den[:, 0:4], den[:, 0:4], [den], [den])
                ob = P.sm()
                for hh in range(4):
                    P.TS(ob[:, hh * 64:(hh + 1) * 64], pO[:, hh * 65:hh * 65 + 64], den[:, hh:hh + 1], ALU.mult, [pO, den], [ob])
                for half in range(2):
                    pT = P.next_ps()
                    P.TR(pT[:, 0:128], ob[:, half * 128:(half + 1) * 128], [ob], [pT])
                    yo = P.sm()
                    P.CP(yo[:, 0:128], pT[:, 0:128], [pT], [yo], eng=P.kb.act)
                    r0 = 512 + g * 256 + half * 128
                    P.DMA(P.yT[r0:r0 + 128, ti * 128:(ti + 1) * 128], yo[:, 0:128], [yo], [P.yT])

    def resid_ln(self, z, xt, ti, xn):
        P = self
        P.STT(z[:], xt[:], ALPHA, z[:], ALU.mult, ALU.add, [xt, z], [z])
        st = P.stat
        for hf in range(2):
            P.kb.op(P.kb.dve, lambda e, hf=hf: e.bn_stats(out=st[:, hf * 6:(hf + 1) * 6], in_=z[:, hf * 512:(hf + 1) * 512]), [z.r], [st.r])
        P.kb.op(P.kb.dve, lambda e: e.bn_aggr(out=st[:, 12:14], in_=st[:, 0:12]), [st.r], [st.r])
        P.ACT(st[:, 14:15], st[:, 13:14], AF.Sqrt, [st, P.cst], [st], bias=P.cst[:, 1:2])
        P.RECIP(st[:, 14:15], st[:, 14:15], [st], [st])
        P.TS(z[:], z[:], st[:, 12:13], ALU.subtract, [z, st], [z], s2=st[:, 14:15], op1=ALU.mult)
        P.TT(z[:], z[:], P.bc[2][:], ALU.mult, [z, P.bc[2]], [z], eng=P.kb.pool)
        P.TT(xn[:], z[:], P.bc[3][:], ALU.add, [z, P.bc[3]], [xn], eng=P.kb.pool)

    def load_ln(self, l, k):
        P = self
        P.DMA(P.bc[2][:], P.w["ln_g"][l, k].partition_broadcast(128), [], [P.bc[2]])
        P.DMA(P.bc[3][:], P.w["ln_b"][l, k].partition_broadcast(128), [], [P.bc[3]])

    def phase_out(self, l, wout_dram, xsrc, xdst, router_j=None):
        P = self
        P.gate_bcast(2, 0, P.bc[0]); P.gate_bcast(2, 1, P.bc[1]); P.load_ln(l, 0)
        if router_j is not None:
            P.DMA(P.rw[:], P.w["moe_router_w"][router_j].rearrange("(c p) n -> p c n", p=128), [], [P.rw])
            P.DMA(P.rb[:], P.w["moe_router_b"][router_j].partition_broadcast(128), [], [P.rb])
        wof, wor = P.gflat(0, 4096, BF16)
        wo = wof.rearrange("p (c n) -> p c n", n=1024)
        P.DMA(wo, wout_dram.rearrange("(c p) n -> p c n", p=128), [], wor, q=P.kb.pool)
        ytb = [P.gflat(2 + i, 512, BF16)[0].rearrange("p (c n) -> p c n", n=128) for i in range(2)]
        for ti in range(NT):
            cond = 1 if ti < 2 else 0
            yt, yr = ytb[ti % 2], P.G[2 + ti % 2]
            P.DMA(yt, P.yT[:, ti * 128:(ti + 1) * 128].rearrange("(c p) n -> p c n", p=128), [P.yT], [yr], q=P.kb.pool)
            xt = P.xb[P.xbi]; P.xbi = (P.xbi + 1) % 3
            P.DMA(xt[:], xsrc[ti * 128:(ti + 1) * 128, :], [xsrc], [xt])
            z = P.zb[ti % 2]
            for hf in range(2):
                po = P.next_ps()
                for c in range(KC):
                    P.MM(po[:, :], yt[:, c, :], wo[:, c, hf * 512:(hf + 1) * 512], [yr] + wor, [po], start=(c == 0), stop=(c == 7))
                P.TT(z[:, hf * 512:(hf + 1) * 512], po[:, :], P.bc[cond][:, hf * 512:(hf + 1) * 512], ALU.mult, [po, P.bc[cond]], [z])
            xn = P.xb[P.xbi]; P.xbi = (P.xbi + 1) % 3
            P.resid_ln(z, xt, ti, xn)
            P.DMA(xdst[ti * 128:(ti + 1) * 128, :], xn[:], [xn], [xdst])
            P.hT_tile(xn, ti, P.sc2p, 3, router=(router_j is not None))

    def ffn_tile_weights(self, wg, wu, wd):
        pass

    def phase_ffn(self, l, j, xsrc, xdst, lat_only_out=None):
        P = self
        P.gate_bcast(5, 0, P.bc[0]); P.gate_bcast(5, 1, P.bc[1]); P.load_ln(l, 1)
        Wg, Wu, Wd = P.w["ffn_w_gate"][j], P.w["ffn_w_up"][j], P.w["ffn_w_down"][j]
        wdf, wdr = P.gflat(0, NFC * 512, BF16)
        wd = wdf.rearrange("p (f n) -> p f n", n=1024)
        P.DMA(wd, Wd.rearrange("(f p) n -> p f n", p=128), [], wdr, q=P.kb.pool)
        acf, acr = P.gflat(5, NFC * 256, BF16)
        actT = acf.rearrange("p (f n) -> p f n", n=512)
        for (t0, n) in NTILES:
            for fc in range(NFC):
                wgb = P.loadw(Wg[:, fc * 128:(fc + 1) * 128], 128)
                wub = P.loadw(Wu[:, fc * 128:(fc + 1) * 128], 128)
                pg = P.next_ps(); pu = P.next_ps()
                for c in range(KC):
                    P.MM(pg[:, 0:n], wgb[:, c, :], P.hT[:, c, t0:t0 + n], [wgb, P.hT], [pg], start=(c == 0), stop=(c == 7))
                for c in range(KC):
                    P.MM(pu[:, 0:n], wub[:, c, :], P.hT[:, c, t0:t0 + n], [wub, P.hT], [pu], start=(c == 0), stop=(c == 7))
                sg = P.sm()
                P.ACT(sg[:, 0:n], pg[:, 0:n], AF.Silu, [pg], [sg])
                P.TT(actT[:, fc, 0:n], sg[:, 0:n], pu[:, 0:n], ALU.mult, [sg, pu], acr)
            for sub in range(n // 128):
                ti = t0 // 128 + sub
                cond = 1 if ti < 2 else 0
                xt = P.xb[P.xbi]; P.xbi = (P.xbi + 1) % 3
                P.DMA(xt[:], xsrc[ti * 128:(ti + 1) * 128, :], [xsrc], [xt])
                z = P.zb[ti % 2]
                for hf in range(2):
                    po = P.next_ps()
                    for fc in range(NFC):
                        P.MM(po[:, :], actT[:, fc, sub * 128:(sub + 1) * 128], wd[:, fc, hf * 512:(hf + 1) * 512], acr + wdr, [po],
                             start=(fc == 0), stop=(fc == NFC - 1))
                    P.TT(z[:, hf * 512:(hf + 1) * 512], po[:, :], P.bc[cond][:, hf * 512:(hf + 1) * 512], ALU.mult, [po, P.bc[cond]], [z])
                xn = P.xb[P.xbi]; P.xbi = (P.xbi + 1) % 3
                P.resid_ln(z, xt, ti, xn)
                P.DMA(xdst[ti * 128:(ti + 1) * 128, :], xn[:], [xn], [xdst])

    def phase_moe(self, l, j, xsrc, xdst):
        P = self
        P.gate_bcast(5, 0, P.bc[0]); P.gate_bcast(5, 1, P.bc[1]); P.load_ln(l, 1)
        wdf, wdr = P.gflat(0, NFC * 512, BF16)
        wd = wdf.rearrange("p (f n) -> p f n", n=1024)
        acf, acr = P.gflat(5, NFC * 256, BF16)
        actT = acf.rearrange("p (f n) -> p f n", n=512)
        accf, accr = P.gflat(8, 4096)
        acc = accf.rearrange("p (s n) -> p s n", n=1024)
        for (t0, n) in NTILES:
            nsub = n // 128
            for ex in range(8):
                Wg, Wu, Wd = P.w["moe_w_gate"][j, ex], P.w["moe_w_up"][j, ex], P.w["moe_w_down"][j, ex]
                P.DMA(wd, Wd.rearrange("(f p) n -> p f n", p=128), [], wdr, q=P.kb.pool)
                for fc in range(NFC):
                    wgb = P.loadw(Wg[:, fc * 128:(fc + 1) * 128], 128)
                    wub = P.loadw(Wu[:, fc * 128:(fc + 1) * 128], 128)
                    pg = P.next_ps(); pu = P.next_ps()
                    for c in range(KC):
                        P.MM(pg[:, 0:n], wgb[:, c, :], P.hT[:, c, t0:t0 + n], [wgb, P.hT], [pg], start=(c == 0), stop=(c == 7))
                    for c in range(KC):
                        P.MM(pu[:, 0:n], wub[:, c, :], P.hT[:, c, t0:t0 + n], [wub, P.hT], [pu], start=(c == 0), stop=(c == 7))
                    sg = P.sm()
                    P.ACT(sg[:, 0:n], pg[:, 0:n], AF.Silu, [pg], [sg])
                    P.TT(actT[:, fc, 0:n], sg[:, 0:n], pu[:, 0:n], ALU.mult, [sg, pu], acr)
                for sub in range(nsub):
                    ti = t0 // 128 + sub
                    for hf in range(2):
                        po = P.next_ps()
                        for fc in range(NFC):
                            P.MM(po[:, :], actT[:, fc, sub * 128:(sub + 1) * 128], wd[:, fc, hf * 512:(hf + 1) * 512], acr + wdr, [po],
                                 start=(fc == 0), stop=(fc == NFC - 1))
                        a = acc[:, sub, hf * 512:(hf + 1) * 512]
                        if ex == 0:
                            P.TS(a, po[:, :], P.gates[:, ti, ex:ex + 1], ALU.mult, [po, P.gates], accr)
                        else:
                            P.STT(a, po[:, :], P.gates[:, ti, ex:ex + 1], a, ALU.mult, ALU.add, [po, P.gates] + accr, accr)
            for sub in range(nsub):
                ti = t0 // 128 + sub
                cond = 1 if ti < 2 else 0
                xt = P.xb[P.xbi]; P.xbi = (P.xbi + 1) % 3
                P.DMA(xt[:], xsrc[ti * 128:(ti + 1) * 128, :], [xsrc], [xt])
                z = P.zb[ti % 2]
                P.TT(z[:], acc[:, sub, :], P.bc[cond][:], ALU.mult, accr + [P.bc[cond]], [z])
                xn = P.xb[P.xbi]; P.xbi = (P.xbi + 1) % 3
                P.resid_ln(z, xt, ti, xn)
                P.DMA(xdst[ti * 128:(ti + 1) * 128, :], xn[:], [xn], [xdst])

    def s5(self, l, j):
        P = self
        G = P.G
        I32 = mybir.dt.int32
        TWO_PI = 2.0 * math.pi
        W = P.w["od_w_in"][j]
        st_ = P.s5t
        R_, W_ = [st_, P.s5i], [st_]
        P.DMA(P.s5d[:], P.w["s5_d_fm"][j], [], [P.s5d])
        LRE, LIM, DT, MAG, THN, SN, CS, CRE, CIM, NCIM, TA, TB = range(12)
        for d in range(2):
            q = lambda k: st_[:, d, k, :]
            P.DMA(q(LRE), P.w["s5_lre"][j, d], [], W_); P.DMA(q(LIM), P.w["s5_lim"][j, d], [], W_); P.DMA(q(DT), P.w["s5_ldt"][j, d], [], W_)
            P.ACT(q(DT), q(DT), AF.Exp, R_, W_)
            P.TS(q(LRE), q(LRE), -1e-4, ALU.min, R_, W_)
            P.TT(q(TA), q(LRE), q(DT), ALU.mult, R_, W_)
            P.ACT(q(MAG), q(TA), AF.Exp, R_, W_)
            P.TT(q(THN), q(LIM), q(DT), ALU.mult, R_, W_)
            P.TS(q(THN), q(THN), 1.0 / TWO_PI, ALU.mult, R_, W_)
            P.CP(P.s5i[:], q(THN), R_, [P.s5i]); P.CP(q(TA), P.s5i[:], R_, W_)
            P.TT(q(TA), q(THN), q(TA), ALU.subtract, R_, W_)
            P.ACT(q(SN), q(TA), AF.Sin, R_, W_, scale=TWO_PI)
            P.TS(q(TA), q(TA), 0.25, ALU.add, R_, W_)
            P.CP(P.s5i[:], q(TA), R_, [P.s5i]); P.CP(q(TB), P.s5i[:], R_, W_)
            P.TT(q(TA), q(TA), q(TB), ALU.subtract, R_, W_)
            P.ACT(q(CS), q(TA), AF.Sin, R_, W_, scale=TWO_PI)
            P.TT(q(CS), q(CS), q(MAG), ALU.mult, R_, W_)
            P.TT(q(SN), q(SN), q(MAG), ALU.mult, R_, W_)
            P.TT(q(TA), q(LRE), q(LRE), ALU.mult, R_, W_); P.TT(q(TB), q(LIM), q(LIM), ALU.mult, R_, W_)
            P.TT(q(TA), q(TA), q(TB), ALU.add, R_, W_); P.RECIP(q(TA), q(TA), R_, W_)
            P.TS(q(CS), q(CS), -1.0, ALU.add, R_, W_)
            P.TT(q(CRE), q(CS), q(LRE), ALU.mult, R_, W_); P.TT(q(TB), q(SN), q(LIM), ALU.mult, R_, W_)
            P.TT(q(CRE), q(CRE), q(TB), ALU.add, R_, W_); P.TT(q(CRE), q(CRE), q(TA), ALU.mult, R_, W_)
            P.TT(q(CIM), q(SN), q(LRE), ALU.mult, R_, W_); P.TT(q(TB), q(CS), q(LIM), ALU.mult, R_, W_)
            P.TT(q(CIM), q(CIM), q(TB), ALU.subtract, R_, W_); P.TT(q(CIM), q(CIM), q(TA), ALU.mult, R_, W_)
            P.TS(q(NCIM), q(CIM), -1.0, ALU.mult, R_, W_)
        uT, yacc, A, B, Cs, Sn, t1, t2 = [G[i] for i in range(8)]
        t2i = P.Gt[:, 7, :].bitcast(I32)
        ygf, ygr = P.gflat(8, 2 * T, BF16)
        ygT = ygf.rearrange("p (c n) -> p c n", n=T)
        for ut in range(4):
            wu = P.loadw(W[:, ut * 128:(ut + 1) * 128], 128)
            P.proj_fm(wu, 128, lambda pb, t0, n: P.CP(uT[:, t0:t0 + n], pb[:, 0:n], [pb], [uT], eng=P.kb.act))
            P.TS(yacc[:], uT[:], P.s5d[:, ut:ut + 1], ALU.mult, [uT, P.s5d], [yacc])
            for sl in range(4):
                stt = ut * 4 + sl
                r0 = sl * 32
                bc_ = P.s5bc
                P.DMA(bc_[:, 0:32], P.w["s5_bT"][j, :, stt].rearrange("p a h -> p (a h)"), [], [bc_])
                P.DMA(bc_[:, 32:64], P.w["s5_cT"][j, :, stt].rearrange("p a h -> p (a h)"), [], [bc_])
                Zc = P.s5zc
                P.MSET(Zc[:, 0:256], 0.0, [Zc])
                for gl in range(2):
                    pr = slice(gl * 64, gl * 64 + 64); cc = slice(r0 + gl * 16, r0 + gl * 16 + 16)
                    P.CP(Zc[pr, cc], bc_[pr, 32:48], [bc_], [Zc])
                    P.TS(Zc[pr, 128 + cc.start:128 + cc.stop], bc_[pr, 48:64], -1.0, ALU.mult, [bc_], [Zc])
                for d in range(2):
                    q = lambda k: st_[:, d, k, stt:stt + 1]
                    Z = P.sm(); tmp = P.sm(); L = P.sm()
                    P.MSET(Z[:, 0:256], 0.0, [Z])
                    for gl in range(2):
                        pr = slice(gl * 64, gl * 64 + 64); c0 = r0 + gl * 16
                        P.TS(tmp[pr, 0:16], bc_[pr, 0:16], q(CRE)[pr], ALU.mult, [bc_, st_], [tmp])
                        P.STT(Z[pr, c0:c0 + 16], bc_[pr, 16:32], q(NCIM)[pr], tmp[pr, 0:16], ALU.mult, ALU.add, [bc_, st_, tmp], [Z])
                        P.TS(tmp[pr, 16:32], bc_[pr, 16:32], q(CRE)[pr], ALU.mult, [bc_, st_], [tmp])
                        P.STT(Z[pr, 128 + c0:128 + c0 + 16], bc_[pr, 0:16], q(CIM)[pr], tmp[pr, 16:32], ALU.mult, ALU.add, [bc_, st_, tmp], [Z])
                    for k in range(2):
                        pz = P.next_ps()
                        P.TR(pz[:, 0:128], Z[:, k * 128:(k + 1) * 128], [Z], [pz])
                        P.CP(L[:, k * 128:(k + 1) * 128], pz[:, 0:128], [pz], [L], eng=P.kb.act)
                    P.DMA(t1[:], P.cd["tauF" if d == 0 else "tauB"][:], [], [t1])
                    P.TS(t1[:], t1[:], q(THN), ALU.mult, [t1, st_], [t1])
                    P.CP(t2i, t1[:], [t1], [t2]); P.CP(Cs[:], t2i, [t2], [Cs], eng=P.kb.pool)
                    P.TT(t1[:], t1[:], Cs[:], ALU.subtract, [t1, Cs], [t1])
                    P.ACT(Sn[:], t1[:], AF.Sin, [t1], [Sn], scale=TWO_PI)
                    P.TS(t1[:], t1[:], 0.25, ALU.add, [t1], [t1])
                    P.CP(t2i, t1[:], [t1], [t2]); P.CP(Cs[:], t2i, [t2], [Cs], eng=P.kb.pool)
                    P.TT(t1[:], t1[:], Cs[:], ALU.subtract, [t1, Cs], [t1])
                    P.ACT(Cs[:], t1[:], AF.Sin, [t1], [Cs], scale=TWO_PI)
                    for (t0, n) in NTILES:
                        pa = P.next_ps(); pb_ = P.next_ps()
                        P.MM(pa[:, 0:n], L[:, 0:128], uT[:, t0:t0 + n], [L, uT], [pa])
                        P.MM(pb_[:, 0:n], L[:, 128:256], uT[:, t0:t0 + n], [L, uT], [pb_])
                        P.CP(A[:, t0:t0 + n], pa[:, 0:n], [pa], [A], eng=P.kb.act)
                        P.CP(B[:, t0:t0 + n], pb_[:, 0:n], [pb_], [B])
                    P.TT(t1[:], A[:], Cs[:], ALU.mult, [A, Cs], [t1]); P.TT(t2[:], B[:], Sn[:], ALU.mult, [B, Sn], [t2], eng=P.kb.pool)
                    P.TT(A[:], A[:], Sn[:], ALU.mult, [A, Sn], [A]); P.TT(B[:], B[:], Cs[:], ALU.mult, [B, Cs], [B], eng=P.kb.pool)
                    P.TT(t1[:], t1[:], t2[:], ALU.add, [t1, t2], [t1]); P.TT(B[:], B[:], A[:], ALU.subtract, [B, A], [B], eng=P.kb.pool)
                    mg = q(MAG)
                    for src, dst in ((t1, t2), (B, A)):
                        if d == 0:
                            P.SCAN(dst[:], mg.broadcast_to([128, T]), src[:], [st_, src], [dst])
                        else:
                            P.SCAN(dst[:, 255::-1], mg.broadcast_to([128, 256]), src[:, 255::-1], [st_, src], [dst])
                            P.SCAN(dst[:, T - 1:255:-1], mg.broadcast_to([128, T - 256]), src[:, T - 1:255:-1], [st_, src, dst], [dst], init=dst[:, 0:1])
                    P.TT(t1[:], t2[:], Cs[:], ALU.mult, [t2, Cs], [t1]); P.TT(B[:], A[:], Sn[:], ALU.mult, [A, Sn], [B], eng=P.kb.pool)
                    P.TT(t1[:], t1[:], B[:], ALU.subtract, [t1, B], [t1])
                    P.TT(t2[:], t2[:], Sn[:], ALU.mult, [t2, Sn], [t2]); P.TT(A[:], A[:], Cs[:], ALU.mult, [A, Cs], [A], eng=P.kb.pool)
                    P.TT(t2[:], t2[:], A[:], ALU.add, [t2, A], [t2])
                    for (t0, n) in NTILES:
                        py = P.next_ps()
                        P.MM(py[:, 0:n], Zc[:, 0:128], t1[:, t0:t0 + n], [Zc, t1], [py], start=True, stop=False)
                        P.MM(py[:, 0:n], Zc[:, 128:256], t2[:, t0:t0 + n], [Zc, t2], [py], start=False, stop=True)
                        P.TT(yacc[:, t0:t0 + n], yacc[:, t0:t0 + n], py[:, 0:n], ALU.add, [yacc, py], [yacc])
            P.ACT(t1[:], yacc[:], AF.Square, [yacc], [t1])
            P.TS(t1[:], t1[:], 0.044715, ALU.mult, [t1], [t1], s2=1.0, op1=ALU.add)
            P.TT(t1[:], t1[:], yacc[:], ALU.mult, [t1, yacc], [t1])
            P.ACT(t1[:], t1[:], AF.Sigmoid, [t1], [t1], scale=2.0 * math.sqrt(2.0 / math.pi))
            P.TT(ygT[:, ut, :], yacc[:], t1[:], ALU.mult, [yacc, t1], ygr)
        for nt in range(4):
            wg = P.loadw(P.w["s5_glu_w"][j][:, nt * 128:(nt + 1) * 128], 128, kc=4)
            for (t0, n) in NTILES:
                pg = P.next_ps()
                for c in range(4):
                    P.MM(pg[:, 0:n], wg[:, c, :], ygT[:, c, t0:t0 + n], [wg] + ygr, [pg], start=(c == 0), stop=(c == 3))
                sg = P.sm()
                P.ACT(sg[:, 0:n], pg[:, 0:n], AF.Sigmoid, [pg], [sg])
                yo = P.sm()
                P.TT(yo[:, 0:n], ygT[:, nt, t0:t0 + n], sg[:, 0:n], ALU.mult, ygr + [sg], [yo])
                P.DMA(P.yT[nt * 128:(nt + 1) * 128, t0:t0 + n], yo[:, 0:n], [yo], [P.yT])

    def rwkv(self, l, j):
        P = self
        G = P.G
        W = P.w["od_w_in"][j]
        C0 = 512
        prm = P.rwp
        P.DMA(prm[:, 0:15], P.w["rw_mu_fm"][j], [], [prm])
        P.DMA(prm[:, 16:20], P.w["rw_kk_fm"][j], [], [prm]); P.DMA(prm[:, 20:24], P.w["rw_ka_fm"][j], [], [prm])
        P.DMA(prm[:, 24:28], P.w["rw_rk_fm"][j], [], [prm])
        for d in range(2):
            P.DMA(prm[:, 28 + d * 4:32 + d * 4], P.w["rw_w0_fm"][j, d], [], [prm])
            P.DMA(prm[:, 36 + d * 4:40 + d * 4], P.w["rw_a0_fm"][j, d], [], [prm])
        MU = lambda t_: prm[:, t_:t_ + 1]
        rT, kT, vtokS, kkT, L_, Gc, E1, E2, As, Oacc = [G[i] for i in range(10)]
        vtok = vtokS[:].rearrange("p (t c) -> p t c", c=128)
        oacc = Oacc[:].rearrange("p (t c) -> p t c", c=128)
        Hbd = P.S

        def proj_shifted(tile_idx, dst, tmp):
            wb = P.loadw(W[:, C0 + tile_idx * 128:C0 + (tile_idx + 1) * 128], 128)
            P.proj_fm(wb, 128, lambda pb, t0, n: P.CP(tmp[:, t0:t0 + n], pb[:, 0:n], [pb], [tmp], eng=P.kb.act))
            for (a, b) in ((0, LC), (LC, T)):
                P.CP(dst[:, a:a + 1], tmp[:, a + 1:a + 2], [tmp], [dst], eng=P.kb.pool)
                P.CP(dst[:, b - 1:b], tmp[:, b - 2:b - 1], [tmp], [dst], eng=P.kb.pool)
                P.TT(dst[:, a + 1:b - 1], tmp[:, a:b - 2], tmp[:, a + 2:b], ALU.add, [tmp], [dst])
            P.STT(dst[:], dst[:], 0.5, tmp[:], ALU.mult, ALU.subtract, [dst, tmp], [dst])
            P.STT(dst[:], dst[:], MU(tile_idx), tmp[:], ALU.mult, ALU.add, [dst, prm, tmp], [dst])

        P.DMA(P.rwm[:], P.cd["rw_masks"][0], [], [P.rwm])
        for pp in range(4):
            proj_shifted(0 + pp, rT, E1)
            proj_shifted(4 + pp, kT, E1)
            proj_shifted(8 + pp, E2, E1)
            for ti in range(NT):
                pv = P.next_ps()
                P.TR(pv[:, 0:128], E2[:, ti * 128:(ti + 1) * 128], [E2], [pv])
                P.CP(vtok[:, ti, :], pv[:, 0:128], [pv], [vtokS], eng=P.kb.act)
            P.TS(kkT[:], kT[:], prm[:, 16 + pp:17 + pp], ALU.mult, [kT, prm], [kkT])
            for (t0, n) in NTILES:
                sq = P.sm()
                P.ACT(sq[:, 0:n], kkT[:, t0:t0 + n], AF.Square, [kkT], [sq])
                pn = P.next_ps()
                P.MM(pn[:, 0:n], P.bdones[:], sq[:, 0:n], [P.bdones, sq], [pn])
                nr_ = P.sm()
                P.ACT(nr_[:, 0:n], pn[:, 0:n], AF.Sqrt, [pn], [nr_])
                P.TS(nr_[:, 0:n], nr_[:, 0:n], 1e-12, ALU.max, [nr_], [nr_])
                P.RECIP(nr_[:, 0:n], nr_[:, 0:n], [nr_], [nr_])
                P.TT(kkT[:, t0:t0 + n], kkT[:, t0:t0 + n], nr_[:, 0:n], ALU.mult, [kkT, nr_], [kkT])
            P.DMA(P.rwln[:, 0, :], P.w["rw_lng"][j, pp * 128:(pp + 1) * 128].partition_broadcast(128), [], [P.rwln])
            P.DMA(P.rwln[:, 1, :], P.w["rw_lnb"][j, pp * 128:(pp + 1) * 128].partition_broadcast(128), [], [P.rwln])
            for d in range(2):
                fwd = d == 0
                P.DMA(P.rwm[:], P.cd["rw_masks"][d], [], [P.rwm])
                for (lt, padk, biasc, dstb, fn) in ((12, "rw_w2pad", 28 + d * 4 + pp, L_, AF.Tanh), (13, "rw_a2pad", 36 + d * 4 + pp, As, None)):
                    proj_shifted(lt, E1, E2)
                    if fn is not None:
                        P.ACT(E1[:], E1[:], fn, [E1], [E1])
                    wpad = P.attm[1]
                    P.DMA(wpad[:, 0:128], P.w[padk][j, d, :, pp * 128:(pp + 1) * 128], [], [wpad])
                    for (t0, n) in NTILES:
                        pz = P.next_ps()
                        P.MM(pz[:, 0:n], wpad[:, 0:128], E1[:, t0:t0 + n], [wpad, E1], [pz])
                        P.ACT(dstb[:, t0:t0 + n], pz[:, 0:n], AF.Sigmoid, [pz, prm], [dstb], bias=prm[:, biasc:biasc + 1])
                P.TS(L_[:], L_[:], -math.exp(-0.5), ALU.mult, [L_], [L_])
                for ti in range(NT):
                    tsl = slice(ti * 128, (ti + 1) * 128)
                    if fwd:
                        P.SCAN(Gc[:, tsl], P.ones[:], L_[:, tsl], [P.ones, L_], [Gc])
                    else:
                        hi, lo = (ti + 1) * 128 - 1, ti * 128 - 1
                        rs = slice(hi, lo if lo >= 0 else None, -1)
                        P.SCAN(Gc[:, rs], P.ones[:], L_[:, rs], [P.ones, L_], [Gc])
                P.ACT(E1[:], Gc[:], AF.Exp, [Gc], [E1])
                ecol = E1[:, 127:T:128] if fwd else E1[:, 0:T:128]
                P.CP(P.rwgc[:], ecol, [E1], [P.rwgc])
                P.TT(E1[:], E1[:], rT[:], ALU.mult, [E1, rT], [E1])
                P.TT(E2[:], Gc[:], L_[:], ALU.subtract, [Gc, L_], [E2])
                P.ACT(E2[:], E2[:], AF.Exp, [E2], [E2])
                P.STT(E2[:], kkT[:], -1.0, E2[:], ALU.mult, ALU.mult, [kkT, E2], [E2])
                P.ACT(Gc[:], Gc[:], AF.Exp, [Gc], [Gc], scale=-1.0)
                P.TT(L_[:], kkT[:], As[:], ALU.mult, [kkT, As], [L_], eng=P.kb.pool)
                P.TT(L_[:], L_[:], Gc[:], ALU.mult, [L_, Gc], [L_], eng=P.kb.pool)
                P.TS(As[:], As[:], -1.0, ALU.add, [As, prm], [As], s2=prm[:, 20 + pp:21 + pp], op1=ALU.mult)
                P.STT(As[:], As[:], 1.0, kT[:], ALU.add, ALU.mult, [As, kT], [As])
                P.TT(As[:], As[:], Gc[:], ALU.mult, [As, Gc], [As])
                rt_, at_, bt_, kt_ = E1, E2, L_, As
                P.MSET(Hbd[:], 0.0, [Hbd])
                tiles = list(range(NT)) if fwd else [1, 0] + list(range(NT - 1, 1, -1))
                hs = [slice(0, 64), slice(64, 128)]
                CB = [P.bc[0], P.bc[1], P.bc[2], P.bc[3], P.xb[0], P.xb[1]]
                XA, ZA, XB, ZB, WT, MRB, LAK, MRK = range(8)
                mat = lambda c, q_: CB[c][:, q_ * 128:(q_ + 1) * 128]
                Ybuf = P.attm[0]
                for g0 in range(0, NT, 3):
                    grp = tiles[g0:g0 + 3]
                    chains = [(ti, hh) for ti in grp for hh in range(2)]
                    for c, (ti, hh) in enumerate(chains):
                        tsl = slice(ti * 128, (ti + 1) * 128)
                        ar = P.rwar[hh]; cb = CB[c]
                        P.TS(ar[:, 0:128], at_[:, tsl], P.halfm[:, hh:hh + 1], ALU.mult, [at_, P.halfm], [ar])
                        P.TS(ar[:, 128:256], rt_[:, tsl], P.halfm[:, hh:hh + 1], ALU.mult, [rt_, P.halfm], [ar], eng=P.kb.pool)
                        p1 = P.next_ps(); p2 = P.next_ps(); p3 = P.next_ps()
                        P.MM(p1[:, 0:256], bt_[:, tsl], ar[:, 0:256], [bt_, ar], [p1])
                        P.MM(p2[:, 0:256], kt_[:, tsl], ar[:, 0:256], [kt_, ar], [p2])
                        P.MM(p3[:, 0:128], ar[:, 0:128], bt_[:, tsl], [ar, bt_], [p3])
                        P.TT(mat(c, XA), p1[:, 0:128], P.rwm[:, 0, :], ALU.mult, [p1, P.rwm], [cb])
                        P.TT(mat(c, MRB), p1[:, 128:256], P.rwm[:, 1, :], ALU.mult, [p1, P.rwm], [cb])
                        P.TT(mat(c, LAK), p2[:, 0:128], P.rwm[:, 0, :], ALU.mult, [p2, P.rwm], [cb])
                        P.TT(mat(c, MRK), p2[:, 128:256], P.rwm[:, 1, :], ALU.mult, [p2, P.rwm], [cb])
                        P.TT(mat(c, ZA), p3[:, 0:128], P.rwm[:, 2, :], ALU.mult, [p3, P.rwm], [cb])
                        P.TT(mat(c, WT), mat(c, XA), P.ident[:], ALU.add, [cb, P.ident], [cb], eng=P.kb.pool)
                    cx, cz, nx, nz = XA, ZA, XB, ZB
                    for k in range(1, 7):
                        for c in range(len(chains)):
                            cb = CB[c]
                            pZ = P.next_ps()
                            P.MM(pZ[:, 0:128], mat(c, cx), mat(c, cz), [cb], [pZ])
                            if k < 6:
                                pX = P.next_ps()
                                P.MM(pX[:, 0:128], mat(c, cz), mat(c, cx), [cb], [pX])
                            P.CP(mat(c, nz), pZ[:, 0:128], [pZ], [cb])
                            if k < 6:
                                P.CP(mat(c, nx), pX[:, 0:128], [pX], [cb], eng=P.kb.act)
                        for c in range(len(chains)):
                            cb = CB[c]
                            pW = P.next_ps()
                            P.MM(pW[:, 0:128], mat(c, nz), mat(c, WT), [cb], [pW])
                            P.TT(mat(c, WT), mat(c, WT), pW[:, 0:128], ALU.add, [cb, pW], [cb])
                        cx, cz, nx, nz = nx, nz, cx, cz
                    for gi, ti in enumerate(grp):
                        tsl = slice(ti * 128, (ti + 1) * 128)
                        U = P.rwu
                        pY = P.next_ps()
                        for hh in range(2):
                            c = gi * 2 + hh
                            P.MM(pY[:, hs[hh]], at_[:, tsl], Hbd[:, hs[hh]], [at_, Hbd], [pY], start=True, stop=False)
                            P.MM(pY[:, hs[hh]], mat(c, LAK), vtok[:, ti, hs[hh]], [CB[c], vtokS], [pY], start=False, stop=True)
                        P.CP(Ybuf[:], pY[:, 0:128], [pY], [Ybuf])
                        pU = P.next_ps()
                        for hh in range(2):
                            c = gi * 2 + hh
                            P.MM(pU[:, hs[hh]], mat(c, WT), Ybuf[:, hs[hh]], [CB[c], Ybuf], [pU])
                        P.CP(U[:], pU[:, 0:128], [pU], [U], eng=P.kb.act)
                        pO = P.next_ps()
                        for hh in range(2):
                            c = gi * 2 + hh
                            P.MM(pO[:, hs[hh]], rt_[:, tsl], Hbd[:, hs[hh]], [rt_, Hbd], [pO], start=True, stop=False)
                            P.MM(pO[:, hs[hh]], mat(c, MRB), U[:, hs[hh]], [CB[c], U], [pO], start=False, stop=False)
                            P.MM(pO[:, hs[hh]], mat(c, MRK), vtok[:, ti, hs[hh]], [CB[c], vtokS], [pO], start=False, stop=True)
                        if fwd:
                            P.CP(oacc[:, ti, :], pO[:, 0:128], [pO], [Oacc], eng=P.kb.act)
                        else:
                            P.TT(oacc[:, ti, :], oacc[:, ti, :], pO[:, 0:128], ALU.add, [Oacc, pO], [Oacc])
                        tk = P.ktok[0]
                        for q_, src in ((0, bt_), (1, kt_)):
                            pt_ = P.next_ps()
                            P.TR(pt_[:, 0:128], src[:, tsl], [src], [pt_])
                            P.CP(tk[:, q_, :], pt_[:, 0:128], [pt_], [tk], eng=P.kb.act)
                        pH = P.next_ps()
                        P.MM(pH[:, 0:128], tk[:, 0, :], U[:], [tk, U], [pH], start=True, stop=False)
                        P.MM(pH[:, 0:128], tk[:, 1, :], vtok[:, ti, :], [tk, vtokS], [pH], start=False, stop=True)
                        P.TT(P.tmpS[:], pH[:, 0:128], P.bdones[:], ALU.mult, [pH, P.bdones], [P.tmpS])
                        P.TT(P.tmpS[:], P.tmpS[:], Hbd[:], ALU.add, [P.tmpS, Hbd], [P.tmpS])
                        P.ACT(Hbd[:], P.tmpS[:], AF.Identity, [P.tmpS, P.rwgc], [Hbd], scale=P.rwgc[:, ti:ti + 1])
            P.STT(E1[:], rT[:], prm[:, 24 + pp:25 + pp], kT[:], ALU.mult, ALU.mult, [rT, prm, kT], [E1])
            proj_shifted(14, E2, Gc)
            P.ACT(E2[:], E2[:], AF.Sigmoid, [E2], [E2])
            g2b = P.attm[0]
            P.DMA(g2b[:, 0:128], P.w["rw_g2"][j, :, pp * 128:(pp + 1) * 128], [], [g2b])
            for ti in range(NT):
                tsl = slice(ti * 128, (ti + 1) * 128)
                o = P.sm(); st = P.stat
                P.CP(o[:, 0:128], oacc[:, ti, :], [Oacc], [o], eng=P.kb.pool)
                o3 = o[:, 0:128].rearrange("p (h i) -> p h i", i=64)
                P.kb.op(P.kb.dve, lambda e, o3=o3: e.reduce_sum(out=st[:, 16:18], in_=o3, axis=AX.X), [o.r], [st.r])
                P.TS(st[:, 16:18], st[:, 16:18], 1.0 / 64.0, ALU.mult, [st], [st])
                for hh in range(2):
                    P.TS(o3[:, hh, :], o3[:, hh, :], st[:, 16 + hh:17 + hh], ALU.subtract, [o, st], [o])
                sq = P.sm()
                P.TT(sq[:, 0:128], o[:, 0:128], o[:, 0:128], ALU.mult, [o], [sq])
                sq3 = sq[:, 0:128].rearrange("p (h i) -> p h i", i=64)
                P.kb.op(P.kb.dve, lambda e, sq3=sq3: e.reduce_sum(out=st[:, 18:20], in_=sq3, axis=AX.X), [sq.r], [st.r])
                P.ACT(st[:, 18:20], st[:, 18:20], AF.Sqrt, [st, P.cst], [st], scale=1.0 / 64.0, bias=P.cst[:, 2:3])
                P.RECIP(st[:, 18:20], st[:, 18:20], [st], [st])
                for hh in range(2):
                    P.TS(o3[:, hh, :], o3[:, hh, :], st[:, 18 + hh:19 + hh], ALU.mult, [o, st], [o])
                P.TT(o[:, 0:128], o[:, 0:128], P.rwln[:, 0, :], ALU.mult, [o, P.rwln], [o])
                P.TT(o[:, 0:128], o[:, 0:128], P.rwln[:, 1, :], ALU.add, [o, P.rwln], [o])
                pb_ = P.next_ps()
                P.MM(pb_[:, 0:2], E1[:, tsl], P.halfm[:], [E1, P.halfm], [pb_])
                P.CP(st[:, 20:22], pb_[:, 0:2], [pb_], [st])
                for hh in range(2):
                    P.STT(o3[:, hh, :], vtok[:, ti, hh * 64:(hh + 1) * 64], st[:, 20 + hh:21 + hh], o3[:, hh, :], ALU.mult, ALU.add, [vtokS, st, o], [o])
                pg = P.next_ps()
                P.MM(pg[:, 0:128], E2[:, tsl], g2b[:, 0:128], [E2, g2b], [pg])
                P.TT(o[:, 0:128], o[:, 0:128], pg[:, 0:128], ALU.mult, [o, pg], [o])
                pT = P.next_ps()
                P.TR(pT[:, 0:128], o[:, 0:128], [o], [pT])
                yo = P.sm()
                P.CP(yo[:, 0:128], pT[:, 0:128], [pT], [yo], eng=P.kb.act)
                P.DMA(P.yT[512 + pp * 128:512 + (pp + 1) * 128, tsl], yo[:, 0:128], [yo], [P.yT])

    def build(self):
        P = self
        P.setup()
        xcur = P.xin
        for li, l in enumerate(P.layers):
            j = l // 2
            last = li == len(P.layers) - 1
            xmid = P.xs[0]
            xnext = P.out if last else P.xs[1]
            if P.stop >= 1:
                P.phase_mod(l)
                if "fm" in P.taps:
                    P.DMA(P.outp("tap_fm", [128, 48, 2]), P.fm[:], [P.fm], [])
            if P.stop >= 2:
                P.phase_hT(xcur)
                if "hTd" in P.taps:
                    P.DMA(P.outp("tap_hT", [128, KC, T]), P.hT[:], [P.hT], [], q=P.kb.pool)
            if l % 2 == 0:
                if P.stop >= 3: P.hgrn2(l, j)
                if P.stop >= 4: P.attention(l, j)
                if P.stop >= 5: P.phase_out(l, P.w["ev_w_out"][j], xcur, xmid)
                if P.stop >= 6: P.phase_ffn(l, j, xmid, xnext)
            else:
                if "inject_y" in P.taps:
                    pass
                else:
                    if P.stop >= 3: P.s5(l, j)
                    if P.stop >= 4: P.rwkv(l, j)
                if P.stop >= 5: P.phase_out(l, P.w["od_w_out"][j], xcur, xmid, router_j=(j if "1" != "0" else None))
                if "gates" in P.taps:
                    P.DMA(P.outp("tap_gates", [128, NT, 8]), P.gates[:], [P.gates], [])
                if P.stop >= 6: P.phase_moe(l, j, xmid, xnext)
            xcur = xnext
        P.kb.emit()
        self.st.close()
        return self.nc


def host_inputs(inp, b):
    m = {}
    m["xin"] = np.ascontiguousarray(np.concatenate([inp["ctx"][b], inp["x"][b]], 0))
    ct = np.stack([inp["c"][b].reshape(8, 128).T, inp["c_ctx"].reshape(8, 128).T], -1)
    m["condT"] = np.ascontiguousarray(ct.astype(np.float32))
    for k, v in host_consts().items():
        m["c_" + k] = v
    m["ada_w"] = inp["ada_w"]
    m["ada_b_fm"] = np.ascontiguousarray(inp["ada_b"].reshape(4, 48, 128).transpose(0, 2, 1))
    m["ln_g"] = inp["ln_g"]; m["ln_b"] = inp["ln_b"]
    m["ev_w_in"] = inp["ev_w_in"]; m["ev_w_out"] = inp["ev_w_out"]
    m["hg_lb_fm"] = np.ascontiguousarray(inp["hg_lb"].reshape(2, 4, 128).transpose(2, 1, 0))
    m["hg_ng_fm"] = np.ascontiguousarray(inp["hg_norm_g"].reshape(2, 4, 128).transpose(0, 2, 1))
    m["attn_sink"] = inp["attn_sink"]
    def fm16(a):
        return np.ascontiguousarray(a.reshape(2, 2, 16, 2, 64).transpose(0, 1, 3, 4, 2).reshape(2, 2, 128, 16))
    m["s5_lre"] = fm16(inp["s5_lam_re"]); m["s5_lim"] = fm16(inp["s5_lam_im"])
    m["s5_ldt"] = fm16(np.ascontiguousarray(np.broadcast_to(inp["s5_log_dt"][..., None], (2, 2, 32, 64))))
    bb = np.stack([inp["s5_b_re"], inp["s5_b_im"]], 3)
    m["s5_bT"] = np.ascontiguousarray(bb.reshape(2, 16, 2, 64, 2, 16).transpose(0, 2, 3, 1, 4, 5).reshape(2, 128, 16, 2, 16))
    cc = np.stack([inp["s5_c_re"], inp["s5_c_im"]], 2).transpose(0, 1, 4, 2, 3)
    m["s5_cT"] = np.ascontiguousarray(cc.reshape(2, 16, 2, 64, 2, 16).transpose(0, 2, 3, 1, 4, 5).reshape(2, 128, 16, 2, 16))
    m["s5_d_fm"] = np.ascontiguousarray(inp["s5_d"].reshape(2, 4, 128).transpose(0, 2, 1))
    m["s5_glu_w"] = inp["s5_glu_w"]
    fm4 = lambda a: np.ascontiguousarray(a.reshape(a.shape[:-1] + (4, 128)).swapaxes(-1, -2))
    m["rw_mu_fm"] = np.ascontiguousarray(inp["rwkv_mu"].reshape(2, 15, 128).transpose(0, 2, 1))
    m["rw_w0_fm"] = fm4(inp["rwkv_w0"]); m["rw_a0_fm"] = fm4(inp["rwkv_a0"])
    def pad2(a):
        o = np.zeros((2, 2, 128, 512), np.float32)
        o[:, 0, 0:64] = a[:, 0]; o[:, 1, 64:128] = a[:, 1]
        return o
    m["rw_w2pad"] = pad2(inp["rwkv_w2"]); m["rw_a2pad"] = pad2(inp["rwkv_a2"]); m["rw_g2"] = inp["rwkv_g2"]
    m["rw_kk_fm"] = fm4(inp["rwkv_k_k"]); m["rw_ka_fm"] = fm4(inp["rwkv_k_a"]); m["rw_rk_fm"] = fm4(inp["rwkv_r_k"].reshape(2, 512))
    m["rw_lng"] = inp["rwkv_ln_g"]; m["rw_lnb"] = inp["rwkv_ln_b"]
    for k in ("ffn_w_gate", "ffn_w_up", "ffn_w_down", "od_w_in", "od_w_out", "moe_router_w", "moe_router_b", "moe_w_gate", "moe_w_up", "moe_w_down"):
        m[k] = inp[k]
    return m


FUSED = os.environ.get("MK_FUSED", "1") == "1"
N_CORES = 8


def _run(layers, inputs, xin_per_core):
    P = Prog(layers)
    nc = P.build()
    in_maps = []
    for b in range(N_CORES):
        m = host_inputs(inputs, b)
        m["xin"] = xin_per_core[b]
        in_maps.append({k: v for k, v in m.items() if k in P.din})
    res = run_bass_kernel_spmd(nc, in_maps, core_ids=list(range(N_CORES)))
    return [r["out"] for r in res.results]


def kernel(**inputs):
    inputs = {k: np.asarray(v) for k, v in inputs.items()}
    xs = [np.ascontiguousarray(np.concatenate([inputs["ctx"][b], inputs["x"][b]], 0)).astype(np.float32) for b in range(N_CORES)]
    if FUSED:
        xs = _run([0, 1, 2, 3], inputs, xs)
    else:
        for l in range(4):
            xs = _run([l], inputs, xs)
    return np.stack([x[LC:] for x in xs], 0).astype(np.float32)
```

```python
import contextlib, math, os
import numpy as np
import concourse.bass as bass
import concourse.mybir as mybir
from concourse.bass_utils import run_bass_kernel_spmd

F32 = mybir.dt.float32
BF16 = mybir.dt.bfloat16
F32R = mybir.dt.float32r
ALU = mybir.AluOpType
AF = mybir.ActivationFunctionType
AX = mybir.AxisListType

EPOCH = 30000
NDMA = 24


class Reg:
    __slots__ = ("w", "r", "name")

    def __init__(self, name=""):
        self.w = {}
        self.r = {}
        self.name = name


class Eng:
    def __init__(self, kb, name, self_sync):
        self.kb, self.name, self.self_sync = kb, name, self_sync
        self.ops = []
        self.seen = {}
        self.sems = [kb.new_sem(f"{name}_e0")]
        self.count = 0

    def cur(self):
        return self.sems[-1]


class KB:
    def __init__(self, nc, stack):
        self.nc, self.stack = nc, stack
        self.semh = {}
        self.nsem = 0
        self.pe = Eng(self, "pe", False)
        self.dve = Eng(self, "dve", True)
        self.act = Eng(self, "act", True)
        self.pool = Eng(self, "pool", True)
        self.sp = Eng(self, "sp", False)
        self.engs = [self.pe, self.dve, self.act, self.pool, self.sp]
        self.dma_sems = [self.new_sem(f"dma{i}") for i in range(NDMA)]
        self.dma_tot = [0] * NDMA
        self.dma_i = 0
        self.n_ops = 0

    def new_sem(self, name):
        h = self.stack.enter_context(self.nc.semaphore(name))
        k = self.nsem
        self.nsem += 1
        self.semh[k] = h
        return k

    def _waits(self, E, reads, writes):
        need = {}
        for r in reads:
            for s, v in r.w.items():
                if need.get(s, 0) < v:
                    need[s] = v
        for w in writes:
            for d in (w.w, w.r):
                for s, v in d.items():
                    if need.get(s, 0) < v:
                        need[s] = v
        out = []
        for s, v in need.items():
            if (not E.self_sync) and s in E.sems:
                continue
            if E.seen.get(s, 0) < v:
                E.seen[s] = v
                out.append((s, v))
        return out

    def _mark(self, ev, reads, writes):
        s, v = ev
        for r in reads:
            r.r[s] = v
        for w in writes:
            w.w = {s: v}
            w.r = {}

    def op(self, E, fn, reads=(), writes=()):
        waits = self._waits(E, reads, writes)
        if E.count >= EPOCH:
            E.sems.append(self.new_sem(f"{E.name}_e{len(E.sems)}"))
            E.count = 0
        E.count += 1
        ev = (E.cur(), E.count)
        E.ops.append((waits, fn, ev[0], 1))
        self._mark(ev, reads, writes)
        self.n_ops += 1
        return ev

    def dma(self, Q, out, in_, reads=(), writes=(), **kw):
        i = self.dma_i
        self.dma_i = (i + 1) % NDMA
        s = self.dma_sems[i]
        waits = self._waits(Q, reads, writes)
        if self.dma_tot[i] > 0 and Q.seen.get(s, 0) < self.dma_tot[i]:
            Q.seen[s] = self.dma_tot[i]
            waits.append((s, self.dma_tot[i]))
        self.dma_tot[i] += 16
        ev = (s, self.dma_tot[i])
        Q.ops.append((waits, lambda e: e.dma_start(out=out, in_=in_, **kw), s, 16))
        self._mark(ev, reads, writes)
        self.n_ops += 1
        return ev

    def emit(self):
        nc = self.nc
        fin = [(self.dma_sems[i], self.dma_tot[i]) for i in range(NDMA) if self.dma_tot[i] > 0]
        semh = self.semh

        def run(E, e):
            for waits, fn, s, inc in E.ops:
                for ws, wv in waits:
                    e.wait_ge(semh[ws], wv)
                inst = fn(e)
                inst.then_inc(semh[s], inc)

        with nc.Block() as block:
            @block.tensor
            def _(e):
                run(self.pe, e)

            @block.vector
            def _(e):
                run(self.dve, e)

            @block.scalar
            def _(e):
                run(self.act, e)

            @block.gpsimd
            def _(e):
                run(self.pool, e)

            @block.sync
            def _(e):
                run(self.sp, e)
                for ws, wv in fin:
                    e.wait_ge(semh[ws], wv)


class _LazyW:
    def __init__(self, prog, shapes):
        self.p, self.shapes, self.c = prog, shapes, {}

    def __getitem__(self, k):
        if k not in self.c:
            self.c[k] = self.p.inp(k, self.shapes[k])
        return self.c[k]


T = 2304; LC = 256; NT = 18; D = 1024; KC = 8; CH = 32; NCH = 72; NG = 10
NTILES = [(0, 256), (256, 512), (768, 512), (1280, 512), (1792, 512)]
ALPHA = 8 ** 0.25
DFF = 2816; NFC = 22


def host_consts():
    c = {}
    c["ident"] = np.eye(128, dtype=np.float32)
    s = np.arange(128)[:, None]; t = np.arange(128)[None, :]
    same = (s // CH) == (t // CH)
    c["tri_le"] = (same & (s <= t)).astype(np.float32)
    c["tri_ge"] = (same & (s >= t)).astype(np.float32)
    tf = np.arange(T, dtype=np.float32)
    tb = np.concatenate([255.0 - np.arange(256), 256.0 + (2303.0 - np.arange(256, T))]).astype(np.float32)
    c["tauF"] = np.ascontiguousarray(np.broadcast_to(tf, (128, T))); c["tauB"] = np.ascontiguousarray(np.broadcast_to(tb, (128, T)))
    a_ = np.arange(128)[:, None]; b_ = np.arange(128)[None, :]
    mf = np.stack([(a_ < b_), (a_ <= b_), (b_ < a_)], 1).astype(np.float32)
    mb = np.stack([(a_ > b_), (a_ >= b_), (b_ > a_)], 1).astype(np.float32)
    c["rw_masks"] = np.ascontiguousarray(np.stack([mf, mb], 0))
    c["bdones"] = ((a_ // 64) == (b_ // 64)).astype(np.float32)
    c["halfm"] = (np.arange(128)[:, None] // 64 == np.arange(2)[None, :]).astype(np.float32)
    c["rowmask"] = (np.arange(128)[:, None] // CH == np.arange(4)[None, :]).astype(np.float32)
    kk = np.arange(128)[:, None]; qq = np.arange(128)[None, :]
    c["mprev4"] = (kk >= qq).astype(np.float32)
    c["mnext4"] = (kk <= qq).astype(np.float32)
    rm = np.ones((128, T), np.float32); rm[:, ::CH] = 0.0
    c["resetm"] = rm
    rows = 2048 // 64
    row = np.repeat(np.arange(rows, dtype=np.float32), 64); col = np.tile(np.arange(64, dtype=np.float32), rows)
    inv = (np.float32(10000.0) ** (-np.arange(16, dtype=np.float32) / np.float32(16))).astype(np.float32)
    ang = np.concatenate([row[:, None] * inv, col[:, None] * inv], axis=-1).astype(np.float32)
    c["cosF"] = np.ascontiguousarray(np.concatenate([np.cos(ang), np.cos(ang)], -1).T.astype(np.float32))
    c["sinF"] = np.ascontiguousarray(np.concatenate([np.sin(ang), np.sin(ang)], -1).T.astype(np.float32))
    pm = np.zeros((64, 64), np.float32)
    for r in range(32):
        pm[r + 32, r] = -1.0
        pm[r, r + 32] = 1.0
    pm2 = np.zeros((128, 128), np.float32); pm2[:64, :64] = pm; pm2[64:, 64:] = pm
    c["Pm2"] = pm2
    c["cosF"] = np.ascontiguousarray(np.concatenate([c["cosF"], c["cosF"]], 0))
    c["sinF"] = np.ascontiguousarray(np.concatenate([c["sinF"], c["sinF"]], 0))
    return c


class Buf:
    def __init__(self, t, name):
        self.t = t; self.r = Reg(name)

    def __getitem__(self, i):
        return self.t[i]


class Prog:
    def __init__(self, layers, taps=(), stop=99):
        self.layers = layers; self.taps = set(taps); self.stop = stop
        self.nc = bass.Bass("TRN2", target_bir_lowering=False)
        self.st = contextlib.ExitStack()
        self.kb = KB(self.nc, self.st)
        self.din = {}; self.dout = {}
        self.psi = 0

    def inp(self, name, shape, dt=F32):
        a = self.nc.dram_tensor(name, list(shape), dt, kind="ExternalInput").ap()
        self.din[name] = a
        return a

    def outp(self, name, shape, dt=F32):
        a = self.nc.dram_tensor(name, list(shape), dt, kind="ExternalOutput").ap()
        self.dout[name] = a
        return a

    def scratch(self, name, shape, dt=F32):
        if name in self.taps:
            return Buf(self.outp(name, shape, dt), name)
        return Buf(self.nc.dram_tensor(name, list(shape), dt, kind="Internal").ap(), name)

    def sb(self, name, shape, dt=F32):
        return Buf(self.st.enter_context(self.nc.sbuf_tensor(name, list(shape), dt)), name)

    def next_ps(self):
        b = self.ps[self.psi]; self.psi = (self.psi + 1) % 8
        return b

    def _rw(self, R, W):
        return [b.r for b in R], [b.r for b in W]

    def MM(self, out, lhsT, rhs, R, W, start=True, stop=True):
        r, w = self._rw(R, W)
        self.kb.op(self.kb.pe, lambda e: e.matmul(out, lhsT=lhsT, rhs=rhs, start=start, stop=stop), r, w)

    def TR(self, out, in_, R, W, n=128):
        r, w = self._rw(R + [self.ident], W)
        idn = self.ident[0:n, 0:n]
        self.kb.op(self.kb.pe, lambda e: e.transpose(out=out, in_=in_, identity=idn), r, w)

    def ACT(self, out, in_, func, R, W, bias=None, scale=None):
        r, w = self._rw(R, W)
        kw = {}
        if bias is not None: kw["bias"] = bias
        if scale is not None: kw["scale"] = scale
        self.kb.op(self.kb.act, lambda e: e.activation(out=out, in_=in_, func=func, **kw), r, w)

    def TT(self, out, a, b, op, R, W, eng=None):
        r, w = self._rw(R, W)
        self.kb.op(eng or self.kb.dve, lambda e: e.tensor_tensor(out=out, in0=a, in1=b, op=op), r, w)

    def TS(self, out, a, s1, op0, R, W, s2=None, op1=None, eng=None):
        r, w = self._rw(R, W)
        if op1 is None:
            self.kb.op(eng or self.kb.dve, lambda e: e.tensor_scalar(out=out, in0=a, scalar1=s1, scalar2=None, op0=op0), r, w)
        else:
            self.kb.op(eng or self.kb.dve, lambda e: e.tensor_scalar(out=out, in0=a, scalar1=s1, scalar2=s2, op0=op0, op1=op1), r, w)

    def STT(self, out, a, s, b, op0, op1, R, W):
        r, w = self._rw(R, W)
        self.kb.op(self.kb.dve, lambda e: e.scalar_tensor_tensor(out=out, in0=a, scalar=s, in1=b, op0=op0, op1=op1), r, w)

    def CP(self, out, in_, R, W, eng=None):
        r, w = self._rw(R, W)
        E = eng or self.kb.dve
        if E is self.kb.act:
            self.kb.op(E, lambda e: e.copy(out=out, in_=in_), r, w)
        else:
            self.kb.op(E, lambda e: e.tensor_copy(out=out, in_=in_), r, w)

    def MSET(self, ap, val, W, eng=None):
        r, w = self._rw([], W)
        self.kb.op(eng or self.kb.pool, lambda e: e.memset(ap, val), r, w)

    def RECIP(self, out, in_, R, W):
        r, w = self._rw(R, W)
        self.kb.op(self.kb.dve, lambda e: e.reciprocal(out=out, in_=in_), r, w)

    def SCAN(self, out, d0, d1, R, W, init=0.0):
        r, w = self._rw(R, W)
        self.kb.op(self.kb.dve, lambda e: e.tensor_tensor_scan(out=out, data0=d0, data1=d1, initial=init, op0=ALU.mult, op1=ALU.add), r, w)

    def DMA(self, out, in_, R, W, q=None, **kw):
        r, w = self._rw(R, W)
        self.kb.dma(q or self.kb.sp, out, in_, r, w, **kw)

    def tap(self, name, src_ap, R, shape):
        if name in self.taps:
            o = self.outp("tap_" + name, shape)
            self.DMA(o, src_ap, R, [])

    def setup(self):
        P = self
        nc = self.nc
        P.xin = Buf(P.inp("xin", [T, D]), "xin")
        P.condT = P.inp("condT", [128, 8, 2])
        hc = host_consts()
        P.cd = {k: Buf(P.inp("c_" + k, v.shape), "c_" + k) for k, v in hc.items()}
        I = lambda name, shape: (name, shape)
        wdecl = dict(
            ada_w=I("ada_w", [4, D, 6 * D]), ada_b_fm=I("ada_b_fm", [4, 128, 48]),
            ln_g=I("ln_g", [4, 2, D]), ln_b=I("ln_b", [4, 2, D]),
            ev_w_in=I("ev_w_in", [2, D, 3328]), ev_w_out=I("ev_w_out", [2, D, D]),
            hg_lb_fm=I("hg_lb_fm", [128, 4, 2]), hg_ng_fm=I("hg_ng_fm", [2, 128, 4]), attn_sink=I("attn_sink", [2, 8]),
            od_w_in=I("od_w_in", [2, D, 2432]), od_w_out=I("od_w_out", [2, D, D]),
            s5_lre=I("s5_lre", [2, 2, 128, 16]), s5_lim=I("s5_lim", [2, 2, 128, 16]), s5_ldt=I("s5_ldt", [2, 2, 128, 16]),
            s5_bT=I("s5_bT", [2, 128, 16, 2, 16]), s5_cT=I("s5_cT", [2, 128, 16, 2, 16]), s5_d_fm=I("s5_d_fm", [2, 128, 4]),
            s5_glu_w=I("s5_glu_w", [2, 512, 512]),
            rw_mu_fm=I("rw_mu_fm", [2, 128, 15]), rw_w0_fm=I("rw_w0_fm", [2, 2, 128, 4]), rw_a0_fm=I("rw_a0_fm", [2, 2, 128, 4]),
            rw_w2pad=I("rw_w2pad", [2, 2, 128, 512]), rw_a2pad=I("rw_a2pad", [2, 2, 128, 512]), rw_g2=I("rw_g2", [2, 128, 512]),
            rw_kk_fm=I("rw_kk_fm", [2, 128, 4]), rw_ka_fm=I("rw_ka_fm", [2, 128, 4]), rw_rk_fm=I("rw_rk_fm", [2, 128, 4]),
            rw_lng=I("rw_lng", [2, 512]), rw_lnb=I("rw_lnb", [2, 512]),
            moe_router_w=I("moe_router_w", [2, D, 8]), moe_router_b=I("moe_router_b", [2, 8]),
            moe_w_gate=I("moe_w_gate", [2, 8, D, DFF]), moe_w_up=I("moe_w_up", [2, 8, D, DFF]), moe_w_down=I("moe_w_down", [2, 8, DFF, D]),
            ffn_w_gate=I("ffn_w_gate", [2, D, DFF]), ffn_w_up=I("ffn_w_up", [2, D, DFF]), ffn_w_down=I("ffn_w_down", [2, DFF, D]),
        )
        P.w = _LazyW(P, {k: v[1] for k, v in wdecl.items()})
        P.out = Buf(P.outp("out", [T, D]), "out")
        P.xs = [P.scratch("xs0", [T, D]), P.scratch("xs1", [T, D])]
        if "inject_y" in P.taps:
            P.yT = Buf(P.inp("yT_inject", [D, T]), "yT_inject")
        else:
            P.yT = P.scratch("yTd", [D, T])
        P.ident = P.sb("ident", [128, 128]); P.ones = P.sb("ones", [128, 128]); P.onesdiv = P.sb("onesdiv", [128, 128])
        P.tri_le = P.sb("tri_le", [128, 128]); P.tri_ge = P.sb("tri_ge", [128, 128])
        P.mprev4 = P.sb("mprev4", [128, 128]); P.mnext4 = P.sb("mnext4", [128, 128])
        P.cst = P.sb("cst", [128, 8])
        P.hT = P.sb("hT", [128, KC, T], BF16)
        P.G = [None] * NG
        P.Gt = self.st.enter_context(nc.sbuf_tensor("G", [128, NG, T], F32))
        for i in range(NG):
            P.G[i] = Buf(P.Gt[:, i, :], f"G{i}")
        P.wA = [P.sb(f"wA{i}", [128, KC, 128], BF16) for i in range(5)]
        P.wAi = 0
        P.xb = [P.sb(f"xb{i}", [128, D]) for i in range(3)]
        P.xbi = 0
        P.zb = [P.sb("zb0", [128, D])] * 2
        P.bc = [P.sb(f"bc{i}", [128, D]) for i in range(4)]
        P.condS = P.sb("condS", [128, 8, 2]); P.adab = P.sb("adab", [128, 48])
        P.fm = P.sb("fm", [128, 48, 2]); P.sc1p = P.sb("sc1p", [128, 8, 2]); P.sc2p = P.sb("sc2p", [128, 8, 2])
        P.small = [P.sb(f"small{i}", [128, 512]) for i in range(5)]
        P.smi = 0
        P.S = P.sb("S", [128, 128]); P.tmpS = P.sb("tmpS", [128, 128])
        P.stat = P.sb("stat", [128, 32])
        P.s5t = P.sb("s5t", [128, 2, 12, 16]); P.s5i = P.sb("s5i", [128, 16], mybir.dt.int32); P.s5d = P.sb("s5d", [128, 4])
        P.s5bc = P.sb("s5bc", [128, 64]); P.s5zc = P.sb("s5zc", [128, 256])
        P.rwm = P.sb("rwm", [128, 3, 128]); P.bdones = P.sb("bdones", [128, 128])
        P.rwar = [P.sb(f"rwar{h}", [128, 256]) for h in range(2)]
        P.rwxm = [P.sb(f"rwxm{h}", [128, 4, 128]) for h in range(2)]
        P.rwxz = [P.sb(f"rwxz{h}", [128, 3, 128]) for h in range(2)]
        P.rwu = P.sb("rwu", [128, 128]); P.rwp = P.sb("rwp", [128, 48]); P.rwgc = P.sb("rwgc", [128, NT]); P.rwln = P.sb("rwln", [128, 2, 128])
        P.gates = P.sb("gates", [128, NT, 8]); P.rw = P.sb("rw", [128, KC, 8]); P.rb = P.sb("rb", [128, 8])
        P.h32 = [P.sb("h32_0", [128, 4, 128])] * 2; P.rt = P.sb("rt", [128, 64])
        P.lbt = P.sb("lbt", [128, 16]); P.ngt = P.sb("ngt", [128, 4]); P.sk = P.sb("sk", [128, 8])
        P.Pm2 = P.sb("Pm2", [128, 128])
        P.attm = [P.sb(f"attm{i}", [128, 128]) for i in range(2)]; P.attmi = 0
        P.ktok = [P.sb(f"ktok{i}", [128, 4, 128]) for i in range(2)]
        P.rowmask = P.sb("rowmask", [128, 4]); P.halfm = P.sb("halfm", [128, 2])
        P.ps = [Buf(self.st.enter_context(nc.psum_tensor(f"ps{i}", [128, 512], F32)), f"ps{i}") for i in range(8)]
        for k, dst in (("ident", P.ident), ("tri_le", P.tri_le), ("tri_ge", P.tri_ge), ("mprev4", P.mprev4),
                       ("mnext4", P.mnext4), ("Pm2", P.Pm2), ("rowmask", P.rowmask), ("halfm", P.halfm), ("bdones", P.bdones)):
            P.DMA(dst[:], P.cd[k][:], [], [dst])
        P.MSET(P.ones[:], 1.0, [P.ones]); P.MSET(P.onesdiv[:], 1.0 / 128.0, [P.onesdiv])
        P.MSET(P.cst[:, 0:1], 1e-6, [P.cst]); P.MSET(P.cst[:, 1:2], 1e-5, [P.cst]); P.MSET(P.cst[:, 2:3], 64e-5, [P.cst])
        P.MSET(P.cst[:, 3:4], 0.0, [P.cst]); P.MSET(P.cst[:, 4:5], 1.0, [P.cst])
        P.DMA(P.condS[:], P.condT, [], [P.condS])
        P.ACT(P.condS[:], P.condS[:], AF.Silu, [P.condS], [P.condS])

    def gflat(self, slot0, nelem, dt=F32):
        flat = self.Gt[:].rearrange("p a n -> p (a n)")[:, slot0 * T:slot0 * T + nelem]
        ns = (nelem + T - 1) // T
        regs = [self.G[slot0 + i] for i in range(ns)]
        if dt is not F32:
            flat = flat.bitcast(dt)
        return flat, regs

    def sm(self):
        b = self.small[self.smi]; self.smi = (self.smi + 1) % len(self.small)
        return b

    def loadw(self, src, ncols, kc=KC):
        b = self.wA[self.wAi]; self.wAi = (self.wAi + 1) % len(self.wA)
        self.DMA(b[:, 0:kc, 0:ncols], src.rearrange("(c p) n -> p c n", p=128), [], [b], q=self.kb.pool)
        return b

    def phase_mod(self, l):
        P = self
        P.DMA(P.adab[:], P.w["ada_b_fm"][l], [], [P.adab])
        pM = P.next_ps()
        for blk in range(12):
            fl, regs = P.gflat((blk % 2) * 2, 4096)
            stg = fl.rearrange("p (c n) -> p c n", n=512)
            for ch in range(8):
                P.DMA(stg[:, ch, :], P.w["ada_w"][l, ch * 128:(ch + 1) * 128, blk * 512:(blk + 1) * 512], [], regs,
                      q=(P.kb.sp if ch % 2 == 0 else P.kb.act))
            for s in range(4):
                k = blk * 4 + s
                for ch in range(8):
                    P.MM(pM[:, 2 * k:2 * k + 2], stg[:, ch, s * 128:(s + 1) * 128], P.condS[:, ch, :], regs + [P.condS], [pM],
                         start=(ch == 0), stop=(ch == 7))
        for cond in range(2):
            P.TT(P.fm[:, :, cond], pM[:, cond:96:2], P.adab[:], ALU.add, [pM, P.adab], [P.fm])
        P.TS(P.sc1p[:], P.fm[:, 8:16, :], 1.0, ALU.add, [P.fm], [P.sc1p])
        P.TS(P.sc2p[:], P.fm[:, 32:40, :], 1.0, ALU.add, [P.fm], [P.sc2p])

    def gate_bcast(self, q, cond, dst):
        P = self
        for half in range(2):
            pg = P.next_ps()
            for cc in range(4):
                c = half * 4 + cc
                dg = P.sm()
                P.TS(dg[:, 0:128], P.ident[:], P.fm[:, q * 8 + c, cond:cond + 1], ALU.mult, [P.ident, P.fm], [dg])
                P.MM(pg[:, cc * 128:(cc + 1) * 128], P.ones[:], dg[:, 0:128], [P.ones, dg], [pg])
            P.CP(dst[:, half * 512:(half + 1) * 512], pg[:, :], [pg], [dst], eng=P.kb.act)

    def hT_tile(self, xt, ti, scp, shq, router=False):
        P = self
        cond = 1 if ti < 2 else 0
        pl = P.next_ps() if router else None
        for half in range(2):
            pt = P.next_ps()
            for cc in range(4):
                c = half * 4 + cc
                P.TR(pt[:, cc * 128:(cc + 1) * 128], xt[:, c * 128:(c + 1) * 128], [xt], [pt])
            h32 = P.h32[half]
            for cc in range(4):
                c = half * 4 + cc
                if router:
                    P.TS(h32[:, cc, :], pt[:, cc * 128:(cc + 1) * 128], scp[:, c, cond:cond + 1], ALU.mult, [pt, scp, P.fm], [h32],
                         s2=P.fm[:, shq * 8 + c, cond:cond + 1], op1=ALU.add)
                    P.CP(P.hT[:, c, ti * 128:(ti + 1) * 128], h32[:, cc, :], [h32], [P.hT], eng=P.kb.act)
                else:
                    P.ACT(P.hT[:, c, ti * 128:(ti + 1) * 128], pt[:, cc * 128:(cc + 1) * 128], AF.Identity,
                          [pt, scp, P.fm], [P.hT], scale=scp[:, c, cond:cond + 1], bias=P.fm[:, shq * 8 + c, cond:cond + 1])
            if router:
                for cc in range(4):
                    c = half * 4 + cc
                    P.MM(pl[:, half * 8:half * 8 + 8], h32[:, cc, :], P.rw[:, c, :], [h32, P.rw], [pl], start=(cc == 0), stop=(cc == 3))
        if router and "1" != "2":
            P.top2(pl, ti)

    def top2(self, pl, ti):
        P = self
        rt = P.rt
        R_, W_ = [rt], [rt]
        lg, e1, l2, e2 = rt[:, 0:8], rt[:, 8:16], rt[:, 16:24], rt[:, 24:32]
        m1, m2, dd, p1, p2 = rt[:, 32:33], rt[:, 33:34], rt[:, 34:35], rt[:, 35:36], rt[:, 36:37]
        P.TT(lg, pl[:, 0:8], P.rb[:], ALU.add, [pl, P.rb], W_)
        P.TT(lg, lg, pl[:, 8:16], ALU.add, [pl, rt], W_)
        P.kb.op(P.kb.dve, lambda e: e.reduce_max(out=m1, in_=lg, axis=AX.X), [rt.r], [rt.r])
        P.TS(e1, lg, m1, ALU.is_equal, R_, W_)
        P.STT(l2, e1, -1e30, lg, ALU.mult, ALU.add, R_, W_)
        P.kb.op(P.kb.dve, lambda e: e.reduce_max(out=m2, in_=l2, axis=AX.X), [rt.r], [rt.r])
        P.TS(e2, l2, m2, ALU.is_equal, R_, W_)
        P.TT(dd, m2, m1, ALU.subtract, R_, W_)
        P.ACT(dd, dd, AF.Exp, R_, W_)
        P.TS(p1, dd, 1.0, ALU.add, R_, W_)
        P.RECIP(p1, p1, R_, W_)
        P.TT(p2, dd, p1, ALU.mult, R_, W_)
        P.TS(e1, e1, p1, ALU.mult, R_, W_)
        P.STT(P.gates[:, ti, :], e2, p2, e1, ALU.mult, ALU.add, R_, [P.gates])

    def phase_hT(self, xsrc):
        P = self
        for ti in range(NT):
            xt = P.xb[P.xbi]; P.xbi = (P.xbi + 1) % 3
            P.DMA(xt[:], xsrc[ti * 128:(ti + 1) * 128, :], [xsrc], [xt])
            P.hT_tile(xt, ti, P.sc1p, 0)

    def proj_fm(self, wb, M, evac, col0=0):
        P = self
        for (t0, n) in NTILES:
            pb = P.next_ps()
            for c in range(KC):
                P.MM(pb[0:M, 0:n], wb[:, c, col0:col0 + M], P.hT[:, c, t0:t0 + n], [wb, P.hT], [pb], start=(c == 0), stop=(c == 7))
            evac(pb, t0, n)

    def hgrn2(self, l, j):
        P = self
        G = P.G
        W = P.w["ev_w_in"][j]
        lbt = P.lbt; ngt = P.ngt
        P.DMA(ngt[:, 0:4], P.w["hg_ng_fm"][j], [], [ngt])
        if j == 0:
            P.MSET(lbt[:, 0:4], 0.0, [lbt]); P.MSET(lbt[:, 4:8], 1.0, [lbt])
        else:
            P.DMA(lbt[:, 8:16], P.w["hg_lb_fm"].rearrange("p h j -> p (h j)"), [], [lbt])
            P.TT(lbt[:, 0:4], lbt[:, 9:16:2], lbt[:, 8:16:2], ALU.subtract, [lbt], [lbt])
            P.ACT(lbt[:, 0:4], lbt[:, 0:4], AF.Sigmoid, [lbt], [lbt])
            P.TS(lbt[:, 4:8], lbt[:, 0:4], -1.0, ALU.mult, [lbt], [lbt], s2=1.0, op1=ALU.add)
        qT, sgT, X1, X2, X3, X4, oacc = G[0], G[1], G[2], G[3], G[4], G[5], G[8]
        P.resetm = G[9]
        P.DMA(P.resetm[:], P.cd["resetm"][:], [], [P.resetm])
        itok = P.Gt[:, 6, :].rearrange("p (b c) -> p b c", c=128)
        itr = [G[6]]
        for hd in range(4):
            cs = lambda k: W[:, k * 512 + hd * 128:k * 512 + (hd + 1) * 128]
            wq = P.loadw(cs(0), 128)
            P.proj_fm(wq, 128, lambda pb, t0, n: P.ACT(qT[:, t0:t0 + n], pb[:, 0:n], AF.Silu, [pb], [qT]))
            wg = P.loadw(cs(4), 128)
            P.proj_fm(wg, 128, lambda pb, t0, n: P.ACT(sgT[:, t0:t0 + n], pb[:, 0:n], AF.Silu, [pb], [sgT]))
            wi = P.loadw(cs(3), 128)
            for b0 in range(0, NT, 4):
                nb = min(4, NT - b0)
                pi = P.next_ps()
                for q in range(nb):
                    ti = b0 + q
                    for c in range(KC):
                        P.MM(pi[:, q * 128:(q + 1) * 128], P.hT[:, c, ti * 128:(ti + 1) * 128], wi[:, c, :], [P.hT, wi], [pi],
                             start=(c == 0), stop=(c == 7))
                P.CP(itok[:, b0:b0 + nb, :], pi[:, 0:nb * 128].rearrange("p (a b) -> p a b", b=128), [pi], itr, eng=P.kb.act)
            for d in range(2):
                fwd = d == 0
                wf = P.loadw(cs(1 + d), 128)
                P.proj_fm(wf, 128, lambda pb, t0, n: P.ACT(X1[:, t0:t0 + n], pb[:, 0:n], AF.Sigmoid, [pb], [X1]))
                P.TS(X1[:], X1[:], lbt[:, 4 + hd:5 + hd], ALU.mult, [X1, lbt], [X1], s2=lbt[:, hd:hd + 1], op1=ALU.add)
                P.TS(X2[:], X1[:], -1.0, ALU.mult, [X1], [X2], s2=1.0, op1=ALU.add)
                P.ACT(X1[:], X1[:], AF.Ln, [X1], [X1])
                if fwd:
                    P.SCAN(X3[:], P.resetm[:], X1[:], [P.resetm, X1], [X3])
                else:
                    P.SCAN(X3[:, ::-1], P.resetm[:], X1[:, ::-1], [P.resetm, X1], [X3])
                P.ACT(X1[:], X3[:], AF.Exp, [X3], [X1])
                P.STT(X4[:], qT[:], 128.0 ** -0.5, X1[:], ALU.mult, ALU.mult, [qT, X1], [X4])
                P.ACT(X3[:], X3[:], AF.Exp, [X3], [X3], scale=-1.0)
                P.TT(X2[:], X2[:], X3[:], ALU.mult, [X2, X3], [X2])
                P.MSET(P.S[:], 0.0, [P.S])
                tiles = list(range(NT)) if fwd else [1, 0] + list(range(NT - 1, 1, -1))
                mask = P.tri_le if fwd else P.tri_ge
                for ti in tiles:
                    tsl = slice(ti * 128, (ti + 1) * 128)
                    pA = P.next_ps()
                    P.MM(pA[:, 0:128], X2[:, tsl], X4[:, tsl], [X2, X4], [pA])
                    attm = P.attm[P.attmi]; ktok = P.ktok[P.attmi]; P.attmi ^= 1
                    P.TT(attm[:], pA[:, 0:128], mask[:], ALU.mult, [pA, mask], [attm])
                    pB = P.next_ps()
                    P.TR(pB[:, 0:128], X2[:, tsl], [X2], [pB])
                    for q in range(4):
                        P.ACT(ktok[:, q, :], pB[:, 0:128], AF.Identity, [pB, P.rowmask], [ktok], scale=P.rowmask[:, q:q + 1])
                    pC = P.next_ps()
                    P.MM(pC[:, 0:128], itok[:, ti, :], attm[:], itr + [attm], [pC], start=True, stop=False)
                    for q in (range(4) if fwd else range(3, -1, -1)):
                        c0 = ti * 128 + q * CH
                        csl = slice(c0, c0 + CH); prt = slice(q * CH, (q + 1) * CH)
                        P.MM(pC[:, q * CH:(q + 1) * CH], P.S[:], X4[:, csl], [P.S, X4], [pC], start=False, stop=True)
                        pD = P.next_ps()
                        P.MM(pD[:, 0:128], ktok[:, q, :], itok[:, ti, :], [ktok] + itr, [pD])
                        P.TT(P.tmpS[:], pD[:, 0:128], P.S[:], ALU.add, [pD, P.S], [P.tmpS])
                        last = c0 + CH - 1 if fwd else c0
                        P.ACT(P.S[:], P.tmpS[:], AF.Identity, [P.tmpS, X1], [P.S], scale=X1[:, last:last + 1])
                    if fwd:
                        P.CP(oacc[:, tsl], pC[:, 0:128], [pC], [oacc], eng=P.kb.act)
                    else:
                        P.TT(oacc[:, tsl], oacc[:, tsl], pC[:, 0:128], ALU.add, [oacc, pC], [oacc])
            for (t0, n) in NTILES:
                sq = P.sm()
                P.ACT(sq[:, 0:n], oacc[:, t0:t0 + n], AF.Square, [oacc], [sq])
                pE = P.next_ps()
                P.MM(pE[:, 0:n], P.onesdiv[:], sq[:, 0:n], [P.onesdiv, sq], [pE])
                rs = P.sm()
                P.ACT(rs[:, 0:n], pE[:, 0:n], AF.Sqrt, [pE, P.cst], [rs], bias=P.cst[:, 0:1])
                P.RECIP(rs[:, 0:n], rs[:, 0:n], [rs], [rs])
                yo = P.sm()
                P.STT(yo[:, 0:n], oacc[:, t0:t0 + n], ngt[:, hd:hd + 1], rs[:, 0:n], ALU.mult, ALU.mult, [oacc, ngt, rs], [yo])
                P.TT(yo[:, 0:n], yo[:, 0:n], sgT[:, t0:t0 + n], ALU.mult, [yo, sgT], [yo])
                P.DMA(P.yT[hd * 128:(hd + 1) * 128, t0:t0 + n], yo[:, 0:n], [yo], [P.yT])

    def attention(self, l, j):
        P = self
        G = P.G
        W = P.w["ev_w_in"][j]
        sk = P.sk
        P.DMA(sk[:, 0:8], P.w["attn_sink"][j].partition_broadcast(128), [], [sk])
        P.ACT(sk[:, 0:8], sk[:, 0:8], AF.Exp, [sk], [sk])
        cosT, sinT, kT2, q2 = G[0], G[1], G[2], G[3]
        qm = [G[4], G[5], G[6], G[7]]
        ptb = [(P.Gt[:, 8, i * 512:(i + 1) * 512], G[8]) for i in range(4)] + [(P.Gt[:, 9, 0:512], G[9])]
        vaug = P.Gt[:, 9, 512:512 + NT * 65].rearrange("p (t e) -> p t e", e=65)
        vreg = [G[9]]
        P.DMA(cosT[:, 0:2048], P.cd["cosF"][:], [], [cosT]); P.DMA(sinT[:, 0:2048], P.cd["sinF"][:], [], [sinT])

        def rope(src):
            for (t0, n) in NTILES[1:]:
                pr = P.next_ps()
                P.MM(pr[:, 0:n], P.Pm2[:], src[:, t0:t0 + n], [P.Pm2, src], [pr])
                t1 = P.sm(); t2 = P.sm()
                P.TT(t1[:, 0:n], src[:, t0:t0 + n], cosT[:, t0 - LC:t0 - LC + n], ALU.mult, [src, cosT], [t1])
                P.TT(t2[:, 0:n], pr[:, 0:n], sinT[:, t0 - LC:t0 - LC + n], ALU.mult, [pr, sinT], [t2])
                P.TT(src[:, t0:t0 + n], t1[:, 0:n], t2[:, 0:n], ALU.add, [t1, t2], [src])

        for g in range(2):
            wv = P.loadw(W[:, 3200 + g * 64:3200 + (g + 1) * 64], 64)
            P.MSET(vaug[:, :, 64:65], 1.0, vreg)
            for ti in range(NT):
                pv = P.next_ps()
                for c in range(KC):
                    P.MM(pv[:, 0:64], P.hT[:, c, ti * 128:(ti + 1) * 128], wv[:, c, 0:64], [P.hT, wv], [pv], start=(c == 0), stop=(c == 7))
                P.CP(vaug[:, ti, 0:64], pv[:, 0:64], [pv], vreg, eng=P.kb.act)
            wk = P.wA[P.wAi]; P.wAi = (P.wAi + 1) % len(P.wA)
            ksrc = W[:, 3072 + g * 64:3072 + (g + 1) * 64].rearrange("(c p) n -> p c n", p=128)
            P.DMA(wk[:, :, 0:64], ksrc, [], [wk], q=P.kb.pool)
            P.DMA(wk[:, :, 64:128], ksrc, [], [wk], q=P.kb.pool)
            P.proj_fm(wk, 128, lambda pb, t0, n: P.CP(kT2[:, t0:t0 + n], pb[:, 0:n], [pb], [kT2], eng=P.kb.act))
            rope(kT2)
            for pair in range(2):
                h0 = g * 4 + pair * 2
                wq = P.loadw(W[:, 2560 + h0 * 64:2560 + (h0 + 2) * 64], 128)
                P.proj_fm(wq, 128, lambda pb, t0, n: P.CP(q2[:, t0:t0 + n], pb[:, 0:n], [pb], [q2], eng=P.kb.act))
                rope(q2)
                for half in range(2):
                    dst = qm[pair * 2 + half]
                    P.TS(dst[:], q2[:], P.halfm[:, half:half + 1], ALU.mult, [q2, P.halfm], [dst])
            for ti in range(NT):
                if ti < 2:
                    keys = [(0, 'c'), (1, 'c')]
                else:
                    keys = [(kt, kd) for kt, kd in ((ti - 1, 'p'), (ti, 's'), (ti + 1, 'n')) if 2 <= kt < NT] + [(0, 'c'), (1, 'c')]
                pts = []
                for ki, (kt, kd) in enumerate(keys):
                    pS = P.next_ps()
                    for hh in range(4):
                        P.MM(pS[:, hh * 128:(hh + 1) * 128], kT2[:, kt * 128:(kt + 1) * 128], qm[hh][:, ti * 128:(ti + 1) * 128],
                             [kT2, qm[hh]], [pS])
                    pt, pr_ = ptb[ki]
                    P.ACT(pt, pS[:, 0:512], AF.Exp, [pS], [pr_], scale=0.125)
                    if kd in ('p', 'n'):
                        mk_ = P.mprev4 if kd == 'p' else P.mnext4
                        for hh in range(4):
                            P.TT(pt[:, hh * 128:(hh + 1) * 128], pt[:, hh * 128:(hh + 1) * 128], mk_[:], ALU.mult, [pr_, mk_], [pr_],
                                 eng=(P.kb.pool if hh % 2 else P.kb.dve))
                    pts.append((pt, pr_, kt))
                pO = P.next_ps()
                for hh in range(4):
                    for ki, (pt, pr_, kt) in enumerate(pts):
                        P.MM(pO[:, hh * 65:(hh + 1) * 65], pt[:, hh * 128:(hh + 1) * 128], vaug[:, kt, :], [pr_] + vreg, [pO],
                             start=(ki == 0), stop=(ki == len(pts) - 1))
                den = P.sm()
                P.TT(den[:, 0:4], pO[:, 64:260:65], sk[:, g * 4:(g + 1) * 4], ALU.add, [pO, sk], [den])
                P.RECIP(den[:, 0:4], den[:, 0:4], [den], [den])
                ob = P.sm()
                for hh in range(4):
                    P.TS(ob[:, hh * 64:(hh + 1) * 64], pO[:, hh * 65:hh * 65 + 64], den[:, hh:hh + 1], ALU.mult, [pO, den], [ob])
                for half in range(2):
                    pT = P.next_ps()
                    P.TR(pT[:, 0:128], ob[:, half * 128:(half + 1) * 128], [ob], [pT])
                    yo = P.sm()
                    P.CP(yo[:, 0:128], pT[:, 0:128], [pT], [yo], eng=P.kb.act)
                    r0 = 512 + g * 256 + half * 128
                    P.DMA(P.yT[r0:r0 + 128, ti * 128:(ti + 1) * 128], yo[:, 0:128], [yo], [P.yT])

    def resid_ln(self, z, xt, ti, xn):
        P = self
        P.STT(z[:], xt[:], ALPHA, z[:], ALU.mult, ALU.add, [xt, z], [z])
        st = P.stat
        for hf in range(2):
            P.kb.op(P.kb.dve, lambda e, hf=hf: e.bn_stats(out=st[:, hf * 6:(hf + 1) * 6], in_=z[:, hf * 512:(hf + 1) * 512]), [z.r], [st.r])
        P.kb.op(P.kb.dve, lambda e: e.bn_aggr(out=st[:, 12:14], in_=st[:, 0:12]), [st.r], [st.r])
        P.ACT(st[:, 14:15], st[:, 13:14], AF.Sqrt, [st, P.cst], [st], bias=P.cst[:, 1:2])
        P.RECIP(st[:, 14:15], st[:, 14:15], [st], [st])
        P.TS(z[:], z[:], st[:, 12:13], ALU.subtract, [z, st], [z], s2=st[:, 14:15], op1=ALU.mult)
        P.TT(z[:], z[:], P.bc[2][:], ALU.mult, [z, P.bc[2]], [z])
        P.TT(xn[:], z[:], P.bc[3][:], ALU.add, [z, P.bc[3]], [xn])

    def load_ln(self, l, k):
        P = self
        P.DMA(P.bc[2][:], P.w["ln_g"][l, k].partition_broadcast(128), [], [P.bc[2]])
        P.DMA(P.bc[3][:], P.w["ln_b"][l, k].partition_broadcast(128), [], [P.bc[3]])

    def phase_out(self, l, wout_dram, xsrc, xdst, router_j=None):
        P = self
        P.gate_bcast(2, 0, P.bc[0]); P.gate_bcast(2, 1, P.bc[1]); P.load_ln(l, 0)
        if router_j is not None:
            P.DMA(P.rw[:], P.w["moe_router_w"][router_j].rearrange("(c p) n -> p c n", p=128), [], [P.rw])
            P.DMA(P.rb[:], P.w["moe_router_b"][router_j].partition_broadcast(128), [], [P.rb])
        wof, wor = P.gflat(0, 4096, BF16)
        wo = wof.rearrange("p (c n) -> p c n", n=1024)
        P.DMA(wo, wout_dram.rearrange("(c p) n -> p c n", p=128), [], wor, q=P.kb.pool)
        ytb = [P.gflat(2 + i, 512, BF16)[0].rearrange("p (c n) -> p c n", n=128) for i in range(2)]
        for ti in range(NT):
            cond = 1 if ti < 2 else 0
            yt, yr = ytb[ti % 2], P.G[2 + ti % 2]
            P.DMA(yt, P.yT[:, ti * 128:(ti + 1) * 128].rearrange("(c p) n -> p c n", p=128), [P.yT], [yr], q=P.kb.pool)
            xt = P.xb[P.xbi]; P.xbi = (P.xbi + 1) % 3
            P.DMA(xt[:], xsrc[ti * 128:(ti + 1) * 128, :], [xsrc], [xt])
            z = P.zb[ti % 2]
            for hf in range(2):
                po = P.next_ps()
                for c in range(KC):
                    P.MM(po[:, :], yt[:, c, :], wo[:, c, hf * 512:(hf + 1) * 512], [yr] + wor, [po], start=(c == 0), stop=(c == 7))
                P.TT(z[:, hf * 512:(hf + 1) * 512], po[:, :], P.bc[cond][:, hf * 512:(hf + 1) * 512], ALU.mult, [po, P.bc[cond]], [z])
            xn = P.xb[P.xbi]; P.xbi = (P.xbi + 1) % 3
            P.resid_ln(z, xt, ti, xn)
            P.DMA(xdst[ti * 128:(ti + 1) * 128, :], xn[:], [xn], [xdst])
            P.hT_tile(xn, ti, P.sc2p, 3, router=(router_j is not None))

    def ffn_tile_weights(self, wg, wu, wd):
        pass

    def phase_ffn(self, l, j, xsrc, xdst, lat_only_out=None):
        P = self
        P.gate_bcast(5, 0, P.bc[0]); P.gate_bcast(5, 1, P.bc[1]); P.load_ln(l, 1)
        Wg, Wu, Wd = P.w["ffn_w_gate"][j], P.w["ffn_w_up"][j], P.w["ffn_w_down"][j]
        wdf, wdr = P.gflat(0, NFC * 512, BF16)
        wd = wdf.rearrange("p (f n) -> p f n", n=1024)
        P.DMA(wd, Wd.rearrange("(f p) n -> p f n", p=128), [], wdr, q=P.kb.pool)
        acf, acr = P.gflat(5, NFC * 256, BF16)
        actT = acf.rearrange("p (f n) -> p f n", n=512)
        for (t0, n) in NTILES:
            for fc in range(NFC):
                wgb = P.loadw(Wg[:, fc * 128:(fc + 1) * 128], 128)
                wub = P.loadw(Wu[:, fc * 128:(fc + 1) * 128], 128)
                pg = P.next_ps(); pu = P.next_ps()
                for c in range(KC):
                    P.MM(pg[:, 0:n], wgb[:, c, :], P.hT[:, c, t0:t0 + n], [wgb, P.hT], [pg], start=(c == 0), stop=(c == 7))
                for c in range(KC):
                    P.MM(pu[:, 0:n], wub[:, c, :], P.hT[:, c, t0:t0 + n], [wub, P.hT], [pu], start=(c == 0), stop=(c == 7))
                sg = P.sm()
                P.ACT(sg[:, 0:n], pg[:, 0:n], AF.Silu, [pg], [sg])
                P.TT(actT[:, fc, 0:n], sg[:, 0:n], pu[:, 0:n], ALU.mult, [sg, pu], acr)
            for sub in range(n // 128):
                ti = t0 // 128 + sub
                cond = 1 if ti < 2 else 0
                xt = P.xb[P.xbi]; P.xbi = (P.xbi + 1) % 3
                P.DMA(xt[:], xsrc[ti * 128:(ti + 1) * 128, :], [xsrc], [xt])
                z = P.zb[ti % 2]
                for hf in range(2):
                    po = P.next_ps()
                    for fc in range(NFC):
                        P.MM(po[:, :], actT[:, fc, sub * 128:(sub + 1) * 128], wd[:, fc, hf * 512:(hf + 1) * 512], acr + wdr, [po],
                             start=(fc == 0), stop=(fc == NFC - 1))
                    P.TT(z[:, hf * 512:(hf + 1) * 512], po[:, :], P.bc[cond][:, hf * 512:(hf + 1) * 512], ALU.mult, [po, P.bc[cond]], [z])
                xn = P.xb[P.xbi]; P.xbi = (P.xbi + 1) % 3
                P.resid_ln(z, xt, ti, xn)
                P.DMA(xdst[ti * 128:(ti + 1) * 128, :], xn[:], [xn], [xdst])

    def phase_moe(self, l, j, xsrc, xdst):
        P = self
        P.gate_bcast(5, 0, P.bc[0]); P.gate_bcast(5, 1, P.bc[1]); P.load_ln(l, 1)
        wdf, wdr = P.gflat(0, NFC * 512, BF16)
        wd = wdf.rearrange("p (f n) -> p f n", n=1024)
        acf, acr = P.gflat(5, NFC * 256, BF16)
        actT = acf.rearrange("p (f n) -> p f n", n=512)
        accf, accr = P.gflat(8, 4096)
        acc = accf.rearrange("p (s n) -> p s n", n=1024)
        for (t0, n) in NTILES:
            nsub = n // 128
            for ex in range(8):
                Wg, Wu, Wd = P.w["moe_w_gate"][j, ex], P.w["moe_w_up"][j, ex], P.w["moe_w_down"][j, ex]
                P.DMA(wd, Wd.rearrange("(f p) n -> p f n", p=128), [], wdr, q=P.kb.pool)
                for fc in range(NFC):
                    wgb = P.loadw(Wg[:, fc * 128:(fc + 1) * 128], 128)
                    wub = P.loadw(Wu[:, fc * 128:(fc + 1) * 128], 128)
                    pg = P.next_ps(); pu = P.next_ps()
                    for c in range(KC):
                        P.MM(pg[:, 0:n], wgb[:, c, :], P.hT[:, c, t0:t0 + n], [wgb, P.hT], [pg], start=(c == 0), stop=(c == 7))
                    for c in range(KC):
                        P.MM(pu[:, 0:n], wub[:, c, :], P.hT[:, c, t0:t0 + n], [wub, P.hT], [pu], start=(c == 0), stop=(c == 7))
                    sg = P.sm()
                    P.ACT(sg[:, 0:n], pg[:, 0:n], AF.Silu, [pg], [sg])
                    P.TT(actT[:, fc, 0:n], sg[:, 0:n], pu[:, 0:n], ALU.mult, [sg, pu], acr)
                for sub in range(nsub):
                    ti = t0 // 128 + sub
                    for hf in range(2):
                        po = P.next_ps()
                        for fc in range(NFC):
                            P.MM(po[:, :], actT[:, fc, sub * 128:(sub + 1) * 128], wd[:, fc, hf * 512:(hf + 1) * 512], acr + wdr, [po],
                                 start=(fc == 0), stop=(fc == NFC - 1))
                        a = acc[:, sub, hf * 512:(hf + 1) * 512]
                        if ex == 0:
                            P.TS(a, po[:, :], P.gates[:, ti, ex:ex + 1], ALU.mult, [po, P.gates], accr)
                        else:
                            P.STT(a, po[:, :], P.gates[:, ti, ex:ex + 1], a, ALU.mult, ALU.add, [po, P.gates] + accr, accr)
            for sub in range(nsub):
                ti = t0 // 128 + sub
                cond = 1 if ti < 2 else 0
                xt = P.xb[P.xbi]; P.xbi = (P.xbi + 1) % 3
                P.DMA(xt[:], xsrc[ti * 128:(ti + 1) * 128, :], [xsrc], [xt])
                z = P.zb[ti % 2]
                P.TT(z[:], acc[:, sub, :], P.bc[cond][:], ALU.mult, accr + [P.bc[cond]], [z])
                xn = P.xb[P.xbi]; P.xbi = (P.xbi + 1) % 3
                P.resid_ln(z, xt, ti, xn)
                P.DMA(xdst[ti * 128:(ti + 1) * 128, :], xn[:], [xn], [xdst])

    def s5(self, l, j):
        P = self
        G = P.G
        I32 = mybir.dt.int32
        TWO_PI = 2.0 * math.pi
        W = P.w["od_w_in"][j]
        st_ = P.s5t
        R_, W_ = [st_, P.s5i], [st_]
        P.DMA(P.s5d[:], P.w["s5_d_fm"][j], [], [P.s5d])
        LRE, LIM, DT, MAG, THN, SN, CS, CRE, CIM, NCIM, TA, TB = range(12)
        for d in range(2):
            q = lambda k: st_[:, d, k, :]
            P.DMA(q(LRE), P.w["s5_lre"][j, d], [], W_); P.DMA(q(LIM), P.w["s5_lim"][j, d], [], W_); P.DMA(q(DT), P.w["s5_ldt"][j, d], [], W_)
            P.ACT(q(DT), q(DT), AF.Exp, R_, W_)
            P.TS(q(LRE), q(LRE), -1e-4, ALU.min, R_, W_)
            P.TT(q(TA), q(LRE), q(DT), ALU.mult, R_, W_)
            P.ACT(q(MAG), q(TA), AF.Exp, R_, W_)
            P.TT(q(THN), q(LIM), q(DT), ALU.mult, R_, W_)
            P.TS(q(THN), q(THN), 1.0 / TWO_PI, ALU.mult, R_, W_)
            P.CP(P.s5i[:], q(THN), R_, [P.s5i]); P.CP(q(TA), P.s5i[:], R_, W_)
            P.TT(q(TA), q(THN), q(TA), ALU.subtract, R_, W_)
            P.ACT(q(SN), q(TA), AF.Sin, R_, W_, scale=TWO_PI)
            P.TS(q(TA), q(TA), 0.25, ALU.add, R_, W_)
            P.CP(P.s5i[:], q(TA), R_, [P.s5i]); P.CP(q(TB), P.s5i[:], R_, W_)
            P.TT(q(TA), q(TA), q(TB), ALU.subtract, R_, W_)
            P.ACT(q(CS), q(TA), AF.Sin, R_, W_, scale=TWO_PI)
            P.TT(q(CS), q(CS), q(MAG), ALU.mult, R_, W_)
            P.TT(q(SN), q(SN), q(MAG), ALU.mult, R_, W_)
            P.TT(q(TA), q(LRE), q(LRE), ALU.mult, R_, W_); P.TT(q(TB), q(LIM), q(LIM), ALU.mult, R_, W_)
            P.TT(q(TA), q(TA), q(TB), ALU.add, R_, W_); P.RECIP(q(TA), q(TA), R_, W_)
            P.TS(q(CS), q(CS), -1.0, ALU.add, R_, W_)
            P.TT(q(CRE), q(CS), q(LRE), ALU.mult, R_, W_); P.TT(q(TB), q(SN), q(LIM), ALU.mult, R_, W_)
            P.TT(q(CRE), q(CRE), q(TB), ALU.add, R_, W_); P.TT(q(CRE), q(CRE), q(TA), ALU.mult, R_, W_)
            P.TT(q(CIM), q(SN), q(LRE), ALU.mult, R_, W_); P.TT(q(TB), q(CS), q(LIM), ALU.mult, R_, W_)
            P.TT(q(CIM), q(CIM), q(TB), ALU.subtract, R_, W_); P.TT(q(CIM), q(CIM), q(TA), ALU.mult, R_, W_)
            P.TS(q(NCIM), q(CIM), -1.0, ALU.mult, R_, W_)
        uT, yacc, A, B, Cs, Sn, t1, t2 = [G[i] for i in range(8)]
        t2i = P.Gt[:, 7, :].bitcast(I32)
        ygf, ygr = P.gflat(8, 2 * T, BF16)
        ygT = ygf.rearrange("p (c n) -> p c n", n=T)
        for ut in range(4):
            wu = P.loadw(W[:, ut * 128:(ut + 1) * 128], 128)
            P.proj_fm(wu, 128, lambda pb, t0, n: P.CP(uT[:, t0:t0 + n], pb[:, 0:n], [pb], [uT], eng=P.kb.act))
            P.TS(yacc[:], uT[:], P.s5d[:, ut:ut + 1], ALU.mult, [uT, P.s5d], [yacc])
            for sl in range(4):
                stt = ut * 4 + sl
                r0 = sl * 32
                bc_ = P.s5bc
                P.DMA(bc_[:, 0:32], P.w["s5_bT"][j, :, stt].rearrange("p a h -> p (a h)"), [], [bc_])
                P.DMA(bc_[:, 32:64], P.w["s5_cT"][j, :, stt].rearrange("p a h -> p (a h)"), [], [bc_])
                Zc = P.s5zc
                P.MSET(Zc[:, 0:256], 0.0, [Zc])
                for gl in range(2):
                    pr = slice(gl * 64, gl * 64 + 64); cc = slice(r0 + gl * 16, r0 + gl * 16 + 16)
                    P.CP(Zc[pr, cc], bc_[pr, 32:48], [bc_], [Zc])
                    P.TS(Zc[pr, 128 + cc.start:128 + cc.stop], bc_[pr, 48:64], -1.0, ALU.mult, [bc_], [Zc])
                for d in range(2):
                    q = lambda k: st_[:, d, k, stt:stt + 1]
                    Z = P.sm(); tmp = P.sm(); L = P.sm()
                    P.MSET(Z[:, 0:256], 0.0, [Z])
                    for gl in range(2):
                        pr = slice(gl * 64, gl * 64 + 64); c0 = r0 + gl * 16
                        P.TS(tmp[pr, 0:16], bc_[pr, 0:16], q(CRE)[pr], ALU.mult, [bc_, st_], [tmp])
                        P.STT(Z[pr, c0:c0 + 16], bc_[pr, 16:32], q(NCIM)[pr], tmp[pr, 0:16], ALU.mult, ALU.add, [bc_, st_, tmp], [Z])
                        P.TS(tmp[pr, 16:32], bc_[pr, 16:32], q(CRE)[pr], ALU.mult, [bc_, st_], [tmp])
                        P.STT(Z[pr, 128 + c0:128 + c0 + 16], bc_[pr, 0:16], q(CIM)[pr], tmp[pr, 16:32], ALU.mult, ALU.add, [bc_, st_, tmp], [Z])
                    for k in range(2):
                        pz = P.next_ps()
                        P.TR(pz[:, 0:128], Z[:, k * 128:(k + 1) * 128], [Z], [pz])
                        P.CP(L[:, k * 128:(k + 1) * 128], pz[:, 0:128], [pz], [L], eng=P.kb.act)
                    P.DMA(t1[:], P.cd["tauF" if d == 0 else "tauB"][:], [], [t1])
                    P.TS(t1[:], t1[:], q(THN), ALU.mult, [t1, st_], [t1])
                    P.CP(t2i, t1[:], [t1], [t2]); P.CP(Cs[:], t2i, [t2], [Cs], eng=P.kb.act)
                    P.TT(t1[:], t1[:], Cs[:], ALU.subtract, [t1, Cs], [t1])
                    P.ACT(Sn[:], t1[:], AF.Sin, [t1], [Sn], scale=TWO_PI)
                    P.TS(t1[:], t1[:], 0.25, ALU.add, [t1], [t1])
                    P.CP(t2i, t1[:], [t1], [t2]); P.CP(Cs[:], t2i, [t2], [Cs], eng=P.kb.act)
                    P.TT(t1[:], t1[:], Cs[:], ALU.subtract, [t1, Cs], [t1])
                    P.ACT(Cs[:], t1[:], AF.Sin, [t1], [Cs], scale=TWO_PI)
                    for (t0, n) in NTILES:
                        pa = P.next_ps(); pb_ = P.next_ps()
                        P.MM(pa[:, 0:n], L[:, 0:128], uT[:, t0:t0 + n], [L, uT], [pa])
                        P.MM(pb_[:, 0:n], L[:, 128:256], uT[:, t0:t0 + n], [L, uT], [pb_])
                        P.CP(A[:, t0:t0 + n], pa[:, 0:n], [pa], [A], eng=P.kb.act)
                        P.CP(B[:, t0:t0 + n], pb_[:, 0:n], [pb_], [B])
                    P.TT(t1[:], A[:], Cs[:], ALU.mult, [A, Cs], [t1]); P.TT(t2[:], B[:], Sn[:], ALU.mult, [B, Sn], [t2])
                    P.TT(A[:], A[:], Sn[:], ALU.mult, [A, Sn], [A]); P.TT(B[:], B[:], Cs[:], ALU.mult, [B, Cs], [B])
                    P.TT(t1[:], t1[:], t2[:], ALU.add, [t1, t2], [t1]); P.TT(B[:], B[:], A[:], ALU.subtract, [B, A], [B])
                    mg = q(MAG)
                    for src, dst in ((t1, t2), (B, A)):
                        if d == 0:
                            P.SCAN(dst[:], mg.broadcast_to([128, T]), src[:], [st_, src], [dst])
                        else:
                            P.SCAN(dst[:, 255::-1], mg.broadcast_to([128, 256]), src[:, 255::-1], [st_, src], [dst])
                            P.SCAN(dst[:, T - 1:255:-1], mg.broadcast_to([128, T - 256]), src[:, T - 1:255:-1], [st_, src, dst], [dst], init=dst[:, 0:1])
                    P.TT(t1[:], t2[:], Cs[:], ALU.mult, [t2, Cs], [t1]); P.TT(B[:], A[:], Sn[:], ALU.mult, [A, Sn], [B])
                    P.TT(t1[:], t1[:], B[:], ALU.subtract, [t1, B], [t1])
                    P.TT(t2[:], t2[:], Sn[:], ALU.mult, [t2, Sn], [t2]); P.TT(A[:], A[:], Cs[:], ALU.mult, [A, Cs], [A])
                    P.TT(t2[:], t2[:], A[:], ALU.add, [t2, A], [t2])
                    for (t0, n) in NTILES:
                        py = P.next_ps()
                        P.MM(py[:, 0:n], Zc[:, 0:128], t1[:, t0:t0 + n], [Zc, t1], [py], start=True, stop=False)
                        P.MM(py[:, 0:n], Zc[:, 128:256], t2[:, t0:t0 + n], [Zc, t2], [py], start=False, stop=True)
                        P.TT(yacc[:, t0:t0 + n], yacc[:, t0:t0 + n], py[:, 0:n], ALU.add, [yacc, py], [yacc])
            P.ACT(t1[:], yacc[:], AF.Square, [yacc], [t1])
            P.TS(t1[:], t1[:], 0.044715, ALU.mult, [t1], [t1], s2=1.0, op1=ALU.add)
            P.TT(t1[:], t1[:], yacc[:], ALU.mult, [t1, yacc], [t1])
            P.ACT(t1[:], t1[:], AF.Sigmoid, [t1], [t1], scale=2.0 * math.sqrt(2.0 / math.pi))
            P.TT(ygT[:, ut, :], yacc[:], t1[:], ALU.mult, [yacc, t1], ygr)
        for nt in range(4):
            wg = P.loadw(P.w["s5_glu_w"][j][:, nt * 128:(nt + 1) * 128], 128, kc=4)
            for (t0, n) in NTILES:
                pg = P.next_ps()
                for c in range(4):
                    P.MM(pg[:, 0:n], wg[:, c, :], ygT[:, c, t0:t0 + n], [wg] + ygr, [pg], start=(c == 0), stop=(c == 3))
                sg = P.sm()
                P.ACT(sg[:, 0:n], pg[:, 0:n], AF.Sigmoid, [pg], [sg])
                yo = P.sm()
                P.TT(yo[:, 0:n], ygT[:, nt, t0:t0 + n], sg[:, 0:n], ALU.mult, ygr + [sg], [yo])
                P.DMA(P.yT[nt * 128:(nt + 1) * 128, t0:t0 + n], yo[:, 0:n], [yo], [P.yT])

    def rwkv(self, l, j):
        P = self
        G = P.G
        W = P.w["od_w_in"][j]
        C0 = 512
        prm = P.rwp
        P.DMA(prm[:, 0:15], P.w["rw_mu_fm"][j], [], [prm])
        P.DMA(prm[:, 16:20], P.w["rw_kk_fm"][j], [], [prm]); P.DMA(prm[:, 20:24], P.w["rw_ka_fm"][j], [], [prm])
        P.DMA(prm[:, 24:28], P.w["rw_rk_fm"][j], [], [prm])
        for d in range(2):
            P.DMA(prm[:, 28 + d * 4:32 + d * 4], P.w["rw_w0_fm"][j, d], [], [prm])
            P.DMA(prm[:, 36 + d * 4:40 + d * 4], P.w["rw_a0_fm"][j, d], [], [prm])
        MU = lambda t_: prm[:, t_:t_ + 1]
        rT, kT, vtokS, kkT, L_, Gc, E1, E2, As, Oacc = [G[i] for i in range(10)]
        vtok = vtokS[:].rearrange("p (t c) -> p t c", c=128)
        oacc = Oacc[:].rearrange("p (t c) -> p t c", c=128)
        Hbd = P.S

        def proj_shifted(tile_idx, dst, tmp):
            wb = P.loadw(W[:, C0 + tile_idx * 128:C0 + (tile_idx + 1) * 128], 128)
            P.proj_fm(wb, 128, lambda pb, t0, n: P.CP(tmp[:, t0:t0 + n], pb[:, 0:n], [pb], [tmp], eng=P.kb.act))
            for (a, b) in ((0, LC), (LC, T)):
                P.CP(dst[:, a:a + 1], tmp[:, a + 1:a + 2], [tmp], [dst], eng=P.kb.pool)
                P.CP(dst[:, b - 1:b], tmp[:, b - 2:b - 1], [tmp], [dst], eng=P.kb.pool)
                P.TT(dst[:, a + 1:b - 1], tmp[:, a:b - 2], tmp[:, a + 2:b], ALU.add, [tmp], [dst])
            P.STT(dst[:], dst[:], 0.5, tmp[:], ALU.mult, ALU.subtract, [dst, tmp], [dst])
            P.STT(dst[:], dst[:], MU(tile_idx), tmp[:], ALU.mult, ALU.add, [dst, prm, tmp], [dst])

        P.DMA(P.rwm[:], P.cd["rw_masks"][0], [], [P.rwm])
        for pp in range(4):
            proj_shifted(0 + pp, rT, E1)
            proj_shifted(4 + pp, kT, E1)
            proj_shifted(8 + pp, E2, E1)
            for ti in range(NT):
                pv = P.next_ps()
                P.TR(pv[:, 0:128], E2[:, ti * 128:(ti + 1) * 128], [E2], [pv])
                P.CP(vtok[:, ti, :], pv[:, 0:128], [pv], [vtokS], eng=P.kb.act)
            P.TS(kkT[:], kT[:], prm[:, 16 + pp:17 + pp], ALU.mult, [kT, prm], [kkT])
            for (t0, n) in NTILES:
                sq = P.sm()
                P.ACT(sq[:, 0:n], kkT[:, t0:t0 + n], AF.Square, [kkT], [sq])
                pn = P.next_ps()
                P.MM(pn[:, 0:n], P.bdones[:], sq[:, 0:n], [P.bdones, sq], [pn])
                nr_ = P.sm()
                P.ACT(nr_[:, 0:n], pn[:, 0:n], AF.Sqrt, [pn], [nr_])
                P.TS(nr_[:, 0:n], nr_[:, 0:n], 1e-12, ALU.max, [nr_], [nr_])
                P.RECIP(nr_[:, 0:n], nr_[:, 0:n], [nr_], [nr_])
                P.TT(kkT[:, t0:t0 + n], kkT[:, t0:t0 + n], nr_[:, 0:n], ALU.mult, [kkT, nr_], [kkT])
            P.DMA(P.rwln[:, 0, :], P.w["rw_lng"][j, pp * 128:(pp + 1) * 128].partition_broadcast(128), [], [P.rwln])
            P.DMA(P.rwln[:, 1, :], P.w["rw_lnb"][j, pp * 128:(pp + 1) * 128].partition_broadcast(128), [], [P.rwln])
            for d in range(2):
                fwd = d == 0
                P.DMA(P.rwm[:], P.cd["rw_masks"][d], [], [P.rwm])
                for (lt, padk, biasc, dstb, fn) in ((12, "rw_w2pad", 28 + d * 4 + pp, L_, AF.Tanh), (13, "rw_a2pad", 36 + d * 4 + pp, As, None)):
                    proj_shifted(lt, E1, E2)
                    if fn is not None:
                        P.ACT(E1[:], E1[:], fn, [E1], [E1])
                    wpad = P.attm[1]
                    P.DMA(wpad[:, 0:128], P.w[padk][j, d, :, pp * 128:(pp + 1) * 128], [], [wpad])
                    for (t0, n) in NTILES:
                        pz = P.next_ps()
                        P.MM(pz[:, 0:n], wpad[:, 0:128], E1[:, t0:t0 + n], [wpad, E1], [pz])
                        P.ACT(dstb[:, t0:t0 + n], pz[:, 0:n], AF.Sigmoid, [pz, prm], [dstb], bias=prm[:, biasc:biasc + 1])
                P.TS(L_[:], L_[:], -math.exp(-0.5), ALU.mult, [L_], [L_])
                for ti in range(NT):
                    tsl = slice(ti * 128, (ti + 1) * 128)
                    if fwd:
                        P.SCAN(Gc[:, tsl], P.ones[:], L_[:, tsl], [P.ones, L_], [Gc])
                    else:
                        hi, lo = (ti + 1) * 128 - 1, ti * 128 - 1
                        rs = slice(hi, lo if lo >= 0 else None, -1)
                        P.SCAN(Gc[:, rs], P.ones[:], L_[:, rs], [P.ones, L_], [Gc])
                P.ACT(E1[:], Gc[:], AF.Exp, [Gc], [E1])
                ecol = E1[:, 127:T:128] if fwd else E1[:, 0:T:128]
                P.CP(P.rwgc[:], ecol, [E1], [P.rwgc])
                P.TT(E1[:], E1[:], rT[:], ALU.mult, [E1, rT], [E1])
                P.TT(E2[:], Gc[:], L_[:], ALU.subtract, [Gc, L_], [E2])
                P.ACT(E2[:], E2[:], AF.Exp, [E2], [E2])
                P.STT(E2[:], kkT[:], -1.0, E2[:], ALU.mult, ALU.mult, [kkT, E2], [E2])
                P.ACT(Gc[:], Gc[:], AF.Exp, [Gc], [Gc], scale=-1.0)
                P.TT(L_[:], kkT[:], As[:], ALU.mult, [kkT, As], [L_])
                P.TT(L_[:], L_[:], Gc[:], ALU.mult, [L_, Gc], [L_])
                P.TS(As[:], As[:], -1.0, ALU.add, [As, prm], [As], s2=prm[:, 20 + pp:21 + pp], op1=ALU.mult)
                P.STT(As[:], As[:], 1.0, kT[:], ALU.add, ALU.mult, [As, kT], [As])
                P.TT(As[:], As[:], Gc[:], ALU.mult, [As, Gc], [As])
                rt_, at_, bt_, kt_ = E1, E2, L_, As
                P.MSET(Hbd[:], 0.0, [Hbd])
                tiles = list(range(NT)) if fwd else [1, 0] + list(range(NT - 1, 1, -1))
                hs = [slice(0, 64), slice(64, 128)]
                CB = [P.bc[0], P.bc[1], P.bc[2], P.bc[3], P.xb[0], P.xb[1]]
                XA, ZA, XB, ZB, WT, MRB, LAK, MRK = range(8)
                mat = lambda c, q_: CB[c][:, q_ * 128:(q_ + 1) * 128]
                Ybuf = P.attm[0]
                for g0 in range(0, NT, 3):
                    grp = tiles[g0:g0 + 3]
                    chains = [(ti, hh) for ti in grp for hh in range(2)]
                    for c, (ti, hh) in enumerate(chains):
                        tsl = slice(ti * 128, (ti + 1) * 128)
                        ar = P.rwar[hh]; cb = CB[c]
                        P.TS(ar[:, 0:128], at_[:, tsl], P.halfm[:, hh:hh + 1], ALU.mult, [at_, P.halfm], [ar])
                        P.TS(ar[:, 128:256], rt_[:, tsl], P.halfm[:, hh:hh + 1], ALU.mult, [rt_, P.halfm], [ar], eng=P.kb.pool)
                        p1 = P.next_ps(); p2 = P.next_ps(); p3 = P.next_ps()
                        P.MM(p1[:, 0:256], bt_[:, tsl], ar[:, 0:256], [bt_, ar], [p1])
                        P.MM(p2[:, 0:256], kt_[:, tsl], ar[:, 0:256], [kt_, ar], [p2])
                        P.MM(p3[:, 0:128], ar[:, 0:128], bt_[:, tsl], [ar, bt_], [p3])
                        P.TT(mat(c, XA), p1[:, 0:128], P.rwm[:, 0, :], ALU.mult, [p1, P.rwm], [cb])
                        P.TT(mat(c, MRB), p1[:, 128:256], P.rwm[:, 1, :], ALU.mult, [p1, P.rwm], [cb])
                        P.TT(mat(c, LAK), p2[:, 0:128], P.rwm[:, 0, :], ALU.mult, [p2, P.rwm], [cb])
                        P.TT(mat(c, MRK), p2[:, 128:256], P.rwm[:, 1, :], ALU.mult, [p2, P.rwm], [cb])
                        P.TT(mat(c, ZA), p3[:, 0:128], P.rwm[:, 2, :], ALU.mult, [p3, P.rwm], [cb])
                        P.TT(mat(c, WT), mat(c, XA), P.ident[:], ALU.add, [cb, P.ident], [cb], eng=P.kb.pool)
                    cx, cz, nx, nz = XA, ZA, XB, ZB
                    for k in range(1, 7):
                        for c in range(len(chains)):
                            cb = CB[c]
                            pZ = P.next_ps()
                            P.MM(pZ[:, 0:128], mat(c, cx), mat(c, cz), [cb], [pZ])
                            if k < 6:
                                pX = P.next_ps()
                                P.MM(pX[:, 0:128], mat(c, cz), mat(c, cx), [cb], [pX])
                            P.CP(mat(c, nz), pZ[:, 0:128], [pZ], [cb])
                            if k < 6:
                                P.CP(mat(c, nx), pX[:, 0:128], [pX], [cb], eng=P.kb.act)
                        for c in range(len(chains)):
                            cb = CB[c]
                            pW = P.next_ps()
                            P.MM(pW[:, 0:128], mat(c, nz), mat(c, WT), [cb], [pW])
                            P.TT(mat(c, WT), mat(c, WT), pW[:, 0:128], ALU.add, [cb, pW], [cb])
                        cx, cz, nx, nz = nx, nz, cx, cz
                    for gi, ti in enumerate(grp):
                        tsl = slice(ti * 128, (ti + 1) * 128)
                        U = P.rwu
                        pY = P.next_ps()
                        for hh in range(2):
                            c = gi * 2 + hh
                            P.MM(pY[:, hs[hh]], at_[:, tsl], Hbd[:, hs[hh]], [at_, Hbd], [pY], start=True, stop=False)
                            P.MM(pY[:, hs[hh]], mat(c, LAK), vtok[:, ti, hs[hh]], [CB[c], vtokS], [pY], start=False, stop=True)
                        P.CP(Ybuf[:], pY[:, 0:128], [pY], [Ybuf])
                        pU = P.next_ps()
                        for hh in range(2):
                            c = gi * 2 + hh
                            P.MM(pU[:, hs[hh]], mat(c, WT), Ybuf[:, hs[hh]], [CB[c], Ybuf], [pU])
                        P.CP(U[:], pU[:, 0:128], [pU], [U], eng=P.kb.act)
                        pO = P.next_ps()
                        for hh in range(2):
                            c = gi * 2 + hh
                            P.MM(pO[:, hs[hh]], rt_[:, tsl], Hbd[:, hs[hh]], [rt_, Hbd], [pO], start=True, stop=False)
                            P.MM(pO[:, hs[hh]], mat(c, MRB), U[:, hs[hh]], [CB[c], U], [pO], start=False, stop=False)
                            P.MM(pO[:, hs[hh]], mat(c, MRK), vtok[:, ti, hs[hh]], [CB[c], vtokS], [pO], start=False, stop=True)
                        if fwd:
                            P.CP(oacc[:, ti, :], pO[:, 0:128], [pO], [Oacc], eng=P.kb.act)
                        else:
                            P.TT(oacc[:, ti, :], oacc[:, ti, :], pO[:, 0:128], ALU.add, [Oacc, pO], [Oacc])
                        tk = P.ktok[0]
                        for q_, src in ((0, bt_), (1, kt_)):
                            pt_ = P.next_ps()
                            P.TR(pt_[:, 0:128], src[:, tsl], [src], [pt_])
                            P.CP(tk[:, q_, :], pt_[:, 0:128], [pt_], [tk], eng=P.kb.act)
                        pH = P.next_ps()
                        P.MM(pH[:, 0:128], tk[:, 0, :], U[:], [tk, U], [pH], start=True, stop=False)
                        P.MM(pH[:, 0:128], tk[:, 1, :], vtok[:, ti, :], [tk, vtokS], [pH], start=False, stop=True)
                        P.TT(P.tmpS[:], pH[:, 0:128], P.bdones[:], ALU.mult, [pH, P.bdones], [P.tmpS])
                        P.TT(P.tmpS[:], P.tmpS[:], Hbd[:], ALU.add, [P.tmpS, Hbd], [P.tmpS])
                        P.ACT(Hbd[:], P.tmpS[:], AF.Identity, [P.tmpS, P.rwgc], [Hbd], scale=P.rwgc[:, ti:ti + 1])
            P.STT(E1[:], rT[:], prm[:, 24 + pp:25 + pp], kT[:], ALU.mult, ALU.mult, [rT, prm, kT], [E1])
            proj_shifted(14, E2, Gc)
            P.ACT(E2[:], E2[:], AF.Sigmoid, [E2], [E2])
            g2b = P.attm[0]
            P.DMA(g2b[:, 0:128], P.w["rw_g2"][j, :, pp * 128:(pp + 1) * 128], [], [g2b])
            for ti in range(NT):
                tsl = slice(ti * 128, (ti + 1) * 128)
                o = P.sm(); st = P.stat
                P.CP(o[:, 0:128], oacc[:, ti, :], [Oacc], [o], eng=P.kb.pool)
                o3 = o[:, 0:128].rearrange("p (h i) -> p h i", i=64)
                P.kb.op(P.kb.dve, lambda e, o3=o3: e.reduce_sum(out=st[:, 16:18], in_=o3, axis=AX.X), [o.r], [st.r])
                P.TS(st[:, 16:18], st[:, 16:18], 1.0 / 64.0, ALU.mult, [st], [st])
                for hh in range(2):
                    P.TS(o3[:, hh, :], o3[:, hh, :], st[:, 16 + hh:17 + hh], ALU.subtract, [o, st], [o])
                sq = P.sm()
                P.TT(sq[:, 0:128], o[:, 0:128], o[:, 0:128], ALU.mult, [o], [sq])
                sq3 = sq[:, 0:128].rearrange("p (h i) -> p h i", i=64)
                P.kb.op(P.kb.dve, lambda e, sq3=sq3: e.reduce_sum(out=st[:, 18:20], in_=sq3, axis=AX.X), [sq.r], [st.r])
                P.ACT(st[:, 18:20], st[:, 18:20], AF.Sqrt, [st, P.cst], [st], scale=1.0 / 64.0, bias=P.cst[:, 2:3])
                P.RECIP(st[:, 18:20], st[:, 18:20], [st], [st])
                for hh in range(2):
                    P.TS(o3[:, hh, :], o3[:, hh, :], st[:, 18 + hh:19 + hh], ALU.mult, [o, st], [o])
                P.TT(o[:, 0:128], o[:, 0:128], P.rwln[:, 0, :], ALU.mult, [o, P.rwln], [o])
                P.TT(o[:, 0:128], o[:, 0:128], P.rwln[:, 1, :], ALU.add, [o, P.rwln], [o])
                pb_ = P.next_ps()
                P.MM(pb_[:, 0:2], E1[:, tsl], P.halfm[:], [E1, P.halfm], [pb_])
                P.CP(st[:, 20:22], pb_[:, 0:2], [pb_], [st])
                for hh in range(2):
                    P.STT(o3[:, hh, :], vtok[:, ti, hh * 64:(hh + 1) * 64], st[:, 20 + hh:21 + hh], o3[:, hh, :], ALU.mult, ALU.add, [vtokS, st, o], [o])
                pg = P.next_ps()
                P.MM(pg[:, 0:128], E2[:, tsl], g2b[:, 0:128], [E2, g2b], [pg])
                P.TT(o[:, 0:128], o[:, 0:128], pg[:, 0:128], ALU.mult, [o, pg], [o])
                pT = P.next_ps()
                P.TR(pT[:, 0:128], o[:, 0:128], [o], [pT])
                yo = P.sm()
                P.CP(yo[:, 0:128], pT[:, 0:128], [pT], [yo], eng=P.kb.act)
                P.DMA(P.yT[512 + pp * 128:512 + (pp + 1) * 128, tsl], yo[:, 0:128], [yo], [P.yT])

    def build(self):
        P = self
        P.setup()
        xcur = P.xin
        for li, l in enumerate(P.layers):
            j = l // 2
            last = li == len(P.layers) - 1
            xmid = P.xs[0]
            xnext = P.out if last else P.xs[1]
            if P.stop >= 1:
                P.phase_mod(l)
                if "fm" in P.taps:
                    P.DMA(P.outp("tap_fm", [128, 48, 2]), P.fm[:], [P.fm], [])
            if P.stop >= 2:
                P.phase_hT(xcur)
                if "hTd" in P.taps:
                    P.DMA(P.outp("tap_hT", [128, KC, T]), P.hT[:], [P.hT], [], q=P.kb.pool)
            if l % 2 == 0:
                if P.stop >= 3: P.hgrn2(l, j)
                if P.stop >= 4: P.attention(l, j)
                if P.stop >= 5: P.phase_out(l, P.w["ev_w_out"][j], xcur, xmid)
                if P.stop >= 6: P.phase_ffn(l, j, xmid, xnext)
            else:
                if "inject_y" in P.taps:
                    pass
                else:
                    if P.stop >= 3: P.s5(l, j)
                    if P.stop >= 4: P.rwkv(l, j)
                if P.stop >= 5: P.phase_out(l, P.w["od_w_out"][j], xcur, xmid, router_j=(j if "1" != "0" else None))
                if "gates" in P.taps:
                    P.DMA(P.outp("tap_gates", [128, NT, 8]), P.gates[:], [P.gates], [])
                if P.stop >= 6: P.phase_moe(l, j, xmid, xnext)
            xcur = xnext
        P.kb.emit()
        self.st.close()
        return self.nc


def host_inputs(inp, b):
    m = {}
    m["xin"] = np.ascontiguousarray(np.concatenate([inp["ctx"][b], inp["x"][b]], 0))
    ct = np.stack([inp["c"][b].reshape(8, 128).T, inp["c_ctx"].reshape(8, 128).T], -1)
    m["condT"] = np.ascontiguousarray(ct.astype(np.float32))
    for k, v in host_consts().items():
        m["c_" + k] = v
    m["ada_w"] = inp["ada_w"]
    m["ada_b_fm"] = np.ascontiguousarray(inp["ada_b"].reshape(4, 48, 128).transpose(0, 2, 1))
    m["ln_g"] = inp["ln_g"]; m["ln_b"] = inp["ln_b"]
    m["ev_w_in"] = inp["ev_w_in"]; m["ev_w_out"] = inp["ev_w_out"]
    m["hg_lb_fm"] = np.ascontiguousarray(inp["hg_lb"].reshape(2, 4, 128).transpose(2, 1, 0))
    m["hg_ng_fm"] = np.ascontiguousarray(inp["hg_norm_g"].reshape(2, 4, 128).transpose(0, 2, 1))
    m["attn_sink"] = inp["attn_sink"]
    def fm16(a):
        return np.ascontiguousarray(a.reshape(2, 2, 16, 2, 64).transpose(0, 1, 3, 4, 2).reshape(2, 2, 128, 16))
    m["s5_lre"] = fm16(inp["s5_lam_re"]); m["s5_lim"] = fm16(inp["s5_lam_im"])
    m["s5_ldt"] = fm16(np.ascontiguousarray(np.broadcast_to(inp["s5_log_dt"][..., None], (2, 2, 32, 64))))
    bb = np.stack([inp["s5_b_re"], inp["s5_b_im"]], 3)
    m["s5_bT"] = np.ascontiguousarray(bb.reshape(2, 16, 2, 64, 2, 16).transpose(0, 2, 3, 1, 4, 5).reshape(2, 128, 16, 2, 16))
    cc = np.stack([inp["s5_c_re"], inp["s5_c_im"]], 2).transpose(0, 1, 4, 2, 3)
    m["s5_cT"] = np.ascontiguousarray(cc.reshape(2, 16, 2, 64, 2, 16).transpose(0, 2, 3, 1, 4, 5).reshape(2, 128, 16, 2, 16))
    m["s5_d_fm"] = np.ascontiguousarray(inp["s5_d"].reshape(2, 4, 128).transpose(0, 2, 1))
    m["s5_glu_w"] = inp["s5_glu_w"]
    fm4 = lambda a: np.ascontiguousarray(a.reshape(a.shape[:-1] + (4, 128)).swapaxes(-1, -2))
    m["rw_mu_fm"] = np.ascontiguousarray(inp["rwkv_mu"].reshape(2, 15, 128).transpose(0, 2, 1))
    m["rw_w0_fm"] = fm4(inp["rwkv_w0"]); m["rw_a0_fm"] = fm4(inp["rwkv_a0"])
    def pad2(a):
        o = np.zeros((2, 2, 128, 512), np.float32)
        o[:, 0, 0:64] = a[:, 0]; o[:, 1, 64:128] = a[:, 1]
        return o
    m["rw_w2pad"] = pad2(inp["rwkv_w2"]); m["rw_a2pad"] = pad2(inp["rwkv_a2"]); m["rw_g2"] = inp["rwkv_g2"]
    m["rw_kk_fm"] = fm4(inp["rwkv_k_k"]); m["rw_ka_fm"] = fm4(inp["rwkv_k_a"]); m["rw_rk_fm"] = fm4(inp["rwkv_r_k"].reshape(2, 512))
    m["rw_lng"] = inp["rwkv_ln_g"]; m["rw_lnb"] = inp["rwkv_ln_b"]
    for k in ("ffn_w_gate", "ffn_w_up", "ffn_w_down", "od_w_in", "od_w_out", "moe_router_w", "moe_router_b", "moe_w_gate", "moe_w_up", "moe_w_down"):
        m[k] = inp[k]
    return m


FUSED = os.environ.get("MK_FUSED", "1") == "1"
N_CORES = 8


def _run(layers, inputs, xin_per_core):
    P = Prog(layers)
    nc = P.build()
    in_maps = []
    for b in range(N_CORES):
        m = host_inputs(inputs, b)
        m["xin"] = xin_per_core[b]
        in_maps.append({k: v for k, v in m.items() if k in P.din})
    res = run_bass_kernel_spmd(nc, in_maps, core_ids=list(range(N_CORES)))
    return [r["out"] for r in res.results]


def kernel(**inputs):
    inputs = {k: np.asarray(v) for k, v in inputs.items()}
    xs = [np.ascontiguousarray(np.concatenate([inputs["ctx"][b], inputs["x"][b]], 0)).astype(np.float32) for b in range(N_CORES)]
    if FUSED:
        xs = _run([0, 1, 2, 3], inputs, xs)
    else:
        for l in range(4):
            xs = _run([l], inputs, xs)
    return np.stack([x[LC:] for x in xs], 0).astype(np.float32)
```

```python
import contextlib, math, os
import numpy as np
import concourse.bass as bass
import concourse.mybir as mybir
from concourse.bass_utils import run_bass_kernel_spmd

F32 = mybir.dt.float32
BF16 = mybir.dt.bfloat16
F32R = mybir.dt.float32r
ALU = mybir.AluOpType
AF = mybir.ActivationFunctionType
AX = mybir.AxisListType

EPOCH = 30000
NDMA = 24


class Reg:
    __slots__ = ("w", "r", "name")

    def __init__(self, name=""):
        self.w = {}
        self.r = {}
        self.name = name


class Eng:
    def __init__(self, kb, name, self_sync):
        self.kb, self.name, self.self_sync = kb, name, self_sync
        self.ops = []
        self.seen = {}
        self.sems = [kb.new_sem(f"{name}_e0")]
        self.count = 0

    def cur(self):
        return self.sems[-1]


class KB:
    def __init__(self, nc, stack):
        self.nc, self.stack = nc, stack
        self.semh = {}
        self.nsem = 0
        self.pe = Eng(self, "pe", False)
        self.dve = Eng(self, "dve", True)
        self.act = Eng(self, "act", True)
        self.pool = Eng(self, "pool", True)
        self.sp = Eng(self, "sp", False)
        self.engs = [self.pe, self.dve, self.act, self.pool, self.sp]
        self.dma_sems = [self.new_sem(f"dma{i}") for i in range(NDMA)]
        self.dma_tot = [0] * NDMA
        self.dma_i = 0
        self.n_ops = 0

    def new_sem(self, name):
        h = self.stack.enter_context(self.nc.semaphore(name))
        k = self.nsem
        self.nsem += 1
        self.semh[k] = h
        return k

    def _waits(self, E, reads, writes):
        need = {}
        for r in reads:
            for s, v in r.w.items():
                if need.get(s, 0) < v:
                    need[s] = v
        for w in writes:
            for d in (w.w, w.r):
                for s, v in d.items():
                    if need.get(s, 0) < v:
                        need[s] = v
        out = []
        for s, v in need.items():
            if (not E.self_sync) and s in E.sems:
                continue
            if E.seen.get(s, 0) < v:
                E.seen[s] = v
                out.append((s, v))
        return out

    def _mark(self, ev, reads, writes):
        s, v = ev
        for r in reads:
            r.r[s] = v
        for w in writes:
            w.w = {s: v}
            w.r = {}

    def op(self, E, fn, reads=(), writes=()):
        waits = self._waits(E, reads, writes)
        if E.count >= EPOCH:
            E.sems.append(self.new_sem(f"{E.name}_e{len(E.sems)}"))
            E.count = 0
        E.count += 1
        ev = (E.cur(), E.count)
        E.ops.append((waits, fn, ev[0], 1))
        self._mark(ev, reads, writes)
        self.n_ops += 1
        return ev

    def dma(self, Q, out, in_, reads=(), writes=(), **kw):
        i = self.dma_i
        self.dma_i = (i + 1) % NDMA
        s = self.dma_sems[i]
        waits = self._waits(Q, reads, writes)
        if self.dma_tot[i] > 0 and Q.seen.get(s, 0) < self.dma_tot[i]:
            Q.seen[s] = self.dma_tot[i]
            waits.append((s, self.dma_tot[i]))
        self.dma_tot[i] += 16
        ev = (s, self.dma_tot[i])
        Q.ops.append((waits, lambda e: e.dma_start(out=out, in_=in_, **kw), s, 16))
        self._mark(ev, reads, writes)
        self.n_ops += 1
        return ev

    def emit(self):
        nc = self.nc
        fin = [(self.dma_sems[i], self.dma_tot[i]) for i in range(NDMA) if self.dma_tot[i] > 0]
        semh = self.semh

        def run(E, e):
            for waits, fn, s, inc in E.ops:
                for ws, wv in waits:
                    e.wait_ge(semh[ws], wv)
                inst = fn(e)
                inst.then_inc(semh[s], inc)

        with nc.Block() as block:
            @block.tensor
            def _(e):
                run(self.pe, e)

            @block.vector
            def _(e):
                run(self.dve, e)

            @block.scalar
            def _(e):
                run(self.act, e)

            @block.gpsimd
            def _(e):
                run(self.pool, e)

            @block.sync
            def _(e):
                run(self.sp, e)
                for ws, wv in fin:
                    e.wait_ge(semh[ws], wv)


class _LazyW:
    def __init__(self, prog, shapes):
        self.p, self.shapes, self.c = prog, shapes, {}

    def __getitem__(self, k):
        if k not in self.c:
            self.c[k] = self.p.inp(k, self.shapes[k])
        return self.c[k]


T = 2304; LC = 256; NT = 18; D = 1024; KC = 8; CH = 32; NCH = 72; NG = 10
NTILES = [(0, 256), (256, 512), (768, 512), (1280, 512), (1792, 512)]
ALPHA = 8 ** 0.25
DFF = 2816; NFC = 22


def host_consts():
    c = {}
    c["ident"] = np.eye(128, dtype=np.float32)
    s = np.arange(128)[:, None]; t = np.arange(128)[None, :]
    same = (s // CH) == (t // CH)
    c["tri_le"] = (same & (s <= t)).astype(np.float32)
    c["tri_ge"] = (same & (s >= t)).astype(np.float32)
    tf = np.arange(T, dtype=np.float32)
    tb = np.concatenate([255.0 - np.arange(256), 256.0 + (2303.0 - np.arange(256, T))]).astype(np.float32)
    c["tauF"] = np.ascontiguousarray(np.broadcast_to(tf, (128, T))); c["tauB"] = np.ascontiguousarray(np.broadcast_to(tb, (128, T)))
    a_ = np.arange(128)[:, None]; b_ = np.arange(128)[None, :]
    mf = np.stack([(a_ < b_), (a_ <= b_), (b_ < a_)], 1).astype(np.float32)
    mb = np.stack([(a_ > b_), (a_ >= b_), (b_ > a_)], 1).astype(np.float32)
    c["rw_masks"] = np.ascontiguousarray(np.stack([mf, mb], 0))
    c["bdones"] = ((a_ // 64) == (b_ // 64)).astype(np.float32)
    c["halfm"] = (np.arange(128)[:, None] // 64 == np.arange(2)[None, :]).astype(np.float32)
    c["rowmask"] = (np.arange(128)[:, None] // CH == np.arange(4)[None, :]).astype(np.float32)
    kk = np.arange(128)[:, None]; qq = np.arange(128)[None, :]
    c["mprev4"] = (kk >= qq).astype(np.float32)
    c["mnext4"] = (kk <= qq).astype(np.float32)
    rm = np.ones((128, T), np.float32); rm[:, ::CH] = 0.0
    c["resetm"] = rm
    rows = 2048 // 64
    row = np.repeat(np.arange(rows, dtype=np.float32), 64); col = np.tile(np.arange(64, dtype=np.float32), rows)
    inv = (np.float32(10000.0) ** (-np.arange(16, dtype=np.float32) / np.float32(16))).astype(np.float32)
    ang = np.concatenate([row[:, None] * inv, col[:, None] * inv], axis=-1).astype(np.float32)
    c["cosF"] = np.ascontiguousarray(np.concatenate([np.cos(ang), np.cos(ang)], -1).T.astype(np.float32))
    c["sinF"] = np.ascontiguousarray(np.concatenate([np.sin(ang), np.sin(ang)], -1).T.astype(np.float32))
    pm = np.zeros((64, 64), np.float32)
    for r in range(32):
        pm[r + 32, r] = -1.0
        pm[r, r + 32] = 1.0
    pm2 = np.zeros((128, 128), np.float32); pm2[:64, :64] = pm; pm2[64:, 64:] = pm
    c["Pm2"] = pm2
    c["cosF"] = np.ascontiguousarray(np.concatenate([c["cosF"], c["cosF"]], 0))
    c["sinF"] = np.ascontiguousarray(np.concatenate([c["sinF"], c["sinF"]], 0))
    return c


class Buf:
    def __init__(self, t, name):
        self.t = t; self.r = Reg(name)

    def __getitem__(self, i):
        return self.t[i]


class Prog:
    def __init__(self, layers, taps=(), stop=99):
        self.layers = layers; self.taps = set(taps); self.stop = stop
        self.nc = bass.Bass("TRN2", target_bir_lowering=False)
        self.st = contextlib.ExitStack()
        self.kb = KB(self.nc, self.st)
        self.din = {}; self.dout = {}
        self.psi = 0

    def inp(self, name, shape, dt=F32):
        a = self.nc.dram_tensor(name, list(shape), dt, kind="ExternalInput").ap()
        self.din[name] = a
        return a

    def outp(self, name, shape, dt=F32):
        a = self.nc.dram_tensor(name, list(shape), dt, kind="ExternalOutput").ap()
        self.dout[name] = a
        return a

    def scratch(self, name, shape, dt=F32):
        if name in self.taps:
            return Buf(self.outp(name, shape, dt), name)
        return Buf(self.nc.dram_tensor(name, list(shape), dt, kind="Internal").ap(), name)

    def sb(self, name, shape, dt=F32):
        return Buf(self.st.enter_context(self.nc.sbuf_tensor(name, list(shape), dt)), name)

    def next_ps(self):
        b = self.ps[self.psi]; self.psi = (self.psi + 1) % 8
        return b

    def _rw(self, R, W):
        return [b.r for b in R], [b.r for b in W]

    def MM(self, out, lhsT, rhs, R, W, start=True, stop=True):
        r, w = self._rw(R, W)
        self.kb.op(self.kb.pe, lambda e: e.matmul(out, lhsT=lhsT, rhs=rhs, start=start, stop=stop), r, w)

    def TR(self, out, in_, R, W, n=128):
        r, w = self._rw(R + [self.ident], W)
        idn = self.ident[0:n, 0:n]
        self.kb.op(self.kb.pe, lambda e: e.transpose(out=out, in_=in_, identity=idn), r, w)

    def ACT(self, out, in_, func, R, W, bias=None, scale=None):
        r, w = self._rw(R, W)
        kw = {}
        if bias is not None: kw["bias"] = bias
        if scale is not None: kw["scale"] = scale
        self.kb.op(self.kb.act, lambda e: e.activation(out=out, in_=in_, func=func, **kw), r, w)

    def TT(self, out, a, b, op, R, W, eng=None):
        r, w = self._rw(R, W)
        self.kb.op(eng or self.kb.dve, lambda e: e.tensor_tensor(out=out, in0=a, in1=b, op=op), r, w)

    def TS(self, out, a, s1, op0, R, W, s2=None, op1=None, eng=None):
        r, w = self._rw(R, W)
        if op1 is None:
            self.kb.op(eng or self.kb.dve, lambda e: e.tensor_scalar(out=out, in0=a, scalar1=s1, scalar2=None, op0=op0), r, w)
        else:
            self.kb.op(eng or self.kb.dve, lambda e: e.tensor_scalar(out=out, in0=a, scalar1=s1, scalar2=s2, op0=op0, op1=op1), r, w)

    def STT(self, out, a, s, b, op0, op1, R, W):
        r, w = self._rw(R, W)
        self.kb.op(self.kb.dve, lambda e: e.scalar_tensor_tensor(out=out, in0=a, scalar=s, in1=b, op0=op0, op1=op1), r, w)

    def CP(self, out, in_, R, W, eng=None):
        r, w = self._rw(R, W)
        E = eng or self.kb.dve
        if E is self.kb.act:
            self.kb.op(E, lambda e: e.copy(out=out, in_=in_), r, w)
        else:
            self.kb.op(E, lambda e: e.tensor_copy(out=out, in_=in_), r, w)

    def MSET(self, ap, val, W, eng=None):
        r, w = self._rw([], W)
        self.kb.op(eng or self.kb.pool, lambda e: e.memset(ap, val), r, w)

    def RECIP(self, out, in_, R, W):
        r, w = self._rw(R, W)
        self.kb.op(self.kb.dve, lambda e: e.reciprocal(out=out, in_=in_), r, w)

    def SCAN(self, out, d0, d1, R, W, init=0.0):
        r, w = self._rw(R, W)
        self.kb.op(self.kb.dve, lambda e: e.tensor_tensor_scan(out=out, data0=d0, data1=d1, initial=init, op0=ALU.mult, op1=ALU.add), r, w)

    def DMA(self, out, in_, R, W, q=None, **kw):
        r, w = self._rw(R, W)
        self.kb.dma(q or self.kb.sp, out, in_, r, w, **kw)

    def tap(self, name, src_ap, R, shape):
        if name in self.taps:
            o = self.outp("tap_" + name, shape)
            self.DMA(o, src_ap, R, [])

    def setup(self):
        P = self
        nc = self.nc
        P.xin = Buf(P.inp("xin", [T, D]), "xin")
        P.condT = P.inp("condT", [128, 8, 2])
        hc = host_consts()
        P.cd = {k: Buf(P.inp("c_" + k, v.shape), "c_" + k) for k, v in hc.items()}
        I = lambda name, shape: (name, shape)
        wdecl = dict(
            ada_w=I("ada_w", [4, D, 6 * D]), ada_b_fm=I("ada_b_fm", [4, 128, 48]),
            ln_g=I("ln_g", [4, 2, D]), ln_b=I("ln_b", [4, 2, D]),
            ev_w_in=I("ev_w_in", [2, D, 3328]), ev_w_out=I("ev_w_out", [2, D, D]),
            hg_lb_fm=I("hg_lb_fm", [128, 4, 2]), hg_ng_fm=I("hg_ng_fm", [2, 128, 4]), attn_sink=I("attn_sink", [2, 8]),
            od_w_in=I("od_w_in", [2, D, 2432]), od_w_out=I("od_w_out", [2, D, D]),
            s5_lre=I("s5_lre", [2, 2, 128, 16]), s5_lim=I("s5_lim", [2, 2, 128, 16]), s5_ldt=I("s5_ldt", [2, 2, 128, 16]),
            s5_bT=I("s5_bT", [2, 128, 16, 2, 16]), s5_cT=I("s5_cT", [2, 128, 16, 2, 16]), s5_d_fm=I("s5_d_fm", [2, 128, 4]),
            s5_glu_w=I("s5_glu_w", [2, 512, 512]),
            rw_mu_fm=I("rw_mu_fm", [2, 128, 15]), rw_w0_fm=I("rw_w0_fm", [2, 2, 128, 4]), rw_a0_fm=I("rw_a0_fm", [2, 2, 128, 4]),
            rw_w2pad=I("rw_w2pad", [2, 2, 128, 512]), rw_a2pad=I("rw_a2pad", [2, 2, 128, 512]), rw_g2=I("rw_g2", [2, 128, 512]),
            rw_kk_fm=I("rw_kk_fm", [2, 128, 4]), rw_ka_fm=I("rw_ka_fm", [2, 128, 4]), rw_rk_fm=I("rw_rk_fm", [2, 128, 4]),
            rw_lng=I("rw_lng", [2, 512]), rw_lnb=I("rw_lnb", [2, 512]),
            moe_router_w=I("moe_router_w", [2, D, 8]), moe_router_b=I("moe_router_b", [2, 8]),
            moe_w_gate=I("moe_w_gate", [2, 8, D, DFF]), moe_w_up=I("moe_w_up", [2, 8, D, DFF]), moe_w_down=I("moe_w_down", [2, 8, DFF, D]),
            ffn_w_gate=I("ffn_w_gate", [2, D, DFF]), ffn_w_up=I("ffn_w_up", [2, D, DFF]), ffn_w_down=I("ffn_w_down", [2, DFF, D]),
        )
        P.w = _LazyW(P, {k: v[1] for k, v in wdecl.items()})
        P.out = Buf(P.outp("out", [T, D]), "out")
        P.xs = [P.scratch("xs0", [T, D]), P.scratch("xs1", [T, D])]
        if "inject_y" in P.taps:
            P.yT = Buf(P.inp("yT_inject", [D, T]), "yT_inject")
        else:
            P.yT = P.scratch("yTd", [D, T])
        P.ident = P.sb("ident", [128, 128]); P.ones = P.sb("ones", [128, 128]); P.onesdiv = P.sb("onesdiv", [128, 128])
        P.tri_le = P.sb("tri_le", [128, 128]); P.tri_ge = P.sb("tri_ge", [128, 128])
        P.mprev4 = P.sb("mprev4", [128, 128]); P.mnext4 = P.sb("mnext4", [128, 128])
        P.cst = P.sb("cst", [128, 8])
        P.hT = P.sb("hT", [128, KC, T], BF16)
        P.G = [None] * NG
        P.Gt = self.st.enter_context(nc.sbuf_tensor("G", [128, NG, T], F32))
        for i in range(NG):
            P.G[i] = Buf(P.Gt[:, i, :], f"G{i}")
        P.wA = [P.sb(f"wA{i}", [128, KC, 128], BF16) for i in range(5)]
        P.wAi = 0
        P.xb = [P.sb(f"xb{i}", [128, D]) for i in range(3)]
        P.xbi = 0
        P.zb = [P.sb("zb0", [128, D])] * 2
        P.bc = [P.sb(f"bc{i}", [128, D]) for i in range(4)]
        P.condS = P.sb("condS", [128, 8, 2]); P.adab = P.sb("adab", [128, 48])
        P.fm = P.sb("fm", [128, 48, 2]); P.sc1p = P.sb("sc1p", [128, 8, 2]); P.sc2p = P.sb("sc2p", [128, 8, 2])
        P.small = [P.sb(f"small{i}", [128, 512]) for i in range(5)]
        P.smi = 0
        P.S = P.sb("S", [128, 128]); P.tmpS = P.sb("tmpS", [128, 128])
        P.stat = P.sb("stat", [128, 32])
        P.s5t = P.sb("s5t", [128, 2, 12, 16]); P.s5i = P.sb("s5i", [128, 16], mybir.dt.int32); P.s5d = P.sb("s5d", [128, 4])
        P.s5bc = P.sb("s5bc", [128, 64]); P.s5zc = P.sb("s5zc", [128, 256])
        P.rwm = P.sb("rwm", [128, 3, 128]); P.bdones = P.sb("bdones", [128, 128])
        P.rwar = [P.sb(f"rwar{h}", [128, 256]) for h in range(2)]
        P.rwxm = [P.sb(f"rwxm{h}", [128, 4, 128]) for h in range(2)]
        P.rwxz = [P.sb(f"rwxz{h}", [128, 3, 128]) for h in range(2)]
        P.rwu = P.sb("rwu", [128, 128]); P.rwp = P.sb("rwp", [128, 48]); P.rwgc = P.sb("rwgc", [128, NT]); P.rwln = P.sb("rwln", [128, 2, 128])
        P.gates = P.sb("gates", [128, NT, 8]); P.rw = P.sb("rw", [128, KC, 8]); P.rb = P.sb("rb", [128, 8])
        P.h32 = [P.sb("h32_0", [128, 4, 128])] * 2; P.rt = P.sb("rt", [128, 64])
        P.lbt = P.sb("lbt", [128, 16]); P.ngt = P.sb("ngt", [128, 4]); P.sk = P.sb("sk", [128, 8])
        P.Pm2 = P.sb("Pm2", [128, 128])
        P.attm = [P.sb(f"attm{i}", [128, 128]) for i in range(2)]; P.attmi = 0
        P.ktok = [P.sb(f"ktok{i}", [128, 4, 128]) for i in range(2)]
        P.rowmask = P.sb("rowmask", [128, 4]); P.halfm = P.sb("halfm", [128, 2])
        P.ps = [Buf(self.st.enter_context(nc.psum_tensor(f"ps{i}", [128, 512], F32)), f"ps{i}") for i in range(8)]
        for k, dst in (("ident", P.ident), ("tri_le", P.tri_le), ("tri_ge", P.tri_ge), ("mprev4", P.mprev4),
                       ("mnext4", P.mnext4), ("Pm2", P.Pm2), ("rowmask", P.rowmask), ("halfm", P.halfm), ("bdones", P.bdones)):
            P.DMA(dst[:], P.cd[k][:], [], [dst])
        P.MSET(P.ones[:], 1.0, [P.ones]); P.MSET(P.onesdiv[:], 1.0 / 128.0, [P.onesdiv])
        P.MSET(P.cst[:, 0:1], 1e-6, [P.cst]); P.MSET(P.cst[:, 1:2], 1e-5, [P.cst]); P.MSET(P.cst[:, 2:3], 64e-5, [P.cst])
        P.MSET(P.cst[:, 3:4], 0.0, [P.cst]); P.MSET(P.cst[:, 4:5], 1.0, [P.cst])
        P.DMA(P.condS[:], P.condT, [], [P.condS])
        P.ACT(P.condS[:], P.condS[:], AF.Silu, [P.condS], [P.condS])

    def gflat(self, slot0, nelem, dt=F32):
        flat = self.Gt[:].rearrange("p a n -> p (a n)")[:, slot0 * T:slot0 * T + nelem]
        ns = (nelem + T - 1) // T
        regs = [self.G[slot0 + i] for i in range(ns)]
        if dt is not F32:
            flat = flat.bitcast(dt)
        return flat, regs

    def sm(self):
        b = self.small[self.smi]; self.smi = (self.smi + 1) % len(self.small)
        return b

    def loadw(self, src, ncols, kc=KC):
        b = self.wA[self.wAi]; self.wAi = (self.wAi + 1) % len(self.wA)
        self.DMA(b[:, 0:kc, 0:ncols], src.rearrange("(c p) n -> p c n", p=128), [], [b], q=self.kb.pool)
        return b

    def phase_mod(self, l):
        P = self
        P.DMA(P.adab[:], P.w["ada_b_fm"][l], [], [P.adab])
        pM = P.next_ps()
        for blk in range(12):
            fl, regs = P.gflat((blk % 2) * 2, 4096)
            stg = fl.rearrange("p (c n) -> p c n", n=512)
            for ch in range(8):
                P.DMA(stg[:, ch, :], P.w["ada_w"][l, ch * 128:(ch + 1) * 128, blk * 512:(blk + 1) * 512], [], regs,
                      q=(P.kb.sp if ch % 2 == 0 else P.kb.act))
            for s in range(4):
                k = blk * 4 + s
                for ch in range(8):
                    P.MM(pM[:, 2 * k:2 * k + 2], stg[:, ch, s * 128:(s + 1) * 128], P.condS[:, ch, :], regs + [P.condS], [pM],
                         start=(ch == 0), stop=(ch == 7))
        for cond in range(2):
            P.TT(P.fm[:, :, cond], pM[:, cond:96:2], P.adab[:], ALU.add, [pM, P.adab], [P.fm])
        P.TS(P.sc1p[:], P.fm[:, 8:16, :], 1.0, ALU.add, [P.fm], [P.sc1p])
        P.TS(P.sc2p[:], P.fm[:, 32:40, :], 1.0, ALU.add, [P.fm], [P.sc2p])

    def gate_bcast(self, q, cond, dst):
        P = self
        for half in range(2):
            pg = P.next_ps()
            for cc in range(4):
                c = half * 4 + cc
                dg = P.sm()
                P.TS(dg[:, 0:128], P.ident[:], P.fm[:, q * 8 + c, cond:cond + 1], ALU.mult, [P.ident, P.fm], [dg])
                P.MM(pg[:, cc * 128:(cc + 1) * 128], P.ones[:], dg[:, 0:128], [P.ones, dg], [pg])
            P.CP(dst[:, half * 512:(half + 1) * 512], pg[:, :], [pg], [dst], eng=P.kb.act)

    def hT_tile(self, xt, ti, scp, shq, router=False):
        P = self
        cond = 1 if ti < 2 else 0
        pl = P.next_ps() if router else None
        for half in range(2):
            pt = P.next_ps()
            for cc in range(4):
                c = half * 4 + cc
                P.TR(pt[:, cc * 128:(cc + 1) * 128], xt[:, c * 128:(c + 1) * 128], [xt], [pt])
            h32 = P.h32[half]
            for cc in range(4):
                c = half * 4 + cc
                if router:
                    P.TS(h32[:, cc, :], pt[:, cc * 128:(cc + 1) * 128], scp[:, c, cond:cond + 1], ALU.mult, [pt, scp, P.fm], [h32],
                         s2=P.fm[:, shq * 8 + c, cond:cond + 1], op1=ALU.add)
                    P.CP(P.hT[:, c, ti * 128:(ti + 1) * 128], h32[:, cc, :], [h32], [P.hT], eng=P.kb.act)
                else:
                    P.ACT(P.hT[:, c, ti * 128:(ti + 1) * 128], pt[:, cc * 128:(cc + 1) * 128], AF.Identity,
                          [pt, scp, P.fm], [P.hT], scale=scp[:, c, cond:cond + 1], bias=P.fm[:, shq * 8 + c, cond:cond + 1])
            if router:
                for cc in range(4):
                    c = half * 4 + cc
                    P.MM(pl[:, half * 8:half * 8 + 8], h32[:, cc, :], P.rw[:, c, :], [h32, P.rw], [pl], start=(cc == 0), stop=(cc == 3))
        if router and "1" != "2":
            P.top2(pl, ti)

    def top2(self, pl, ti):
        P = self
        rt = P.rt
        R_, W_ = [rt], [rt]
        lg, e1, l2, e2 = rt[:, 0:8], rt[:, 8:16], rt[:, 16:24], rt[:, 24:32]
        m1, m2, dd, p1, p2 = rt[:, 32:33], rt[:, 33:34], rt[:, 34:35], rt[:, 35:36], rt[:, 36:37]
        P.TT(lg, pl[:, 0:8], P.rb[:], ALU.add, [pl, P.rb], W_)
        P.TT(lg, lg, pl[:, 8:16], ALU.add, [pl, rt], W_)
        P.kb.op(P.kb.dve, lambda e: e.reduce_max(out=m1, in_=lg, axis=AX.X), [rt.r], [rt.r])
        P.TS(e1, lg, m1, ALU.is_equal, R_, W_)
        P.STT(l2, e1, -1e30, lg, ALU.mult, ALU.add, R_, W_)
        P.kb.op(P.kb.dve, lambda e: e.reduce_max(out=m2, in_=l2, axis=AX.X), [rt.r], [rt.r])
        P.TS(e2, l2, m2, ALU.is_equal, R_, W_)
        P.TT(dd, m2, m1, ALU.subtract, R_, W_)
        P.ACT(dd, dd, AF.Exp, R_, W_)
        P.TS(p1, dd, 1.0, ALU.add, R_, W_)
        P.RECIP(p1, p1, R_, W_)
        P.TT(p2, dd, p1, ALU.mult, R_, W_)
        P.TS(e1, e1, p1, ALU.mult, R_, W_)
        P.STT(P.gates[:, ti, :], e2, p2, e1, ALU.mult, ALU.add, R_, [P.gates])

    def phase_hT(self, xsrc):
        P = self
        for ti in range(NT):
            xt = P.xb[P.xbi]; P.xbi = (P.xbi + 1) % 3
            P.DMA(xt[:], xsrc[ti * 128:(ti + 1) * 128, :], [xsrc], [xt])
            P.hT_tile(xt, ti, P.sc1p, 0)

    def proj_fm(self, wb, M, evac, col0=0):
        P = self
        for (t0, n) in NTILES:
            pb = P.next_ps()
            for c in range(KC):
                P.MM(pb[0:M, 0:n], wb[:, c, col0:col0 + M], P.hT[:, c, t0:t0 + n], [wb, P.hT], [pb], start=(c == 0), stop=(c == 7))
            evac(pb, t0, n)

    def hgrn2(self, l, j):
        P = self
        G = P.G
        W = P.w["ev_w_in"][j]
        lbt = P.lbt; ngt = P.ngt
        P.DMA(ngt[:, 0:4], P.w["hg_ng_fm"][j], [], [ngt])
        if j == 0:
            P.MSET(lbt[:, 0:4], 0.0, [lbt]); P.MSET(lbt[:, 4:8], 1.0, [lbt])
        else:
            P.DMA(lbt[:, 8:16], P.w["hg_lb_fm"].rearrange("p h j -> p (h j)"), [], [lbt])
            P.TT(lbt[:, 0:4], lbt[:, 9:16:2], lbt[:, 8:16:2], ALU.subtract, [lbt], [lbt])
            P.ACT(lbt[:, 0:4], lbt[:, 0:4], AF.Sigmoid, [lbt], [lbt])
            P.TS(lbt[:, 4:8], lbt[:, 0:4], -1.0, ALU.mult, [lbt], [lbt], s2=1.0, op1=ALU.add)
        qT, sgT, X1, X2, X3, X4, oacc = G[0], G[1], G[2], G[3], G[4], G[5], G[8]
        P.resetm = G[9]
        P.DMA(P.resetm[:], P.cd["resetm"][:], [], [P.resetm])
        itok = P.Gt[:, 6, :].rearrange("p (b c) -> p b c", c=128)
        itr = [G[6]]
        for hd in range(4):
            cs = lambda k: W[:, k * 512 + hd * 128:k * 512 + (hd + 1) * 128]
            wq = P.loadw(cs(0), 128)
            P.proj_fm(wq, 128, lambda pb, t0, n: P.ACT(qT[:, t0:t0 + n], pb[:, 0:n], AF.Silu, [pb], [qT]))
            wg = P.loadw(cs(4), 128)
            P.proj_fm(wg, 128, lambda pb, t0, n: P.ACT(sgT[:, t0:t0 + n], pb[:, 0:n], AF.Silu, [pb], [sgT]))
            wi = P.loadw(cs(3), 128)
            for b0 in range(0, NT, 4):
                nb = min(4, NT - b0)
                pi = P.next_ps()
                for q in range(nb):
                    ti = b0 + q
                    for c in range(KC):
                        P.MM(pi[:, q * 128:(q + 1) * 128], P.hT[:, c, ti * 128:(ti + 1) * 128], wi[:, c, :], [P.hT, wi], [pi],
                             start=(c == 0), stop=(c == 7))
                P.CP(itok[:, b0:b0 + nb, :], pi[:, 0:nb * 128].rearrange("p (a b) -> p a b", b=128), [pi], itr, eng=P.kb.act)
            for d in range(2):
                fwd = d == 0
                wf = P.loadw(cs(1 + d), 128)
                P.proj_fm(wf, 128, lambda pb, t0, n: P.ACT(X1[:, t0:t0 + n], pb[:, 0:n], AF.Sigmoid, [pb], [X1]))
                P.TS(X1[:], X1[:], lbt[:, 4 + hd:5 + hd], ALU.mult, [X1, lbt], [X1], s2=lbt[:, hd:hd + 1], op1=ALU.add)
                P.TS(X2[:], X1[:], -1.0, ALU.mult, [X1], [X2], s2=1.0, op1=ALU.add)
                P.ACT(X1[:], X1[:], AF.Ln, [X1], [X1])
                if fwd:
                    P.SCAN(X3[:], P.resetm[:], X1[:], [P.resetm, X1], [X3])
                else:
                    P.SCAN(X3[:, ::-1], P.resetm[:], X1[:, ::-1], [P.resetm, X1], [X3])
                P.ACT(X1[:], X3[:], AF.Exp, [X3], [X1])
                P.STT(X4[:], qT[:], 128.0 ** -0.5, X1[:], ALU.mult, ALU.mult, [qT, X1], [X4])
                P.ACT(X3[:], X3[:], AF.Exp, [X3], [X3], scale=-1.0)
                P.TT(X2[:], X2[:], X3[:], ALU.mult, [X2, X3], [X2])
                ring = [P.S, P.rwu, P.rwar[0], P.rwar[1], P.s5zc]
                rpos = [0]
                P.MSET(ring[0][:, 0:128], 0.0, [ring[0]])
                tiles = list(range(NT)) if fwd else [1, 0] + list(range(NT - 1, 1, -1))
                mask = P.tri_le if fwd else P.tri_ge
                for ti in tiles:
                    tsl = slice(ti * 128, (ti + 1) * 128)
                    pA = P.next_ps()
                    P.MM(pA[:, 0:128], X2[:, tsl], X4[:, tsl], [X2, X4], [pA])
                    attm = P.attm[P.attmi]; ktok = P.ktok[P.attmi]; P.attmi ^= 1
                    P.TT(attm[:], pA[:, 0:128], mask[:], ALU.mult, [pA, mask], [attm])
                    pB = P.next_ps()
                    P.TR(pB[:, 0:128], X2[:, tsl], [X2], [pB])
                    for q in range(4):
                        P.ACT(ktok[:, q, :], pB[:, 0:128], AF.Identity, [pB, P.rowmask], [ktok], scale=P.rowmask[:, q:q + 1])
                    pC = P.next_ps()
                    P.MM(pC[:, 0:128], itok[:, ti, :], attm[:], itr + [attm], [pC], start=True, stop=False)
                    qorder = list(range(4) if fwd else range(3, -1, -1))
                    pDs = []
                    for q in qorder:
                        pD = P.next_ps()
                        P.MM(pD[:, 0:128], ktok[:, q, :], itok[:, ti, :], [ktok] + itr, [pD])
                        pDs.append(pD)
                    kvb = P.rwxm[P.attmi]
                    lasts = []
                    for n_, q in enumerate(qorder):
                        c0 = ti * 128 + q * CH
                        last = c0 + CH - 1 if fwd else c0
                        lasts.append(last)
                        P.ACT(kvb[:, n_, :], pDs[n_][:, 0:128], AF.Identity, [pDs[n_], X1], [kvb], scale=X1[:, last:last + 1])
                    Ss = []
                    for n_, q in enumerate(qorder):
                        Sin = ring[rpos[0] % len(ring)]; Sout = ring[(rpos[0] + 1) % len(ring)]
                        Ss.append(Sin)
                        P.STT(Sout[:, 0:128], Sin[:, 0:128], X1[:, lasts[n_]:lasts[n_] + 1], kvb[:, n_, :], ALU.mult, ALU.add, [Sin, X1, kvb], [Sout])
                        rpos[0] += 1
                    for n_, q in enumerate(qorder):
                        c0 = ti * 128 + q * CH
                        P.MM(pC[:, q * CH:(q + 1) * CH], Ss[n_][:, 0:128], X4[:, c0:c0 + CH], [Ss[n_], X4], [pC], start=False, stop=True)
                    if fwd:
                        P.CP(oacc[:, tsl], pC[:, 0:128], [pC], [oacc], eng=P.kb.act)
                    else:
                        P.TT(oacc[:, tsl], oacc[:, tsl], pC[:, 0:128], ALU.add, [oacc, pC], [oacc])
            for (t0, n) in NTILES:
                sq = P.sm()
                P.ACT(sq[:, 0:n], oacc[:, t0:t0 + n], AF.Square, [oacc], [sq])
                pE = P.next_ps()
                P.MM(pE[:, 0:n], P.onesdiv[:], sq[:, 0:n], [P.onesdiv, sq], [pE])
                rs = P.sm()
                P.ACT(rs[:, 0:n], pE[:, 0:n], AF.Sqrt, [pE, P.cst], [rs], bias=P.cst[:, 0:1])
                P.RECIP(rs[:, 0:n], rs[:, 0:n], [rs], [rs])
                yo = P.sm()
                P.STT(yo[:, 0:n], oacc[:, t0:t0 + n], ngt[:, hd:hd + 1], rs[:, 0:n], ALU.mult, ALU.mult, [oacc, ngt, rs], [yo])
                P.TT(yo[:, 0:n], yo[:, 0:n], sgT[:, t0:t0 + n], ALU.mult, [yo, sgT], [yo])
                P.DMA(P.yT[hd * 128:(hd + 1) * 128, t0:t0 + n], yo[:, 0:n], [yo], [P.yT])

    def attention(self, l, j):
        P = self
        G = P.G
        W = P.w["ev_w_in"][j]
        sk = P.sk
        P.DMA(sk[:, 0:8], P.w["attn_sink"][j].partition_broadcast(128), [], [sk])
        P.ACT(sk[:, 0:8], sk[:, 0:8], AF.Exp, [sk], [sk])
        cosT, sinT, kT2, q2 = G[0], G[1], G[2], G[3]
        qm = [G[4], G[5], G[6], G[7]]
        ptb = [(P.Gt[:, 8, i * 512:(i + 1) * 512], G[8]) for i in range(4)] + [(P.Gt[:, 9, 0:512], G[9])]
        vaug = P.Gt[:, 9, 512:512 + NT * 65].rearrange("p (t e) -> p t e", e=65)
        vreg = [G[9]]
        P.DMA(cosT[:, 0:2048], P.cd["cosF"][:], [], [cosT]); P.DMA(sinT[:, 0:2048], P.cd["sinF"][:], [], [sinT])

        def rope(src):
            for (t0, n) in NTILES[1:]:
                pr = P.next_ps()
                P.MM(pr[:, 0:n], P.Pm2[:], src[:, t0:t0 + n], [P.Pm2, src], [pr])
                t1 = P.sm(); t2 = P.sm()
                P.TT(t1[:, 0:n], src[:, t0:t0 + n], cosT[:, t0 - LC:t0 - LC + n], ALU.mult, [src, cosT], [t1])
                P.TT(t2[:, 0:n], pr[:, 0:n], sinT[:, t0 - LC:t0 - LC + n], ALU.mult, [pr, sinT], [t2])
                P.TT(src[:, t0:t0 + n], t1[:, 0:n], t2[:, 0:n], ALU.add, [t1, t2], [src])

        for g in range(2):
            wv = P.loadw(W[:, 3200 + g * 64:3200 + (g + 1) * 64], 64)
            P.MSET(vaug[:, :, 64:65], 1.0, vreg)
            for ti in range(NT):
                pv = P.next_ps()
                for c in range(KC):
                    P.MM(pv[:, 0:64], P.hT[:, c, ti * 128:(ti + 1) * 128], wv[:, c, 0:64], [P.hT, wv], [pv], start=(c == 0), stop=(c == 7))
                P.CP(vaug[:, ti, 0:64], pv[:, 0:64], [pv], vreg, eng=P.kb.act)
            wk = P.wA[P.wAi]; P.wAi = (P.wAi + 1) % len(P.wA)
            ksrc = W[:, 3072 + g * 64:3072 + (g + 1) * 64].rearrange("(c p) n -> p c n", p=128)
            P.DMA(wk[:, :, 0:64], ksrc, [], [wk], q=P.kb.pool)
            P.DMA(wk[:, :, 64:128], ksrc, [], [wk], q=P.kb.pool)
            P.proj_fm(wk, 128, lambda pb, t0, n: P.CP(kT2[:, t0:t0 + n], pb[:, 0:n], [pb], [kT2], eng=P.kb.act))
            rope(kT2)
            for pair in range(2):
                h0 = g * 4 + pair * 2
                wq = P.loadw(W[:, 2560 + h0 * 64:2560 + (h0 + 2) * 64], 128)
                P.proj_fm(wq, 128, lambda pb, t0, n: P.CP(q2[:, t0:t0 + n], pb[:, 0:n], [pb], [q2], eng=P.kb.act))
                rope(q2)
                for half in range(2):
                    dst = qm[pair * 2 + half]
                    P.TS(dst[:], q2[:], P.halfm[:, half:half + 1], ALU.mult, [q2, P.halfm], [dst])
            for ti in range(NT):
                if ti < 2:
                    keys = [(0, 'c'), (1, 'c')]
                else:
                    keys = [(kt, kd) for kt, kd in ((ti - 1, 'p'), (ti, 's'), (ti + 1, 'n')) if 2 <= kt < NT] + [(0, 'c'), (1, 'c')]
                pts = []
                for ki, (kt, kd) in enumerate(keys):
                    pS = P.next_ps()
                    for hh in range(4):
                        P.MM(pS[:, hh * 128:(hh + 1) * 128], kT2[:, kt * 128:(kt + 1) * 128], qm[hh][:, ti * 128:(ti + 1) * 128],
                             [kT2, qm[hh]], [pS])
                    pt, pr_ = ptb[ki]
                    P.ACT(pt, pS[:, 0:512], AF.Exp, [pS], [pr_], scale=0.125)
                    if kd in ('p', 'n'):
                        mk_ = P.mprev4 if kd == 'p' else P.mnext4
                        for hh in range(4):
                            P.TT(pt[:, hh * 128:(hh + 1) * 128], pt[:, hh * 128:(hh + 1) * 128], mk_[:], ALU.mult, [pr_, mk_], [pr_],
                                 eng=(P.kb.pool if hh % 2 else P.kb.dve))
                    pts.append((pt, pr_, kt))
                pO = P.next_ps()
                for hh in range(4):
                    for ki, (pt, pr_, kt) in enumerate(pts):
                        P.MM(pO[:, hh * 65:(hh + 1) * 65], pt[:, hh * 128:(hh + 1) * 128], vaug[:, kt, :], [pr_] + vreg, [pO],
                             start=(ki == 0), stop=(ki == len(pts) - 1))
                den = P.sm()
                P.TT(den[:, 0:4], pO[:, 64:260:65], sk[:, g * 4:(g + 1) * 4], ALU.add, [pO, sk], [den])
                P.RECIP(den[:, 0:4], den[:, 0:4], [den], [den])
                ob = P.sm()
                for hh in range(4):
                    P.TS(ob[:, hh * 64:(hh + 1) * 64], pO[:, hh * 65:hh * 65 + 64], den[:, hh:hh + 1], ALU.mult, [pO, den], [ob])
                for half in range(2):
                    pT = P.next_ps()
                    P.TR(pT[:, 0:128], ob[:, half * 128:(half + 1) * 128], [ob], [pT])
                    yo = P.sm()
                    P.CP(yo[:, 0:128], pT[:, 0:128], [pT], [yo], eng=P.kb.act)
                    r0 = 512 + g * 256 + half * 128
                    P.DMA(P.yT[r0:r0 + 128, ti * 128:(ti + 1) * 128], yo[:, 0:128], [yo], [P.yT])

    def resid_ln(self, z, xt, ti, xn):
        P = self
        P.STT(z[:], xt[:], ALPHA, z[:], ALU.mult, ALU.add, [xt, z], [z])
        st = P.stat
        for hf in range(2):
            P.kb.op(P.kb.dve, lambda e, hf=hf: e.bn_stats(out=st[:, hf * 6:(hf + 1) * 6], in_=z[:, hf * 512:(hf + 1) * 512]), [z.r], [st.r])
        P.kb.op(P.kb.dve, lambda e: e.bn_aggr(out=st[:, 12:14], in_=st[:, 0:12]), [st.r], [st.r])
        P.ACT(st[:, 14:15], st[:, 13:14], AF.Sqrt, [st, P.cst], [st], bias=P.cst[:, 1:2])
        P.RECIP(st[:, 14:15], st[:, 14:15], [st], [st])
        P.TS(z[:], z[:], st[:, 12:13], ALU.subtract, [z, st], [z], s2=st[:, 14:15], op1=ALU.mult)
        P.TT(z[:], z[:], P.bc[2][:], ALU.mult, [z, P.bc[2]], [z])
        P.TT(xn[:], z[:], P.bc[3][:], ALU.add, [z, P.bc[3]], [xn])

    def load_ln(self, l, k):
        P = self
        P.DMA(P.bc[2][:], P.w["ln_g"][l, k].partition_broadcast(128), [], [P.bc[2]])
        P.DMA(P.bc[3][:], P.w["ln_b"][l, k].partition_broadcast(128), [], [P.bc[3]])

    def phase_out(self, l, wout_dram, xsrc, xdst, router_j=None):
        P = self
        P.gate_bcast(2, 0, P.bc[0]); P.gate_bcast(2, 1, P.bc[1]); P.load_ln(l, 0)
        if router_j is not None:
            P.DMA(P.rw[:], P.w["moe_router_w"][router_j].rearrange("(c p) n -> p c n", p=128), [], [P.rw])
            P.DMA(P.rb[:], P.w["moe_router_b"][router_j].partition_broadcast(128), [], [P.rb])
        wof, wor = P.gflat(0, 4096, BF16)
        wo = wof.rearrange("p (c n) -> p c n", n=1024)
        P.DMA(wo, wout_dram.rearrange("(c p) n -> p c n", p=128), [], wor, q=P.kb.pool)
        ytb = [P.gflat(2 + i, 512, BF16)[0].rearrange("p (c n) -> p c n", n=128) for i in range(2)]
        for ti in range(NT):
            cond = 1 if ti < 2 else 0
            yt, yr = ytb[ti % 2], P.G[2 + ti % 2]
            P.DMA(yt, P.yT[:, ti * 128:(ti + 1) * 128].rearrange("(c p) n -> p c n", p=128), [P.yT], [yr], q=P.kb.pool)
            xt = P.xb[P.xbi]; P.xbi = (P.xbi + 1) % 3
            P.DMA(xt[:], xsrc[ti * 128:(ti + 1) * 128, :], [xsrc], [xt])
            z = P.zb[ti % 2]
            for hf in range(2):
                po = P.next_ps()
                for c in range(KC):
                    P.MM(po[:, :], yt[:, c, :], wo[:, c, hf * 512:(hf + 1) * 512], [yr] + wor, [po], start=(c == 0), stop=(c == 7))
                P.TT(z[:, hf * 512:(hf + 1) * 512], po[:, :], P.bc[cond][:, hf * 512:(hf + 1) * 512], ALU.mult, [po, P.bc[cond]], [z])
            xn = P.xb[P.xbi]; P.xbi = (P.xbi + 1) % 3
            P.resid_ln(z, xt, ti, xn)
            P.DMA(xdst[ti * 128:(ti + 1) * 128, :], xn[:], [xn], [xdst])
            P.hT_tile(xn, ti, P.sc2p, 3, router=(router_j is not None))

    def ffn_tile_weights(self, wg, wu, wd):
        pass

    def phase_ffn(self, l, j, xsrc, xdst, lat_only_out=None):
        P = self
        P.gate_bcast(5, 0, P.bc[0]); P.gate_bcast(5, 1, P.bc[1]); P.load_ln(l, 1)
        Wg, Wu, Wd = P.w["ffn_w_gate"][j], P.w["ffn_w_up"][j], P.w["ffn_w_down"][j]
        wdf, wdr = P.gflat(0, NFC * 512, BF16)
        wd = wdf.rearrange("p (f n) -> p f n", n=1024)
        P.DMA(wd, Wd.rearrange("(f p) n -> p f n", p=128), [], wdr, q=P.kb.pool)
        acf, acr = P.gflat(5, NFC * 256, BF16)
        actT = acf.rearrange("p (f n) -> p f n", n=512)
        for (t0, n) in NTILES:
            for fc in range(NFC):
                wgb = P.loadw(Wg[:, fc * 128:(fc + 1) * 128], 128)
                wub = P.loadw(Wu[:, fc * 128:(fc + 1) * 128], 128)
                pg = P.next_ps(); pu = P.next_ps()
                for c in range(KC):
                    P.MM(pg[:, 0:n], wgb[:, c, :], P.hT[:, c, t0:t0 + n], [wgb, P.hT], [pg], start=(c == 0), stop=(c == 7))
                for c in range(KC):
                    P.MM(pu[:, 0:n], wub[:, c, :], P.hT[:, c, t0:t0 + n], [wub, P.hT], [pu], start=(c == 0), stop=(c == 7))
                sg = P.sm()
                P.ACT(sg[:, 0:n], pg[:, 0:n], AF.Silu, [pg], [sg])
                P.TT(actT[:, fc, 0:n], sg[:, 0:n], pu[:, 0:n], ALU.mult, [sg, pu], acr)
            for sub in range(n // 128):
                ti = t0 // 128 + sub
                cond = 1 if ti < 2 else 0
                xt = P.xb[P.xbi]; P.xbi = (P.xbi + 1) % 3
                P.DMA(xt[:], xsrc[ti * 128:(ti + 1) * 128, :], [xsrc], [xt])
                z = P.zb[ti % 2]
                for hf in range(2):
                    po = P.next_ps()
                    for fc in range(NFC):
                        P.MM(po[:, :], actT[:, fc, sub * 128:(sub + 1) * 128], wd[:, fc, hf * 512:(hf + 1) * 512], acr + wdr, [po],
                             start=(fc == 0), stop=(fc == NFC - 1))
                    P.TT(z[:, hf * 512:(hf + 1) * 512], po[:, :], P.bc[cond][:, hf * 512:(hf + 1) * 512], ALU.mult, [po, P.bc[cond]], [z])
                xn = P.xb[P.xbi]; P.xbi = (P.xbi + 1) % 3
                P.resid_ln(z, xt, ti, xn)
                P.DMA(xdst[ti * 128:(ti + 1) * 128, :], xn[:], [xn], [xdst])

    def phase_moe(self, l, j, xsrc, xdst):
        P = self
        P.gate_bcast(5, 0, P.bc[0]); P.gate_bcast(5, 1, P.bc[1]); P.load_ln(l, 1)
        wdf, wdr = P.gflat(0, NFC * 512, BF16)
        wd = wdf.rearrange("p (f n) -> p f n", n=1024)
        acf, acr = P.gflat(5, NFC * 256, BF16)
        actT = acf.rearrange("p (f n) -> p f n", n=512)
        accf, accr = P.gflat(8, 4096)
        acc = accf.rearrange("p (s n) -> p s n", n=1024)
        for (t0, n) in NTILES:
            nsub = n // 128
            for ex in range(8):
                Wg, Wu, Wd = P.w["moe_w_gate"][j, ex], P.w["moe_w_up"][j, ex], P.w["moe_w_down"][j, ex]
                P.DMA(wd, Wd.rearrange("(f p) n -> p f n", p=128), [], wdr, q=P.kb.pool)
                for fc in range(NFC):
                    wgb = P.loadw(Wg[:, fc * 128:(fc + 1) * 128], 128)
                    wub = P.loadw(Wu[:, fc * 128:(fc + 1) * 128], 128)
                    pg = P.next_ps(); pu = P.next_ps()
                    for c in range(KC):
                        P.MM(pg[:, 0:n], wgb[:, c, :], P.hT[:, c, t0:t0 + n], [wgb, P.hT], [pg], start=(c == 0), stop=(c == 7))
                    for c in range(KC):
                        P.MM(pu[:, 0:n], wub[:, c, :], P.hT[:, c, t0:t0 + n], [wub, P.hT], [pu], start=(c == 0), stop=(c == 7))
                    sg = P.sm()
                    P.ACT(sg[:, 0:n], pg[:, 0:n], AF.Silu, [pg], [sg])
                    P.TT(actT[:, fc, 0:n], sg[:, 0:n], pu[:, 0:n], ALU.mult, [sg, pu], acr)
                for sub in range(nsub):
                    ti = t0 // 128 + sub
                    for hf in range(2):
                        po = P.next_ps()
                        for fc in range(NFC):
                            P.MM(po[:, :], actT[:, fc, sub * 128:(sub + 1) * 128], wd[:, fc, hf * 512:(hf + 1) * 512], acr + wdr, [po],
                                 start=(fc == 0), stop=(fc == NFC - 1))
                        a = acc[:, sub, hf * 512:(hf + 1) * 512]
                        if ex == 0:
                            P.TS(a, po[:, :], P.gates[:, ti, ex:ex + 1], ALU.mult, [po, P.gates], accr)
                        else:
                            P.STT(a, po[:, :], P.gates[:, ti, ex:ex + 1], a, ALU.mult, ALU.add, [po, P.gates] + accr, accr)
            for sub in range(nsub):
                ti = t0 // 128 + sub
                cond = 1 if ti < 2 else 0
                xt = P.xb[P.xbi]; P.xbi = (P.xbi + 1) % 3
                P.DMA(xt[:], xsrc[ti * 128:(ti + 1) * 128, :], [xsrc], [xt])
                z = P.zb[ti % 2]
                P.TT(z[:], acc[:, sub, :], P.bc[cond][:], ALU.mult, accr + [P.bc[cond]], [z])
                xn = P.xb[P.xbi]; P.xbi = (P.xbi + 1) % 3
                P.resid_ln(z, xt, ti, xn)
                P.DMA(xdst[ti * 128:(ti + 1) * 128, :], xn[:], [xn], [xdst])

    def s5(self, l, j):
        P = self
        G = P.G
        I32 = mybir.dt.int32
        TWO_PI = 2.0 * math.pi
        W = P.w["od_w_in"][j]
        st_ = P.s5t
        R_, W_ = [st_, P.s5i], [st_]
        P.DMA(P.s5d[:], P.w["s5_d_fm"][j], [], [P.s5d])
        LRE, LIM, DT, MAG, THN, SN, CS, CRE, CIM, NCIM, TA, TB = range(12)
        for d in range(2):
            q = lambda k: st_[:, d, k, :]
            P.DMA(q(LRE), P.w["s5_lre"][j, d], [], W_); P.DMA(q(LIM), P.w["s5_lim"][j, d], [], W_); P.DMA(q(DT), P.w["s5_ldt"][j, d], [], W_)
            P.ACT(q(DT), q(DT), AF.Exp, R_, W_)
            P.TS(q(LRE), q(LRE), -1e-4, ALU.min, R_, W_)
            P.TT(q(TA), q(LRE), q(DT), ALU.mult, R_, W_)
            P.ACT(q(MAG), q(TA), AF.Exp, R_, W_)
            P.TT(q(THN), q(LIM), q(DT), ALU.mult, R_, W_)
            P.TS(q(THN), q(THN), 1.0 / TWO_PI, ALU.mult, R_, W_)
            P.CP(P.s5i[:], q(THN), R_, [P.s5i]); P.CP(q(TA), P.s5i[:], R_, W_)
            P.TT(q(TA), q(THN), q(TA), ALU.subtract, R_, W_)
            P.ACT(q(SN), q(TA), AF.Sin, R_, W_, scale=TWO_PI)
            P.TS(q(TA), q(TA), 0.25, ALU.add, R_, W_)
            P.CP(P.s5i[:], q(TA), R_, [P.s5i]); P.CP(q(TB), P.s5i[:], R_, W_)
            P.TT(q(TA), q(TA), q(TB), ALU.subtract, R_, W_)
            P.ACT(q(CS), q(TA), AF.Sin, R_, W_, scale=TWO_PI)
            P.TT(q(CS), q(CS), q(MAG), ALU.mult, R_, W_)
            P.TT(q(SN), q(SN), q(MAG), ALU.mult, R_, W_)
            P.TT(q(TA), q(LRE), q(LRE), ALU.mult, R_, W_); P.TT(q(TB), q(LIM), q(LIM), ALU.mult, R_, W_)
            P.TT(q(TA), q(TA), q(TB), ALU.add, R_, W_); P.RECIP(q(TA), q(TA), R_, W_)
            P.TS(q(CS), q(CS), -1.0, ALU.add, R_, W_)
            P.TT(q(CRE), q(CS), q(LRE), ALU.mult, R_, W_); P.TT(q(TB), q(SN), q(LIM), ALU.mult, R_, W_)
            P.TT(q(CRE), q(CRE), q(TB), ALU.add, R_, W_); P.TT(q(CRE), q(CRE), q(TA), ALU.mult, R_, W_)
            P.TT(q(CIM), q(SN), q(LRE), ALU.mult, R_, W_); P.TT(q(TB), q(CS), q(LIM), ALU.mult, R_, W_)
            P.TT(q(CIM), q(CIM), q(TB), ALU.subtract, R_, W_); P.TT(q(CIM), q(CIM), q(TA), ALU.mult, R_, W_)
            P.TS(q(NCIM), q(CIM), -1.0, ALU.mult, R_, W_)
        uT, yacc, A, B, Cs, Sn, t1, t2 = [G[i] for i in range(8)]
        t2i = P.Gt[:, 7, :].bitcast(I32)
        ygf, ygr = P.gflat(8, 2 * T, BF16)
        ygT = ygf.rearrange("p (c n) -> p c n", n=T)
        for ut in range(4):
            wu = P.loadw(W[:, ut * 128:(ut + 1) * 128], 128)
            P.proj_fm(wu, 128, lambda pb, t0, n: P.CP(uT[:, t0:t0 + n], pb[:, 0:n], [pb], [uT], eng=P.kb.act))
            P.TS(yacc[:], uT[:], P.s5d[:, ut:ut + 1], ALU.mult, [uT, P.s5d], [yacc])
            for sl in range(4):
                stt = ut * 4 + sl
                r0 = sl * 32
                bc_ = P.s5bc
                P.DMA(bc_[:, 0:32], P.w["s5_bT"][j, :, stt].rearrange("p a h -> p (a h)"), [], [bc_])
                P.DMA(bc_[:, 32:64], P.w["s5_cT"][j, :, stt].rearrange("p a h -> p (a h)"), [], [bc_])
                Zc = P.s5zc
                P.MSET(Zc[:, 0:256], 0.0, [Zc])
                for gl in range(2):
                    pr = slice(gl * 64, gl * 64 + 64); cc = slice(r0 + gl * 16, r0 + gl * 16 + 16)
                    P.CP(Zc[pr, cc], bc_[pr, 32:48], [bc_], [Zc])
                    P.TS(Zc[pr, 128 + cc.start:128 + cc.stop], bc_[pr, 48:64], -1.0, ALU.mult, [bc_], [Zc])
                for d in range(2):
                    q = lambda k: st_[:, d, k, stt:stt + 1]
                    Z = P.sm(); tmp = P.sm(); L = P.sm()
                    P.MSET(Z[:, 0:256], 0.0, [Z])
                    for gl in range(2):
                        pr = slice(gl * 64, gl * 64 + 64); c0 = r0 + gl * 16
                        P.TS(tmp[pr, 0:16], bc_[pr, 0:16], q(CRE)[pr], ALU.mult, [bc_, st_], [tmp])
                        P.STT(Z[pr, c0:c0 + 16], bc_[pr, 16:32], q(NCIM)[pr], tmp[pr, 0:16], ALU.mult, ALU.add, [bc_, st_, tmp], [Z])
                        P.TS(tmp[pr, 16:32], bc_[pr, 16:32], q(CRE)[pr], ALU.mult, [bc_, st_], [tmp])
                        P.STT(Z[pr, 128 + c0:128 + c0 + 16], bc_[pr, 0:16], q(CIM)[pr], tmp[pr, 16:32], ALU.mult, ALU.add, [bc_, st_, tmp], [Z])
                    for k in range(2):
                        pz = P.next_ps()
                        P.TR(pz[:, 0:128], Z[:, k * 128:(k + 1) * 128], [Z], [pz])
                        P.CP(L[:, k * 128:(k + 1) * 128], pz[:, 0:128], [pz], [L], eng=P.kb.act)
                    P.DMA(t1[:], P.cd["tauF" if d == 0 else "tauB"][:], [], [t1])
                    P.TS(t1[:], t1[:], q(THN), ALU.mult, [t1, st_], [t1])
                    P.CP(t2i, t1[:], [t1], [t2]); P.CP(Cs[:], t2i, [t2], [Cs], eng=P.kb.act)
                    P.TT(t1[:], t1[:], Cs[:], ALU.subtract, [t1, Cs], [t1])
                    P.ACT(Sn[:], t1[:], AF.Sin, [t1], [Sn], scale=TWO_PI)
                    P.TS(t1[:], t1[:], 0.25, ALU.add, [t1], [t1])
                    P.CP(t2i, t1[:], [t1], [t2]); P.CP(Cs[:], t2i, [t2], [Cs], eng=P.kb.act)
                    P.TT(t1[:], t1[:], Cs[:], ALU.subtract, [t1, Cs], [t1])
                    P.ACT(Cs[:], t1[:], AF.Sin, [t1], [Cs], scale=TWO_PI)
                    for (t0, n) in NTILES:
                        pa = P.next_ps(); pb_ = P.next_ps()
                        P.MM(pa[:, 0:n], L[:, 0:128], uT[:, t0:t0 + n], [L, uT], [pa])
                        P.MM(pb_[:, 0:n], L[:, 128:256], uT[:, t0:t0 + n], [L, uT], [pb_])
                        P.CP(A[:, t0:t0 + n], pa[:, 0:n], [pa], [A], eng=P.kb.act)
                        P.CP(B[:, t0:t0 + n], pb_[:, 0:n], [pb_], [B])
                    P.TT(t1[:], A[:], Cs[:], ALU.mult, [A, Cs], [t1]); P.TT(t2[:], B[:], Sn[:], ALU.mult, [B, Sn], [t2])
                    P.TT(A[:], A[:], Sn[:], ALU.mult, [A, Sn], [A]); P.TT(B[:], B[:], Cs[:], ALU.mult, [B, Cs], [B])
                    P.TT(t1[:], t1[:], t2[:], ALU.add, [t1, t2], [t1]); P.TT(B[:], B[:], A[:], ALU.subtract, [B, A], [B])
                    mg = q(MAG)
                    for src, dst in ((t1, t2), (B, A)):
                        if d == 0:
                            P.SCAN(dst[:], mg.broadcast_to([128, T]), src[:], [st_, src], [dst])
                        else:
                            P.SCAN(dst[:, 255::-1], mg.broadcast_to([128, 256]), src[:, 255::-1], [st_, src], [dst])
                            P.SCAN(dst[:, T - 1:255:-1], mg.broadcast_to([128, T - 256]), src[:, T - 1:255:-1], [st_, src, dst], [dst], init=dst[:, 0:1])
                    P.TT(t1[:], t2[:], Cs[:], ALU.mult, [t2, Cs], [t1]); P.TT(B[:], A[:], Sn[:], ALU.mult, [A, Sn], [B])
                    P.TT(t1[:], t1[:], B[:], ALU.subtract, [t1, B], [t1])
                    P.TT(t2[:], t2[:], Sn[:], ALU.mult, [t2, Sn], [t2]); P.TT(A[:], A[:], Cs[:], ALU.mult, [A, Cs], [A])
                    P.TT(t2[:], t2[:], A[:], ALU.add, [t2, A], [t2])
                    for (t0, n) in NTILES:
                        py = P.next_ps()
                        P.MM(py[:, 0:n], Zc[:, 0:128], t1[:, t0:t0 + n], [Zc, t1], [py], start=True, stop=False)
                        P.MM(py[:, 0:n], Zc[:, 128:256], t2[:, t0:t0 + n], [Zc, t2], [py], start=False, stop=True)
                        P.TT(yacc[:, t0:t0 + n], yacc[:, t0:t0 + n], py[:, 0:n], ALU.add, [yacc, py], [yacc])
            P.ACT(t1[:], yacc[:], AF.Square, [yacc], [t1])
            P.TS(t1[:], t1[:], 0.044715, ALU.mult, [t1], [t1], s2=1.0, op1=ALU.add)
            P.TT(t1[:], t1[:], yacc[:], ALU.mult, [t1, yacc], [t1])
            P.ACT(t1[:], t1[:], AF.Sigmoid, [t1], [t1], scale=2.0 * math.sqrt(2.0 / math.pi))
            P.TT(ygT[:, ut, :], yacc[:], t1[:], ALU.mult, [yacc, t1], ygr)
        for nt in range(4):
            wg = P.loadw(P.w["s5_glu_w"][j][:, nt * 128:(nt + 1) * 128], 128, kc=4)
            for (t0, n) in NTILES:
                pg = P.next_ps()
                for c in range(4):
                    P.MM(pg[:, 0:n], wg[:, c, :], ygT[:, c, t0:t0 + n], [wg] + ygr, [pg], start=(c == 0), stop=(c == 3))
                sg = P.sm()
                P.ACT(sg[:, 0:n], pg[:, 0:n], AF.Sigmoid, [pg], [sg])
                yo = P.sm()
                P.TT(yo[:, 0:n], ygT[:, nt, t0:t0 + n], sg[:, 0:n], ALU.mult, ygr + [sg], [yo])
                P.DMA(P.yT[nt * 128:(nt + 1) * 128, t0:t0 + n], yo[:, 0:n], [yo], [P.yT])

    def rwkv(self, l, j):
        P = self
        G = P.G
        W = P.w["od_w_in"][j]
        C0 = 512
        prm = P.rwp
        P.DMA(prm[:, 0:15], P.w["rw_mu_fm"][j], [], [prm])
        P.DMA(prm[:, 16:20], P.w["rw_kk_fm"][j], [], [prm]); P.DMA(prm[:, 20:24], P.w["rw_ka_fm"][j], [], [prm])
        P.DMA(prm[:, 24:28], P.w["rw_rk_fm"][j], [], [prm])
        for d in range(2):
            P.DMA(prm[:, 28 + d * 4:32 + d * 4], P.w["rw_w0_fm"][j, d], [], [prm])
            P.DMA(prm[:, 36 + d * 4:40 + d * 4], P.w["rw_a0_fm"][j, d], [], [prm])
        MU = lambda t_: prm[:, t_:t_ + 1]
        rT, kT, vtokS, kkT, L_, Gc, E1, E2, As, Oacc = [G[i] for i in range(10)]
        vtok = vtokS[:].rearrange("p (t c) -> p t c", c=128)
        oacc = Oacc[:].rearrange("p (t c) -> p t c", c=128)
        Hbd = P.S

        def proj_shifted(tile_idx, dst, tmp):
            wb = P.loadw(W[:, C0 + tile_idx * 128:C0 + (tile_idx + 1) * 128], 128)
            P.proj_fm(wb, 128, lambda pb, t0, n: P.CP(tmp[:, t0:t0 + n], pb[:, 0:n], [pb], [tmp], eng=P.kb.act))
            for (a, b) in ((0, LC), (LC, T)):
                P.CP(dst[:, a:a + 1], tmp[:, a + 1:a + 2], [tmp], [dst], eng=P.kb.pool)
                P.CP(dst[:, b - 1:b], tmp[:, b - 2:b - 1], [tmp], [dst], eng=P.kb.pool)
                P.TT(dst[:, a + 1:b - 1], tmp[:, a:b - 2], tmp[:, a + 2:b], ALU.add, [tmp], [dst])
            P.STT(dst[:], dst[:], 0.5, tmp[:], ALU.mult, ALU.subtract, [dst, tmp], [dst])
            P.STT(dst[:], dst[:], MU(tile_idx), tmp[:], ALU.mult, ALU.add, [dst, prm, tmp], [dst])

        P.DMA(P.rwm[:], P.cd["rw_masks"][0], [], [P.rwm])
        for pp in range(4):
            proj_shifted(0 + pp, rT, E1)
            proj_shifted(4 + pp, kT, E1)
            proj_shifted(8 + pp, E2, E1)
            for ti in range(NT):
                pv = P.next_ps()
                P.TR(pv[:, 0:128], E2[:, ti * 128:(ti + 1) * 128], [E2], [pv])
                P.CP(vtok[:, ti, :], pv[:, 0:128], [pv], [vtokS], eng=P.kb.act)
            P.TS(kkT[:], kT[:], prm[:, 16 + pp:17 + pp], ALU.mult, [kT, prm], [kkT])
            for (t0, n) in NTILES:
                sq = P.sm()
                P.ACT(sq[:, 0:n], kkT[:, t0:t0 + n], AF.Square, [kkT], [sq])
                pn = P.next_ps()
                P.MM(pn[:, 0:n], P.bdones[:], sq[:, 0:n], [P.bdones, sq], [pn])
                nr_ = P.sm()
                P.ACT(nr_[:, 0:n], pn[:, 0:n], AF.Sqrt, [pn], [nr_])
                P.TS(nr_[:, 0:n], nr_[:, 0:n], 1e-12, ALU.max, [nr_], [nr_])
                P.RECIP(nr_[:, 0:n], nr_[:, 0:n], [nr_], [nr_])
                P.TT(kkT[:, t0:t0 + n], kkT[:, t0:t0 + n], nr_[:, 0:n], ALU.mult, [kkT, nr_], [kkT])
            P.DMA(P.rwln[:, 0, :], P.w["rw_lng"][j, pp * 128:(pp + 1) * 128].partition_broadcast(128), [], [P.rwln])
            P.DMA(P.rwln[:, 1, :], P.w["rw_lnb"][j, pp * 128:(pp + 1) * 128].partition_broadcast(128), [], [P.rwln])
            for d in range(2):
                fwd = d == 0
                P.DMA(P.rwm[:], P.cd["rw_masks"][d], [], [P.rwm])
                for (lt, padk, biasc, dstb, fn) in ((12, "rw_w2pad", 28 + d * 4 + pp, L_, AF.Tanh), (13, "rw_a2pad", 36 + d * 4 + pp, As, None)):
                    proj_shifted(lt, E1, E2)
                    if fn is not None:
                        P.ACT(E1[:], E1[:], fn, [E1], [E1])
                    wpad = P.attm[1]
                    P.DMA(wpad[:, 0:128], P.w[padk][j, d, :, pp * 128:(pp + 1) * 128], [], [wpad])
                    for (t0, n) in NTILES:
                        pz = P.next_ps()
                        P.MM(pz[:, 0:n], wpad[:, 0:128], E1[:, t0:t0 + n], [wpad, E1], [pz])
                        P.ACT(dstb[:, t0:t0 + n], pz[:, 0:n], AF.Sigmoid, [pz, prm], [dstb], bias=prm[:, biasc:biasc + 1])
                P.TS(L_[:], L_[:], -math.exp(-0.5), ALU.mult, [L_], [L_])
                for ti in range(NT):
                    tsl = slice(ti * 128, (ti + 1) * 128)
                    if fwd:
                        P.SCAN(Gc[:, tsl], P.ones[:], L_[:, tsl], [P.ones, L_], [Gc])
                    else:
                        hi, lo = (ti + 1) * 128 - 1, ti * 128 - 1
                        rs = slice(hi, lo if lo >= 0 else None, -1)
                        P.SCAN(Gc[:, rs], P.ones[:], L_[:, rs], [P.ones, L_], [Gc])
                P.ACT(E1[:], Gc[:], AF.Exp, [Gc], [E1])
                ecol = E1[:, 127:T:128] if fwd else E1[:, 0:T:128]
                P.CP(P.rwgc[:], ecol, [E1], [P.rwgc])
                P.TT(E1[:], E1[:], rT[:], ALU.mult, [E1, rT], [E1])
                P.TT(E2[:], Gc[:], L_[:], ALU.subtract, [Gc, L_], [E2])
                P.ACT(E2[:], E2[:], AF.Exp, [E2], [E2])
                P.STT(E2[:], kkT[:], -1.0, E2[:], ALU.mult, ALU.mult, [kkT, E2], [E2])
                P.ACT(Gc[:], Gc[:], AF.Exp, [Gc], [Gc], scale=-1.0)
                P.TT(L_[:], kkT[:], As[:], ALU.mult, [kkT, As], [L_])
                P.TT(L_[:], L_[:], Gc[:], ALU.mult, [L_, Gc], [L_])
                P.TS(As[:], As[:], -1.0, ALU.add, [As, prm], [As], s2=prm[:, 20 + pp:21 + pp], op1=ALU.mult)
                P.STT(As[:], As[:], 1.0, kT[:], ALU.add, ALU.mult, [As, kT], [As])
                P.TT(As[:], As[:], Gc[:], ALU.mult, [As, Gc], [As])
                rt_, at_, bt_, kt_ = E1, E2, L_, As
                P.MSET(Hbd[:], 0.0, [Hbd])
                tiles = list(range(NT)) if fwd else [1, 0] + list(range(NT - 1, 1, -1))
                hs = [slice(0, 64), slice(64, 128)]
                CB = [P.bc[0], P.bc[1], P.bc[2], P.bc[3], P.xb[0], P.xb[1]]
                XA, ZA, XB, ZB, WT, MRB, LAK, MRK = range(8)
                mat = lambda c, q_: CB[c][:, q_ * 128:(q_ + 1) * 128]
                Ybuf = P.attm[0]
                for g0 in range(0, NT, 3):
                    grp = tiles[g0:g0 + 3]
                    chains = [(ti, hh) for ti in grp for hh in range(2)]
                    for c, (ti, hh) in enumerate(chains):
                        tsl = slice(ti * 128, (ti + 1) * 128)
                        ar = P.rwar[hh]; cb = CB[c]
                        P.TS(ar[:, 0:128], at_[:, tsl], P.halfm[:, hh:hh + 1], ALU.mult, [at_, P.halfm], [ar])
                        P.TS(ar[:, 128:256], rt_[:, tsl], P.halfm[:, hh:hh + 1], ALU.mult, [rt_, P.halfm], [ar], eng=P.kb.pool)
                        p1 = P.next_ps(); p2 = P.next_ps(); p3 = P.next_ps()
                        P.MM(p1[:, 0:256], bt_[:, tsl], ar[:, 0:256], [bt_, ar], [p1])
                        P.MM(p2[:, 0:256], kt_[:, tsl], ar[:, 0:256], [kt_, ar], [p2])
                        P.MM(p3[:, 0:128], ar[:, 0:128], bt_[:, tsl], [ar, bt_], [p3])
                        P.TT(mat(c, XA), p1[:, 0:128], P.rwm[:, 0, :], ALU.mult, [p1, P.rwm], [cb])
                        P.TT(mat(c, MRB), p1[:, 128:256], P.rwm[:, 1, :], ALU.mult, [p1, P.rwm], [cb])
                        P.TT(mat(c, LAK), p2[:, 0:128], P.rwm[:, 0, :], ALU.mult, [p2, P.rwm], [cb])
                        P.TT(mat(c, MRK), p2[:, 128:256], P.rwm[:, 1, :], ALU.mult, [p2, P.rwm], [cb])
                        P.TT(mat(c, ZA), p3[:, 0:128], P.rwm[:, 2, :], ALU.mult, [p3, P.rwm], [cb])
                        P.TT(mat(c, WT), mat(c, XA), P.ident[:], ALU.add, [cb, P.ident], [cb], eng=P.kb.pool)
                    cx, cz, nx, nz = XA, ZA, XB, ZB
                    for k in range(1, 7):
                        for c in range(len(chains)):
                            cb = CB[c]
                            pZ = P.next_ps()
                            P.MM(pZ[:, 0:128], mat(c, cx), mat(c, cz), [cb], [pZ])
                            if k < 6:
                                pX = P.next_ps()
                                P.MM(pX[:, 0:128], mat(c, cz), mat(c, cx), [cb], [pX])
                            P.CP(mat(c, nz), pZ[:, 0:128], [pZ], [cb])
                            if k < 6:
                                P.CP(mat(c, nx), pX[:, 0:128], [pX], [cb], eng=P.kb.act)
                        for c in range(len(chains)):
                            cb = CB[c]
                            pW = P.next_ps()
                            P.MM(pW[:, 0:128], mat(c, nz), mat(c, WT), [cb], [pW])
                            P.TT(mat(c, WT), mat(c, WT), pW[:, 0:128], ALU.add, [cb, pW], [cb])
                        cx, cz, nx, nz = nx, nz, cx, cz
                    for gi, ti in enumerate(grp):
                        tsl = slice(ti * 128, (ti + 1) * 128)
                        U = P.rwu
                        pY = P.next_ps()
                        for hh in range(2):
                            c = gi * 2 + hh
                            P.MM(pY[:, hs[hh]], at_[:, tsl], Hbd[:, hs[hh]], [at_, Hbd], [pY], start=True, stop=False)
                            P.MM(pY[:, hs[hh]], mat(c, LAK), vtok[:, ti, hs[hh]], [CB[c], vtokS], [pY], start=False, stop=True)
                        P.CP(Ybuf[:], pY[:, 0:128], [pY], [Ybuf])
                        pU = P.next_ps()
                        for hh in range(2):
                            c = gi * 2 + hh
                            P.MM(pU[:, hs[hh]], mat(c, WT), Ybuf[:, hs[hh]], [CB[c], Ybuf], [pU])
                        P.CP(U[:], pU[:, 0:128], [pU], [U], eng=P.kb.act)
                        pO = P.next_ps()
                        for hh in range(2):
                            c = gi * 2 + hh
                            P.MM(pO[:, hs[hh]], rt_[:, tsl], Hbd[:, hs[hh]], [rt_, Hbd], [pO], start=True, stop=False)
                            P.MM(pO[:, hs[hh]], mat(c, MRB), U[:, hs[hh]], [CB[c], U], [pO], start=False, stop=False)
                            P.MM(pO[:, hs[hh]], mat(c, MRK), vtok[:, ti, hs[hh]], [CB[c], vtokS], [pO], start=False, stop=True)
                        if fwd:
                            P.CP(oacc[:, ti, :], pO[:, 0:128], [pO], [Oacc], eng=P.kb.act)
                        else:
                            P.TT(oacc[:, ti, :], oacc[:, ti, :], pO[:, 0:128], ALU.add, [Oacc, pO], [Oacc])
                        tk = P.ktok[0]
                        for q_, src in ((0, bt_), (1, kt_)):
                            pt_ = P.next_ps()
                            P.TR(pt_[:, 0:128], src[:, tsl], [src], [pt_])
                            P.CP(tk[:, q_, :], pt_[:, 0:128], [pt_], [tk], eng=P.kb.act)
                        pH = P.next_ps()
                        P.MM(pH[:, 0:128], tk[:, 0, :], U[:], [tk, U], [pH], start=True, stop=False)
                        P.MM(pH[:, 0:128], tk[:, 1, :], vtok[:, ti, :], [tk, vtokS], [pH], start=False, stop=True)
                        P.STT(P.tmpS[:], pH[:, 0:128], P.rwgc[:, ti:ti + 1], P.bdones[:], ALU.mult, ALU.mult, [pH, P.rwgc, P.bdones], [P.tmpS])
                        P.STT(Hbd[:], Hbd[:], P.rwgc[:, ti:ti + 1], P.tmpS[:], ALU.mult, ALU.add, [Hbd, P.rwgc, P.tmpS], [Hbd])
            P.STT(E1[:], rT[:], prm[:, 24 + pp:25 + pp], kT[:], ALU.mult, ALU.mult, [rT, prm, kT], [E1])
            proj_shifted(14, E2, Gc)
            P.ACT(E2[:], E2[:], AF.Sigmoid, [E2], [E2])
            g2b = P.attm[0]
            P.DMA(g2b[:, 0:128], P.w["rw_g2"][j, :, pp * 128:(pp + 1) * 128], [], [g2b])
            for ti in range(NT):
                tsl = slice(ti * 128, (ti + 1) * 128)
                o = P.sm(); st = P.stat
                P.CP(o[:, 0:128], oacc[:, ti, :], [Oacc], [o], eng=P.kb.pool)
                o3 = o[:, 0:128].rearrange("p (h i) -> p h i", i=64)
                P.kb.op(P.kb.dve, lambda e, o3=o3: e.reduce_sum(out=st[:, 16:18], in_=o3, axis=AX.X), [o.r], [st.r])
                P.TS(st[:, 16:18], st[:, 16:18], 1.0 / 64.0, ALU.mult, [st], [st])
                for hh in range(2):
                    P.TS(o3[:, hh, :], o3[:, hh, :], st[:, 16 + hh:17 + hh], ALU.subtract, [o, st], [o])
                sq = P.sm()
                P.TT(sq[:, 0:128], o[:, 0:128], o[:, 0:128], ALU.mult, [o], [sq])
                sq3 = sq[:, 0:128].rearrange("p (h i) -> p h i", i=64)
                P.kb.op(P.kb.dve, lambda e, sq3=sq3: e.reduce_sum(out=st[:, 18:20], in_=sq3, axis=AX.X), [sq.r], [st.r])
                P.ACT(st[:, 18:20], st[:, 18:20], AF.Sqrt, [st, P.cst], [st], scale=1.0 / 64.0, bias=P.cst[:, 2:3])
                P.RECIP(st[:, 18:20], st[:, 18:20], [st], [st])
                for hh in range(2):
                    P.TS(o3[:, hh, :], o3[:, hh, :], st[:, 18 + hh:19 + hh], ALU.mult, [o, st], [o])
                P.TT(o[:, 0:128], o[:, 0:128], P.rwln[:, 0, :], ALU.mult, [o, P.rwln], [o])
                P.TT(o[:, 0:128], o[:, 0:128], P.rwln[:, 1, :], ALU.add, [o, P.rwln], [o])
                pb_ = P.next_ps()
                P.MM(pb_[:, 0:2], E1[:, tsl], P.halfm[:], [E1, P.halfm], [pb_])
                P.CP(st[:, 20:22], pb_[:, 0:2], [pb_], [st])
                for hh in range(2):
                    P.STT(o3[:, hh, :], vtok[:, ti, hh * 64:(hh + 1) * 64], st[:, 20 + hh:21 + hh], o3[:, hh, :], ALU.mult, ALU.add, [vtokS, st, o], [o])
                pg = P.next_ps()
                P.MM(pg[:, 0:128], E2[:, tsl], g2b[:, 0:128], [E2, g2b], [pg])
                P.TT(o[:, 0:128], o[:, 0:128], pg[:, 0:128], ALU.mult, [o, pg], [o])
                pT = P.next_ps()
                P.TR(pT[:, 0:128], o[:, 0:128], [o], [pT])
                yo = P.sm()
                P.CP(yo[:, 0:128], pT[:, 0:128], [pT], [yo], eng=P.kb.act)
                P.DMA(P.yT[512 + pp * 128:512 + (pp + 1) * 128, tsl], yo[:, 0:128], [yo], [P.yT])

    def build(self):
        P = self
        P.setup()
        xcur = P.xin
        for li, l in enumerate(P.layers):
            j = l // 2
            last = li == len(P.layers) - 1
            xmid = P.xs[0]
            xnext = P.out if last else P.xs[1]
            if P.stop >= 1:
                P.phase_mod(l)
                if "fm" in P.taps:
                    P.DMA(P.outp("tap_fm", [128, 48, 2]), P.fm[:], [P.fm], [])
            if P.stop >= 2:
                P.phase_hT(xcur)
                if "hTd" in P.taps:
                    P.DMA(P.outp("tap_hT", [128, KC, T]), P.hT[:], [P.hT], [], q=P.kb.pool)
            if l % 2 == 0:
                if P.stop >= 3: P.hgrn2(l, j)
                if P.stop >= 4: P.attention(l, j)
                if P.stop >= 5: P.phase_out(l, P.w["ev_w_out"][j], xcur, xmid)
                if P.stop >= 6: P.phase_ffn(l, j, xmid, xnext)
            else:
                if "inject_y" in P.taps:
                    pass
                else:
                    if P.stop >= 3: P.s5(l, j)
                    if P.stop >= 4: P.rwkv(l, j)
                if P.stop >= 5: P.phase_out(l, P.w["od_w_out"][j], xcur, xmid, router_j=(j if "1" != "0" else None))
                if "gates" in P.taps:
                    P.DMA(P.outp("tap_gates", [128, NT, 8]), P.gates[:], [P.gates], [])
                if P.stop >= 6: P.phase_moe(l, j, xmid, xnext)
            xcur = xnext
        P.kb.emit()
        self.st.close()
        return self.nc


def host_inputs(inp, b):
    m = {}
    m["xin"] = np.ascontiguousarray(np.concatenate([inp["ctx"][b], inp["x"][b]], 0))
    ct = np.stack([inp["c"][b].reshape(8, 128).T, inp["c_ctx"].reshape(8, 128).T], -1)
    m["condT"] = np.ascontiguousarray(ct.astype(np.float32))
    for k, v in host_consts().items():
        m["c_" + k] = v
    m["ada_w"] = inp["ada_w"]
    m["ada_b_fm"] = np.ascontiguousarray(inp["ada_b"].reshape(4, 48, 128).transpose(0, 2, 1))
    m["ln_g"] = inp["ln_g"]; m["ln_b"] = inp["ln_b"]
    m["ev_w_in"] = inp["ev_w_in"]; m["ev_w_out"] = inp["ev_w_out"]
    m["hg_lb_fm"] = np.ascontiguousarray(inp["hg_lb"].reshape(2, 4, 128).transpose(2, 1, 0))
    m["hg_ng_fm"] = np.ascontiguousarray(inp["hg_norm_g"].reshape(2, 4, 128).transpose(0, 2, 1))
    m["attn_sink"] = inp["attn_sink"]
    def fm16(a):
        return np.ascontiguousarray(a.reshape(2, 2, 16, 2, 64).transpose(0, 1, 3, 4, 2).reshape(2, 2, 128, 16))
    m["s5_lre"] = fm16(inp["s5_lam_re"]); m["s5_lim"] = fm16(inp["s5_lam_im"])
    m["s5_ldt"] = fm16(np.ascontiguousarray(np.broadcast_to(inp["s5_log_dt"][..., None], (2, 2, 32, 64))))
    bb = np.stack([inp["s5_b_re"], inp["s5_b_im"]], 3)
    m["s5_bT"] = np.ascontiguousarray(bb.reshape(2, 16, 2, 64, 2, 16).transpose(0, 2, 3, 1, 4, 5).reshape(2, 128, 16, 2, 16))
    cc = np.stack([inp["s5_c_re"], inp["s5_c_im"]], 2).transpose(0, 1, 4, 2, 3)
    m["s5_cT"] = np.ascontiguousarray(cc.reshape(2, 16, 2, 64, 2, 16).transpose(0, 2, 3, 1, 4, 5).reshape(2, 128, 16, 2, 16))
    m["s5_d_fm"] = np.ascontiguousarray(inp["s5_d"].reshape(2, 4, 128).transpose(0, 2, 1))
    m["s5_glu_w"] = inp["s5_glu_w"]
    fm4 = lambda a: np.ascontiguousarray(a.reshape(a.shape[:-1] + (4, 128)).swapaxes(-1, -2))
    m["rw_mu_fm"] = np.ascontiguousarray(inp["rwkv_mu"].reshape(2, 15, 128).transpose(0, 2, 1))
    m["rw_w0_fm"] = fm4(inp["rwkv_w0"]); m["rw_a0_fm"] = fm4(inp["rwkv_a0"])
    def pad2(a):
        o = np.zeros((2, 2, 128, 512), np.float32)
        o[:, 0, 0:64] = a[:, 0]; o[:, 1, 64:128] = a[:, 1]
        return o
    m["rw_w2pad"] = pad2(inp["rwkv_w2"]); m["rw_a2pad"] = pad2(inp["rwkv_a2"]); m["rw_g2"] = inp["rwkv_g2"]
    m["rw_kk_fm"] = fm4(inp["rwkv_k_k"]); m["rw_ka_fm"] = fm4(inp["rwkv_k_a"]); m["rw_rk_fm"] = fm4(inp["rwkv_r_k"].reshape(2, 512))
    m["rw_lng"] = inp["rwkv_ln_g"]; m["rw_lnb"] = inp["rwkv_ln_b"]
    for k in ("ffn_w_gate", "ffn_w_up", "ffn_w_down", "od_w_in", "od_w_out", "moe_router_w", "moe_router_b", "moe_w_gate", "moe_w_up", "moe_w_down"):
        m[k] = inp[k]
    return m


FUSED = os.environ.get("MK_FUSED", "1") == "1"
N_CORES = 8


def _run(layers, inputs, xin_per_core):
    P = Prog(layers)
    nc = P.build()
    in_maps = []
    for b in range(N_CORES):
        m = host_inputs(inputs, b)
        m["xin"] = xin_per_core[b]
        in_maps.append({k: v for k, v in m.items() if k in P.din})
    res = run_bass_kernel_spmd(nc, in_maps, core_ids=list(range(N_CORES)))
    return [r["out"] for r in res.results]


def kernel(**inputs):
    inputs = {k: np.asarray(v) for k, v in inputs.items()}
    xs = [np.ascontiguousarray(np.concatenate([inputs["ctx"][b], inputs["x"][b]], 0)).astype(np.float32) for b in range(N_CORES)]
    if FUSED:
        xs = _run([0, 1, 2, 3], inputs, xs)
    else:
        for l in range(4):
            xs = _run([l], inputs, xs)
    return np.stack([x[LC:] for x in xs], 0).astype(np.float32)
```

```python
import contextlib, math, os
import numpy as np
import concourse.bass as bass
import concourse.mybir as mybir
from concourse.bass_utils import run_bass_kernel_spmd

F32 = mybir.dt.float32
BF16 = mybir.dt.bfloat16
F32R = mybir.dt.float32r
ALU = mybir.AluOpType
AF = mybir.ActivationFunctionType
AX = mybir.AxisListType

EPOCH = 30000
NDMA = 24


class Reg:
    __slots__ = ("w", "r", "name")

    def __init__(self, name=""):
        self.w = {}
        self.r = {}
        self.name = name


class Eng:
    def __init__(self, kb, name, self_sync):
        self.kb, self.name, self.self_sync = kb, name, self_sync
        self.ops = []
        self.seen = {}
        self.sems = [kb.new_sem(f"{name}_e0")]
        self.count = 0

    def cur(self):
        return self.sems[-1]


class KB:
    def __init__(self, nc, stack):
        self.nc, self.stack = nc, stack
        self.semh = {}
        self.nsem = 0
        self.pe = Eng(self, "pe", False)
        self.dve = Eng(self, "dve", True)
        self.act = Eng(self, "act", True)
        self.pool = Eng(self, "pool", True)
        self.sp = Eng(self, "sp", False)
        self.engs = [self.pe, self.dve, self.act, self.pool, self.sp]
        self.dma_sems = [self.new_sem(f"dma{i}") for i in range(NDMA)]
        self.dma_tot = [0] * NDMA
        self.dma_i = 0
        self.n_ops = 0

    def new_sem(self, name):
        h = self.stack.enter_context(self.nc.semaphore(name))
        k = self.nsem
        self.nsem += 1
        self.semh[k] = h
        return k

    def _waits(self, E, reads, writes):
        need = {}
        for r in reads:
            for s, v in r.w.items():
                if need.get(s, 0) < v:
                    need[s] = v
        for w in writes:
            for d in (w.w, w.r):
                for s, v in d.items():
                    if need.get(s, 0) < v:
                        need[s] = v
        out = []
        for s, v in need.items():
            if (not E.self_sync) and s in E.sems:
                continue
            if E.seen.get(s, 0) < v:
                E.seen[s] = v
                out.append((s, v))
        return out

    def _mark(self, ev, reads, writes):
        s, v = ev
        for r in reads:
            r.r[s] = v
        for w in writes:
            w.w = {s: v}
            w.r = {}

    def op(self, E, fn, reads=(), writes=()):
        waits = self._waits(E, reads, writes)
        if E.count >= EPOCH:
            E.sems.append(self.new_sem(f"{E.name}_e{len(E.sems)}"))
            E.count = 0
        E.count += 1
        ev = (E.cur(), E.count)
        E.ops.append((waits, fn, ev[0], 1))
        self._mark(ev, reads, writes)
        self.n_ops += 1
        return ev

    def dma(self, Q, out, in_, reads=(), writes=(), **kw):
        i = self.dma_i
        self.dma_i = (i + 1) % NDMA
        s = self.dma_sems[i]
        waits = self._waits(Q, reads, writes)
        if self.dma_tot[i] > 0 and Q.seen.get(s, 0) < self.dma_tot[i]:
            Q.seen[s] = self.dma_tot[i]
            waits.append((s, self.dma_tot[i]))
        self.dma_tot[i] += 16
        ev = (s, self.dma_tot[i])
        Q.ops.append((waits, lambda e: e.dma_start(out=out, in_=in_, **kw), s, 16))
        self._mark(ev, reads, writes)
        self.n_ops += 1
        return ev

    def emit(self):
        nc = self.nc
        fin = [(self.dma_sems[i], self.dma_tot[i]) for i in range(NDMA) if self.dma_tot[i] > 0]
        semh = self.semh

        def run(E, e):
            for waits, fn, s, inc in E.ops:
                for ws, wv in waits:
                    e.wait_ge(semh[ws], wv)
                inst = fn(e)
                inst.then_inc(semh[s], inc)

        with nc.Block() as block:
            @block.tensor
            def _(e):
                run(self.pe, e)

            @block.vector
            def _(e):
                run(self.dve, e)

            @block.scalar
            def _(e):
                run(self.act, e)

            @block.gpsimd
            def _(e):
                run(self.pool, e)

            @block.sync
            def _(e):
                run(self.sp, e)
                for ws, wv in fin:
                    e.wait_ge(semh[ws], wv)


class _LazyW:
    def __init__(self, prog, shapes):
        self.p, self.shapes, self.c = prog, shapes, {}

    def __getitem__(self, k):
        if k not in self.c:
            self.c[k] = self.p.inp(k, self.shapes[k])
        return self.c[k]


T = 2304; LC = 256; NT = 18; D = 1024; KC = 8; CH = 32; NCH = 72; NG = 10
NTILES = [(0, 256), (256, 512), (768, 512), (1280, 512), (1792, 512)]
ALPHA = 8 ** 0.25
DFF = 2816; NFC = 22


def host_consts():
    c = {}
    c["ident"] = np.eye(128, dtype=np.float32)
    s = np.arange(128)[:, None]; t = np.arange(128)[None, :]
    same = (s // CH) == (t // CH)
    c["tri_le"] = (same & (s <= t)).astype(np.float32)
    c["tri_ge"] = (same & (s >= t)).astype(np.float32)
    tf = np.arange(T, dtype=np.float32)
    tb = np.concatenate([255.0 - np.arange(256), 256.0 + (2303.0 - np.arange(256, T))]).astype(np.float32)
    c["tauF"] = np.ascontiguousarray(np.broadcast_to(tf, (128, T))); c["tauB"] = np.ascontiguousarray(np.broadcast_to(tb, (128, T)))
    a_ = np.arange(128)[:, None]; b_ = np.arange(128)[None, :]
    mf = np.stack([(a_ < b_), (a_ <= b_), (b_ < a_)], 1).astype(np.float32)
    mb = np.stack([(a_ > b_), (a_ >= b_), (b_ > a_)], 1).astype(np.float32)
    c["rw_masks"] = np.ascontiguousarray(np.stack([mf, mb], 0))
    c["bdones"] = ((a_ // 64) == (b_ // 64)).astype(np.float32)
    c["halfm"] = (np.arange(128)[:, None] // 64 == np.arange(2)[None, :]).astype(np.float32)
    c["rowmask"] = (np.arange(128)[:, None] // CH == np.arange(4)[None, :]).astype(np.float32)
    kk = np.arange(128)[:, None]; qq = np.arange(128)[None, :]
    c["mprev4"] = (kk >= qq).astype(np.float32)
    c["mnext4"] = (kk <= qq).astype(np.float32)
    rm = np.ones((128, T), np.float32); rm[:, ::CH] = 0.0
    c["resetm"] = rm
    rows = 2048 // 64
    row = np.repeat(np.arange(rows, dtype=np.float32), 64); col = np.tile(np.arange(64, dtype=np.float32), rows)
    inv = (np.float32(10000.0) ** (-np.arange(16, dtype=np.float32) / np.float32(16))).astype(np.float32)
    ang = np.concatenate([row[:, None] * inv, col[:, None] * inv], axis=-1).astype(np.float32)
    c["cosF"] = np.ascontiguousarray(np.concatenate([np.cos(ang), np.cos(ang)], -1).T.astype(np.float32))
    c["sinF"] = np.ascontiguousarray(np.concatenate([np.sin(ang), np.sin(ang)], -1).T.astype(np.float32))
    pm = np.zeros((64, 64), np.float32)
    for r in range(32):
        pm[r + 32, r] = -1.0
        pm[r, r + 32] = 1.0
    pm2 = np.zeros((128, 128), np.float32); pm2[:64, :64] = pm; pm2[64:, 64:] = pm
    c["Pm2"] = pm2
    c["cosF"] = np.ascontiguousarray(np.concatenate([c["cosF"], c["cosF"]], 0))
    c["sinF"] = np.ascontiguousarray(np.concatenate([c["sinF"], c["sinF"]], 0))
    return c


class Buf:
    def __init__(self, t, name):
        self.t = t; self.r = Reg(name)

    def __getitem__(self, i):
        return self.t[i]


class Prog:
    def __init__(self, layers, taps=(), stop=99):
        self.layers = layers; self.taps = set(taps); self.stop = stop
        self.nc = bass.Bass("TRN2", target_bir_lowering=False)
        self.st = contextlib.ExitStack()
        self.kb = KB(self.nc, self.st)
        self.din = {}; self.dout = {}
        self.psi = 0

    def inp(self, name, shape, dt=F32):
        a = self.nc.dram_tensor(name, list(shape), dt, kind="ExternalInput").ap()
        self.din[name] = a
        return a

    def outp(self, name, shape, dt=F32):
        a = self.nc.dram_tensor(name, list(shape), dt, kind="ExternalOutput").ap()
        self.dout[name] = a
        return a

    def scratch(self, name, shape, dt=F32):
        if name in self.taps:
            return Buf(self.outp(name, shape, dt), name)
        return Buf(self.nc.dram_tensor(name, list(shape), dt, kind="Internal").ap(), name)

    def sb(self, name, shape, dt=F32):
        return Buf(self.st.enter_context(self.nc.sbuf_tensor(name, list(shape), dt)), name)

    def next_ps(self):
        b = self.ps[self.psi]; self.psi = (self.psi + 1) % 8
        return b

    def _rw(self, R, W):
        return [b.r for b in R], [b.r for b in W]

    def MM(self, out, lhsT, rhs, R, W, start=True, stop=True):
        r, w = self._rw(R, W)
        self.kb.op(self.kb.pe, lambda e: e.matmul(out, lhsT=lhsT, rhs=rhs, start=start, stop=stop), r, w)

    def TR(self, out, in_, R, W, n=128):
        r, w = self._rw(R + [self.ident], W)
        idn = self.ident[0:n, 0:n]
        self.kb.op(self.kb.pe, lambda e: e.transpose(out=out, in_=in_, identity=idn), r, w)

    def ACT(self, out, in_, func, R, W, bias=None, scale=None):
        r, w = self._rw(R, W)
        kw = {}
        if bias is not None: kw["bias"] = bias
        if scale is not None: kw["scale"] = scale
        self.kb.op(self.kb.act, lambda e: e.activation(out=out, in_=in_, func=func, **kw), r, w)

    def TT(self, out, a, b, op, R, W, eng=None):
        r, w = self._rw(R, W)
        self.kb.op(eng or self.kb.dve, lambda e: e.tensor_tensor(out=out, in0=a, in1=b, op=op), r, w)

    def TS(self, out, a, s1, op0, R, W, s2=None, op1=None, eng=None):
        r, w = self._rw(R, W)
        if op1 is None:
            self.kb.op(eng or self.kb.dve, lambda e: e.tensor_scalar(out=out, in0=a, scalar1=s1, scalar2=None, op0=op0), r, w)
        else:
            self.kb.op(eng or self.kb.dve, lambda e: e.tensor_scalar(out=out, in0=a, scalar1=s1, scalar2=s2, op0=op0, op1=op1), r, w)

    def STT(self, out, a, s, b, op0, op1, R, W):
        r, w = self._rw(R, W)
        self.kb.op(self.kb.dve, lambda e: e.scalar_tensor_tensor(out=out, in0=a, scalar=s, in1=b, op0=op0, op1=op1), r, w)

    def CP(self, out, in_, R, W, eng=None):
        r, w = self._rw(R, W)
        E = eng or self.kb.dve
        if E is self.kb.act:
            self.kb.op(E, lambda e: e.copy(out=out, in_=in_), r, w)
        else:
            self.kb.op(E, lambda e: e.tensor_copy(out=out, in_=in_), r, w)

    def MSET(self, ap, val, W, eng=None):
        r, w = self._rw([], W)
        self.kb.op(eng or self.kb.pool, lambda e: e.memset(ap, val), r, w)

    def RECIP(self, out, in_, R, W):
        r, w = self._rw(R, W)
        self.kb.op(self.kb.dve, lambda e: e.reciprocal(out=out, in_=in_), r, w)

    def SCAN(self, out, d0, d1, R, W, init=0.0):
        r, w = self._rw(R, W)
        self.kb.op(self.kb.dve, lambda e: e.tensor_tensor_scan(out=out, data0=d0, data1=d1, initial=init, op0=ALU.mult, op1=ALU.add), r, w)

    def DMA(self, out, in_, R, W, q=None, **kw):
        r, w = self._rw(R, W)
        self.kb.dma(q or self.kb.sp, out, in_, r, w, **kw)

    def tap(self, name, src_ap, R, shape):
        if name in self.taps:
            o = self.outp("tap_" + name, shape)
            self.DMA(o, src_ap, R, [])

    def setup(self):
        P = self
        nc = self.nc
        P.xin = Buf(P.inp("xin", [T, D]), "xin")
        P.condT = P.inp("condT", [128, 8, 2])
        hc = host_consts()
        P.cd = {k: Buf(P.inp("c_" + k, v.shape), "c_" + k) for k, v in hc.items()}
        I = lambda name, shape: (name, shape)
        wdecl = dict(
            ada_w=I("ada_w", [4, D, 6 * D]), ada_b_fm=I("ada_b_fm", [4, 128, 48]),
            ln_g=I("ln_g", [4, 2, D]), ln_b=I("ln_b", [4, 2, D]),
            ev_w_in=I("ev_w_in", [2, D, 3328]), ev_w_out=I("ev_w_out", [2, D, D]),
            hg_lb_fm=I("hg_lb_fm", [128, 4, 2]), hg_ng_fm=I("hg_ng_fm", [2, 128, 4]), attn_sink=I("attn_sink", [2, 8]),
            od_w_in=I("od_w_in", [2, D, 2432]), od_w_out=I("od_w_out", [2, D, D]),
            s5_lre=I("s5_lre", [2, 2, 128, 16]), s5_lim=I("s5_lim", [2, 2, 128, 16]), s5_ldt=I("s5_ldt", [2, 2, 128, 16]),
            s5_bT=I("s5_bT", [2, 128, 16, 2, 16]), s5_cT=I("s5_cT", [2, 128, 16, 2, 16]), s5_d_fm=I("s5_d_fm", [2, 128, 4]),
            s5_glu_w=I("s5_glu_w", [2, 512, 512]),
            rw_mu_fm=I("rw_mu_fm", [2, 128, 15]), rw_w0_fm=I("rw_w0_fm", [2, 2, 128, 4]), rw_a0_fm=I("rw_a0_fm", [2, 2, 128, 4]),
            rw_w2pad=I("rw_w2pad", [2, 2, 128, 512]), rw_a2pad=I("rw_a2pad", [2, 2, 128, 512]), rw_g2=I("rw_g2", [2, 128, 512]),
            rw_kk_fm=I("rw_kk_fm", [2, 128, 4]), rw_ka_fm=I("rw_ka_fm", [2, 128, 4]), rw_rk_fm=I("rw_rk_fm", [2, 128, 4]),
            rw_lng=I("rw_lng", [2, 512]), rw_lnb=I("rw_lnb", [2, 512]),
            moe_router_w=I("moe_router_w", [2, D, 8]), moe_router_b=I("moe_router_b", [2, 8]),
            moe_w_gate=I("moe_w_gate", [2, 8, D, DFF]), moe_w_up=I("moe_w_up", [2, 8, D, DFF]), moe_w_down=I("moe_w_down", [2, 8, DFF, D]),
            ffn_w_gate=I("ffn_w_gate", [2, D, DFF]), ffn_w_up=I("ffn_w_up", [2, D, DFF]), ffn_w_down=I("ffn_w_down", [2, DFF, D]),
        )
        P.w = _LazyW(P, {k: v[1] for k, v in wdecl.items()})
        P.out = Buf(P.outp("out", [T, D]), "out")
        P.xs = [P.scratch("xs0", [T, D]), P.scratch("xs1", [T, D])]
        P.rwscr = P.scratch("rwscr", [3, 128, T])
        if "inject_y" in P.taps:
            P.yT = Buf(P.inp("yT_inject", [D, T]), "yT_inject")
        else:
            P.yT = P.scratch("yTd", [D, T])
        P.ident = P.sb("ident", [128, 128]); P.ones = P.sb("ones", [128, 128]); P.onesdiv = P.sb("onesdiv", [128, 128])
        P.tri_le = P.sb("tri_le", [128, 128]); P.tri_ge = P.sb("tri_ge", [128, 128])
        P.mprev4 = P.sb("mprev4", [128, 128]); P.mnext4 = P.sb("mnext4", [128, 128])
        P.cst = P.sb("cst", [128, 8])
        P.hT = P.sb("hT", [128, KC, T], BF16)
        P.G = [None] * NG
        P.Gt = self.st.enter_context(nc.sbuf_tensor("G", [128, NG, T], F32))
        for i in range(NG):
            P.G[i] = Buf(P.Gt[:, i, :], f"G{i}")
        P.wA = [P.sb(f"wA{i}", [128, KC, 128], BF16) for i in range(5)]
        P.wAi = 0
        P.xb = [P.sb(f"xb{i}", [128, D]) for i in range(3)]
        P.xbi = 0
        P.zb = [P.sb("zb0", [128, D])] * 2
        P.bc = [P.sb(f"bc{i}", [128, D]) for i in range(4)]
        P.condS = P.sb("condS", [128, 8, 2]); P.adab = P.sb("adab", [128, 48])
        P.fm = P.sb("fm", [128, 48, 2]); P.sc1p = P.sb("sc1p", [128, 8, 2]); P.sc2p = P.sb("sc2p", [128, 8, 2])
        P.small = [P.sb(f"small{i}", [128, 512]) for i in range(5)]
        P.smi = 0
        P.S = P.sb("S", [128, 128]); P.tmpS = P.sb("tmpS", [128, 128])
        P.stat = P.sb("stat", [128, 32])
        P.s5t = P.sb("s5t", [128, 2, 12, 16]); P.s5i = P.sb("s5i", [128, 16], mybir.dt.int32); P.s5d = P.sb("s5d", [128, 4])
        P.s5bc = P.sb("s5bc", [128, 64]); P.s5zc = P.sb("s5zc", [128, 256])
        P.rwm = P.sb("rwm", [128, 3, 128]); P.bdones = P.sb("bdones", [128, 128])
        P.rwar = [P.sb(f"rwar{h}", [128, 256]) for h in range(2)]
        P.rwxm = [P.sb(f"rwxm{h}", [128, 4, 128]) for h in range(2)]
        P.rwxz = [P.sb(f"rwxz{h}", [128, 3, 128]) for h in range(2)]
        P.rwu = P.sb("rwu", [128, 128]); P.rwp = P.sb("rwp", [128, 48]); P.rwgc = P.sb("rwgc", [128, NT]); P.rwln = P.sb("rwln", [128, 2, 128])
        P.gates = P.sb("gates", [128, NT, 8]); P.rw = P.sb("rw", [128, KC, 8]); P.rb = P.sb("rb", [128, 8])
        P.h32 = [P.sb("h32_0", [128, 4, 128])] * 2; P.rt = P.sb("rt", [128, 64])
        P.lbt = P.sb("lbt", [128, 16]); P.ngt = P.sb("ngt", [128, 4]); P.sk = P.sb("sk", [128, 8])
        P.Pm2 = P.sb("Pm2", [128, 128])
        P.attm = [P.sb(f"attm{i}", [128, 128]) for i in range(2)]; P.attmi = 0
        P.ktok = [P.sb(f"ktok{i}", [128, 4, 128]) for i in range(2)]
        P.rowmask = P.sb("rowmask", [128, 4]); P.halfm = P.sb("halfm", [128, 2])
        P.ps = [Buf(self.st.enter_context(nc.psum_tensor(f"ps{i}", [128, 512], F32)), f"ps{i}") for i in range(8)]
        for k, dst in (("ident", P.ident), ("tri_le", P.tri_le), ("tri_ge", P.tri_ge), ("mprev4", P.mprev4),
                       ("mnext4", P.mnext4), ("Pm2", P.Pm2), ("rowmask", P.rowmask), ("halfm", P.halfm), ("bdones", P.bdones)):
            P.DMA(dst[:], P.cd[k][:], [], [dst])
        P.MSET(P.ones[:], 1.0, [P.ones]); P.MSET(P.onesdiv[:], 1.0 / 128.0, [P.onesdiv])
        P.MSET(P.cst[:, 0:1], 1e-6, [P.cst]); P.MSET(P.cst[:, 1:2], 1e-5, [P.cst]); P.MSET(P.cst[:, 2:3], 64e-5, [P.cst])
        P.MSET(P.cst[:, 3:4], 0.0, [P.cst]); P.MSET(P.cst[:, 4:5], 1.0, [P.cst])
        P.DMA(P.condS[:], P.condT, [], [P.condS])
        P.ACT(P.condS[:], P.condS[:], AF.Silu, [P.condS], [P.condS])

    def gflat(self, slot0, nelem, dt=F32):
        flat = self.Gt[:].rearrange("p a n -> p (a n)")[:, slot0 * T:slot0 * T + nelem]
        ns = (nelem + T - 1) // T
        regs = [self.G[slot0 + i] for i in range(ns)]
        if dt is not F32:
            flat = flat.bitcast(dt)
        return flat, regs

    def sm(self):
        b = self.small[self.smi]; self.smi = (self.smi + 1) % len(self.small)
        return b

    def loadw(self, src, ncols, kc=KC):
        b = self.wA[self.wAi]; self.wAi = (self.wAi + 1) % len(self.wA)
        self.DMA(b[:, 0:kc, 0:ncols], src.rearrange("(c p) n -> p c n", p=128), [], [b], q=self.kb.pool)
        return b

    def phase_mod(self, l):
        P = self
        P.DMA(P.adab[:], P.w["ada_b_fm"][l], [], [P.adab])
        pM = P.next_ps()
        for blk in range(12):
            fl, regs = P.gflat((blk % 2) * 2, 4096)
            stg = fl.rearrange("p (c n) -> p c n", n=512)
            for ch in range(8):
                P.DMA(stg[:, ch, :], P.w["ada_w"][l, ch * 128:(ch + 1) * 128, blk * 512:(blk + 1) * 512], [], regs,
                      q=(P.kb.sp if ch % 2 == 0 else P.kb.act))
            for s in range(4):
                k = blk * 4 + s
                for ch in range(8):
                    P.MM(pM[:, 2 * k:2 * k + 2], stg[:, ch, s * 128:(s + 1) * 128], P.condS[:, ch, :], regs + [P.condS], [pM],
                         start=(ch == 0), stop=(ch == 7))
        for cond in range(2):
            P.TT(P.fm[:, :, cond], pM[:, cond:96:2], P.adab[:], ALU.add, [pM, P.adab], [P.fm])
        P.TS(P.sc1p[:], P.fm[:, 8:16, :], 1.0, ALU.add, [P.fm], [P.sc1p])
        P.TS(P.sc2p[:], P.fm[:, 32:40, :], 1.0, ALU.add, [P.fm], [P.sc2p])

    def gate_bcast(self, q, cond, dst):
        P = self
        for half in range(2):
            pg = P.next_ps()
            for cc in range(4):
                c = half * 4 + cc
                dg = P.sm()
                P.TS(dg[:, 0:128], P.ident[:], P.fm[:, q * 8 + c, cond:cond + 1], ALU.mult, [P.ident, P.fm], [dg])
                P.MM(pg[:, cc * 128:(cc + 1) * 128], P.ones[:], dg[:, 0:128], [P.ones, dg], [pg])
            P.CP(dst[:, half * 512:(half + 1) * 512], pg[:, :], [pg], [dst], eng=P.kb.act)

    def hT_tile(self, xt, ti, scp, shq, router=False):
        P = self
        cond = 1 if ti < 2 else 0
        pl = P.next_ps() if router else None
        for half in range(2):
            pt = P.next_ps()
            for cc in range(4):
                c = half * 4 + cc
                P.TR(pt[:, cc * 128:(cc + 1) * 128], xt[:, c * 128:(c + 1) * 128], [xt], [pt])
            h32 = P.h32[half]
            for cc in range(4):
                c = half * 4 + cc
                if router:
                    P.TS(h32[:, cc, :], pt[:, cc * 128:(cc + 1) * 128], scp[:, c, cond:cond + 1], ALU.mult, [pt, scp, P.fm], [h32],
                         s2=P.fm[:, shq * 8 + c, cond:cond + 1], op1=ALU.add)
                    P.CP(P.hT[:, c, ti * 128:(ti + 1) * 128], h32[:, cc, :], [h32], [P.hT], eng=P.kb.act)
                else:
                    P.ACT(P.hT[:, c, ti * 128:(ti + 1) * 128], pt[:, cc * 128:(cc + 1) * 128], AF.Identity,
                          [pt, scp, P.fm], [P.hT], scale=scp[:, c, cond:cond + 1], bias=P.fm[:, shq * 8 + c, cond:cond + 1])
            if router:
                for cc in range(4):
                    c = half * 4 + cc
                    P.MM(pl[:, half * 8:half * 8 + 8], h32[:, cc, :], P.rw[:, c, :], [h32, P.rw], [pl], start=(cc == 0), stop=(cc == 3))
        if router and "1" != "2":
            P.top2(pl, ti)

    def top2(self, pl, ti):
        P = self
        rt = P.rt
        R_, W_ = [rt], [rt]
        lg, e1, l2, e2 = rt[:, 0:8], rt[:, 8:16], rt[:, 16:24], rt[:, 24:32]
        m1, m2, dd, p1, p2 = rt[:, 32:33], rt[:, 33:34], rt[:, 34:35], rt[:, 35:36], rt[:, 36:37]
        P.TT(lg, pl[:, 0:8], P.rb[:], ALU.add, [pl, P.rb], W_)
        P.TT(lg, lg, pl[:, 8:16], ALU.add, [pl, rt], W_)
        P.kb.op(P.kb.dve, lambda e: e.reduce_max(out=m1, in_=lg, axis=AX.X), [rt.r], [rt.r])
        P.TS(e1, lg, m1, ALU.is_equal, R_, W_)
        P.STT(l2, e1, -1e30, lg, ALU.mult, ALU.add, R_, W_)
        P.kb.op(P.kb.dve, lambda e: e.reduce_max(out=m2, in_=l2, axis=AX.X), [rt.r], [rt.r])
        P.TS(e2, l2, m2, ALU.is_equal, R_, W_)
        P.TT(dd, m2, m1, ALU.subtract, R_, W_)
        P.ACT(dd, dd, AF.Exp, R_, W_)
        P.TS(p1, dd, 1.0, ALU.add, R_, W_)
        P.RECIP(p1, p1, R_, W_)
        P.TT(p2, dd, p1, ALU.mult, R_, W_)
        P.TS(e1, e1, p1, ALU.mult, R_, W_)
        P.STT(P.gates[:, ti, :], e2, p2, e1, ALU.mult, ALU.add, R_, [P.gates])

    def phase_hT(self, xsrc):
        P = self
        for ti in range(NT):
            xt = P.xb[P.xbi]; P.xbi = (P.xbi + 1) % 3
            P.DMA(xt[:], xsrc[ti * 128:(ti + 1) * 128, :], [xsrc], [xt])
            P.hT_tile(xt, ti, P.sc1p, 0)

    def proj_fm(self, wb, M, evac, col0=0):
        P = self
        for (t0, n) in NTILES:
            pb = P.next_ps()
            for c in range(KC):
                P.MM(pb[0:M, 0:n], wb[:, c, col0:col0 + M], P.hT[:, c, t0:t0 + n], [wb, P.hT], [pb], start=(c == 0), stop=(c == 7))
            evac(pb, t0, n)

    def hgrn2(self, l, j):
        P = self
        G = P.G
        W = P.w["ev_w_in"][j]
        lbt = P.lbt; ngt = P.ngt
        P.DMA(ngt[:, 0:4], P.w["hg_ng_fm"][j], [], [ngt])
        if j == 0:
            P.MSET(lbt[:, 0:4], 0.0, [lbt]); P.MSET(lbt[:, 4:8], 1.0, [lbt])
        else:
            P.DMA(lbt[:, 8:16], P.w["hg_lb_fm"].rearrange("p h j -> p (h j)"), [], [lbt])
            P.TT(lbt[:, 0:4], lbt[:, 9:16:2], lbt[:, 8:16:2], ALU.subtract, [lbt], [lbt])
            P.ACT(lbt[:, 0:4], lbt[:, 0:4], AF.Sigmoid, [lbt], [lbt])
            P.TS(lbt[:, 4:8], lbt[:, 0:4], -1.0, ALU.mult, [lbt], [lbt], s2=1.0, op1=ALU.add)
        qT, sgT, X1, X2, X3, X4, oacc = G[0], G[1], G[2], G[3], G[4], G[5], G[8]
        P.resetm = G[9]
        P.DMA(P.resetm[:], P.cd["resetm"][:], [], [P.resetm])
        itok = P.Gt[:, 6, :].rearrange("p (b c) -> p b c", c=128)
        itr = [G[6]]
        for hd in range(4):
            cs = lambda k: W[:, k * 512 + hd * 128:k * 512 + (hd + 1) * 128]
            wq = P.loadw(cs(0), 128)
            P.proj_fm(wq, 128, lambda pb, t0, n: P.ACT(qT[:, t0:t0 + n], pb[:, 0:n], AF.Silu, [pb], [qT]))
            wg = P.loadw(cs(4), 128)
            P.proj_fm(wg, 128, lambda pb, t0, n: P.ACT(sgT[:, t0:t0 + n], pb[:, 0:n], AF.Silu, [pb], [sgT]))
            wi = P.loadw(cs(3), 128)
            for b0 in range(0, NT, 4):
                nb = min(4, NT - b0)
                pi = P.next_ps()
                for q in range(nb):
                    ti = b0 + q
                    for c in range(KC):
                        P.MM(pi[:, q * 128:(q + 1) * 128], P.hT[:, c, ti * 128:(ti + 1) * 128], wi[:, c, :], [P.hT, wi], [pi],
                             start=(c == 0), stop=(c == 7))
                P.CP(itok[:, b0:b0 + nb, :], pi[:, 0:nb * 128].rearrange("p (a b) -> p a b", b=128), [pi], itr, eng=P.kb.act)
            for d in range(2):
                fwd = d == 0
                wf = P.loadw(cs(1 + d), 128)
                P.proj_fm(wf, 128, lambda pb, t0, n: P.ACT(X1[:, t0:t0 + n], pb[:, 0:n], AF.Sigmoid, [pb], [X1]))
                P.TS(X1[:], X1[:], lbt[:, 4 + hd:5 + hd], ALU.mult, [X1, lbt], [X1], s2=lbt[:, hd:hd + 1], op1=ALU.add)
                P.TS(X2[:], X1[:], -1.0, ALU.mult, [X1], [X2], s2=1.0, op1=ALU.add)
                P.ACT(X1[:], X1[:], AF.Ln, [X1], [X1])
                if fwd:
                    P.SCAN(X3[:], P.resetm[:], X1[:], [P.resetm, X1], [X3])
                else:
                    P.SCAN(X3[:, ::-1], P.resetm[:], X1[:, ::-1], [P.resetm, X1], [X3])
                P.ACT(X1[:], X3[:], AF.Exp, [X3], [X1])
                P.STT(X4[:], qT[:], 128.0 ** -0.5, X1[:], ALU.mult, ALU.mult, [qT, X1], [X4])
                P.ACT(X3[:], X3[:], AF.Exp, [X3], [X3], scale=-1.0)
                P.TT(X2[:], X2[:], X3[:], ALU.mult, [X2, X3], [X2])
                ring = [P.S, P.rwu, P.rwar[0], P.rwar[1], P.s5zc]
                rpos = [0]
                P.MSET(ring[0][:, 0:128], 0.0, [ring[0]])
                tiles = list(range(NT)) if fwd else [1, 0] + list(range(NT - 1, 1, -1))
                mask = P.tri_le if fwd else P.tri_ge
                for ti in tiles:
                    tsl = slice(ti * 128, (ti + 1) * 128)
                    pA = P.next_ps()
                    P.MM(pA[:, 0:128], X2[:, tsl], X4[:, tsl], [X2, X4], [pA])
                    attm = P.attm[P.attmi]; ktok = P.ktok[P.attmi]; P.attmi ^= 1
                    P.TT(attm[:], pA[:, 0:128], mask[:], ALU.mult, [pA, mask], [attm])
                    pB = P.next_ps()
                    P.TR(pB[:, 0:128], X2[:, tsl], [X2], [pB])
                    for q in range(4):
                        P.ACT(ktok[:, q, :], pB[:, 0:128], AF.Identity, [pB, P.rowmask], [ktok], scale=P.rowmask[:, q:q + 1])
                    pC = P.next_ps()
                    P.MM(pC[:, 0:128], itok[:, ti, :], attm[:], itr + [attm], [pC], start=True, stop=False)
                    qorder = list(range(4) if fwd else range(3, -1, -1))
                    pDs = []
                    for q in qorder:
                        pD = P.next_ps()
                        P.MM(pD[:, 0:128], ktok[:, q, :], itok[:, ti, :], [ktok] + itr, [pD])
                        pDs.append(pD)
                    kvb = P.rwxm[P.attmi]
                    lasts = []
                    for n_, q in enumerate(qorder):
                        c0 = ti * 128 + q * CH
                        last = c0 + CH - 1 if fwd else c0
                        lasts.append(last)
                        P.ACT(kvb[:, n_, :], pDs[n_][:, 0:128], AF.Identity, [pDs[n_], X1], [kvb], scale=X1[:, last:last + 1])
                    Ss = []
                    for n_, q in enumerate(qorder):
                        Sin = ring[rpos[0] % len(ring)]; Sout = ring[(rpos[0] + 1) % len(ring)]
                        Ss.append(Sin)
                        P.STT(Sout[:, 0:128], Sin[:, 0:128], X1[:, lasts[n_]:lasts[n_] + 1], kvb[:, n_, :], ALU.mult, ALU.add, [Sin, X1, kvb], [Sout])
                        rpos[0] += 1
                    for n_, q in enumerate(qorder):
                        c0 = ti * 128 + q * CH
                        P.MM(pC[:, q * CH:(q + 1) * CH], Ss[n_][:, 0:128], X4[:, c0:c0 + CH], [Ss[n_], X4], [pC], start=False, stop=True)
                    if fwd:
                        P.CP(oacc[:, tsl], pC[:, 0:128], [pC], [oacc], eng=P.kb.act)
                    else:
                        P.TT(oacc[:, tsl], oacc[:, tsl], pC[:, 0:128], ALU.add, [oacc, pC], [oacc])
            for (t0, n) in NTILES:
                sq = P.sm()
                P.ACT(sq[:, 0:n], oacc[:, t0:t0 + n], AF.Square, [oacc], [sq])
                pE = P.next_ps()
                P.MM(pE[:, 0:n], P.onesdiv[:], sq[:, 0:n], [P.onesdiv, sq], [pE])
                rs = P.sm()
                P.ACT(rs[:, 0:n], pE[:, 0:n], AF.Sqrt, [pE, P.cst], [rs], bias=P.cst[:, 0:1])
                P.RECIP(rs[:, 0:n], rs[:, 0:n], [rs], [rs])
                yo = P.sm()
                P.STT(yo[:, 0:n], oacc[:, t0:t0 + n], ngt[:, hd:hd + 1], rs[:, 0:n], ALU.mult, ALU.mult, [oacc, ngt, rs], [yo])
                P.TT(yo[:, 0:n], yo[:, 0:n], sgT[:, t0:t0 + n], ALU.mult, [yo, sgT], [yo])
                P.DMA(P.yT[hd * 128:(hd + 1) * 128, t0:t0 + n], yo[:, 0:n], [yo], [P.yT])

    def attention(self, l, j):
        P = self
        G = P.G
        W = P.w["ev_w_in"][j]
        sk = P.sk
        P.DMA(sk[:, 0:8], P.w["attn_sink"][j].partition_broadcast(128), [], [sk])
        P.ACT(sk[:, 0:8], sk[:, 0:8], AF.Exp, [sk], [sk])
        cosT, sinT, kT2, q2 = G[0], G[1], G[2], G[3]
        qm = [G[4], G[5], G[6], G[7]]
        ptb = [(P.Gt[:, 8, i * 512:(i + 1) * 512], G[8]) for i in range(4)] + [(P.Gt[:, 9, 0:512], G[9])]
        vaug = P.Gt[:, 9, 512:512 + NT * 65].rearrange("p (t e) -> p t e", e=65)
        vreg = [G[9]]
        P.DMA(cosT[:, 0:2048], P.cd["cosF"][:], [], [cosT]); P.DMA(sinT[:, 0:2048], P.cd["sinF"][:], [], [sinT])

        def rope(src):
            for (t0, n) in NTILES[1:]:
                pr = P.next_ps()
                P.MM(pr[:, 0:n], P.Pm2[:], src[:, t0:t0 + n], [P.Pm2, src], [pr])
                t1 = P.sm(); t2 = P.sm()
                P.TT(t1[:, 0:n], src[:, t0:t0 + n], cosT[:, t0 - LC:t0 - LC + n], ALU.mult, [src, cosT], [t1])
                P.TT(t2[:, 0:n], pr[:, 0:n], sinT[:, t0 - LC:t0 - LC + n], ALU.mult, [pr, sinT], [t2])
                P.TT(src[:, t0:t0 + n], t1[:, 0:n], t2[:, 0:n], ALU.add, [t1, t2], [src])

        for g in range(2):
            wv = P.loadw(W[:, 3200 + g * 64:3200 + (g + 1) * 64], 64)
            P.MSET(vaug[:, :, 64:65], 1.0, vreg)
            for ti in range(NT):
                pv = P.next_ps()
                for c in range(KC):
                    P.MM(pv[:, 0:64], P.hT[:, c, ti * 128:(ti + 1) * 128], wv[:, c, 0:64], [P.hT, wv], [pv], start=(c == 0), stop=(c == 7))
                P.CP(vaug[:, ti, 0:64], pv[:, 0:64], [pv], vreg, eng=P.kb.act)
            wk = P.wA[P.wAi]; P.wAi = (P.wAi + 1) % len(P.wA)
            ksrc = W[:, 3072 + g * 64:3072 + (g + 1) * 64].rearrange("(c p) n -> p c n", p=128)
            P.DMA(wk[:, :, 0:64], ksrc, [], [wk], q=P.kb.pool)
            P.DMA(wk[:, :, 64:128], ksrc, [], [wk], q=P.kb.pool)
            P.proj_fm(wk, 128, lambda pb, t0, n: P.CP(kT2[:, t0:t0 + n], pb[:, 0:n], [pb], [kT2], eng=P.kb.act))
            rope(kT2)
            for pair in range(2):
                h0 = g * 4 + pair * 2
                wq = P.loadw(W[:, 2560 + h0 * 64:2560 + (h0 + 2) * 64], 128)
                P.proj_fm(wq, 128, lambda pb, t0, n: P.CP(q2[:, t0:t0 + n], pb[:, 0:n], [pb], [q2], eng=P.kb.act))
                rope(q2)
                for half in range(2):
                    dst = qm[pair * 2 + half]
                    P.TS(dst[:], q2[:], P.halfm[:, half:half + 1], ALU.mult, [q2, P.halfm], [dst])
            for ti in range(NT):
                if ti < 2:
                    keys = [(0, 'c'), (1, 'c')]
                else:
                    keys = [(kt, kd) for kt, kd in ((ti - 1, 'p'), (ti, 's'), (ti + 1, 'n')) if 2 <= kt < NT] + [(0, 'c'), (1, 'c')]
                pts = []
                for ki, (kt, kd) in enumerate(keys):
                    pS = P.next_ps()
                    for hh in range(4):
                        P.MM(pS[:, hh * 128:(hh + 1) * 128], kT2[:, kt * 128:(kt + 1) * 128], qm[hh][:, ti * 128:(ti + 1) * 128],
                             [kT2, qm[hh]], [pS])
                    pt, pr_ = ptb[ki]
                    P.ACT(pt, pS[:, 0:512], AF.Exp, [pS], [pr_], scale=0.125)
                    if kd in ('p', 'n'):
                        mk_ = P.mprev4 if kd == 'p' else P.mnext4
                        for hh in range(4):
                            P.TT(pt[:, hh * 128:(hh + 1) * 128], pt[:, hh * 128:(hh + 1) * 128], mk_[:], ALU.mult, [pr_, mk_], [pr_],
                                 eng=(P.kb.pool if hh % 2 else P.kb.dve))
                    pts.append((pt, pr_, kt))
                pO = P.next_ps()
                for hh in range(4):
                    for ki, (pt, pr_, kt) in enumerate(pts):
                        P.MM(pO[:, hh * 65:(hh + 1) * 65], pt[:, hh * 128:(hh + 1) * 128], vaug[:, kt, :], [pr_] + vreg, [pO],
                             start=(ki == 0), stop=(ki == len(pts) - 1))
                den = P.sm()
                P.TT(den[:, 0:4], pO[:, 64:260:65], sk[:, g * 4:(g + 1) * 4], ALU.add, [pO, sk], [den])
                P.RECIP(den[:, 0:4], den[:, 0:4], [den], [den])
                ob = P.sm()
                for hh in range(4):
                    P.TS(ob[:, hh * 64:(hh + 1) * 64], pO[:, hh * 65:hh * 65 + 64], den[:, hh:hh + 1], ALU.mult, [pO, den], [ob])
                for half in range(2):
                    pT = P.next_ps()
                    P.TR(pT[:, 0:128], ob[:, half * 128:(half + 1) * 128], [ob], [pT])
                    yo = P.sm()
                    P.CP(yo[:, 0:128], pT[:, 0:128], [pT], [yo], eng=P.kb.act)
                    r0 = 512 + g * 256 + half * 128
                    P.DMA(P.yT[r0:r0 + 128, ti * 128:(ti + 1) * 128], yo[:, 0:128], [yo], [P.yT])

    def resid_ln(self, z, xt, ti, xn):
        P = self
        P.STT(z[:], xt[:], ALPHA, z[:], ALU.mult, ALU.add, [xt, z], [z])
        st = P.stat
        for hf in range(2):
            P.kb.op(P.kb.dve, lambda e, hf=hf: e.bn_stats(out=st[:, hf * 6:(hf + 1) * 6], in_=z[:, hf * 512:(hf + 1) * 512]), [z.r], [st.r])
        P.kb.op(P.kb.dve, lambda e: e.bn_aggr(out=st[:, 12:14], in_=st[:, 0:12]), [st.r], [st.r])
        P.ACT(st[:, 14:15], st[:, 13:14], AF.Sqrt, [st, P.cst], [st], bias=P.cst[:, 1:2])
        P.RECIP(st[:, 14:15], st[:, 14:15], [st], [st])
        P.TS(z[:], z[:], st[:, 12:13], ALU.subtract, [z, st], [z], s2=st[:, 14:15], op1=ALU.mult)
        P.TT(z[:], z[:], P.bc[2][:], ALU.mult, [z, P.bc[2]], [z])
        P.TT(xn[:], z[:], P.bc[3][:], ALU.add, [z, P.bc[3]], [xn])

    def load_ln(self, l, k):
        P = self
        P.DMA(P.bc[2][:], P.w["ln_g"][l, k].partition_broadcast(128), [], [P.bc[2]])
        P.DMA(P.bc[3][:], P.w["ln_b"][l, k].partition_broadcast(128), [], [P.bc[3]])

    def phase_out(self, l, wout_dram, xsrc, xdst, router_j=None):
        P = self
        P.gate_bcast(2, 0, P.bc[0]); P.gate_bcast(2, 1, P.bc[1]); P.load_ln(l, 0)
        if router_j is not None:
            P.DMA(P.rw[:], P.w["moe_router_w"][router_j].rearrange("(c p) n -> p c n", p=128), [], [P.rw])
            P.DMA(P.rb[:], P.w["moe_router_b"][router_j].partition_broadcast(128), [], [P.rb])
        wof, wor = P.gflat(0, 4096, BF16)
        wo = wof.rearrange("p (c n) -> p c n", n=1024)
        P.DMA(wo, wout_dram.rearrange("(c p) n -> p c n", p=128), [], wor, q=P.kb.pool)
        ytb = [P.gflat(2 + i, 512, BF16)[0].rearrange("p (c n) -> p c n", n=128) for i in range(2)]
        for ti in range(NT):
            cond = 1 if ti < 2 else 0
            yt, yr = ytb[ti % 2], P.G[2 + ti % 2]
            P.DMA(yt, P.yT[:, ti * 128:(ti + 1) * 128].rearrange("(c p) n -> p c n", p=128), [P.yT], [yr], q=P.kb.pool)
            xt = P.xb[P.xbi]; P.xbi = (P.xbi + 1) % 3
            P.DMA(xt[:], xsrc[ti * 128:(ti + 1) * 128, :], [xsrc], [xt])
            z = P.zb[ti % 2]
            for hf in range(2):
                po = P.next_ps()
                for c in range(KC):
                    P.MM(po[:, :], yt[:, c, :], wo[:, c, hf * 512:(hf + 1) * 512], [yr] + wor, [po], start=(c == 0), stop=(c == 7))
                P.TT(z[:, hf * 512:(hf + 1) * 512], po[:, :], P.bc[cond][:, hf * 512:(hf + 1) * 512], ALU.mult, [po, P.bc[cond]], [z])
            xn = P.xb[P.xbi]; P.xbi = (P.xbi + 1) % 3
            P.resid_ln(z, xt, ti, xn)
            P.DMA(xdst[ti * 128:(ti + 1) * 128, :], xn[:], [xn], [xdst])
            P.hT_tile(xn, ti, P.sc2p, 3, router=(router_j is not None))

    def ffn_tile_weights(self, wg, wu, wd):
        pass

    def phase_ffn(self, l, j, xsrc, xdst, lat_only_out=None):
        P = self
        P.gate_bcast(5, 0, P.bc[0]); P.gate_bcast(5, 1, P.bc[1]); P.load_ln(l, 1)
        Wg, Wu, Wd = P.w["ffn_w_gate"][j], P.w["ffn_w_up"][j], P.w["ffn_w_down"][j]
        wdf, wdr = P.gflat(0, NFC * 512, BF16)
        wd = wdf.rearrange("p (f n) -> p f n", n=1024)
        P.DMA(wd, Wd.rearrange("(f p) n -> p f n", p=128), [], wdr, q=P.kb.pool)
        acf, acr = P.gflat(5, NFC * 256, BF16)
        actT = acf.rearrange("p (f n) -> p f n", n=512)
        for (t0, n) in NTILES:
            for fc in range(NFC):
                wgb = P.loadw(Wg[:, fc * 128:(fc + 1) * 128], 128)
                wub = P.loadw(Wu[:, fc * 128:(fc + 1) * 128], 128)
                pg = P.next_ps(); pu = P.next_ps()
                for c in range(KC):
                    P.MM(pg[:, 0:n], wgb[:, c, :], P.hT[:, c, t0:t0 + n], [wgb, P.hT], [pg], start=(c == 0), stop=(c == 7))
                for c in range(KC):
                    P.MM(pu[:, 0:n], wub[:, c, :], P.hT[:, c, t0:t0 + n], [wub, P.hT], [pu], start=(c == 0), stop=(c == 7))
                sg = P.sm()
                P.ACT(sg[:, 0:n], pg[:, 0:n], AF.Silu, [pg], [sg])
                P.TT(actT[:, fc, 0:n], sg[:, 0:n], pu[:, 0:n], ALU.mult, [sg, pu], acr)
            for sub in range(n // 128):
                ti = t0 // 128 + sub
                cond = 1 if ti < 2 else 0
                xt = P.xb[P.xbi]; P.xbi = (P.xbi + 1) % 3
                P.DMA(xt[:], xsrc[ti * 128:(ti + 1) * 128, :], [xsrc], [xt])
                z = P.zb[ti % 2]
                for hf in range(2):
                    po = P.next_ps()
                    for fc in range(NFC):
                        P.MM(po[:, :], actT[:, fc, sub * 128:(sub + 1) * 128], wd[:, fc, hf * 512:(hf + 1) * 512], acr + wdr, [po],
                             start=(fc == 0), stop=(fc == NFC - 1))
                    P.TT(z[:, hf * 512:(hf + 1) * 512], po[:, :], P.bc[cond][:, hf * 512:(hf + 1) * 512], ALU.mult, [po, P.bc[cond]], [z])
                xn = P.xb[P.xbi]; P.xbi = (P.xbi + 1) % 3
                P.resid_ln(z, xt, ti, xn)
                P.DMA(xdst[ti * 128:(ti + 1) * 128, :], xn[:], [xn], [xdst])

    def phase_moe(self, l, j, xsrc, xdst):
        P = self
        P.gate_bcast(5, 0, P.bc[0]); P.gate_bcast(5, 1, P.bc[1]); P.load_ln(l, 1)
        wdf, wdr = P.gflat(0, NFC * 512, BF16)
        wd = wdf.rearrange("p (f n) -> p f n", n=1024)
        acf, acr = P.gflat(5, NFC * 256, BF16)
        actT = acf.rearrange("p (f n) -> p f n", n=512)
        accf, accr = P.gflat(8, 4096)
        acc = accf.rearrange("p (s n) -> p s n", n=1024)
        for (t0, n) in NTILES:
            nsub = n // 128
            for ex in range(8):
                Wg, Wu, Wd = P.w["moe_w_gate"][j, ex], P.w["moe_w_up"][j, ex], P.w["moe_w_down"][j, ex]
                P.DMA(wd, Wd.rearrange("(f p) n -> p f n", p=128), [], wdr, q=P.kb.pool)
                for fc in range(NFC):
                    wgb = P.loadw(Wg[:, fc * 128:(fc + 1) * 128], 128)
                    wub = P.loadw(Wu[:, fc * 128:(fc + 1) * 128], 128)
                    pg = P.next_ps(); pu = P.next_ps()
                    for c in range(KC):
                        P.MM(pg[:, 0:n], wgb[:, c, :], P.hT[:, c, t0:t0 + n], [wgb, P.hT], [pg], start=(c == 0), stop=(c == 7))
                    for c in range(KC):
                        P.MM(pu[:, 0:n], wub[:, c, :], P.hT[:, c, t0:t0 + n], [wub, P.hT], [pu], start=(c == 0), stop=(c == 7))
                    sg = P.sm()
                    P.ACT(sg[:, 0:n], pg[:, 0:n], AF.Silu, [pg], [sg])
                    P.TT(actT[:, fc, 0:n], sg[:, 0:n], pu[:, 0:n], ALU.mult, [sg, pu], acr)
                for sub in range(nsub):
                    ti = t0 // 128 + sub
                    for hf in range(2):
                        po = P.next_ps()
                        for fc in range(NFC):
                            P.MM(po[:, :], actT[:, fc, sub * 128:(sub + 1) * 128], wd[:, fc, hf * 512:(hf + 1) * 512], acr + wdr, [po],
                                 start=(fc == 0), stop=(fc == NFC - 1))
                        a = acc[:, sub, hf * 512:(hf + 1) * 512]
                        if ex == 0:
                            P.TS(a, po[:, :], P.gates[:, ti, ex:ex + 1], ALU.mult, [po, P.gates], accr)
                        else:
                            P.STT(a, po[:, :], P.gates[:, ti, ex:ex + 1], a, ALU.mult, ALU.add, [po, P.gates] + accr, accr)
            for sub in range(nsub):
                ti = t0 // 128 + sub
                cond = 1 if ti < 2 else 0
                xt = P.xb[P.xbi]; P.xbi = (P.xbi + 1) % 3
                P.DMA(xt[:], xsrc[ti * 128:(ti + 1) * 128, :], [xsrc], [xt])
                z = P.zb[ti % 2]
                P.TT(z[:], acc[:, sub, :], P.bc[cond][:], ALU.mult, accr + [P.bc[cond]], [z])
                xn = P.xb[P.xbi]; P.xbi = (P.xbi + 1) % 3
                P.resid_ln(z, xt, ti, xn)
                P.DMA(xdst[ti * 128:(ti + 1) * 128, :], xn[:], [xn], [xdst])

    def s5(self, l, j):
        P = self
        G = P.G
        I32 = mybir.dt.int32
        TWO_PI = 2.0 * math.pi
        W = P.w["od_w_in"][j]
        st_ = P.s5t
        R_, W_ = [st_, P.s5i], [st_]
        P.DMA(P.s5d[:], P.w["s5_d_fm"][j], [], [P.s5d])
        LRE, LIM, DT, MAG, THN, SN, CS, CRE, CIM, NCIM, TA, TB = range(12)
        for d in range(2):
            q = lambda k: st_[:, d, k, :]
            P.DMA(q(LRE), P.w["s5_lre"][j, d], [], W_); P.DMA(q(LIM), P.w["s5_lim"][j, d], [], W_); P.DMA(q(DT), P.w["s5_ldt"][j, d], [], W_)
            P.ACT(q(DT), q(DT), AF.Exp, R_, W_)
            P.TS(q(LRE), q(LRE), -1e-4, ALU.min, R_, W_)
            P.TT(q(TA), q(LRE), q(DT), ALU.mult, R_, W_)
            P.ACT(q(MAG), q(TA), AF.Exp, R_, W_)
            P.TT(q(THN), q(LIM), q(DT), ALU.mult, R_, W_)
            P.TS(q(THN), q(THN), 1.0 / TWO_PI, ALU.mult, R_, W_)
            P.CP(P.s5i[:], q(THN), R_, [P.s5i]); P.CP(q(TA), P.s5i[:], R_, W_)
            P.TT(q(TA), q(THN), q(TA), ALU.subtract, R_, W_)
            P.ACT(q(SN), q(TA), AF.Sin, R_, W_, scale=TWO_PI)
            P.TS(q(TA), q(TA), 0.25, ALU.add, R_, W_)
            P.CP(P.s5i[:], q(TA), R_, [P.s5i]); P.CP(q(TB), P.s5i[:], R_, W_)
            P.TT(q(TA), q(TA), q(TB), ALU.subtract, R_, W_)
            P.ACT(q(CS), q(TA), AF.Sin, R_, W_, scale=TWO_PI)
            P.TT(q(CS), q(CS), q(MAG), ALU.mult, R_, W_)
            P.TT(q(SN), q(SN), q(MAG), ALU.mult, R_, W_)
            P.TT(q(TA), q(LRE), q(LRE), ALU.mult, R_, W_); P.TT(q(TB), q(LIM), q(LIM), ALU.mult, R_, W_)
            P.TT(q(TA), q(TA), q(TB), ALU.add, R_, W_); P.RECIP(q(TA), q(TA), R_, W_)
            P.TS(q(CS), q(CS), -1.0, ALU.add, R_, W_)
            P.TT(q(CRE), q(CS), q(LRE), ALU.mult, R_, W_); P.TT(q(TB), q(SN), q(LIM), ALU.mult, R_, W_)
            P.TT(q(CRE), q(CRE), q(TB), ALU.add, R_, W_); P.TT(q(CRE), q(CRE), q(TA), ALU.mult, R_, W_)
            P.TT(q(CIM), q(SN), q(LRE), ALU.mult, R_, W_); P.TT(q(TB), q(CS), q(LIM), ALU.mult, R_, W_)
            P.TT(q(CIM), q(CIM), q(TB), ALU.subtract, R_, W_); P.TT(q(CIM), q(CIM), q(TA), ALU.mult, R_, W_)
            P.TS(q(NCIM), q(CIM), -1.0, ALU.mult, R_, W_)
        uT, yacc, A, B, Cs, Sn, t1, t2 = [G[i] for i in range(8)]
        t2i = P.Gt[:, 7, :].bitcast(I32)
        ygf, ygr = P.gflat(8, 2 * T, BF16)
        ygT = ygf.rearrange("p (c n) -> p c n", n=T)
        for ut in range(4):
            wu = P.loadw(W[:, ut * 128:(ut + 1) * 128], 128)
            P.proj_fm(wu, 128, lambda pb, t0, n: P.CP(uT[:, t0:t0 + n], pb[:, 0:n], [pb], [uT], eng=P.kb.act))
            P.TS(yacc[:], uT[:], P.s5d[:, ut:ut + 1], ALU.mult, [uT, P.s5d], [yacc])
            for sl in range(4):
                stt = ut * 4 + sl
                r0 = sl * 32
                bc_ = P.s5bc
                P.DMA(bc_[:, 0:32], P.w["s5_bT"][j, :, stt].rearrange("p a h -> p (a h)"), [], [bc_])
                P.DMA(bc_[:, 32:64], P.w["s5_cT"][j, :, stt].rearrange("p a h -> p (a h)"), [], [bc_])
                Zc = P.s5zc
                P.MSET(Zc[:, 0:256], 0.0, [Zc])
                for gl in range(2):
                    pr = slice(gl * 64, gl * 64 + 64); cc = slice(r0 + gl * 16, r0 + gl * 16 + 16)
                    P.CP(Zc[pr, cc], bc_[pr, 32:48], [bc_], [Zc])
                    P.TS(Zc[pr, 128 + cc.start:128 + cc.stop], bc_[pr, 48:64], -1.0, ALU.mult, [bc_], [Zc])
                for d in range(2):
                    q = lambda k: st_[:, d, k, stt:stt + 1]
                    Z = P.sm(); tmp = P.sm(); L = P.sm()
                    P.MSET(Z[:, 0:256], 0.0, [Z])
                    for gl in range(2):
                        pr = slice(gl * 64, gl * 64 + 64); c0 = r0 + gl * 16
                        P.TS(tmp[pr, 0:16], bc_[pr, 0:16], q(CRE)[pr], ALU.mult, [bc_, st_], [tmp])
                        P.STT(Z[pr, c0:c0 + 16], bc_[pr, 16:32], q(NCIM)[pr], tmp[pr, 0:16], ALU.mult, ALU.add, [bc_, st_, tmp], [Z])
                        P.TS(tmp[pr, 16:32], bc_[pr, 16:32], q(CRE)[pr], ALU.mult, [bc_, st_], [tmp])
                        P.STT(Z[pr, 128 + c0:128 + c0 + 16], bc_[pr, 0:16], q(CIM)[pr], tmp[pr, 16:32], ALU.mult, ALU.add, [bc_, st_, tmp], [Z])
                    for k in range(2):
                        pz = P.next_ps()
                        P.TR(pz[:, 0:128], Z[:, k * 128:(k + 1) * 128], [Z], [pz])
                        P.CP(L[:, k * 128:(k + 1) * 128], pz[:, 0:128], [pz], [L], eng=P.kb.act)
                    P.DMA(t1[:], P.cd["tauF" if d == 0 else "tauB"][:], [], [t1])
                    P.TS(t1[:], t1[:], q(THN), ALU.mult, [t1, st_], [t1])
                    P.CP(t2i, t1[:], [t1], [t2]); P.CP(Cs[:], t2i, [t2], [Cs], eng=P.kb.act)
                    P.TT(t1[:], t1[:], Cs[:], ALU.subtract, [t1, Cs], [t1])
                    P.ACT(Sn[:], t1[:], AF.Sin, [t1], [Sn], scale=TWO_PI)
                    P.TS(t1[:], t1[:], 0.25, ALU.add, [t1], [t1])
                    P.CP(t2i, t1[:], [t1], [t2]); P.CP(Cs[:], t2i, [t2], [Cs], eng=P.kb.act)
                    P.TT(t1[:], t1[:], Cs[:], ALU.subtract, [t1, Cs], [t1])
                    P.ACT(Cs[:], t1[:], AF.Sin, [t1], [Cs], scale=TWO_PI)
                    for (t0, n) in NTILES:
                        pa = P.next_ps(); pb_ = P.next_ps()
                        P.MM(pa[:, 0:n], L[:, 0:128], uT[:, t0:t0 + n], [L, uT], [pa])
                        P.MM(pb_[:, 0:n], L[:, 128:256], uT[:, t0:t0 + n], [L, uT], [pb_])
                        P.CP(A[:, t0:t0 + n], pa[:, 0:n], [pa], [A], eng=P.kb.act)
                        P.CP(B[:, t0:t0 + n], pb_[:, 0:n], [pb_], [B])
                    P.TT(t1[:], A[:], Cs[:], ALU.mult, [A, Cs], [t1]); P.TT(t2[:], B[:], Sn[:], ALU.mult, [B, Sn], [t2])
                    P.TT(A[:], A[:], Sn[:], ALU.mult, [A, Sn], [A]); P.TT(B[:], B[:], Cs[:], ALU.mult, [B, Cs], [B])
                    P.TT(t1[:], t1[:], t2[:], ALU.add, [t1, t2], [t1]); P.TT(B[:], B[:], A[:], ALU.subtract, [B, A], [B])
                    mg = q(MAG)
                    for src, dst in ((t1, t2), (B, A)):
                        if d == 0:
                            P.SCAN(dst[:], mg.broadcast_to([128, T]), src[:], [st_, src], [dst])
                        else:
                            P.SCAN(dst[:, 255::-1], mg.broadcast_to([128, 256]), src[:, 255::-1], [st_, src], [dst])
                            P.SCAN(dst[:, T - 1:255:-1], mg.broadcast_to([128, T - 256]), src[:, T - 1:255:-1], [st_, src, dst], [dst], init=dst[:, 0:1])
                    P.TT(t1[:], t2[:], Cs[:], ALU.mult, [t2, Cs], [t1]); P.TT(B[:], A[:], Sn[:], ALU.mult, [A, Sn], [B])
                    P.TT(t1[:], t1[:], B[:], ALU.subtract, [t1, B], [t1])
                    P.TT(t2[:], t2[:], Sn[:], ALU.mult, [t2, Sn], [t2]); P.TT(A[:], A[:], Cs[:], ALU.mult, [A, Cs], [A])
                    P.TT(t2[:], t2[:], A[:], ALU.add, [t2, A], [t2])
                    for (t0, n) in NTILES:
                        py = P.next_ps()
                        P.MM(py[:, 0:n], Zc[:, 0:128], t1[:, t0:t0 + n], [Zc, t1], [py], start=True, stop=False)
                        P.MM(py[:, 0:n], Zc[:, 128:256], t2[:, t0:t0 + n], [Zc, t2], [py], start=False, stop=True)
                        P.TT(yacc[:, t0:t0 + n], yacc[:, t0:t0 + n], py[:, 0:n], ALU.add, [yacc, py], [yacc])
            P.ACT(t1[:], yacc[:], AF.Square, [yacc], [t1])
            P.TS(t1[:], t1[:], 0.044715, ALU.mult, [t1], [t1], s2=1.0, op1=ALU.add)
            P.TT(t1[:], t1[:], yacc[:], ALU.mult, [t1, yacc], [t1])
            P.ACT(t1[:], t1[:], AF.Sigmoid, [t1], [t1], scale=2.0 * math.sqrt(2.0 / math.pi))
            P.TT(ygT[:, ut, :], yacc[:], t1[:], ALU.mult, [yacc, t1], ygr)
        for nt in range(4):
            wg = P.loadw(P.w["s5_glu_w"][j][:, nt * 128:(nt + 1) * 128], 128, kc=4)
            for (t0, n) in NTILES:
                pg = P.next_ps()
                for c in range(4):
                    P.MM(pg[:, 0:n], wg[:, c, :], ygT[:, c, t0:t0 + n], [wg] + ygr, [pg], start=(c == 0), stop=(c == 3))
                sg = P.sm()
                P.ACT(sg[:, 0:n], pg[:, 0:n], AF.Sigmoid, [pg], [sg])
                yo = P.sm()
                P.TT(yo[:, 0:n], ygT[:, nt, t0:t0 + n], sg[:, 0:n], ALU.mult, ygr + [sg], [yo])
                P.DMA(P.yT[nt * 128:(nt + 1) * 128, t0:t0 + n], yo[:, 0:n], [yo], [P.yT])

    def rwkv(self, l, j):
        P = self
        G = P.G
        W = P.w["od_w_in"][j]
        C0 = 512
        prm = P.rwp
        P.DMA(prm[:, 0:15], P.w["rw_mu_fm"][j], [], [prm])
        P.DMA(prm[:, 16:20], P.w["rw_kk_fm"][j], [], [prm]); P.DMA(prm[:, 20:24], P.w["rw_ka_fm"][j], [], [prm])
        P.DMA(prm[:, 24:28], P.w["rw_rk_fm"][j], [], [prm])
        for d in range(2):
            P.DMA(prm[:, 28 + d * 4:32 + d * 4], P.w["rw_w0_fm"][j, d], [], [prm])
            P.DMA(prm[:, 36 + d * 4:40 + d * 4], P.w["rw_a0_fm"][j, d], [], [prm])
        MU = lambda t_: prm[:, t_:t_ + 1]
        rT, kT, vtokS, kkT, L_, Gc, E1, E2, As, Oacc = [G[i] for i in range(10)]
        vtok = vtokS[:].rearrange("p (t c) -> p t c", c=128)
        oacc = Oacc[:].rearrange("p (t c) -> p t c", c=128)
        Hbd = P.S

        def proj_shifted(tile_idx, dst, tmp):
            wb = P.loadw(W[:, C0 + tile_idx * 128:C0 + (tile_idx + 1) * 128], 128)
            P.proj_fm(wb, 128, lambda pb, t0, n: P.CP(tmp[:, t0:t0 + n], pb[:, 0:n], [pb], [tmp], eng=P.kb.act))
            for (a, b) in ((0, LC), (LC, T)):
                P.CP(dst[:, a:a + 1], tmp[:, a + 1:a + 2], [tmp], [dst], eng=P.kb.pool)
                P.CP(dst[:, b - 1:b], tmp[:, b - 2:b - 1], [tmp], [dst], eng=P.kb.pool)
                P.TT(dst[:, a + 1:b - 1], tmp[:, a:b - 2], tmp[:, a + 2:b], ALU.add, [tmp], [dst])
            P.STT(dst[:], dst[:], 0.5, tmp[:], ALU.mult, ALU.subtract, [dst, tmp], [dst])
            P.STT(dst[:], dst[:], MU(tile_idx), tmp[:], ALU.mult, ALU.add, [dst, prm, tmp], [dst])

        P.DMA(P.rwm[:], P.cd["rw_masks"][0], [], [P.rwm])
        for idx, (lt, fn) in enumerate(((12, AF.Tanh), (13, None), (14, AF.Sigmoid))):
            proj_shifted(lt, E1, E2)
            if fn is not None:
                P.ACT(E1[:], E1[:], fn, [E1], [E1])
            P.DMA(P.rwscr[idx], E1[:], [E1], [P.rwscr])
        for pp in range(4):
            proj_shifted(0 + pp, rT, E1)
            proj_shifted(4 + pp, kT, E1)
            proj_shifted(8 + pp, E2, E1)
            for ti in range(NT):
                pv = P.next_ps()
                P.TR(pv[:, 0:128], E2[:, ti * 128:(ti + 1) * 128], [E2], [pv])
                P.CP(vtok[:, ti, :], pv[:, 0:128], [pv], [vtokS], eng=P.kb.act)
            P.TS(kkT[:], kT[:], prm[:, 16 + pp:17 + pp], ALU.mult, [kT, prm], [kkT])
            for (t0, n) in NTILES:
                sq = P.sm()
                P.ACT(sq[:, 0:n], kkT[:, t0:t0 + n], AF.Square, [kkT], [sq])
                pn = P.next_ps()
                P.MM(pn[:, 0:n], P.bdones[:], sq[:, 0:n], [P.bdones, sq], [pn])
                nr_ = P.sm()
                P.ACT(nr_[:, 0:n], pn[:, 0:n], AF.Sqrt, [pn], [nr_])
                P.TS(nr_[:, 0:n], nr_[:, 0:n], 1e-12, ALU.max, [nr_], [nr_])
                P.RECIP(nr_[:, 0:n], nr_[:, 0:n], [nr_], [nr_])
                P.TT(kkT[:, t0:t0 + n], kkT[:, t0:t0 + n], nr_[:, 0:n], ALU.mult, [kkT, nr_], [kkT])
            P.DMA(P.rwln[:, 0, :], P.w["rw_lng"][j, pp * 128:(pp + 1) * 128].partition_broadcast(128), [], [P.rwln])
            P.DMA(P.rwln[:, 1, :], P.w["rw_lnb"][j, pp * 128:(pp + 1) * 128].partition_broadcast(128), [], [P.rwln])
            for d in range(2):
                fwd = d == 0
                P.DMA(P.rwm[:], P.cd["rw_masks"][d], [], [P.rwm])
                for (lt, padk, biasc, dstb, fn) in ((12, "rw_w2pad", 28 + d * 4 + pp, L_, AF.Tanh), (13, "rw_a2pad", 36 + d * 4 + pp, As, None)):
                    P.DMA(E1[:], P.rwscr[lt - 12], [P.rwscr], [E1])
                    wpad = P.attm[1]
                    P.DMA(wpad[:, 0:128], P.w[padk][j, d, :, pp * 128:(pp + 1) * 128], [], [wpad])
                    for (t0, n) in NTILES:
                        pz = P.next_ps()
                        P.MM(pz[:, 0:n], wpad[:, 0:128], E1[:, t0:t0 + n], [wpad, E1], [pz])
                        P.ACT(dstb[:, t0:t0 + n], pz[:, 0:n], AF.Sigmoid, [pz, prm], [dstb], bias=prm[:, biasc:biasc + 1])
                P.TS(L_[:], L_[:], -math.exp(-0.5), ALU.mult, [L_], [L_])
                for ti in range(NT):
                    tsl = slice(ti * 128, (ti + 1) * 128)
                    if fwd:
                        P.SCAN(Gc[:, tsl], P.ones[:], L_[:, tsl], [P.ones, L_], [Gc])
                    else:
                        hi, lo = (ti + 1) * 128 - 1, ti * 128 - 1
                        rs = slice(hi, lo if lo >= 0 else None, -1)
                        P.SCAN(Gc[:, rs], P.ones[:], L_[:, rs], [P.ones, L_], [Gc])
                P.ACT(E1[:], Gc[:], AF.Exp, [Gc], [E1])
                ecol = E1[:, 127:T:128] if fwd else E1[:, 0:T:128]
                P.CP(P.rwgc[:], ecol, [E1], [P.rwgc])
                P.TT(E1[:], E1[:], rT[:], ALU.mult, [E1, rT], [E1])
                P.TT(E2[:], Gc[:], L_[:], ALU.subtract, [Gc, L_], [E2])
                P.ACT(E2[:], E2[:], AF.Exp, [E2], [E2])
                P.STT(E2[:], kkT[:], -1.0, E2[:], ALU.mult, ALU.mult, [kkT, E2], [E2])
                P.ACT(Gc[:], Gc[:], AF.Exp, [Gc], [Gc], scale=-1.0)
                P.TT(L_[:], kkT[:], As[:], ALU.mult, [kkT, As], [L_])
                P.TT(L_[:], L_[:], Gc[:], ALU.mult, [L_, Gc], [L_])
                P.TS(As[:], As[:], -1.0, ALU.add, [As, prm], [As], s2=prm[:, 20 + pp:21 + pp], op1=ALU.mult)
                P.STT(As[:], As[:], 1.0, kT[:], ALU.add, ALU.mult, [As, kT], [As])
                P.TT(As[:], As[:], Gc[:], ALU.mult, [As, Gc], [As])
                rt_, at_, bt_, kt_ = E1, E2, L_, As
                P.MSET(Hbd[:], 0.0, [Hbd])
                tiles = list(range(NT)) if fwd else [1, 0] + list(range(NT - 1, 1, -1))
                hs = [slice(0, 64), slice(64, 128)]
                CB = [P.bc[0], P.bc[1], P.bc[2], P.bc[3], P.xb[0], P.xb[1]]
                XA, ZA, XB, ZB, WT, MRB, LAK, MRK = range(8)
                mat = lambda c, q_: CB[c][:, q_ * 128:(q_ + 1) * 128]
                Ybuf = P.attm[0]
                for g0 in range(0, NT, 3):
                    grp = tiles[g0:g0 + 3]
                    chains = [(ti, hh) for ti in grp for hh in range(2)]
                    for c, (ti, hh) in enumerate(chains):
                        tsl = slice(ti * 128, (ti + 1) * 128)
                        ar = P.rwar[hh]; cb = CB[c]
                        P.TS(ar[:, 0:128], at_[:, tsl], P.halfm[:, hh:hh + 1], ALU.mult, [at_, P.halfm], [ar])
                        P.TS(ar[:, 128:256], rt_[:, tsl], P.halfm[:, hh:hh + 1], ALU.mult, [rt_, P.halfm], [ar], eng=P.kb.pool)
                        p1 = P.next_ps(); p2 = P.next_ps(); p3 = P.next_ps()
                        P.MM(p1[:, 0:256], bt_[:, tsl], ar[:, 0:256], [bt_, ar], [p1])
                        P.MM(p2[:, 0:256], kt_[:, tsl], ar[:, 0:256], [kt_, ar], [p2])
                        P.MM(p3[:, 0:128], ar[:, 0:128], bt_[:, tsl], [ar, bt_], [p3])
                        P.TT(mat(c, XA), p1[:, 0:128], P.rwm[:, 0, :], ALU.mult, [p1, P.rwm], [cb])
                        P.TT(mat(c, MRB), p1[:, 128:256], P.rwm[:, 1, :], ALU.mult, [p1, P.rwm], [cb])
                        P.TT(mat(c, LAK), p2[:, 0:128], P.rwm[:, 0, :], ALU.mult, [p2, P.rwm], [cb])
                        P.TT(mat(c, MRK), p2[:, 128:256], P.rwm[:, 1, :], ALU.mult, [p2, P.rwm], [cb])
                        P.TT(mat(c, ZA), p3[:, 0:128], P.rwm[:, 2, :], ALU.mult, [p3, P.rwm], [cb])
                        P.TT(mat(c, WT), mat(c, XA), P.ident[:], ALU.add, [cb, P.ident], [cb], eng=P.kb.pool)
                    cx, cz, nx, nz = XA, ZA, XB, ZB
                    for k in range(1, 7):
                        for c in range(len(chains)):
                            cb = CB[c]
                            pZ = P.next_ps()
                            P.MM(pZ[:, 0:128], mat(c, cx), mat(c, cz), [cb], [pZ])
                            if k < 6:
                                pX = P.next_ps()
                                P.MM(pX[:, 0:128], mat(c, cz), mat(c, cx), [cb], [pX])
                            P.CP(mat(c, nz), pZ[:, 0:128], [pZ], [cb])
                            if k < 6:
                                P.CP(mat(c, nx), pX[:, 0:128], [pX], [cb], eng=P.kb.act)
                        for c in range(len(chains)):
                            cb = CB[c]
                            pW = P.next_ps()
                            P.MM(pW[:, 0:128], mat(c, nz), mat(c, WT), [cb], [pW])
                            P.TT(mat(c, WT), mat(c, WT), pW[:, 0:128], ALU.add, [cb, pW], [cb])
                        cx, cz, nx, nz = nx, nz, cx, cz
                    for gi, ti in enumerate(grp):
                        tsl = slice(ti * 128, (ti + 1) * 128)
                        U = P.rwu
                        pY = P.next_ps()
                        for hh in range(2):
                            c = gi * 2 + hh
                            P.MM(pY[:, hs[hh]], at_[:, tsl], Hbd[:, hs[hh]], [at_, Hbd], [pY], start=True, stop=False)
                            P.MM(pY[:, hs[hh]], mat(c, LAK), vtok[:, ti, hs[hh]], [CB[c], vtokS], [pY], start=False, stop=True)
                        P.CP(Ybuf[:], pY[:, 0:128], [pY], [Ybuf])
                        pU = P.next_ps()
                        for hh in range(2):
                            c = gi * 2 + hh
                            P.MM(pU[:, hs[hh]], mat(c, WT), Ybuf[:, hs[hh]], [CB[c], Ybuf], [pU])
                        P.CP(U[:], pU[:, 0:128], [pU], [U], eng=P.kb.act)
                        pO = P.next_ps()
                        for hh in range(2):
                            c = gi * 2 + hh
                            P.MM(pO[:, hs[hh]], rt_[:, tsl], Hbd[:, hs[hh]], [rt_, Hbd], [pO], start=True, stop=False)
                            P.MM(pO[:, hs[hh]], mat(c, MRB), U[:, hs[hh]], [CB[c], U], [pO], start=False, stop=False)
                            P.MM(pO[:, hs[hh]], mat(c, MRK), vtok[:, ti, hs[hh]], [CB[c], vtokS], [pO], start=False, stop=True)
                        if fwd:
                            P.CP(oacc[:, ti, :], pO[:, 0:128], [pO], [Oacc], eng=P.kb.act)
                        else:
                            P.TT(oacc[:, ti, :], oacc[:, ti, :], pO[:, 0:128], ALU.add, [Oacc, pO], [Oacc])
                        tk = P.ktok[0]
                        for q_, src in ((0, bt_), (1, kt_)):
                            pt_ = P.next_ps()
                            P.TR(pt_[:, 0:128], src[:, tsl], [src], [pt_])
                            P.CP(tk[:, q_, :], pt_[:, 0:128], [pt_], [tk], eng=P.kb.act)
                        pH = P.next_ps()
                        P.MM(pH[:, 0:128], tk[:, 0, :], U[:], [tk, U], [pH], start=True, stop=False)
                        P.MM(pH[:, 0:128], tk[:, 1, :], vtok[:, ti, :], [tk, vtokS], [pH], start=False, stop=True)
                        P.STT(P.tmpS[:], pH[:, 0:128], P.rwgc[:, ti:ti + 1], P.bdones[:], ALU.mult, ALU.mult, [pH, P.rwgc, P.bdones], [P.tmpS])
                        P.STT(Hbd[:], Hbd[:], P.rwgc[:, ti:ti + 1], P.tmpS[:], ALU.mult, ALU.add, [Hbd, P.rwgc, P.tmpS], [Hbd])
            P.STT(E1[:], rT[:], prm[:, 24 + pp:25 + pp], kT[:], ALU.mult, ALU.mult, [rT, prm, kT], [E1])
            P.DMA(E2[:], P.rwscr[2], [P.rwscr], [E2])
            g2b = P.attm[0]
            P.DMA(g2b[:, 0:128], P.w["rw_g2"][j, :, pp * 128:(pp + 1) * 128], [], [g2b])
            for ti in range(NT):
                tsl = slice(ti * 128, (ti + 1) * 128)
                o = P.sm(); st = P.stat
                P.CP(o[:, 0:128], oacc[:, ti, :], [Oacc], [o], eng=P.kb.pool)
                o3 = o[:, 0:128].rearrange("p (h i) -> p h i", i=64)
                P.kb.op(P.kb.dve, lambda e, o3=o3: e.reduce_sum(out=st[:, 16:18], in_=o3, axis=AX.X), [o.r], [st.r])
                P.TS(st[:, 16:18], st[:, 16:18], 1.0 / 64.0, ALU.mult, [st], [st])
                for hh in range(2):
                    P.TS(o3[:, hh, :], o3[:, hh, :], st[:, 16 + hh:17 + hh], ALU.subtract, [o, st], [o])
                sq = P.sm()
                P.TT(sq[:, 0:128], o[:, 0:128], o[:, 0:128], ALU.mult, [o], [sq])
                sq3 = sq[:, 0:128].rearrange("p (h i) -> p h i", i=64)
                P.kb.op(P.kb.dve, lambda e, sq3=sq3: e.reduce_sum(out=st[:, 18:20], in_=sq3, axis=AX.X), [sq.r], [st.r])
                P.ACT(st[:, 18:20], st[:, 18:20], AF.Sqrt, [st, P.cst], [st], scale=1.0 / 64.0, bias=P.cst[:, 2:3])
                P.RECIP(st[:, 18:20], st[:, 18:20], [st], [st])
                for hh in range(2):
                    P.TS(o3[:, hh, :], o3[:, hh, :], st[:, 18 + hh:19 + hh], ALU.mult, [o, st], [o])
                P.TT(o[:, 0:128], o[:, 0:128], P.rwln[:, 0, :], ALU.mult, [o, P.rwln], [o])
                P.TT(o[:, 0:128], o[:, 0:128], P.rwln[:, 1, :], ALU.add, [o, P.rwln], [o])
                pb_ = P.next_ps()
                P.MM(pb_[:, 0:2], E1[:, tsl], P.halfm[:], [E1, P.halfm], [pb_])
                P.CP(st[:, 20:22], pb_[:, 0:2], [pb_], [st])
                for hh in range(2):
                    P.STT(o3[:, hh, :], vtok[:, ti, hh * 64:(hh + 1) * 64], st[:, 20 + hh:21 + hh], o3[:, hh, :], ALU.mult, ALU.add, [vtokS, st, o], [o])
                pg = P.next_ps()
                P.MM(pg[:, 0:128], E2[:, tsl], g2b[:, 0:128], [E2, g2b], [pg])
                P.TT(o[:, 0:128], o[:, 0:128], pg[:, 0:128], ALU.mult, [o, pg], [o])
                pT = P.next_ps()
                P.TR(pT[:, 0:128], o[:, 0:128], [o], [pT])
                yo = P.sm()
                P.CP(yo[:, 0:128], pT[:, 0:128], [pT], [yo], eng=P.kb.act)
                P.DMA(P.yT[512 + pp * 128:512 + (pp + 1) * 128, tsl], yo[:, 0:128], [yo], [P.yT])

    def build(self):
        P = self
        P.setup()
        xcur = P.xin
        for li, l in enumerate(P.layers):
            j = l // 2
            last = li == len(P.layers) - 1
            xmid = P.xs[0]
            xnext = P.out if last else P.xs[1]
            if P.stop >= 1:
                P.phase_mod(l)
                if "fm" in P.taps:
                    P.DMA(P.outp("tap_fm", [128, 48, 2]), P.fm[:], [P.fm], [])
            if P.stop >= 2:
                P.phase_hT(xcur)
                if "hTd" in P.taps:
                    P.DMA(P.outp("tap_hT", [128, KC, T]), P.hT[:], [P.hT], [], q=P.kb.pool)
            if l % 2 == 0:
                if P.stop >= 3: P.hgrn2(l, j)
                if P.stop >= 4: P.attention(l, j)
                if P.stop >= 5: P.phase_out(l, P.w["ev_w_out"][j], xcur, xmid)
                if P.stop >= 6: P.phase_ffn(l, j, xmid, xnext)
            else:
                if "inject_y" in P.taps:
                    pass
                else:
                    if P.stop >= 3: P.s5(l, j)
                    if P.stop >= 4: P.rwkv(l, j)
                if P.stop >= 5: P.phase_out(l, P.w["od_w_out"][j], xcur, xmid, router_j=(j if "1" != "0" else None))
                if "gates" in P.taps:
                    P.DMA(P.outp("tap_gates", [128, NT, 8]), P.gates[:], [P.gates], [])
                if P.stop >= 6: P.phase_moe(l, j, xmid, xnext)
            xcur = xnext
        P.kb.emit()
        self.st.close()
        return self.nc


def host_inputs(inp, b):
    m = {}
    m["xin"] = np.ascontiguousarray(np.concatenate([inp["ctx"][b], inp["x"][b]], 0))
    ct = np.stack([inp["c"][b].reshape(8, 128).T, inp["c_ctx"].reshape(8, 128).T], -1)
    m["condT"] = np.ascontiguousarray(ct.astype(np.float32))
    for k, v in host_consts().items():
        m["c_" + k] = v
    m["ada_w"] = inp["ada_w"]
    m["ada_b_fm"] = np.ascontiguousarray(inp["ada_b"].reshape(4, 48, 128).transpose(0, 2, 1))
    m["ln_g"] = inp["ln_g"]; m["ln_b"] = inp["ln_b"]
    m["ev_w_in"] = inp["ev_w_in"]; m["ev_w_out"] = inp["ev_w_out"]
    m["hg_lb_fm"] = np.ascontiguousarray(inp["hg_lb"].reshape(2, 4, 128).transpose(2, 1, 0))
    m["hg_ng_fm"] = np.ascontiguousarray(inp["hg_norm_g"].reshape(2, 4, 128).transpose(0, 2, 1))
    m["attn_sink"] = inp["attn_sink"]
    def fm16(a):
        return np.ascontiguousarray(a.reshape(2, 2, 16, 2, 64).transpose(0, 1, 3, 4, 2).reshape(2, 2, 128, 16))
    m["s5_lre"] = fm16(inp["s5_lam_re"]); m["s5_lim"] = fm16(inp["s5_lam_im"])
    m["s5_ldt"] = fm16(np.ascontiguousarray(np.broadcast_to(inp["s5_log_dt"][..., None], (2, 2, 32, 64))))
    bb = np.stack([inp["s5_b_re"], inp["s5_b_im"]], 3)
    m["s5_bT"] = np.ascontiguousarray(bb.reshape(2, 16, 2, 64, 2, 16).transpose(0, 2, 3, 1, 4, 5).reshape(2, 128, 16, 2, 16))
    cc = np.stack([inp["s5_c_re"], inp["s5_c_im"]], 2).transpose(0, 1, 4, 2, 3)
    m["s5_cT"] = np.ascontiguousarray(cc.reshape(2, 16, 2, 64, 2, 16).transpose(0, 2, 3, 1, 4, 5).reshape(2, 128, 16, 2, 16))
    m["s5_d_fm"] = np.ascontiguousarray(inp["s5_d"].reshape(2, 4, 128).transpose(0, 2, 1))
    m["s5_glu_w"] = inp["s5_glu_w"]
    fm4 = lambda a: np.ascontiguousarray(a.reshape(a.shape[:-1] + (4, 128)).swapaxes(-1, -2))
    m["rw_mu_fm"] = np.ascontiguousarray(inp["rwkv_mu"].reshape(2, 15, 128).transpose(0, 2, 1))
    m["rw_w0_fm"] = fm4(inp["rwkv_w0"]); m["rw_a0_fm"] = fm4(inp["rwkv_a0"])
    def pad2(a):
        o = np.zeros((2, 2, 128, 512), np.float32)
        o[:, 0, 0:64] = a[:, 0]; o[:, 1, 64:128] = a[:, 1]
        return o
    m["rw_w2pad"] = pad2(inp["rwkv_w2"]); m["rw_a2pad"] = pad2(inp["rwkv_a2"]); m["rw_g2"] = inp["rwkv_g2"]
    m["rw_kk_fm"] = fm4(inp["rwkv_k_k"]); m["rw_ka_fm"] = fm4(inp["rwkv_k_a"]); m["rw_rk_fm"] = fm4(inp["rwkv_r_k"].reshape(2, 512))
    m["rw_lng"] = inp["rwkv_ln_g"]; m["rw_lnb"] = inp["rwkv_ln_b"]
    for k in ("ffn_w_gate", "ffn_w_up", "ffn_w_down", "od_w_in", "od_w_out", "moe_router_w", "moe_router_b", "moe_w_gate", "moe_w_up", "moe_w_down"):
        m[k] = inp[k]
    return m


FUSED = os.environ.get("MK_FUSED", "1") == "1"
N_CORES = 8


def _run(layers, inputs, xin_per_core):
    P = Prog(layers)
    nc = P.build()
    in_maps = []
    for b in range(N_CORES):
        m = host_inputs(inputs, b)
        m["xin"] = xin_per_core[b]
        in_maps.append({k: v for k, v in m.items() if k in P.din})
    res = run_bass_kernel_spmd(nc, in_maps, core_ids=list(range(N_CORES)))
    return [r["out"] for r in res.results]


def kernel(**inputs):
    inputs = {k: np.asarray(v) for k, v in inputs.items()}
    xs = [np.ascontiguousarray(np.concatenate([inputs["ctx"][b], inputs["x"][b]], 0)).astype(np.float32) for b in range(N_CORES)]
    if FUSED:
        xs = _run([0, 1, 2, 3], inputs, xs)
    else:
        for l in range(4):
            xs = _run([l], inputs, xs)
    return np.stack([x[LC:] for x in xs], 0).astype(np.float32)
```
